# Optimizing a Trainium2 kernel written in Bass

```python
import math
import jax, jax.numpy as jnp
from jax import lax
import numpy as np

D_MODEL = 1024
BATCH = 8
SEQ = 4096
DEPTH = 2

HEAD_DIM = 64
MIX_W = D_MODEL // 4
RWKV_W = MIX_W
RWKV_HEADS = RWKV_W // HEAD_DIM
RWKV_W_LORA = 64
RWKV_A_LORA = 64
RWKV_LNX_EPS = 64e-5
DSA_W = MIX_W
DSA_HEADS = DSA_W // HEAD_DIM
IDX_HEADS = 4
IDX_DIM = 64
TOPK_MAX = 256
Q_BLOCK = 128
RET_W = MIX_W
RET_HEADS = RET_W // HEAD_DIM
RET_CHUNK = 128
S5_W = MIX_W
S5_GROUP = 16
S5_GROUPS = S5_W // S5_GROUP
S5_STATE = 64
XATT_W = MIX_W
XATT_HEADS = XATT_W // HEAD_DIM
N_MEM = 256
N_BRANCH = 5
ROPE_THETA = 10000.0
NORM_EPS = 1e-6

RWKV_SIZES = (RWKV_W, RWKV_W, RWKV_W, RWKV_W_LORA, RWKV_A_LORA, RWKV_W)
DSA_SIZES = (DSA_W, DSA_W, DSA_W, IDX_HEADS * IDX_DIM, IDX_DIM, IDX_HEADS, DSA_W)
RET_SIZES = (RET_W, RET_W, RET_W, RET_W)
S5_SIZES = (S5_W, S5_W)
XATT_SIZES = (XATT_W, XATT_W)
RWKV_COLS = 4 * RWKV_W + RWKV_W_LORA + RWKV_A_LORA
DSA_COLS = 4 * DSA_W + IDX_HEADS * IDX_DIM + IDX_DIM + IDX_HEADS
RET_COLS = 4 * RET_W
S5_COLS = 2 * S5_W
XATT_COLS = 2 * XATT_W
GATE_COLS = N_BRANCH * D_MODEL
IN_SIZES = (RWKV_COLS, DSA_COLS, RET_COLS, S5_COLS, XATT_COLS, GATE_COLS)
N_IN = RWKV_COLS + DSA_COLS + RET_COLS + S5_COLS + XATT_COLS + GATE_COLS

kernel_name = 'hybrid_gated_rwkv7_dsa_retention_s5_block'


def _split(t, sizes):
    out, start = [], 0
    for s in sizes:
        out.append(t[..., start:start + s])
        start += s
    return out


def rms_norm(x, g):
    xf = x.astype(jnp.float32)
    y = xf * lax.rsqrt(jnp.mean(xf * xf, -1, keepdims=True) + NORM_EPS)
    return (y * g.astype(jnp.float32)).astype(x.dtype)


def head_norm(y, w, b, eps):
    bsz, seq, nh, hd = y.shape
    y = y.astype(jnp.float32)
    mu = jnp.mean(y, -1, keepdims=True)
    var = jnp.mean(jnp.square(y - mu), -1, keepdims=True)
    y = ((y - mu) * lax.rsqrt(var + eps)).reshape(bsz, seq, nh * hd) * w
    return y if b is None else y + b


def rope(x, pos):
    half = x.shape[-1] // 2
    inv = ROPE_THETA ** (-jnp.arange(half, dtype=jnp.float32) / half)
    ang = pos.astype(jnp.float32)[..., None] * inv
    cos, sin = jnp.cos(ang)[:, :, None, :], jnp.sin(ang)[:, :, None, :]
    xf = x.astype(jnp.float32)
    x1, x2 = xf[..., :half], xf[..., half:]
    return jnp.concatenate([x1 * cos - x2 * sin, x2 * cos + x1 * sin], -1)


def rwkv7_branch(c, mu, w0, w2, a0, a2, k_k, k_a, r_k, lnx_w, lnx_b):
    bsz, seq, _ = c.shape
    c = c.astype(jnp.float32)
    prev = jnp.pad(c, ((0, 0), (1, 0), (0, 0)))[:, :-1]
    c = c + mu * (prev - c)
    r, k, v, wl, al, g = _split(c, RWKV_SIZES)
    w_log = -jax.nn.softplus(-(w0 + jnp.tanh(wl) @ w2)) - 0.5
    decay = jnp.exp(-jnp.exp(w_log))
    a = jax.nn.sigmoid(a0 + al @ a2)
    hs = lambda t: t.reshape(bsz, seq, RWKV_HEADS, HEAD_DIM)
    kk = hs(k * k_k)
    kk = kk / jnp.maximum(jnp.sqrt(jnp.sum(kk * kk, -1, keepdims=True)), 1e-12)
    k = k * (1.0 + (a - 1.0) * k_a)
    r, k, v, decay, a = hs(r), hs(k), hs(v), hs(decay), hs(a)

    def step(state, inp):
        r_t, w_t, k_t, v_t, kk_t, a_t = inp
        sa = jnp.einsum('bhvk,bhk->bhv', state, -kk_t)
        state = (state * w_t[:, :, None, :] + sa[..., None] * (kk_t * a_t)[:, :, None, :]
                 + v_t[..., None] * k_t[:, :, None, :])
        return state, jnp.einsum('bhvk,bhk->bhv', state, r_t)

    tm = lambda t: jnp.moveaxis(t, 1, 0)
    s0 = jnp.zeros((bsz, RWKV_HEADS, HEAD_DIM, HEAD_DIM), jnp.float32)
    _, y = lax.scan(step, s0, (tm(r), tm(decay), tm(k), tm(v), tm(kk), tm(a)))
    y = head_norm(jnp.moveaxis(y, 0, 1), lnx_w, lnx_b, RWKV_LNX_EPS)
    bonus = jnp.sum(r * k * r_k, -1, keepdims=True) * v
    return (y + bonus.reshape(bsz, seq, RWKV_W)) * jax.nn.silu(g)


def dsa_branch(c, pos):
    bsz, seq, _ = c.shape
    q, k, v, iq, ik, iw, g = _split(c, DSA_SIZES)
    q = rope(q.reshape(bsz, seq, DSA_HEADS, HEAD_DIM), pos)
    k = rope(k.reshape(bsz, seq, DSA_HEADS, HEAD_DIM), pos)
    v = v.reshape(bsz, seq, DSA_HEADS, HEAD_DIM).astype(jnp.float32)
    iq = rope(iq.reshape(bsz, seq, IDX_HEADS, IDX_DIM), pos) * IDX_DIM ** -0.5
    ik = rope(ik[:, :, None, :], pos)[:, :, 0]
    iw = iw.astype(jnp.float32) * IDX_HEADS ** -0.5
    n_sel = min(TOPK_MAX, seq // 4)
    n_blk = seq // Q_BLOCK
    key_idx = jnp.arange(seq)

    def block(args):
        qb, iqb, iwb, tb = args
        s_idx = jnp.einsum('bqhd,bsd->bqhs', iqb, ik)
        score = jnp.einsum('bqhs,bqh->bqs', jax.nn.relu(s_idx), iwb)
        causal = key_idx[None, None, :] <= tb[None, :, None]
        score = jnp.where(causal, score, -jnp.inf)
        _, sel = lax.top_k(score, n_sel)
        gather = jax.vmap(lambda tb_, ib_: tb_[ib_])
        k_sel = gather(k, sel)
        v_sel = gather(v, sel)
        logits = jnp.einsum('bqhd,bqnhd->bhqn', qb, k_sel) * HEAD_DIM ** -0.5
        valid = (sel <= tb[None, :, None])[:, None]
        p = jax.nn.softmax(jnp.where(valid, logits, -1e30), -1)
        return jnp.einsum('bhqn,bqnhd->bqhd', p, v_sel)

    blocks = lambda t: jnp.moveaxis(t.reshape((bsz, n_blk, Q_BLOCK) + t.shape[2:]), 1, 0)
    out = lax.map(block, (blocks(q), blocks(iq), blocks(iw), key_idx.reshape(n_blk, Q_BLOCK)))
    out = jnp.moveaxis(out, 0, 1).reshape(bsz, seq, DSA_W)
    return out * jax.nn.silu(g.astype(jnp.float32))


def retention_branch(c, pos, gn_w):
    bsz, seq, _ = c.shape
    q, k, v, g = _split(c, RET_SIZES)
    q = rope(q.reshape(bsz, seq, RET_HEADS, HEAD_DIM), pos)
    k = rope(k.reshape(bsz, seq, RET_HEADS, HEAD_DIM), pos) * HEAD_DIM ** -0.5
    v = v.reshape(bsz, seq, RET_HEADS, HEAD_DIM).astype(jnp.float32)
    log_g = jnp.log(1.0 - jnp.exp(jnp.linspace(math.log(1.0 / 32), math.log(1.0 / 512), RET_HEADS)))
    n_ch = seq // RET_CHUNK
    j = jnp.arange(RET_CHUNK, dtype=jnp.float32)
    rel = j[:, None] - j[None, :]
    inner_decay = jnp.where(rel >= 0, jnp.exp(log_g[:, None, None] * jnp.maximum(rel, 0.0)), 0.0)
    q_decay = jnp.exp(log_g[:, None] * (j + 1.0))[..., None]
    k_decay = jnp.exp(log_g[:, None] * (RET_CHUNK - 1.0 - j))[..., None]
    chunk_decay = jnp.exp(log_g * RET_CHUNK)[:, None, None]

    def step(R, inp):
        qc, kc, vc = inp
        a = jnp.einsum('bhqd,bhkd->bhqk', qc, kc) * inner_decay
        o = jnp.einsum('bhqk,bhkv->bhqv', a, vc) + jnp.einsum('bhqd,bhdv->bhqv', qc, R) * q_decay
        R = chunk_decay * R + jnp.einsum('bhkd,bhkv->bhdv', kc * k_decay, vc)
        return R, o

    chunks = lambda t: t.reshape(bsz, n_ch, RET_CHUNK, RET_HEADS, HEAD_DIM).transpose(1, 0, 3, 2, 4)
    r0 = jnp.zeros((bsz, RET_HEADS, HEAD_DIM, HEAD_DIM), jnp.float32)
    _, o = lax.scan(step, r0, (chunks(q), chunks(k), chunks(v)))
    o = o.transpose(1, 0, 3, 2, 4).reshape(bsz, seq, RET_HEADS, HEAD_DIM)
    return head_norm(o, gn_w, None, NORM_EPS) * jax.nn.silu(g.astype(jnp.float32))


def _complex_affine_combine(e1, e2):
    a1r, a1i, b1r, b1i = e1
    a2r, a2i, b2r, b2i = e2
    return (a2r * a1r - a2i * a1i, a2r * a1i + a2i * a1r,
            a2r * b1r - a2i * b1i + b2r, a2r * b1i + a2i * b1r + b2i)


def s5_branch(c, lam_re, lam_im, log_dt, b_re, b_im, c_re, c_im, d_skip, w_glu):
    bsz, seq, _ = c.shape
    u, g = _split(c.astype(jnp.float32), S5_SIZES)
    ug = u.reshape(bsz, seq, S5_GROUPS, S5_GROUP)
    lr = jnp.minimum(lam_re.astype(jnp.float32), -1e-4)
    li = lam_im.astype(jnp.float32)
    dt = jnp.exp(log_dt.astype(jnp.float32))[:, None]
    mag = jnp.exp(lr * dt)
    ab_re, ab_im = mag * jnp.cos(li * dt), mag * jnp.sin(li * dt)
    den = lr * lr + li * li
    f_re = ((ab_re - 1.0) * lr + ab_im * li) / den
    f_im = (ab_im * lr - (ab_re - 1.0) * li) / den
    bb_re = f_re[..., None] * b_re - f_im[..., None] * b_im
    bb_im = f_re[..., None] * b_im + f_im[..., None] * b_re
    bu_re = jnp.einsum('gpc,bsgc->bsgp', bb_re, ug)
    bu_im = jnp.einsum('gpc,bsgc->bsgp', bb_im, ug)
    a_re = jnp.broadcast_to(ab_re, bu_re.shape)
    a_im = jnp.broadcast_to(ab_im, bu_im.shape)
    _, _, xs_re, xs_im = lax.associative_scan(_complex_affine_combine, (a_re, a_im, bu_re, bu_im), axis=1)
    y = jnp.einsum('gcp,bsgp->bsgc', c_re, xs_re) - jnp.einsum('gcp,bsgp->bsgc', c_im, xs_im)
    y = y.reshape(bsz, seq, S5_W) + d_skip * u
    y = jax.nn.gelu(y)
    y = y * jax.nn.sigmoid(y @ w_glu)
    return y * jax.nn.silu(g)


def memory_branch(c, mem_kv):
    bsz, seq, _ = c.shape
    q, g = _split(c, XATT_SIZES)
    q = q.reshape(bsz, seq, XATT_HEADS, HEAD_DIM).astype(jnp.float32)
    km, vm = _split(mem_kv.astype(jnp.float32), (XATT_W, XATT_W))
    km = km.reshape(bsz, -1, XATT_HEADS, HEAD_DIM)
    vm = vm.reshape(bsz, -1, XATT_HEADS, HEAD_DIM)
    p = jax.nn.softmax(jnp.einsum('bshd,bmhd->bhsm', q, km) * HEAD_DIM ** -0.5, -1)
    o = jnp.einsum('bhsm,bmhd->bshd', p, vm).reshape(bsz, seq, XATT_W)
    return o * jax.nn.silu(g.astype(jnp.float32))


def setup_inputs(seed: int = 0) -> dict:
    key = jax.random.key(seed)
    ks = jax.random.split(key, 32)
    L, D = DEPTH, D_MODEL
    nrm = lambda k, shape, s: jax.random.normal(k, shape, jnp.float32) * s
    offsets = jax.random.randint(ks[2], (BATCH,), 0, SEQ, dtype=jnp.int32)
    return {
        'x': nrm(ks[0], (BATCH, SEQ, D), 1.0),
        'mem': nrm(ks[1], (BATCH, N_MEM, D), 1.0),
        'positions': offsets[:, None] + jnp.arange(SEQ, dtype=jnp.int32)[None, :],
        'norm_pre': 1.0 + nrm(ks[3], (L, D), 0.05),
        'norm_post': 1.0 + nrm(ks[4], (L, D), 0.05),
        'norm_mem': 1.0 + nrm(ks[5], (L, D), 0.05),
        'w_in': nrm(ks[6], (L, D, N_IN), D ** -0.5),
        'rwkv_mu': jax.random.uniform(ks[7], (L, RWKV_COLS), jnp.float32),
        'rwkv_w0': jax.random.uniform(ks[8], (L, RWKV_W), jnp.float32, -2.0, 1.0),
        'rwkv_w2': nrm(ks[9], (L, RWKV_W_LORA, RWKV_W), 0.5 * RWKV_W_LORA ** -0.5),
        'rwkv_a0': nrm(ks[10], (L, RWKV_W), 0.3),
        'rwkv_a2': nrm(ks[11], (L, RWKV_A_LORA, RWKV_W), 0.5 * RWKV_A_LORA ** -0.5),
        'rwkv_k_k': 0.85 + nrm(ks[12], (L, RWKV_W), 0.05),
        'rwkv_k_a': 1.0 + nrm(ks[13], (L, RWKV_W), 0.05),
        'rwkv_r_k': nrm(ks[14], (L, RWKV_HEADS, HEAD_DIM), 0.3),
        'rwkv_lnx_w': 1.0 + nrm(ks[15], (L, RWKV_W), 0.05),
        'rwkv_lnx_b': nrm(ks[16], (L, RWKV_W), 0.02),
        'ret_gn_w': 1.0 + nrm(ks[17], (L, RET_W), 0.05),
        's5_lam_re': -0.5 + nrm(ks[18], (L, S5_GROUPS, S5_STATE), 0.01),
        's5_lam_im': math.pi * jnp.arange(S5_STATE, dtype=jnp.float32)[None, None, :] + nrm(ks[19], (L, S5_GROUPS, S5_STATE), 0.01),
        's5_log_dt': jax.random.uniform(ks[20], (L, S5_GROUPS), jnp.float32, math.log(1e-3), math.log(1e-1)),
        's5_b_re': nrm(ks[21], (L, S5_GROUPS, S5_STATE, S5_GROUP), (2 * S5_GROUP) ** -0.5),
        's5_b_im': nrm(ks[22], (L, S5_GROUPS, S5_STATE, S5_GROUP), (2 * S5_GROUP) ** -0.5),
        's5_c_re': nrm(ks[23], (L, S5_GROUPS, S5_GROUP, S5_STATE), S5_STATE ** -0.5),
        's5_c_im': nrm(ks[24], (L, S5_GROUPS, S5_GROUP, S5_STATE), S5_STATE ** -0.5),
        's5_d': nrm(ks[25], (L, S5_W), 0.5),
        's5_w_glu': nrm(ks[26], (L, S5_W, S5_W), S5_W ** -0.5),
        'w_mem_kv': nrm(ks[27], (L, D, 2 * XATT_W), D ** -0.5),
        'w_branch': nrm(ks[28], (L, N_BRANCH, MIX_W, D), MIX_W ** -0.5),
        'w_out': nrm(ks[29], (L, D, D), D ** -0.5),
    }


def reference(x, mem, positions, norm_pre, norm_post, norm_mem, w_in, rwkv_mu, rwkv_w0, rwkv_w2, rwkv_a0,
              rwkv_a2, rwkv_k_k, rwkv_k_a, rwkv_r_k, rwkv_lnx_w, rwkv_lnx_b, ret_gn_w, s5_lam_re, s5_lam_im,
              s5_log_dt, s5_b_re, s5_b_im, s5_c_re, s5_c_im, s5_d, s5_w_glu, w_mem_kv, w_branch, w_out):
    bsz, seq, _ = x.shape
    for l in range(DEPTH):
        h = rms_norm(x, norm_pre[l])
        cols = jnp.einsum('bsd,dn->bsn', h, w_in[l])
        c_rwkv, c_dsa, c_ret, c_s5, c_x, c_gate = _split(cols, IN_SIZES)
        mem_kv = jnp.einsum('bmd,dn->bmn', rms_norm(mem, norm_mem[l]), w_mem_kv[l])
        ys = (
            rwkv7_branch(c_rwkv, rwkv_mu[l], rwkv_w0[l], rwkv_w2[l], rwkv_a0[l], rwkv_a2[l], rwkv_k_k[l],
                         rwkv_k_a[l], rwkv_r_k[l], rwkv_lnx_w[l], rwkv_lnx_b[l]),
            dsa_branch(c_dsa, positions),
            retention_branch(c_ret, positions, ret_gn_w[l]),
            s5_branch(c_s5, s5_lam_re[l], s5_lam_im[l], s5_log_dt[l], s5_b_re[l], s5_b_im[l], s5_c_re[l],
                      s5_c_im[l], s5_d[l], s5_w_glu[l]),
            memory_branch(c_x, mem_kv),
        )
        gates = jax.nn.sigmoid(c_gate.astype(jnp.float32)).reshape(bsz, seq, N_BRANCH, D_MODEL)
        merged = None
        for i in range(N_BRANCH):
            term = gates[:, :, i] * jnp.einsum('bsw,wd->bsd', ys[i], w_branch[l, i])
            merged = term if merged is None else merged + term
        out = jnp.einsum('bsd,de->bse', merged, w_out[l])
        x = x + rms_norm(out, norm_post[l]).astype(x.dtype)
    return x
```

```python
from contextlib import ExitStack
import math
import numpy as np
import ml_dtypes
import concourse.bass as bass
import concourse.mybir as mybir
from concourse.bass_utils import run_bass_kernel_spmd

F32 = mybir.dt.float32
BF16 = mybir.dt.bfloat16
I32 = mybir.dt.int32
AF = mybir.ActivationFunctionType
ALU = mybir.AluOpType
AX = mybir.AxisListType

ENGS = ['sp', 'act', 'dve', 'pool', 'pe']
EPOCH = 16000
NDMASEM = 24
SB_BASE = 16640
SBUF_BYTES = 229000


class Buf:
    __slots__ = ('name', 'wev', 'rev', 'tracked')

    def __init__(self, name, tracked=True):
        self.name = name
        self.wev = {}
        self.rev = {}
        self.tracked = tracked


class V:
    __slots__ = ('buf', 'ap')

    def __init__(self, buf, ap):
        self.buf = buf
        self.ap = ap

    def __getitem__(self, k):
        return V(self.buf, self.ap[k])

    def m(self, fn):
        return V(self.buf, fn(self.ap))

    def r(self, s, **kw):
        return V(self.buf, self.ap.rearrange(s, **kw))

    def bc(self, shape):
        return V(self.buf, self.ap.to_broadcast(list(shape)))

    def bitcast(self, dt):
        return V(self.buf, self.ap.bitcast(dt))

    @property
    def shape(self):
        return tuple(self.ap.shape)


def _ap(x):
    return x.ap if isinstance(x, V) else x


class Prog:
    def __init__(self, nc):
        self.nc = nc
        self.q = {e: [] for e in ENGS}
        self.cnt = {e: 0 for e in ENGS}
        self.noinc = {e: False for e in ENGS}
        self.known = {e: {} for e in ENGS}
        self.dma_n = {e: 0 for e in ENGS}
        self.nbar = 0
        self.bufs = []
        self.sb_off = SB_BASE
        self.sb_id = 0
        self.sb_mark = 0
        self.psum = []
        for i in range(8):
            h = nc.alloc_psum_tensor(f"ps{i}", [128, 512], F32)
            self.psum.append(V(self._newbuf(f"ps{i}"), h[:]))
        self.ps_rr = 0

    def _newbuf(self, name, tracked=True):
        b = Buf(name, tracked)
        if tracked:
            self.bufs.append(b)
        return b

    def sb(self, shape, dtype=F32, name="t"):
        esz = {F32: 4, BF16: 2, I32: 4}[dtype]
        per = esz * int(np.prod(shape[1:]))
        per = (per + 63) // 64 * 64
        off = self.sb_off
        assert off + per <= SBUF_BYTES, f"SBUF overflow {name} {off}+{per}"
        self.sb_off += per
        self.sb_id += 1
        nm = f"{name}_{self.sb_id}"
        h = self.nc.alloc_sbuf_tensor_at(nm, list(shape), dtype, offset=off)
        return V(self._newbuf(nm), h[:])

    def mark(self):
        self.sb_mark = self.sb_off

    def release(self):
        self.sb_off = self.sb_mark

    def ps(self):
        v = self.psum[self.ps_rr % 7]
        self.ps_rr += 1
        return v

    def ps_acc(self):
        return self.psum[7]

    def dram(self, name, shape, dtype=F32, kind="Internal"):
        h = self.nc.dram_tensor(name, list(shape), dtype, kind=kind)
        return V(self._newbuf(name, tracked=False), h.ap())

    def _collect(self, eng, reads, writes, extra=None):
        waits = {}

        def need(evs, skip_own):
            for sk, v in evs.items():
                if skip_own and sk[0] == eng:
                    continue
                if waits.get(sk, 0) < v:
                    waits[sk] = v
        for x in reads:
            if x.buf.tracked:
                need(x.buf.wev, False)
        for x in writes:
            if x.buf.tracked:
                need(x.buf.wev, True)
                need(x.buf.rev, True)
        if extra:
            need(extra, False)
        kn = self.known[eng]
        wl = []
        for sk, v in waits.items():
            if kn.get(sk, 0) < v:
                kn[sk] = v
                wl.append((sk, v))
        return wl

    def emit(self, eng, fn, reads=(), writes=(), inc=True):
        reads = [x for x in reads if isinstance(x, V)]
        writes = [x for x in writes if isinstance(x, V)]
        wl = self._collect(eng, reads, writes)
        idx = self.cnt[eng] + 1
        if inc:
            self.cnt[eng] = idx
            self.noinc[eng] = False
        else:
            self.noinc[eng] = True
        sk = (eng, (idx - 1) // EPOCH)
        val = (idx - 1) % EPOCH + 1
        self.q[eng].append((wl, fn, (sk, 1) if inc else None))
        for x in reads:
            b = x.buf
            if b.tracked and b.rev.get(sk, 0) < val:
                b.rev[sk] = val
        for x in writes:
            b = x.buf
            if b.tracked:
                b.wev = {sk: val}
                b.rev = {}

    def dma(self, out, in_, eng='sp'):
        n = self.dma_n[eng]
        self.dma_n[eng] = n + 1
        slot, k = n % NDMASEM, n // NDMASEM
        sk = ('dma', eng, slot)
        val = 16 * (k + 1)
        extra = {sk: 16 * k} if k > 0 else None
        wl = self._collect(eng, [in_], [out], extra)
        oa, ia = out.ap, in_.ap
        self.q[eng].append((wl, lambda e: e.dma_start(out=oa, in_=ia), (sk, 16)))
        b = in_.buf
        if b.tracked:
            b.rev[sk] = val
        b = out.buf
        if b.tracked:
            b.wev = {sk: val}
            b.rev = {}

    def barrier(self):
        for e in ENGS:
            assert not self.noinc[e], f"dangling no-inc instruction on {e}"
        waits = {}
        for e in ENGS:
            idx = self.cnt[e]
            if idx > 0:
                waits[(e, (idx - 1) // EPOCH)] = (idx - 1) % EPOCH + 1
            n = self.dma_n[e]
            for slot in range(min(n, NDMASEM)):
                k = (n - 1 - slot) // NDMASEM
                waits[('dma', e, slot)] = 16 * (k + 1)
        kn = self.known['sp']
        wl = []
        for sk, v in waits.items():
            if sk[0] == 'sp':
                continue
            if kn.get(sk, 0) < v:
                kn[sk] = v
                wl.append((sk, v))
        self.nbar += 1
        nb = self.nbar
        bk = ('bar', 0)
        self.q['sp'].append((wl, 'seminc', (bk, 1)))
        for e in ENGS:
            if e != 'sp':
                self.q[e].append(([(bk, nb)], None, None))
                for sk, v in waits.items():
                    if self.known[e].get(sk, 0) < v:
                        self.known[e][sk] = v
        for b in self.bufs:
            b.wev = {}
            b.rev = {}

    def finish(self):
        self.barrier()
        nc = self.nc
        keys = set()
        for e in ENGS:
            for wl, fn, inc in self.q[e]:
                for sk, v in wl:
                    keys.add(sk)
                if inc is not None:
                    keys.add(inc[0])
        stack = ExitStack()
        sems = {}
        for i, sk in enumerate(sorted(keys, key=str)):
            sems[sk] = stack.enter_context(nc.semaphore(f"s{i}"))
        self.nsem = len(sems)
        q = self.q

        def mk(en):
            def body(e):
                for wl, fn, inc in q[en]:
                    for sk, v in wl:
                        e.wait_ge(sems[sk], v)
                    if fn is None:
                        continue
                    if fn == 'seminc':
                        e.sem_inc(sems[inc[0]], inc[1])
                        continue
                    ins = fn(e)
                    if inc is not None:
                        ins.then_inc(sems[inc[0]], inc[1])
            return body
        with stack:
            with nc.Block() as block:
                block.sync(mk('sp'))
                block.scalar(mk('act'))
                block.vector(mk('dve'))
                block.gpsimd(mk('pool'))
                block.tensor(mk('pe'))

    def act(self, out, in_, func, bias=None, scale=1.0, accum=None):
        o, i, b, s, a = _ap(out), _ap(in_), _ap(bias), _ap(scale), _ap(accum)
        kw = {}
        if b is not None:
            kw['bias'] = b
        if a is not None:
            kw['accum_out'] = a
        self.emit('act', lambda e: e.activation(out=o, in_=i, func=func, scale=s, **kw),
                  [in_, bias, scale], [out, accum])

    def ts(self, out, in0, s1, op0, s2=None, op1=None, accum=None, eng='dve'):
        o, i, a1, a2, ac = _ap(out), _ap(in0), _ap(s1), _ap(s2), _ap(accum)
        kw = {}
        if op1 is not None:
            kw['op1'] = op1
        if ac is not None:
            kw['accum_out'] = ac
        self.emit(eng, lambda e: e.tensor_scalar(out=o, in0=i, scalar1=a1, scalar2=a2, op0=op0, **kw),
                  [in0, s1, s2], [out, accum])

    def tt(self, out, in0, in1, op, eng='dve'):
        o, a, b = _ap(out), _ap(in0), _ap(in1)
        self.emit(eng, lambda e: e.tensor_tensor(out=o, in0=a, in1=b, op=op), [in0, in1], [out])

    def stt(self, out, in0, scalar, in1, op0, op1, eng='dve'):
        o, a, s, b = _ap(out), _ap(in0), _ap(scalar), _ap(in1)
        self.emit(eng, lambda e: e.scalar_tensor_tensor(out=o, in0=a, scalar=s, in1=b, op0=op0, op1=op1),
                  [in0, scalar, in1], [out])

    def copy(self, out, in_, eng='dve'):
        o, i = _ap(out), _ap(in_)
        if eng == 'act':
            self.emit('act', lambda e: e.copy(out=o, in_=i), [in_], [out])
        else:
            self.emit(eng, lambda e: e.tensor_copy(out=o, in_=i), [in_], [out])

    def memset(self, out, val, eng='dve'):
        o = _ap(out)
        self.emit(eng, lambda e: e.memset(o, val), [], [out])

    def recip(self, out, in_):
        o, i = _ap(out), _ap(in_)
        self.emit('dve', lambda e: e.reciprocal(out=o, in_=i), [in_], [out])

    def reduce(self, out, in_, op, axis=AX.X):
        o, i = _ap(out), _ap(in_)
        self.emit('dve', lambda e: e.tensor_reduce(out=o, in_=i, axis=axis, op=op), [in_], [out])

    def scan(self, out, d0, d1, initial, op0, op1):
        o, a, b, ini = _ap(out), _ap(d0), _ap(d1), _ap(initial)
        self.emit('dve', lambda e: e.tensor_tensor_scan(out=o, data0=a, data1=b, initial=ini, op0=op0, op1=op1),
                  [d0, d1, initial], [out])

    def mm(self, out, lhsT, rhs, start=True, stop=True, inc=None):
        o, l, r = _ap(out), _ap(lhsT), _ap(rhs)
        if inc is None:
            inc = stop
        self.emit('pe', lambda e: e.matmul(o, l, r, start=start, stop=stop), [lhsT, rhs], [out], inc=inc)

    def transpose(self, out, in_, ident):
        o, i, d = _ap(out), _ap(in_), _ap(ident)
        self.emit('pe', lambda e: e.transpose(o, i, d), [in_, ident], [out])


S = 4096
D = 1024
TT = 512
NTT = S // TT
NP_ROWS = 5376
NT_COLS = 516
B_RWKV, B_DSA, B_RET, B_S5, B_X, B_GATE = 0, 1152, 2500, 3524, 4036, 4548
C_RWKV = 0
C_DQ, C_DQS, C_DK, C_DKS, C_IQ, C_IQS, C_IK, C_DG = 9, 11, 13, 15, 17, 19, 21, 22
C_RQ, C_RQS, C_RK, C_RKS, C_RG = 24, 26, 28, 30, 32
C_SU, C_SG = 34, 36
C_XQ, C_XG = 38, 40


def _swap_idx(base, nheads):
    idx = []
    for h in range(nheads):
        for j in range(64):
            idx.append(base + h * 64 + (j + 32) % 64)
    return idx


def proj_col_indices():
    r = lambda a, n: list(range(a, a + n))
    f = []
    f += r(B_RWKV, 1152)
    f += r(B_DSA, 256) + _swap_idx(B_DSA, 4)
    f += r(B_DSA + 256, 256) + _swap_idx(B_DSA + 256, 4)
    f += r(B_DSA + 768, 256) + _swap_idx(B_DSA + 768, 4)
    f += r(B_DSA + 1024, 64) + _swap_idx(B_DSA + 1024, 1)
    f += r(B_DSA + 1092, 256)
    f += r(B_RET, 256) + _swap_idx(B_RET, 4)
    f += r(B_RET + 256, 256) + _swap_idx(B_RET + 256, 4)
    f += r(B_RET + 768, 256)
    f += r(B_S5, 512)
    f += r(B_X, 512)
    assert len(f) == NP_ROWS
    t = r(B_DSA + 512, 256) + r(B_RET + 512, 256) + r(B_DSA + 1088, 4)
    assert len(t) == NT_COLS
    return np.array(f), np.array(t)


def load_weight_bf16(P, dst, src_dram, gcol, nk, ncols, blk=1344):
    src = src_dram.r("(k p) n -> p k n", p=128)
    stg = [P.sb([128, blk], F32, "wstg") for _ in range(2)]
    i = 0
    for k in range(nk):
        for c0 in range(0, ncols, blk):
            c1 = min(ncols, c0 + blk)
            s = stg[i % 2]
            P.dma(s[:, 0:c1 - c0], src[:, k, c0:c1])
            eng = 'dve' if i % 2 == 0 else 'pool'
            if gcol is not None:
                P.ts(dst[:, k, c0:c1], s[:, 0:c1 - c0], gcol[:, k:k + 1], ALU.mult, eng=eng)
            else:
                P.copy(dst[:, k, c0:c1], s[:, 0:c1 - c0], eng=eng)
            i += 1


def rsqrt_ps(P, out, src, scale, eps):
    P.ts(out, src, scale, ALU.mult, eps, ALU.add)
    P.act(out, out, AF.Sqrt)
    P.recip(out, out)


def rms_tile(P, xt, hT, sq, rstd, ones, nk, n, width):
    P.act(sq, xt, AF.Square)
    ps = P.ps()
    for k in range(nk):
        P.mm(ps[:, 0:width], ones, sq[:, k, :], start=(k == 0), stop=(k == nk - 1))
    rsqrt_ps(P, rstd, ps[:, 0:width], 1.0 / n, 1e-6)
    for k in range(nk):
        P.tt(hT[:, k, :], xt[:, k, :], rstd, ALU.mult)


def stage_P(P, l, Dm, xT):
    P.sb_off = SB_BASE
    npre = P.sb([128, 8], F32, "npre")
    P.dma(npre, Dm[f'npre{l}'])
    ones = P.sb([128, 128], F32, "ones")
    P.memset(ones, 1.0)
    wp = P.sb([128, 8, NP_ROWS], BF16, "wp")
    wt = P.sb([128, 8, NT_COLS], BF16, "wt")
    wf = P.sb([128, 8, 644], F32, "wf")
    wfsrc = Dm[f'wpf{l}'].r("(k p) n -> p k n", p=128)
    for k in range(8):
        P.dma(wf[:, k, :], wfsrc[:, k, :])
    for k in range(8):
        P.ts(wf[:, k, :], wf[:, k, :], npre[:, k:k + 1], ALU.mult, eng=('dve' if k % 2 else 'pool'))
    m0 = P.sb_off
    load_weight_bf16(P, wp, Dm[f'wp{l}'], npre, 8, NP_ROWS)
    load_weight_bf16(P, wt, Dm[f'wt{l}'], npre, 8, NT_COLS, blk=NT_COLS)
    P.barrier()
    P.sb_off = m0
    xts = [P.sb([128, 8, TT], F32, "xt") for _ in range(2)]
    sq = P.sb([128, 8, TT], F32, "sq")
    hTs = [P.sb([128, 8, TT], BF16, "hT") for _ in range(2)]
    rstd = P.sb([128, TT], F32, "rstd")
    ostg = [P.sb([128, 4, TT], F32, "ostg") for _ in range(2)]
    tstg = [P.sb([128, NT_COLS], F32, "tstg") for _ in range(2)]
    xsrc = xT.r("(k p) t -> p k t", p=128)
    cdst = Dm['colsT'].r("(c p) t -> p c t", p=128)
    ctok = Dm['colsTok']
    for tt in range(NTT):
        t0 = tt * TT
        xt, hT = xts[tt % 2], hTs[tt % 2]
        P.dma(xt, xsrc[:, :, t0:t0 + TT])
        rms_tile(P, xt, hT, sq, rstd, ones, 8, D, TT)
        for k in range(8):
            P.tt(sq[:, k, :], xt[:, k, :], rstd, ALU.mult, eng='pool')
        for c in range(42):
            ps = P.ps()
            for k in range(8):
                if C_IQ <= c <= C_IK:
                    P.mm(ps, wf[:, k, (c - C_IQ) * 128:(c - C_IQ + 1) * 128], sq[:, k, :], start=(k == 0), stop=(k == 7))
                else:
                    P.mm(ps, wp[:, k, c * 128:(c + 1) * 128], hT[:, k, :], start=(k == 0), stop=(k == 7))
            stg = ostg[(c // 4) % 2]
            if c % 3 == 2:
                P.copy(stg[:, c % 4, :], ps, eng='dve')
            else:
                P.act(stg[:, c % 4, :], ps, AF.Copy)
            if c % 4 == 3 or c == 41:
                c0 = c - c % 4
                P.dma(cdst[:, c0:c + 1, t0:t0 + TT], stg[:, 0:c % 4 + 1, :])
        for s in range(4):
            ps = P.ps()
            ps2 = P.ps()
            for k in range(8):
                P.mm(ps, hT[:, k, s * 128:(s + 1) * 128], wt[:, k, 0:512], start=(k == 0), stop=(k == 7))
            for k in range(8):
                P.mm(ps2[:, 0:4], sq[:, k, s * 128:(s + 1) * 128], wf[:, k, 640:644], start=(k == 0), stop=(k == 7))
            ts_ = tstg[s % 2]
            P.act(ts_[:, 0:512], ps, AF.Copy)
            P.copy(ts_[:, 512:516], ps2[:, 0:4], eng='dve')
            P.dma(ctok[t0 + s * 128:t0 + (s + 1) * 128, :], ts_)
    P.barrier()


BR_NAMES = ['rwkv', 'dsa', 'ret', 's5', 'xatt']


def stage_M(P, l, Dm, xT, xT_out):
    P.sb_off = SB_BASE
    npre = P.sb([128, 8], F32, "npre")
    npost = P.sb([128, 8], F32, "npost")
    P.dma(npre, Dm[f'npre{l}'])
    P.dma(npost, Dm[f'npost{l}'])
    ones = P.sb([128, 128], F32, "ones")
    P.memset(ones, 1.0)
    wg = P.sb([128, 8, 5120], BF16, "wg")
    wbr = P.sb([128, 10, 1024], BF16, "wbr")
    wout = P.sb([128, 8, 1024], BF16, "wout")
    m0 = P.sb_off
    load_weight_bf16(P, wg, Dm[f'wg{l}'], npre, 8, 5120, blk=1280)
    load_weight_bf16(P, wbr, Dm[f'wbr{l}'], None, 10, 1024, blk=1024)
    load_weight_bf16(P, wout, Dm[f'wout{l}'], None, 8, 1024, blk=1024)
    P.barrier()
    P.sb_off = m0
    xt = P.sb([128, 8, TT], F32, "xt")
    sq = P.sb([128, 8, TT], F32, "sq")
    hT = P.sb([128, 8, TT], BF16, "hT")
    rstd = P.sb([128, TT], F32, "rstd")
    yts = [P.sb([128, 2, TT], BF16, f"y{i}") for i in range(5)]
    sg = [P.sb([128, TT], F32, "sg") for _ in range(2)]
    term = [P.sb([128, TT], F32, "term") for _ in range(2)]
    macc = P.sb([128, TT], F32, "macc")
    mT = P.sb([128, 8, TT], BF16, "mT")
    osb = sq
    osq = P.sb([128, TT], F32, "osq")
    xsrc = xT.r("(k p) t -> p k t", p=128)
    xdst = xT_out.r("(k p) t -> p k t", p=128)
    for tt in range(NTT):
        t0 = tt * TT
        P.dma(xt, xsrc[:, :, t0:t0 + TT])
        for i in range(5):
            P.dma(yts[i], Dm[f'yT_{BR_NAMES[i]}'].r("(c p) t -> p c t", p=128)[:, :, t0:t0 + TT])
        rms_tile(P, xt, hT, sq, rstd, ones, 8, D, TT)
        j = 0
        for dc in range(8):
            for i in range(5):
                psg = P.ps()
                for k in range(8):
                    P.mm(psg, wg[:, k, i * 1024 + dc * 128:i * 1024 + (dc + 1) * 128], hT[:, k, :],
                         start=(k == 0), stop=(k == 7))
                psb = P.ps()
                for kk in range(2):
                    P.mm(psb, wbr[:, i * 2 + kk, dc * 128:(dc + 1) * 128], yts[i][:, kk, :],
                         start=(kk == 0), stop=(kk == 1))
                s_, t_ = sg[j % 2], term[j % 2]
                j += 1
                P.act(s_, psg, AF.Sigmoid)
                if i == 0:
                    P.tt(macc, s_, psb, ALU.mult)
                elif i < 4:
                    P.tt(t_, s_, psb, ALU.mult)
                    P.tt(macc, macc, t_, ALU.add, eng='pool')
                else:
                    P.tt(t_, s_, psb, ALU.mult)
                    P.tt(mT[:, dc, :], macc, t_, ALU.add, eng='pool')
        pss = P.ps_acc()
        for ec in range(8):
            ps = P.ps()
            for k in range(8):
                P.mm(ps, wout[:, k, ec * 128:(ec + 1) * 128], mT[:, k, :], start=(k == 0), stop=(k == 7))
            P.act(osb[:, ec, :], ps, AF.Copy)
            P.act(osq, ps, AF.Square)
            P.mm(pss, ones, osq, start=(ec == 0), stop=(ec == 7))
        rsqrt_ps(P, rstd, pss, 1.0 / D, 1e-6)
        for ec in range(8):
            P.stt(osb[:, ec, :], osb[:, ec, :], npost[:, ec:ec + 1], rstd, ALU.mult, ALU.mult)
            P.tt(xt[:, ec, :], xt[:, ec, :], osb[:, ec, :], ALU.add, eng='pool')
        P.dma(xdst[:, :, t0:t0 + TT], xt)
    P.barrier()


def stage_X(P, l, Dm):
    P.sb_off = SB_BASE
    nmem = P.sb([128, 8], F32, "nmem")
    P.dma(nmem, Dm[f'nmem{l}'])
    ones = P.sb([128, 128], F32, "ones")
    P.memset(ones, 1.0)
    wm = P.sb([128, 8, 512], BF16, "wm")
    m0 = P.sb_off
    load_weight_bf16(P, wm, Dm[f'wmem{l}'], nmem, 8, 512, blk=512)
    P.barrier()
    P.sb_off = m0
    mt = P.sb([128, 8, 256], F32, "mt")
    msq = P.sb([128, 8, 256], F32, "msq")
    mh = P.sb([128, 8, 256], BF16, "mh")
    mr = P.sb([128, 256], F32, "mr")
    P.dma(mt, Dm['memT'].r("(k p) m -> p k m", p=128))
    rms_tile(P, mt, mh, msq, mr, ones, 8, D, 256)
    kmT = [P.sb([128, 256], BF16, "kmT") for _ in range(2)]
    for c in range(2):
        ps = P.ps()
        for k in range(8):
            P.mm(ps[:, 0:256], wm[:, k, c * 128:(c + 1) * 128], mh[:, k, :], start=(k == 0), stop=(k == 7))
        P.copy(kmT[c], ps[:, 0:256])
    vpad = [[P.sb([128, 128], BF16, "vpad") for _ in range(4)] for _ in range(2)]
    opad = [P.sb([128, 128], BF16, "opad") for _ in range(2)]
    for hh in range(2):
        P.memset(opad[hh], 0.0)
        P.memset(opad[hh][:, hh * 64:(hh + 1) * 64], 1.0)
    for mc in range(2):
        ps = P.ps()
        for k in range(8):
            P.mm(ps[:, 0:256], mh[:, k, mc * 128:(mc + 1) * 128], wm[:, k, 256:512], start=(k == 0), stop=(k == 7))
        for h in range(4):
            hh = h % 2
            P.memset(vpad[mc][h], 0.0)
            P.copy(vpad[mc][h][:, hh * 64:(hh + 1) * 64], ps[:, h * 64:(h + 1) * 64])
    qf = P.sb([128, 2, TT], F32, "qf")
    gf = P.sb([128, 2, TT], F32, "gf")
    qb = P.sb([128, 2, TT], BF16, "qb")
    E = [[P.sb([128, TT], BF16, "E") for _ in range(2)] for _ in range(2)]
    rs = P.sb([128, TT], F32, "rs")
    o = P.sb([128, TT], F32, "o")
    sgl = P.sb([128, TT], F32, "sgl")
    yst = P.sb([128, 2, TT], BF16, "yst")
    csrc = Dm['colsT'].r("(c p) t -> p c t", p=128)
    ydst = Dm['yT_xatt'].r("(c p) t -> p c t", p=128)
    for tt in range(NTT):
        t0 = tt * TT
        P.dma(qf, csrc[:, C_XQ:C_XQ + 2, t0:t0 + TT])
        P.dma(gf, csrc[:, C_XG:C_XG + 2, t0:t0 + TT])
        P.copy(qb, qf)
        for p in range(2):
            for hh in range(2):
                for mc in range(2):
                    ps = P.ps()
                    P.mm(ps, kmT[p][hh * 64:(hh + 1) * 64, mc * 128:(mc + 1) * 128],
                         qb[hh * 64:(hh + 1) * 64, p, :])
                    P.act(E[hh][mc], ps, AF.Exp, scale=0.125)
            pso = P.ps()
            pss = P.ps()
            n = 0
            for hh in range(2):
                for mc in range(2):
                    P.mm(pso, vpad[mc][2 * p + hh], E[hh][mc], start=(n == 0), stop=(n == 3))
                    n += 1
            n = 0
            for hh in range(2):
                for mc in range(2):
                    P.mm(pss, opad[hh], E[hh][mc], start=(n == 0), stop=(n == 3))
                    n += 1
            P.recip(rs, pss)
            P.tt(o, pso, rs, ALU.mult)
            P.act(sgl, gf[:, p, :], AF.Silu)
            P.tt(yst[:, p, :], o, sgl, ALU.mult)
        P.dma(ydst[:, :, t0:t0 + TT], yst)
    P.barrier()


def dram_specs():
    sp = {
        'xT': ([D, S], F32, 'in'), 'memT': ([D, 256], F32, 'in'), 'pos': ([1, S], I32, 'in'),
        'colsT': ([NP_ROWS, S], F32, 'scratch'), 'colsTok': ([S, NT_COLS], F32, 'scratch'),
        'xT1': ([D, S], F32, 'scratch'),
    }
    for n in BR_NAMES:
        sp[f'yT_{n}'] = ([256, S], BF16, 'scratch')
    sp['iqR'] = ([256, S], F32, 'scratch')
    sp['ropeC'] = ([64, S], F32, 'scratch')
    sp['ropeS'] = ([64, S], F32, 'scratch')
    sp['ropeconst'] = ([64, 2], F32, 'in')
    sp['ident'] = ([128, 128], F32, 'in')
    sp['ret_idT'] = ([128, 4, 128], F32, 'in')
    sp['ret_qd'] = ([64, 4, 128], F32, 'in')
    sp['ret_kd'] = ([128, 4], F32, 'in')
    sp['ret_cd'] = ([64, 256], F32, 'in')
    sp['s5mask'] = ([128, 8, 8], F32, 'in')
    sp['rw_masks'] = ([64, 3, 64], F32, 'in')
    sp['dsa_cb'] = ([128, 128], F32, 'in')
    for l in range(2):
        sp[f'rwprm{l}'] = ([64, 8, 4], F32, 'in')
        sp[f'rwmu{l}'] = ([64, 18], F32, 'in')
        sp[f'rww2{l}'] = ([64, 256], F32, 'in')
        sp[f'rwa2{l}'] = ([64, 256], F32, 'in')
    sp['s5tau'] = ([128, 512], F32, 'in')
    for l in range(2):
        sp[f'retgn{l}'] = ([64, 4], F32, 'in')
        sp[f's5lam{l}'] = ([128, 8, 3], F32, 'in')
        sp[f's5b{l}'] = ([128, 8, 2, 16], F32, 'in')
        sp[f's5c{l}'] = ([128, 8, 2, 16], F32, 'in')
        sp[f's5d{l}'] = ([128, 2], F32, 'in')
        sp[f's5wglu{l}'] = ([256, 256], F32, 'in')
    for l in range(2):
        sp[f'wp{l}'] = ([D, NP_ROWS], F32, 'in')
        sp[f'wt{l}'] = ([D, NT_COLS], F32, 'in')
        sp[f'wpf{l}'] = ([D, 644], F32, 'in')
        sp[f'wg{l}'] = ([D, 5120], F32, 'in')
        sp[f'wbr{l}'] = ([1280, D], F32, 'in')
        sp[f'wout{l}'] = ([D, D], F32, 'in')
        sp[f'wmem{l}'] = ([D, 512], F32, 'in')
        for n in ['npre', 'npost', 'nmem']:
            sp[f'{n}{l}'] = ([128, 8], F32, 'in')
    return sp


def host_inputs(inputs, b):
    f_idx, t_idx = proj_col_indices()
    d = {}
    d['xT'] = np.ascontiguousarray(inputs['x'][b].T)
    d['memT'] = np.ascontiguousarray(inputs['mem'][b].T)
    d['pos'] = np.ascontiguousarray(inputs['positions'][b][None, :]).astype(np.int32)
    pk = lambda v: np.ascontiguousarray(v.reshape(8, 128).T)
    jj = np.arange(64)
    inv = (10000.0 ** (-(np.arange(32, dtype=np.float32)) / 32)).astype(np.float32)
    d['ropeconst'] = np.stack([inv[jj % 32], np.where(jj < 32, -1.0, 1.0)], 1).astype(np.float32)
    d['ident'] = np.eye(128, dtype=np.float32)
    d['ret_idT'], d['ret_qd'], d['ret_kd'], d['ret_cd'] = ret_consts()
    ii = np.arange(64)
    rm = np.zeros((64, 3, 64), np.float32)
    rm[:, 0, :] = (ii[None, :] > ii[:, None])
    rm[:, 1, :] = (ii[None, :] >= ii[:, None])
    rm[:, 2, :] = (ii[None, :] < ii[:, None])
    d['rw_masks'] = rm
    i128 = np.arange(128)
    d['dsa_cb'] = np.where(i128[None, :] <= i128[:, None], 0.0, -1e30).astype(np.float32)
    for l in range(2):
        hd = lambda v: np.ascontiguousarray(v.reshape(4, 64).T)
        z = np.zeros((64, 4), np.float32)
        d[f'rwprm{l}'] = np.ascontiguousarray(np.stack([hd(inputs['rwkv_w0'][l]), hd(inputs['rwkv_a0'][l]), hd(inputs['rwkv_k_k'][l]),
                                   hd(inputs['rwkv_k_a'][l]), hd(inputs['rwkv_r_k'][l].reshape(256)), hd(inputs['rwkv_lnx_w'][l]),
                                   hd(inputs['rwkv_lnx_b'][l]), z], 1).astype(np.float32))
        d[f'rwmu{l}'] = np.ascontiguousarray(inputs['rwkv_mu'][l].reshape(18, 64).T)
        d[f'rww2{l}'] = np.ascontiguousarray(inputs['rwkv_w2'][l])
        d[f'rwa2{l}'] = np.ascontiguousarray(inputs['rwkv_a2'][l])
    sidx = np.arange(128)
    mk = np.zeros((128, 8, 8), np.float32)
    for j in range(8):
        mk[sidx, j, (2 * j + sidx // 64) % 8] = 1.0
    d['s5mask'] = mk
    d['s5tau'] = np.ascontiguousarray(np.broadcast_to(np.arange(1, 513, dtype=np.float32)[None, :], (128, 512)))
    sj = lambda a: np.ascontiguousarray(a.reshape((8, 128) + a.shape[1:]).swapaxes(0, 1))
    for l in range(2):
        d[f'retgn{l}'] = np.ascontiguousarray(inputs['ret_gn_w'][l].reshape(4, 64).T)
        lam3 = np.stack([inputs['s5_lam_re'][l].reshape(1024), inputs['s5_lam_im'][l].reshape(1024),
                         np.repeat(inputs['s5_log_dt'][l], 64)], 1).astype(np.float32)
        d[f's5lam{l}'] = sj(lam3)
        d[f's5b{l}'] = sj(np.stack([inputs['s5_b_re'][l].reshape(1024, 16), inputs['s5_b_im'][l].reshape(1024, 16)], 1))
        ct = lambda c: np.ascontiguousarray(c.transpose(0, 2, 1)).reshape(1024, 16)
        d[f's5c{l}'] = sj(np.stack([ct(inputs['s5_c_re'][l]), ct(inputs['s5_c_im'][l])], 1))
        d[f's5d{l}'] = np.ascontiguousarray(inputs['s5_d'][l].reshape(2, 128).T)
        d[f's5wglu{l}'] = np.ascontiguousarray(inputs['s5_w_glu'][l])
    for l in range(2):
        w = inputs['w_in'][l]
        d[f'wp{l}'] = np.ascontiguousarray(w[:, f_idx])
        d[f'wt{l}'] = np.ascontiguousarray(w[:, t_idx])
        d[f'wpf{l}'] = np.ascontiguousarray(w[:, np.concatenate([f_idx[C_IQ * 128:(C_IK + 1) * 128], t_idx[512:516]])])
        d[f'wg{l}'] = np.ascontiguousarray(w[:, B_GATE:B_GATE + 5120])
        d[f'wbr{l}'] = np.ascontiguousarray(inputs['w_branch'][l].reshape(1280, D))
        d[f'wout{l}'] = np.ascontiguousarray(inputs['w_out'][l])
        d[f'wmem{l}'] = np.ascontiguousarray(inputs['w_mem_kv'][l])
        d[f'npre{l}'] = pk(inputs['norm_pre'][l])
        d[f'npost{l}'] = pk(inputs['norm_post'][l])
        d[f'nmem{l}'] = pk(inputs['norm_mem'][l])
    return d


STAGE_FNS = {}


def build(plan, ext_in=(), ext_out=()):
    nc = bass.Bass("TRN2", target_bir_lowering=False)
    P = Prog(nc)
    Dm = {}
    used_in = []
    for name, (shape, dtype, role) in dram_specs().items():
        if role == 'in' or name in ext_in:
            kind = "ExternalInput"
            used_in.append(name)
        elif name in ext_out:
            kind = "ExternalOutput"
        else:
            kind = "Internal"
        Dm[name] = P.dram(name, shape, dtype, kind=kind)
    Dm['outT'] = P.dram('outT', [D, S], F32, kind="ExternalOutput")
    for st, l in plan:
        xin = Dm['xT'] if l == 0 else Dm['xT1']
        xout = Dm['xT1'] if l == 0 else Dm['outT']
        if st == 'P':
            stage_P(P, l, Dm, xin)
        elif st == 'M':
            stage_M(P, l, Dm, xin, xout)
        elif st == 'X':
            stage_X(P, l, Dm)
        else:
            STAGE_FNS[st](P, l, Dm)
    P.finish()
    return nc, P, used_in


def sin_reduced(P, out, ang, kq, ki, m1):
    P.ts(kq, ang, 1.0 / (2 * math.pi), ALU.mult)
    P.copy(ki, kq)
    P.copy(kq, ki)
    P.stt(ang, kq, -2 * math.pi, ang, ALU.mult, ALU.add)
    P.ts(m1, ang, math.pi, ALU.is_gt, -2 * math.pi, ALU.mult)
    P.tt(ang, ang, m1, ALU.add)
    P.ts(m1, ang, -math.pi, ALU.is_lt, 2 * math.pi, ALU.mult)
    P.tt(ang, ang, m1, ALU.add)
    P.act(out, ang, AF.Sin)


def stage_R(P, l, Dm):
    P.sb_off = SB_BASE
    W = 2048
    rc = P.sb([64, 2], F32, "rc")
    P.dma(rc, Dm['ropeconst'])
    posi = P.sb([64, W], I32, "posi")
    posf = P.sb([64, W], F32, "posf")
    ang = P.sb([64, W], F32, "ang")
    kq = P.sb([64, W], F32, "kq")
    ki = P.sb([64, W], I32, "ki")
    m1 = P.sb([64, W], F32, "m1")
    o = P.sb([64, W], F32, "o")
    for half in range(S // W):
        sl = slice(half * W, (half + 1) * W)
        P.dma(posi, Dm['pos'][:, sl].m(lambda x: x.to_broadcast([64, W])))
        P.copy(posf, posi)
        P.ts(ang, posf, rc[:, 0:1], ALU.mult)
        sin_reduced(P, o, ang, kq, ki, m1)
        P.ts(o, o, rc[:, 1:2], ALU.mult)
        P.dma(Dm['ropeS'][:, sl], o)
        P.ts(ang, posf, rc[:, 0:1], ALU.mult, math.pi / 2, ALU.add)
        sin_reduced(P, o, ang, kq, ki, m1)
        P.dma(Dm['ropeC'][:, sl], o)
    P.barrier()


def rope_heads(P, dst, Dm, c_base, c_swap, ropeC, ropeS, nheads=4, scale=None, dram_dst=None):
    a = P.sb([64, nheads, TT], F32, "ra")
    b = P.sb([64, nheads, TT], F32, "rb")
    if dram_dst is not None:
        ro = [P.sb([64, nheads, TT], F32, "ro") for _ in range(2)]
    src = Dm['colsT']
    for tt in range(NTT):
        sl = slice(tt * TT, (tt + 1) * TT)
        rb_ = c_base * 128 if isinstance(c_base, int) else c_base[0]
        rs_ = c_swap * 128 if isinstance(c_swap, int) else c_swap[0]
        P.dma(a, src[rb_:rb_ + nheads * 64, sl].r("(h d) t -> d h t", d=64))
        P.dma(b, src[rs_:rs_ + nheads * 64, sl].r("(h d) t -> d h t", d=64))
        cb = ropeC[:, sl].m(lambda x: x.unsqueeze(1).to_broadcast([64, nheads, TT]))
        sb_ = ropeS[:, sl].m(lambda x: x.unsqueeze(1).to_broadcast([64, nheads, TT]))
        P.tt(a, a, cb, ALU.mult)
        P.tt(b, b, sb_, ALU.mult, eng='pool')
        if dram_dst is None:
            P.tt(dst[:, :, sl], a, b, ALU.add)
        else:
            o_ = ro[tt % 2]
            P.tt(o_, a, b, ALU.add)
            P.dma(dram_dst.r("(h d) t -> d h t", d=64)[:, :, sl], o_)


RET_LOGG = [math.log(1.0 - math.exp(v)) for v in np.linspace(math.log(1.0 / 32), math.log(1.0 / 512), 4)]


def ret_consts():
    j = np.arange(128, dtype=np.float64)
    idT = np.zeros((128, 4, 128), np.float32)
    qd = np.zeros((64, 4, 128), np.float32)
    kd = np.zeros((128, 4), np.float32)
    cd = np.zeros((64, 256), np.float32)
    for h in range(4):
        lg = RET_LOGG[h]
        rel = j[None, :] - j[:, None]
        idT[:, h, :] = np.where(rel >= 0, np.exp(lg * np.maximum(rel, 0.0)), 0.0) * 0.125
        qd[:, h, :] = np.exp(lg * (j + 1.0))[None, :]
        kd[:, h] = np.exp(lg * (127.0 - j)) * 0.125
        cd[:, h * 64:(h + 1) * 64] = math.exp(lg * 128)
    return idT, qd, kd, cd


def stage_RET(P, l, Dm):
    P.sb_off = SB_BASE
    ropeC = P.sb([64, S], F32, "ropeC")
    ropeS = P.sb([64, S], F32, "ropeS")
    P.dma(ropeC, Dm['ropeC'])
    P.dma(ropeS, Dm['ropeS'])
    idT = P.sb([128, 4, 128], F32, "idT")
    qd = P.sb([64, 4, 128], F32, "qd")
    kd = P.sb([128, 4], F32, "kd")
    cd = P.sb([64, 256], F32, "cd")
    gn = P.sb([64, 4], F32, "gn")
    identb = P.sb([128, 128], BF16, "identb")
    identf = P.sb([128, 128], F32, "identf")
    ones64 = P.sb([64, 64], F32, "ones64")
    P.dma(idT, Dm['ret_idT'])
    P.dma(qd, Dm['ret_qd'])
    P.dma(kd, Dm['ret_kd'])
    P.dma(cd, Dm['ret_cd'])
    P.dma(gn, Dm[f'retgn{l}'])
    P.dma(identf, Dm['ident'])
    P.copy(identb, identf)
    P.memset(ones64, 1.0 / 64)
    qT = P.sb([64, 4, S], BF16, "qT")
    kT = P.sb([64, 4, S], BF16, "kT")
    qdT = P.sb([64, 4, S], BF16, "qdT")
    m0 = P.sb_off
    rope_heads(P, qT, Dm, C_RQ, C_RQS, ropeC, ropeS)
    rope_heads(P, kT, Dm, C_RK, C_RKS, ropeC, ropeS)
    for c in range(32):
        cs = slice(c * 128, (c + 1) * 128)
        P.tt(qdT[:, :, cs], qT[:, :, cs], qd, ALU.mult, eng=('dve' if c % 2 else 'pool'))
    P.barrier()
    P.sb_off = m0
    Vt = P.sb([128, 32, 256], BF16, "Vt")
    Kd = P.sb([128, 32, 256], BF16, "Kd")
    vst = [P.sb([128, 4, 256], F32, "vst") for _ in range(2)]
    vsrc = Dm['colsTok'].r("(c p) n -> p c n", p=128)
    for i in range(8):
        v_ = vst[i % 2]
        P.dma(v_, vsrc[:, i * 4:(i + 1) * 4, 256:512])
        P.copy(Vt[:, i * 4:(i + 1) * 4, :], v_, eng=('dve' if i % 2 else 'pool'))
    for c in range(32):
        cs = slice(c * 128, (c + 1) * 128)
        ps = P.ps()
        psb = ps.bitcast(BF16)
        for h in range(4):
            P.transpose(psb[:, h * 64:(h + 1) * 64], kT[:, h, cs], identb[0:64, 0:64])
        P.tt(Kd[:, c, :].r("p (h d) -> p h d", h=4), psb[:, 0:256].r("p (h d) -> p h d", h=4),
             kd.m(lambda x: x.unsqueeze(2).to_broadcast([128, 4, 64])), ALU.mult)
    R = P.sb([64, 256], F32, "R")
    Rb = P.sb([64, 256], BF16, "Rb")
    P.memset(R, 0.0)
    P.memset(Rb, 0.0)
    AT = [P.sb([128, 4, 128], BF16, "AT") for _ in range(2)]
    Osb = P.sb([64, 512], F32, "Osb")
    dd = P.sb([64, 512], F32, "dd")
    dsq = P.sb([64, 512], F32, "dsq")
    rstd = P.sb([64, 512], F32, "rstd")
    gt = [P.sb([64, 4, 128], F32, "gt") for _ in range(2)]
    sg = P.sb([64, 4, 128], F32, "sg")
    yo = [P.sb([64, 4, 128], BF16, "yo") for _ in range(2)]
    gsrc = Dm['colsT'][C_RG * 128:C_RG * 128 + 256, :].r("(h d) t -> d h t", d=64)
    ydst = Dm['yT_ret'].r("(h d) t -> d h t", d=64)
    for c in range(32):
        cs = slice(c * 128, (c + 1) * 128)
        g_ = gt[c % 2]
        P.dma(g_, gsrc[:, :, cs])
        psA = P.ps()
        for h in range(4):
            P.mm(psA[:, h * 128:(h + 1) * 128], kT[:, h, cs], qT[:, h, cs])
        at = AT[c % 2]
        P.tt(at, psA.r("p (h q) -> p h q", h=4), idT, ALU.mult)
        psO = P.ps()
        for h in range(4):
            P.mm(psO[0:64, h * 128:(h + 1) * 128], Vt[:, c, h * 64:(h + 1) * 64], at[:, h, :], start=True, stop=False, inc=False)
            P.mm(psO[0:64, h * 128:(h + 1) * 128], Rb[:, h * 64:(h + 1) * 64], qdT[:, h, cs], start=False, stop=True)
        psKV = P.ps()
        for h in range(4):
            P.mm(psKV[0:64, h * 64:(h + 1) * 64], Kd[:, c, h * 64:(h + 1) * 64], Vt[:, c, h * 64:(h + 1) * 64])
        P.tt(R, R, cd, ALU.mult)
        P.tt(R, R, psKV[0:64, 0:256], ALU.add)
        P.copy(Rb, R, eng='pool')
        P.act(Osb, psO[0:64, :], AF.Copy)
        psM = P.ps()
        P.mm(psM[0:64, :], ones64, Osb)
        P.tt(dd, Osb, psM[0:64, :], ALU.subtract)
        P.act(dsq, dd, AF.Square)
        psV = P.ps()
        P.mm(psV[0:64, :], ones64, dsq)
        P.ts(rstd, psV[0:64, :], 1e-6, ALU.add)
        P.act(rstd, rstd, AF.Sqrt)
        P.recip(rstd, rstd)
        P.tt(dd, dd, rstd, ALU.mult)
        P.tt(dd.r("p (h q) -> p h q", h=4), dd.r("p (h q) -> p h q", h=4),
             gn.m(lambda x: x.unsqueeze(2).to_broadcast([64, 4, 128])), ALU.mult)
        P.act(sg, g_, AF.Silu)
        y_ = yo[c % 2]
        P.tt(y_, dd.r("p (h q) -> p h q", h=4), sg, ALU.mult)
        P.dma(ydst[:, :, cs], y_)
    P.barrier()


STAGE_FNS['R'] = stage_R
STAGE_FNS['RET'] = stage_RET


def stage_S5(P, l, Dm):
    P.sb_off = SB_BASE
    W = TT
    lam = P.sb([128, 8, 3], F32, "lam")
    bsb = P.sb([128, 8, 2, 16], F32, "bsb")
    csb = P.sb([128, 8, 2, 16], F32, "csb")
    msk = P.sb([128, 8, 8], F32, "msk")
    tau = P.sb([128, W], F32, "tau")
    dsk = P.sb([128, 2], F32, "dsk")
    identf = P.sb([128, 128], F32, "identf")
    P.dma(lam, Dm[f's5lam{l}'])
    P.dma(bsb, Dm[f's5b{l}'])
    P.dma(csb, Dm[f's5c{l}'])
    P.dma(msk, Dm['s5mask'])
    P.dma(tau, Dm['s5tau'])
    P.dma(dsk, Dm[f's5d{l}'])
    P.dma(identf, Dm['ident'])
    wglu = P.sb([128, 2, 256], BF16, "wglu")
    cosT = P.sb([128, 8, W], F32, "cosT")
    sinT = P.sb([128, 8, W], F32, "sinT")
    mag = P.sb([128, 8], F32, "mag")
    BT = P.sb([128, 8, 2, 128], BF16, "BT")
    CX = P.sb([128, 8, 2, 128], BF16, "CX")
    m0 = P.sb_off
    load_weight_bf16(P, wglu, Dm[f's5wglu{l}'], None, 2, 256, blk=256)
    lr = P.sb([128, 8], F32, "lr")
    li = P.sb([128, 8], F32, "li")
    dt = P.sb([128, 8], F32, "dt")
    th = P.sb([128, 8], F32, "th")
    P.ts(lr, lam[:, :, 0], -1e-4, ALU.min)
    P.copy(li, lam[:, :, 1])
    P.act(dt, lam[:, :, 2], AF.Exp)
    P.tt(th, li, dt, ALU.mult)
    P.tt(mag, lr, dt, ALU.mult)
    P.act(mag, mag, AF.Exp)
    ang = P.sb([128, W], F32, "ang")
    kq = P.sb([128, W], F32, "kq")
    ki = P.sb([128, W], I32, "ki")
    m1 = P.sb([128, W], F32, "m1")
    for j in range(8):
        P.ts(ang, tau, th[:, j:j + 1], ALU.mult)
        sin_reduced(P, sinT[:, j, :], ang, kq, ki, m1)
        P.ts(ang, tau, th[:, j:j + 1], ALU.mult, math.pi / 2, ALU.add)
        sin_reduced(P, cosT[:, j, :], ang, kq, ki, m1)
    abr = P.sb([128, 8], F32, "abr")
    abi = P.sb([128, 8], F32, "abi")
    den = P.sb([128, 8], F32, "den")
    t8 = P.sb([128, 8], F32, "t8")
    fre = P.sb([128, 8], F32, "fre")
    fim = P.sb([128, 8], F32, "fim")
    P.tt(abr, mag, cosT[:, :, 0], ALU.mult)
    P.tt(abi, mag, sinT[:, :, 0], ALU.mult)
    P.ts(abr, abr, -1.0, ALU.add)
    P.tt(den, lr, lr, ALU.mult)
    P.tt(t8, li, li, ALU.mult)
    P.tt(den, den, t8, ALU.add)
    P.recip(den, den)
    P.tt(fre, abr, lr, ALU.mult)
    P.tt(t8, abi, li, ALU.mult)
    P.tt(fre, fre, t8, ALU.add)
    P.tt(fre, fre, den, ALU.mult)
    P.tt(fim, abi, lr, ALU.mult)
    P.tt(t8, abr, li, ALU.mult)
    P.tt(fim, fim, t8, ALU.subtract)
    P.tt(fim, fim, den, ALU.mult)
    bb = P.sb([128, 8, 2, 16], F32, "bb")
    tb = P.sb([128, 8, 16], F32, "tb")
    bc16 = lambda v: v.m(lambda x: x.unsqueeze(2).to_broadcast([128, 8, 16]))
    P.tt(bb[:, :, 0, :], bsb[:, :, 0, :], bc16(fre), ALU.mult)
    P.tt(tb, bsb[:, :, 1, :], bc16(fim), ALU.mult)
    P.tt(bb[:, :, 0, :], bb[:, :, 0, :], tb, ALU.subtract)
    P.tt(bb[:, :, 1, :], bsb[:, :, 1, :], bc16(fre), ALU.mult)
    P.tt(tb, bsb[:, :, 0, :], bc16(fim), ALU.mult)
    P.tt(bb[:, :, 1, :], bb[:, :, 1, :], tb, ALU.add)
    P.ts(csb[:, :, 1, :], csb[:, :, 1, :], -1.0, ALU.mult)
    bx = P.sb([128, 8, 16], F32, "bx")
    for j in range(8):
        mj = msk[:, j, :].m(lambda x: x.unsqueeze(2).to_broadcast([128, 8, 16]))
        for ri in range(2):
            P.tt(bx, bb[:, j, ri, :].m(lambda x: x.unsqueeze(1).to_broadcast([128, 8, 16])), mj, ALU.mult)
            ps = P.ps()
            P.transpose(ps[:, 0:128], bx.r("p a b -> p (a b)"), identf)
            P.copy(BT[:, j, ri, :], ps[:, 0:128])
            P.tt(CX[:, j, ri, :].r("p (a b) -> p a b", a=8),
                 csb[:, j, ri, :].m(lambda x: x.unsqueeze(1).to_broadcast([128, 8, 16])), mj, ALU.mult)
    P.barrier()
    P.sb_off = m0
    A = P.sb([128, 8, W], F32, "A")
    B = P.sb([128, 8, W], F32, "B")
    t1 = P.sb([128, 8, W], F32, "t1")
    t2 = P.sb([128, 8, W], F32, "t2")
    wre = P.sb([128, 8, W], F32, "wre")
    wim = P.sb([128, 8, W], F32, "wim")
    xre = P.sb([128, 8, W], BF16, "xre")
    xim = P.sb([128, 8, W], BF16, "xim")
    cre = P.sb([128, 8], F32, "cre")
    cim = P.sb([128, 8], F32, "cim")
    P.memset(cre, 0.0)
    P.memset(cim, 0.0)
    uf = P.sb([128, 2, W], F32, "uf")
    ub = P.sb([128, 2, W], BF16, "ub")
    gf = P.sb([128, 2, W], F32, "gf")
    y = P.sb([128, 2, W], F32, "y")
    y2 = P.sb([128, 2, W], F32, "y2")
    glb = P.sb([128, 2, W], BF16, "glb")
    yo = P.sb([128, 2, W], BF16, "yo")
    csrc = Dm['colsT'].r("(c p) t -> p c t", p=128)
    ydst = Dm['yT_s5'].r("(c p) t -> p c t", p=128)
    for tt in range(NTT):
        sl = slice(tt * W, (tt + 1) * W)
        P.dma(uf, csrc[:, C_SU:C_SU + 2, sl])
        P.dma(gf, csrc[:, C_SG:C_SG + 2, sl])
        P.copy(ub, uf, eng='pool')
        for j in range(8):
            for ri, dst in ((0, A), (1, B)):
                ps = P.ps()
                P.mm(ps, BT[:, j, ri, :], ub[:, j // 4, :])
                P.act(dst[:, j, :], ps, AF.Copy)
        P.tt(t1, A, cosT, ALU.mult)
        P.tt(t2, B, sinT, ALU.mult, eng='pool')
        P.tt(t1, t1, t2, ALU.add)
        P.tt(t2, A, sinT, ALU.mult, eng='pool')
        P.tt(B, B, cosT, ALU.mult)
        P.tt(t2, B, t2, ALU.subtract, eng='pool')
        for j in range(8):
            mb = mag[:, j:j + 1].bc([128, W])
            P.scan(wre[:, j, :], mb, t1[:, j, :], cre[:, j:j + 1], ALU.mult, ALU.add)
            P.scan(wim[:, j, :], mb, t2[:, j, :], cim[:, j:j + 1], ALU.mult, ALU.add)
        P.tt(t1, wre, cosT, ALU.mult)
        P.tt(A, wim, sinT, ALU.mult, eng='pool')
        P.tt(xre, t1, A, ALU.subtract)
        P.tt(cre, t1[:, :, W - 1], A[:, :, W - 1], ALU.subtract)
        P.tt(t2, wre, sinT, ALU.mult, eng='pool')
        P.tt(B, wim, cosT, ALU.mult)
        P.tt(xim, t2, B, ALU.add, eng='pool')
        P.tt(cim, t2[:, :, W - 1], B[:, :, W - 1], ALU.add)
        for jc in range(2):
            ps = P.ps()
            n = 0
            for j in range(4 * jc, 4 * jc + 4):
                for ri, xx in ((0, xre), (1, xim)):
                    P.mm(ps, CX[:, j, ri, :], xx[:, j, :], start=(n == 0), stop=(n == 7))
                    n += 1
            P.stt(y[:, jc, :], uf[:, jc, :], dsk[:, jc:jc + 1], ps, ALU.mult, ALU.add)
        P.tt(y2, y, y, ALU.mult)
        P.ts(y2, y2, 0.044715, ALU.mult, 1.0, ALU.add)
        P.tt(y2, y2, y, ALU.mult)
        P.act(y2, y2, AF.Sigmoid, scale=1.5957691216057308)
        P.tt(y, y, y2, ALU.mult)
        P.copy(glb, y, eng='pool')
        for oc in range(2):
            ps = P.ps()
            for kc in range(2):
                P.mm(ps, wglu[:, kc, oc * 128:(oc + 1) * 128], glb[:, kc, :], start=(kc == 0), stop=(kc == 1))
            P.act(y2[:, oc, :], ps, AF.Sigmoid)
        P.tt(y, y, y2, ALU.mult)
        P.act(y2, gf, AF.Silu)
        P.tt(yo, y, y2, ALU.mult)
        P.dma(ydst[:, :, sl], yo)
    P.barrier()


STAGE_FNS['S5'] = stage_S5


import os
RW_DEBUG = int(os.environ.get('RW_DEBUG', '3'))


def stage_RWKV(P, l, Dm):
    P.sb_off = SB_BASE
    W = 256
    H4 = 4
    NCH = W // 64
    HC = H4 * NCH
    prm = P.sb([64, 8, 4], F32, "prm")
    mu = P.sb([64, 18], F32, "mu")
    w2 = P.sb([64, 256], F32, "w2")
    a2 = P.sb([64, 256], F32, "a2")
    msks = P.sb([64, 3, 64], F32, "msks")
    identf = P.sb([128, 128], F32, "identf")
    ones64 = P.sb([64, 64], F32, "ones64")
    onesw = P.sb([64, 1], F32, "onesw")
    P.dma(prm, Dm[f'rwprm{l}'])
    P.dma(mu, Dm[f'rwmu{l}'])
    P.dma(w2, Dm[f'rww2{l}'])
    P.dma(a2, Dm[f'rwa2{l}'])
    P.dma(msks, Dm['rw_masks'])
    P.dma(identf, Dm['ident'])
    P.memset(ones64, 1.0)
    P.memset(onesw, 1.0)
    id64 = identf[0:64, 0:64]
    hb = lambda v, n=W: v.m(lambda x: x.unsqueeze(2).to_broadcast([64, H4, n]))
    mb = lambda k: msks[:, k, :].m(lambda x: x.unsqueeze(1).to_broadcast([64, H4, 64]))
    idb = id64.m(lambda x: x.unsqueeze(1).to_broadcast([64, H4, 64]))
    cin = P.sb([64, 18, W + 1], F32, "cin")
    cs = P.sb([64, 18, W], F32, "cs")
    f = lambda nm: P.sb([64, H4, W], F32, nm)
    twl = P.sb([64, W], F32, "twl")
    sgz, av, kx, t0, kp, beta = f("sgz"), f("av"), f("kx"), f("t0"), f("kp"), f("beta")
    kkn, lw, cw, e1, e2 = f("kkn"), f("lw"), f("cw"), f("e1"), f("e2")
    rt, at, bt, kt, Bh, Kh = f("rt"), f("at"), f("bt"), f("kt"), f("Bh"), f("Kh")
    bonus, Yt = f("bonus"), f("Yt")
    base = P.sb([64, HC], F32, "base")
    cwC = P.sb([64, HC], F32, "cwC")
    gC = P.sb([64, HC], F32, "gC")
    S0 = P.sb([64, H4, 64], F32, "S0")
    P.memset(S0, 0.0)
    g4 = lambda nm: P.sb([64, H4, 64], F32, nm)
    X, XT, PaT, AakT, ArbT, ArkT = g4("X"), g4("XT"), g4("PaT"), g4("AakT"), g4("ArbT"), g4("ArkT")
    Vt, BhT, KhT, Wsb, Usb = g4("Vt"), g4("BhT"), g4("KhT"), g4("Wsb"), g4("Usb")
    yo = P.sb([64, H4, W], BF16, "yo")
    src = Dm['colsT'][0:1152, :].r("(g d) t -> d g t", d=64)
    ydst = Dm['yT_rwkv'].r("(h d) t -> d h t", d=64)
    ps4 = lambda ps: ps[0:64, 0:256].r("p (h x) -> p h x", h=H4)
    for tt in range(S // W):
        t_0 = tt * W
        if tt == 0:
            P.dma(cin[:, :, 1:W + 1], src[:, :, t_0:t_0 + W])
            P.memset(cin[:, :, 0:1], 0.0)
        else:
            P.dma(cin, src[:, :, t_0 - 1:t_0 + W])
        P.tt(cs, cin[:, :, 0:W], cin[:, :, 1:W + 1], ALU.subtract)
        P.tt(cs, cs, mu.m(lambda x: x.unsqueeze(2).to_broadcast([64, 18, W])), ALU.mult)
        P.tt(cs, cs, cin[:, :, 1:W + 1], ALU.add)
        Rr, Kk, Vv, G = cs[:, 0:4, :], cs[:, 4:8, :], cs[:, 8:12, :], cs[:, 14:18, :]
        P.act(twl, cs[:, 12, :], AF.Tanh)
        for h in range(H4):
            ps = P.ps()
            P.mm(ps[0:64, 0:W], w2[:, h * 64:(h + 1) * 64], twl)
            P.act(sgz[:, h, :], ps[0:64, 0:W], AF.Sigmoid, bias=prm[:, 0, h:h + 1])
            ps = P.ps()
            P.mm(ps[0:64, 0:W], a2[:, h * 64:(h + 1) * 64], cs[:, 13, :])
            P.act(av[:, h, :], ps[0:64, 0:W], AF.Sigmoid, bias=prm[:, 1, h:h + 1])
        P.tt(kx, Kk, hb(prm[:, 2, :]), ALU.mult)
        P.tt(t0, kx, kx, ALU.mult, eng='pool')
        for h in range(H4):
            ps = P.ps()
            P.mm(ps[0:64, 0:W], ones64, t0[:, h, :])
            P.ts(kkn[:, h, :], ps[0:64, 0:W], 1e-24, ALU.add)
        P.act(kkn, kkn, AF.Sqrt)
        P.recip(kkn, kkn)
        P.tt(kkn, kkn, kx, ALU.mult)
        P.ts(t0, av, -1.0, ALU.add)
        P.tt(t0, t0, hb(prm[:, 3, :]), ALU.mult)
        P.stt(kp, t0, 1.0, Kk, ALU.add, ALU.mult)
        P.tt(beta, kkn, av, ALU.mult, eng='pool')
        P.tt(t0, Rr, kp, ALU.mult)
        P.tt(t0, t0, hb(prm[:, 4, :]), ALU.mult)
        for h in range(H4):
            ps = P.ps()
            P.mm(ps[0:64, 0:W], ones64, t0[:, h, :])
            P.tt(bonus[:, h, :], ps[0:64, 0:W], Vv[:, h, :], ALU.mult)
        P.ts(lw, sgz, -math.exp(-0.5), ALU.mult)
        for h in range(H4):
            P.scan(cw[:, h, :], onesw[:, 0:1].bc([64, W]), lw[:, h, :], 0.0, ALU.mult, ALU.add)
        cw3 = cw.r("p h (c i) -> p (h c) i", i=64)
        P.memset(base, 0.0)
        P.copy(base.r("p (h c) -> p h c", h=H4)[:, :, 1:NCH], cw.r("p h (c i) -> p h c i", i=64)[:, :, 0:NCH - 1, 63])
        P.tt(cw3, cw3, base.m(lambda x: x.unsqueeze(2).to_broadcast([64, HC, 64])), ALU.subtract)
        P.copy(cwC, cw3[:, :, 63])
        P.act(gC, cwC, AF.Exp)
        P.act(e1, cw, AF.Exp)
        P.tt(rt, Rr, e1, ALU.mult)
        P.act(e1, cw, AF.Exp, scale=-1.0)
        P.tt(bt, beta, e1, ALU.mult)
        P.tt(kt, kp, e1, ALU.mult, eng='pool')
        P.tt(e2, cw, lw, ALU.subtract)
        P.act(e2, e2, AF.Exp)
        P.stt(at, kkn, -1.0, e2, ALU.mult, ALU.mult)
        e13 = e1.r("p h (c i) -> p (h c) i", i=64)
        P.tt(e13, cw3, cwC.m(lambda x: x.unsqueeze(2).to_broadcast([64, HC, 64])), ALU.subtract)
        P.act(e1, e1, AF.Exp, scale=-1.0)
        P.tt(Bh, beta, e1, ALU.mult)
        P.tt(Kh, kp, e1, ALU.mult, eng='pool')
        for c in range(NCH if RW_DEBUG >= 1 else 0):
            sl = slice(c * 64, (c + 1) * 64)
            def mm4(lh, rh):
                ps = P.ps()
                for h in range(H4):
                    P.mm(ps[0:64, h * 64:(h + 1) * 64], lh[:, h, sl], rh[:, h, sl], inc=(h == 3))
                return ps4(ps)
            P.tt(X, mm4(at, bt), mb(2), ALU.mult)
            P.tt(XT, mm4(bt, at), mb(0), ALU.mult)
            P.tt(AakT, mm4(kt, at), mb(0), ALU.mult)
            P.tt(ArbT, mm4(bt, rt), mb(1), ALU.mult)
            P.tt(ArkT, mm4(kt, rt), mb(1), ALU.mult)
            P.tt(PaT, XT, idb, ALU.add, eng='pool')
            for srcT, dstT, eng in ((Vv, Vt, 'act'), (Bh, BhT, 'dve'), (Kh, KhT, 'act')):
                ps = P.ps()
                for h in range(H4):
                    P.transpose(ps[0:64, h * 64:(h + 1) * 64], srcT[:, h, sl], id64)
                if eng == 'act':
                    P.act(dstT, ps4(ps), AF.Copy)
                else:
                    P.copy(dstT, ps4(ps))
            for it in range(5 if RW_DEBUG >= 2 else 0):
                psx = P.ps()
                psxt = P.ps()
                for h in range(H4):
                    P.mm(psx[0:64, h * 64:(h + 1) * 64], XT[:, h, :], X[:, h, :], inc=(h == 3))
                for h in range(H4):
                    P.mm(psxt[0:64, h * 64:(h + 1) * 64], X[:, h, :], XT[:, h, :], inc=(h == 3))
                P.act(X, ps4(psx), AF.Copy)
                P.copy(XT, ps4(psxt))
                psp = P.ps()
                for h in range(H4):
                    P.mm(psp[0:64, h * 64:(h + 1) * 64], X[:, h, :], PaT[:, h, :], inc=(h == 3))
                P.tt(PaT, PaT, ps4(psp), ALU.add)
            if RW_DEBUG < 3:
                continue
            psw = P.ps()
            for h in range(H4):
                o_ = psw[0:64, h * 64:(h + 1) * 64]
                P.mm(o_, at[:, h, sl], S0[:, h, :], start=True, stop=False, inc=False)
                P.mm(o_, AakT[:, h, :], Vt[:, h, :], start=False, stop=True, inc=(h == 3))
            P.act(Wsb, ps4(psw), AF.Copy)
            psu = P.ps()
            for h in range(H4):
                P.mm(psu[0:64, h * 64:(h + 1) * 64], PaT[:, h, :], Wsb[:, h, :], inc=(h == 3))
            P.copy(Usb, ps4(psu))
            psy = P.ps()
            for h in range(H4):
                o_ = psy[0:64, h * 64:(h + 1) * 64]
                P.mm(o_, S0[:, h, :], rt[:, h, sl], start=True, stop=False, inc=False)
                P.mm(o_, Usb[:, h, :], ArbT[:, h, :], start=False, stop=False, inc=False)
                P.mm(o_, Vt[:, h, :], ArkT[:, h, :], start=False, stop=True, inc=(h == 3))
            P.act(Yt[:, :, sl], ps4(psy), AF.Copy)
            pss = P.ps()
            for h in range(H4):
                o_ = pss[0:64, h * 64:(h + 1) * 64]
                P.mm(o_, BhT[:, h, :], Usb[:, h, :], start=True, stop=False, inc=False)
                P.mm(o_, KhT[:, h, :], Vt[:, h, :], start=False, stop=True, inc=(h == 3))
            gcb = gC.r("p (h c) -> p h c", h=H4)[:, :, c].m(lambda x: x.unsqueeze(2).to_broadcast([64, H4, 64]))
            P.tt(S0, S0, gcb, ALU.mult)
            P.tt(S0, S0, ps4(pss), ALU.add)
        for h in range(H4):
            ps = P.ps()
            P.mm(ps[0:64, 0:W], ones64, Yt[:, h, :])
            P.stt(e1[:, h, :], ps[0:64, 0:W], -1.0 / 64, Yt[:, h, :], ALU.mult, ALU.add)
        P.tt(e2, e1, e1, ALU.mult, eng='pool')
        for h in range(H4):
            ps = P.ps()
            P.mm(ps[0:64, 0:W], ones64, e2[:, h, :])
            P.ts(t0[:, h, :], ps[0:64, 0:W], 1.0 / 64, ALU.mult, 64e-5, ALU.add)
        P.act(t0, t0, AF.Sqrt)
        P.recip(t0, t0)
        P.tt(e1, e1, t0, ALU.mult)
        P.tt(e1, e1, hb(prm[:, 5, :]), ALU.mult)
        P.tt(e1, e1, hb(prm[:, 6, :]), ALU.add)
        P.tt(e1, e1, bonus, ALU.add)
        P.act(e2, G, AF.Silu)
        P.tt(yo, e1, e2, ALU.mult)
        P.dma(ydst[:, :, t_0:t_0 + W], yo)
    P.barrier()


STAGE_FNS['RWKV'] = stage_RWKV


N_BISECT = 24


def stage_DSA(P, l, Dm):
    P.sb_off = SB_BASE
    qT = P.sb([64, 4, S], BF16, "qT")
    kT = P.sb([64, 4, S], BF16, "kT")
    ikT = P.sb([64, 1, S], F32, "ikT")
    m0 = P.sb_off
    ropeC = P.sb([64, S], F32, "ropeC")
    ropeS = P.sb([64, S], F32, "ropeS")
    P.dma(ropeC, Dm['ropeC'])
    P.dma(ropeS, Dm['ropeS'])
    rope_heads(P, qT, Dm, C_DQ, C_DQS, ropeC, ropeS)
    rope_heads(P, kT, Dm, C_DK, C_DKS, ropeC, ropeS)
    rope_heads(P, None, Dm, C_IQ, C_IQS, ropeC, ropeS, dram_dst=Dm['iqR'])
    rope_heads(P, ikT, Dm, (C_IK * 128,), (C_IK * 128 + 64,), ropeC, ropeS, nheads=1)
    P.barrier()
    P.sb_off = m0
    identf = P.sb([128, 128], F32, "identf")
    identb = P.sb([128, 128], BF16, "identb")
    cb = P.sb([128, 128], F32, "cb")
    P.dma(identf, Dm['ident'])
    P.copy(identb, identf)
    P.dma(cb, Dm['dsa_cb'])
    Vaug = P.sb([128, 32, 4, 65], BF16, "Vaug")
    iwt = P.sb([128, 32, 4], F32, "iwt")
    vst = [P.sb([128, 4, 256], F32, "vst") for _ in range(2)]
    tsrc = Dm['colsTok'].r("(c p) n -> p c n", p=128)
    P.memset(Vaug[:, :, :, 64:65], 1.0)
    for i in range(8):
        v_ = vst[i % 2]
        P.dma(v_, tsrc[:, i * 4:(i + 1) * 4, 0:256])
        P.copy(Vaug[:, i * 4:(i + 1) * 4, :, 0:64], v_.r("p c (h d) -> p c h d", h=4), eng=('dve' if i % 2 else 'pool'))
    P.dma(iwt, tsrc[:, :, 512:516])
    P.ts(iwt, iwt, 1.0 / 16, ALU.mult)
    score = P.sb([128, S], F32, "score")
    junk = P.sb([128, S], BF16, "junk")
    mask01 = P.sb([128, S], BF16, "mask01")
    maskT = P.sb([128, 32, 128], BF16, "maskT")
    rl = [P.sb([128, 512], F32, "rl") for _ in range(4)]
    E = [P.sb([128, 512], BF16, "E") for _ in range(2)]
    lo = P.sb([128, 1], F32, "lo")
    hi = P.sb([128, 1], F32, "hi")
    mid = P.sb([128, 1], F32, "mid")
    cnt = P.sb([128, 1], F32, "cnt")
    sel = P.sb([128, 1], F32, "sel")
    dlt = P.sb([128, 1], F32, "dlt")
    zt = P.sb([128, S], F32, "zt")
    cz = P.sb([128, S], F32, "cz")
    nz = P.sb([128, 1], F32, "nz")
    npos = P.sb([128, 1], F32, "npos")
    flag = P.sb([128, 1], F32, "flag")
    f2 = P.sb([128, 1], F32, "f2")
    rr = P.sb([128, 1], F32, "rr")
    onesw = P.sb([128, 1], F32, "onesw")
    P.memset(onesw, 1.0)
    osb = P.sb([128, 4, 64], F32, "osb")
    rs = P.sb([128, 4, 1], F32, "rs")
    gt = [P.sb([128, 2, 128], F32, "gt") for _ in range(2)]
    sg = P.sb([128, 2, 128], F32, "sg")
    yo = [P.sb([128, 2, 128], BF16, "yo") for _ in range(2)]
    iqt = [P.sb([64, 4, 128], F32, "iqt") for _ in range(2)]
    iqsrc = Dm['iqR'].r("(h d) t -> d h t", d=64)
    gsrc = Dm['colsT'].r("(c p) t -> p c t", p=128)
    ydst = Dm['yT_dsa'].r("(c p) t -> p c t", p=128)
    ne = 0
    for i in range(32):
        qs = slice(i * 128, (i + 1) * 128)
        Nk = 128 * (i + 1)
        g_ = gt[i % 2]
        P.dma(g_, gsrc[:, C_DG:C_DG + 2, qs])
        iq_ = iqt[i % 2]
        P.dma(iq_, iqsrc[:, :, qs])
        for k0 in range(0, Nk, 512):
            kw = min(512, Nk - k0)
            pss = []
            for h in range(4):
                ps = P.ps()
                P.mm(ps[:, 0:kw], iq_[:, h, :], ikT[:, 0, k0:k0 + kw])
                pss.append(ps)
            for h in range(4):
                P.act(rl[h][:, 0:kw], pss[h][:, 0:kw], AF.Relu)
            P.ts(score[:, k0:k0 + kw], rl[0][:, 0:kw], iwt[:, i, 0:1], ALU.mult)
            for h in range(1, 4):
                P.stt(score[:, k0:k0 + kw], rl[h][:, 0:kw], iwt[:, i, h:h + 1], score[:, k0:k0 + kw], ALU.mult, ALU.add)
        P.tt(score[:, i * 128:Nk], score[:, i * 128:Nk], cb, ALU.add)
        if Nk > 256:
            P.reduce(hi, score[:, 0:Nk], ALU.max)
            P.reduce(lo, score[:, 0:i * 128], ALU.min)
            P.ts(hi, hi, 1.0, ALU.add)
            P.ts(lo, lo, -1.0, ALU.add)
            for it in range(N_BISECT):
                P.ts(mid, lo, hi[:, 0:1], ALU.add, 0.5, ALU.mult)
                P.ts(junk[:, 0:Nk], score[:, 0:Nk], mid[:, 0:1], ALU.is_ge, 0.0, ALU.add, accum=cnt)
                P.ts(sel, cnt, 255.5, ALU.is_ge)
                P.tt(dlt, mid, lo, ALU.subtract)
                P.stt(lo, dlt, sel[:, 0:1], lo, ALU.mult, ALU.add)
                P.tt(dlt, hi, mid, ALU.subtract)
                P.stt(hi, dlt, sel[:, 0:1], mid, ALU.mult, ALU.add)
        else:
            P.memset(lo, -1e29)
        P.ts(zt[:, 0:Nk], score[:, 0:Nk], 0.0, ALU.is_equal, 0.0, ALU.add, accum=nz)
        P.ts(junk[:, 0:Nk], score[:, 0:Nk], 0.0, ALU.is_gt, 0.0, ALU.add, accum=npos)
        P.ts(flag, npos, 255.5, ALU.is_lt)
        P.tt(f2, npos, nz, ALU.add)
        P.ts(f2, f2, 255.5, ALU.is_ge)
        P.tt(flag, flag, f2, ALU.mult)
        P.ts(rr, npos, -1.0, ALU.mult, 256.0, ALU.add)
        P.scan(cz[:, 0:Nk], onesw[:, 0:1].bc([128, Nk]), zt[:, 0:Nk], 0.0, ALU.mult, ALU.add)
        P.ts(cz[:, 0:Nk], cz[:, 0:Nk], rr[:, 0:1], ALU.is_le, flag[:, 0:1], ALU.mult)
        P.tt(zt[:, 0:Nk], zt[:, 0:Nk], cz[:, 0:Nk], ALU.mult)
        P.ts(f2, flag, -1.0, ALU.mult, 1.0, ALU.add)
        P.tt(lo, lo, f2, ALU.mult)
        P.stt(lo, flag, 1e-30, lo, ALU.mult, ALU.add)
        P.ts(mask01[:, 0:Nk], score[:, 0:Nk], lo[:, 0:1], ALU.is_ge)
        P.tt(mask01[:, 0:Nk], mask01[:, 0:Nk], zt[:, 0:Nk], ALU.add)
        for c0 in range(0, i + 1, 4):
            nc_ = min(4, i + 1 - c0)
            ps = P.ps()
            psb = ps.bitcast(BF16)
            for cl in range(nc_):
                c = c0 + cl
                P.transpose(psb[:, cl * 128:(cl + 1) * 128], mask01[:, c * 128:(c + 1) * 128], identb, )
            P.act(maskT[:, c0:c0 + nc_, :], psb[:, 0:nc_ * 128].r("p (c q) -> p c q", q=128), AF.Copy)
        psO = P.ps_acc()
        for h in range(4):
            for c0 in range(0, i + 1, 4):
                nc_ = min(4, i + 1 - c0)
                ps = P.ps()
                for cl in range(nc_):
                    c = c0 + cl
                    P.mm(ps[:, cl * 128:(cl + 1) * 128], kT[:, h, c * 128:(c + 1) * 128], qT[:, h, qs], inc=(cl == nc_ - 1))
                e_ = E[ne % 2]
                ne += 1
                P.act(e_[:, 0:nc_ * 128], ps[:, 0:nc_ * 128], AF.Exp, scale=0.125)
                P.tt(e_[:, 0:nc_ * 128].r("p (c q) -> p c q", q=128), e_[:, 0:nc_ * 128].r("p (c q) -> p c q", q=128),
                     maskT[:, c0:c0 + nc_, :], ALU.mult, eng=('dve' if ne % 2 else 'pool'))
                for cl in range(nc_):
                    c = c0 + cl
                    P.mm(psO[:, h * 65:(h + 1) * 65], e_[:, cl * 128:(cl + 1) * 128], Vaug[:, c, h, :],
                         start=(c == 0), stop=(c == i), inc=(cl == nc_ - 1))
        pv = psO[:, 0:260].r("p (h x) -> p h x", h=4)
        P.recip(rs, pv[:, :, 64:65])
        P.tt(osb, pv[:, :, 0:64], rs.m(lambda x: x.to_broadcast([128, 4, 64])), ALU.mult)
        P.act(sg, g_, AF.Silu)
        y_ = yo[i % 2]
        for p in range(2):
            ps = P.ps()
            P.transpose(ps[:, 0:128], osb[:, 2 * p:2 * p + 2, :].r("p a b -> p (a b)"), identf)
            P.tt(y_[:, p, :], ps[:, 0:128], sg[:, p, :], ALU.mult)
        P.dma(ydst[:, :, qs], y_)
    P.barrier()


STAGE_FNS['DSA'] = stage_DSA


FULL_PLAN = [('R', 0)] + [(st, l) for l in range(2) for st in ('P', 'X', 'RET', 'S5', 'RWKV', 'DSA', 'M')]


def kernel(**inputs):
    inputs = {k: np.asarray(v) for k, v in inputs.items()}
    nb = inputs['x'].shape[0]
    nc, P, used_in = build(FULL_PLAN)
    in_maps = []
    for b in range(nb):
        d = host_inputs(inputs, b)
        in_maps.append({k: v for k, v in d.items() if k in used_in})
    res = run_bass_kernel_spmd(nc, in_maps, core_ids=list(range(nb)))
    out = np.stack([np.ascontiguousarray(np.asarray(r['outT']).T) for r in res.results], 0)
    return out.astype(np.float32)
```

```python
from contextlib import ExitStack
import math
import numpy as np
import ml_dtypes
import concourse.bass as bass
import concourse.mybir as mybir
from concourse.bass_utils import run_bass_kernel_spmd

F32 = mybir.dt.float32
BF16 = mybir.dt.bfloat16
I32 = mybir.dt.int32
AF = mybir.ActivationFunctionType
ALU = mybir.AluOpType
AX = mybir.AxisListType

ENGS = ['sp', 'act', 'dve', 'pool', 'pe']
EPOCH = 16000
NDMASEM = 24
SB_BASE = 16640
SBUF_BYTES = 229000


class Buf:
    __slots__ = ('name', 'wev', 'rev', 'tracked')

    def __init__(self, name, tracked=True):
        self.name = name
        self.wev = {}
        self.rev = {}
        self.tracked = tracked


class V:
    __slots__ = ('buf', 'ap')

    def __init__(self, buf, ap):
        self.buf = buf
        self.ap = ap

    def __getitem__(self, k):
        return V(self.buf, self.ap[k])

    def m(self, fn):
        return V(self.buf, fn(self.ap))

    def r(self, s, **kw):
        return V(self.buf, self.ap.rearrange(s, **kw))

    def bc(self, shape):
        return V(self.buf, self.ap.to_broadcast(list(shape)))

    def bitcast(self, dt):
        return V(self.buf, self.ap.bitcast(dt))

    @property
    def shape(self):
        return tuple(self.ap.shape)


def _ap(x):
    return x.ap if isinstance(x, V) else x


class Prog:
    def __init__(self, nc):
        self.nc = nc
        self.q = {e: [] for e in ENGS}
        self.cnt = {e: 0 for e in ENGS}
        self.noinc = {e: False for e in ENGS}
        self.known = {e: {} for e in ENGS}
        self.dma_n = {e: 0 for e in ENGS}
        self.nbar = 0
        self.bufs = []
        self.sb_off = SB_BASE
        self.sb_id = 0
        self.sb_mark = 0
        self.psum = []
        for i in range(8):
            h = nc.alloc_psum_tensor(f"ps{i}", [128, 512], F32)
            self.psum.append(V(self._newbuf(f"ps{i}"), h[:]))
        self.ps_rr = 0

    def _newbuf(self, name, tracked=True):
        b = Buf(name, tracked)
        if tracked:
            self.bufs.append(b)
        return b

    def sb(self, shape, dtype=F32, name="t"):
        esz = {F32: 4, BF16: 2, I32: 4}[dtype]
        per = esz * int(np.prod(shape[1:]))
        per = (per + 63) // 64 * 64
        off = self.sb_off
        assert off + per <= SBUF_BYTES, f"SBUF overflow {name} {off}+{per}"
        self.sb_off += per
        self.sb_id += 1
        nm = f"{name}_{self.sb_id}"
        h = self.nc.alloc_sbuf_tensor_at(nm, list(shape), dtype, offset=off)
        return V(self._newbuf(nm), h[:])

    def mark(self):
        self.sb_mark = self.sb_off

    def release(self):
        self.sb_off = self.sb_mark

    def ps(self):
        v = self.psum[self.ps_rr % 7]
        self.ps_rr += 1
        return v

    def ps_acc(self):
        return self.psum[7]

    def dram(self, name, shape, dtype=F32, kind="Internal"):
        h = self.nc.dram_tensor(name, list(shape), dtype, kind=kind)
        return V(self._newbuf(name, tracked=False), h.ap())

    def _collect(self, eng, reads, writes, extra=None):
        waits = {}

        def need(evs, skip_own):
            for sk, v in evs.items():
                if skip_own and sk[0] == eng:
                    continue
                if waits.get(sk, 0) < v:
                    waits[sk] = v
        for x in reads:
            if x.buf.tracked:
                need(x.buf.wev, False)
        for x in writes:
            if x.buf.tracked:
                need(x.buf.wev, True)
                need(x.buf.rev, True)
        if extra:
            need(extra, False)
        kn = self.known[eng]
        wl = []
        for sk, v in waits.items():
            if kn.get(sk, 0) < v:
                kn[sk] = v
                wl.append((sk, v))
        return wl

    def emit(self, eng, fn, reads=(), writes=(), inc=True):
        reads = [x for x in reads if isinstance(x, V)]
        writes = [x for x in writes if isinstance(x, V)]
        wl = self._collect(eng, reads, writes)
        idx = self.cnt[eng] + 1
        if inc:
            self.cnt[eng] = idx
            self.noinc[eng] = False
        else:
            self.noinc[eng] = True
        sk = (eng, (idx - 1) // EPOCH)
        val = (idx - 1) % EPOCH + 1
        self.q[eng].append((wl, fn, (sk, 1) if inc else None))
        for x in reads:
            b = x.buf
            if b.tracked and b.rev.get(sk, 0) < val:
                b.rev[sk] = val
        for x in writes:
            b = x.buf
            if b.tracked:
                b.wev = {sk: val}
                b.rev = {}

    def dma(self, out, in_, eng='sp'):
        n = self.dma_n[eng]
        self.dma_n[eng] = n + 1
        slot, k = n % NDMASEM, n // NDMASEM
        sk = ('dma', eng, slot)
        val = 16 * (k + 1)
        extra = {sk: 16 * k} if k > 0 else None
        wl = self._collect(eng, [in_], [out], extra)
        oa, ia = out.ap, in_.ap
        self.q[eng].append((wl, lambda e: e.dma_start(out=oa, in_=ia), (sk, 16)))
        b = in_.buf
        if b.tracked:
            b.rev[sk] = val
        b = out.buf
        if b.tracked:
            b.wev = {sk: val}
            b.rev = {}

    def barrier(self):
        for e in ENGS:
            assert not self.noinc[e], f"dangling no-inc instruction on {e}"
        waits = {}
        for e in ENGS:
            idx = self.cnt[e]
            if idx > 0:
                waits[(e, (idx - 1) // EPOCH)] = (idx - 1) % EPOCH + 1
            n = self.dma_n[e]
            for slot in range(min(n, NDMASEM)):
                k = (n - 1 - slot) // NDMASEM
                waits[('dma', e, slot)] = 16 * (k + 1)
        kn = self.known['sp']
        wl = []
        for sk, v in waits.items():
            if sk[0] == 'sp':
                continue
            if kn.get(sk, 0) < v:
                kn[sk] = v
                wl.append((sk, v))
        self.nbar += 1
        nb = self.nbar
        bk = ('bar', 0)
        self.q['sp'].append((wl, 'seminc', (bk, 1)))
        for e in ENGS:
            if e != 'sp':
                self.q[e].append(([(bk, nb)], None, None))
                for sk, v in waits.items():
                    if self.known[e].get(sk, 0) < v:
                        self.known[e][sk] = v
        for b in self.bufs:
            b.wev = {}
            b.rev = {}

    def finish(self):
        self.barrier()
        nc = self.nc
        keys = set()
        for e in ENGS:
            for wl, fn, inc in self.q[e]:
                for sk, v in wl:
                    keys.add(sk)
                if inc is not None:
                    keys.add(inc[0])
        stack = ExitStack()
        sems = {}
        for i, sk in enumerate(sorted(keys, key=str)):
            sems[sk] = stack.enter_context(nc.semaphore(f"s{i}"))
        self.nsem = len(sems)
        q = self.q

        def mk(en):
            def body(e):
                for wl, fn, inc in q[en]:
                    for sk, v in wl:
                        e.wait_ge(sems[sk], v)
                    if fn is None:
                        continue
                    if fn == 'seminc':
                        e.sem_inc(sems[inc[0]], inc[1])
                        continue
                    ins = fn(e)
                    if inc is not None:
                        ins.then_inc(sems[inc[0]], inc[1])
            return body
        with stack:
            with nc.Block() as block:
                block.sync(mk('sp'))
                block.scalar(mk('act'))
                block.vector(mk('dve'))
                block.gpsimd(mk('pool'))
                block.tensor(mk('pe'))

    def act(self, out, in_, func, bias=None, scale=1.0, accum=None):
        o, i, b, s, a = _ap(out), _ap(in_), _ap(bias), _ap(scale), _ap(accum)
        kw = {}
        if b is not None:
            kw['bias'] = b
        if a is not None:
            kw['accum_out'] = a
        self.emit('act', lambda e: e.activation(out=o, in_=i, func=func, scale=s, **kw),
                  [in_, bias, scale], [out, accum])

    def ts(self, out, in0, s1, op0, s2=None, op1=None, accum=None, eng='dve'):
        o, i, a1, a2, ac = _ap(out), _ap(in0), _ap(s1), _ap(s2), _ap(accum)
        kw = {}
        if op1 is not None:
            kw['op1'] = op1
        if ac is not None:
            kw['accum_out'] = ac
        self.emit(eng, lambda e: e.tensor_scalar(out=o, in0=i, scalar1=a1, scalar2=a2, op0=op0, **kw),
                  [in0, s1, s2], [out, accum])

    def tt(self, out, in0, in1, op, eng='dve'):
        o, a, b = _ap(out), _ap(in0), _ap(in1)
        self.emit(eng, lambda e: e.tensor_tensor(out=o, in0=a, in1=b, op=op), [in0, in1], [out])

    def stt(self, out, in0, scalar, in1, op0, op1, eng='dve'):
        o, a, s, b = _ap(out), _ap(in0), _ap(scalar), _ap(in1)
        self.emit(eng, lambda e: e.scalar_tensor_tensor(out=o, in0=a, scalar=s, in1=b, op0=op0, op1=op1),
                  [in0, scalar, in1], [out])

    def copy(self, out, in_, eng='dve'):
        o, i = _ap(out), _ap(in_)
        if eng == 'act':
            self.emit('act', lambda e: e.copy(out=o, in_=i), [in_], [out])
        else:
            self.emit(eng, lambda e: e.tensor_copy(out=o, in_=i), [in_], [out])

    def memset(self, out, val, eng='dve'):
        o = _ap(out)
        self.emit(eng, lambda e: e.memset(o, val), [], [out])

    def recip(self, out, in_):
        o, i = _ap(out), _ap(in_)
        self.emit('dve', lambda e: e.reciprocal(out=o, in_=i), [in_], [out])

    def reduce(self, out, in_, op, axis=AX.X):
        o, i = _ap(out), _ap(in_)
        self.emit('dve', lambda e: e.tensor_reduce(out=o, in_=i, axis=axis, op=op), [in_], [out])

    def scan(self, out, d0, d1, initial, op0, op1):
        o, a, b, ini = _ap(out), _ap(d0), _ap(d1), _ap(initial)
        self.emit('dve', lambda e: e.tensor_tensor_scan(out=o, data0=a, data1=b, initial=ini, op0=op0, op1=op1),
                  [d0, d1, initial], [out])

    def mm(self, out, lhsT, rhs, start=True, stop=True, inc=None):
        o, l, r = _ap(out), _ap(lhsT), _ap(rhs)
        if inc is None:
            inc = stop
        self.emit('pe', lambda e: e.matmul(o, l, r, start=start, stop=stop), [lhsT, rhs], [out], inc=inc)

    def transpose(self, out, in_, ident, inc=True):
        o, i, d = _ap(out), _ap(in_), _ap(ident)
        self.emit('pe', lambda e: e.transpose(o, i, d), [in_, ident], [out], inc=inc)


S = 4096
D = 1024
TT = 512
NTT = S // TT
NP_ROWS = 5376
NT_COLS = 516
B_RWKV, B_DSA, B_RET, B_S5, B_X, B_GATE = 0, 1152, 2500, 3524, 4036, 4548
C_RWKV = 0
C_DQ, C_DQS, C_DK, C_DKS, C_IQ, C_IQS, C_IK, C_DG = 9, 11, 13, 15, 17, 19, 21, 22
C_RQ, C_RQS, C_RK, C_RKS, C_RG = 24, 26, 28, 30, 32
C_SU, C_SG = 34, 36
C_XQ, C_XG = 38, 40


def _swap_idx(base, nheads):
    idx = []
    for h in range(nheads):
        for j in range(64):
            idx.append(base + h * 64 + (j + 32) % 64)
    return idx


def proj_col_indices():
    r = lambda a, n: list(range(a, a + n))
    f = []
    f += r(B_RWKV, 1152)
    f += r(B_DSA, 256) + _swap_idx(B_DSA, 4)
    f += r(B_DSA + 256, 256) + _swap_idx(B_DSA + 256, 4)
    f += r(B_DSA + 768, 256) + _swap_idx(B_DSA + 768, 4)
    f += r(B_DSA + 1024, 64) + _swap_idx(B_DSA + 1024, 1)
    f += r(B_DSA + 1092, 256)
    f += r(B_RET, 256) + _swap_idx(B_RET, 4)
    f += r(B_RET + 256, 256) + _swap_idx(B_RET + 256, 4)
    f += r(B_RET + 768, 256)
    f += r(B_S5, 512)
    f += r(B_X, 512)
    assert len(f) == NP_ROWS
    t = r(B_DSA + 512, 256) + r(B_RET + 512, 256) + r(B_DSA + 1088, 4)
    assert len(t) == NT_COLS
    return np.array(f), np.array(t)


def load_weight_bf16(P, dst, src_dram, gcol, nk, ncols, blk=1344):
    src = src_dram.r("(k p) n -> p k n", p=128)
    stg = [P.sb([128, blk], F32, "wstg") for _ in range(2)]
    i = 0
    for k in range(nk):
        for c0 in range(0, ncols, blk):
            c1 = min(ncols, c0 + blk)
            s = stg[i % 2]
            P.dma(s[:, 0:c1 - c0], src[:, k, c0:c1])
            eng = 'dve' if i % 2 == 0 else 'pool'
            if gcol is not None:
                P.ts(dst[:, k, c0:c1], s[:, 0:c1 - c0], gcol[:, k:k + 1], ALU.mult, eng=eng)
            else:
                P.copy(dst[:, k, c0:c1], s[:, 0:c1 - c0], eng=eng)
            i += 1


def rsqrt_ps(P, out, src, scale, eps):
    P.ts(out, src, scale, ALU.mult, eps, ALU.add)
    P.act(out, out, AF.Sqrt)
    P.recip(out, out)


def rms_tile(P, xt, hT, sq, rstd, ones, nk, n, width):
    P.act(sq, xt, AF.Square)
    ps = P.ps()
    for k in range(nk):
        P.mm(ps[:, 0:width], ones, sq[:, k, :], start=(k == 0), stop=(k == nk - 1))
    rsqrt_ps(P, rstd, ps[:, 0:width], 1.0 / n, 1e-6)
    for k in range(nk):
        P.tt(hT[:, k, :], xt[:, k, :], rstd, ALU.mult)


def stage_P(P, l, Dm, xT):
    P.sb_off = SB_BASE
    npre = P.sb([128, 8], F32, "npre")
    P.dma(npre, Dm[f'npre{l}'])
    ones = P.sb([128, 128], F32, "ones")
    P.memset(ones, 1.0)
    wp = P.sb([128, 8, NP_ROWS], BF16, "wp")
    wt = P.sb([128, 8, NT_COLS], BF16, "wt")
    wf = P.sb([128, 8, 644], F32, "wf")
    wfsrc = Dm[f'wpf{l}'].r("(k p) n -> p k n", p=128)
    for k in range(8):
        P.dma(wf[:, k, :], wfsrc[:, k, :])
    for k in range(8):
        P.ts(wf[:, k, :], wf[:, k, :], npre[:, k:k + 1], ALU.mult, eng=('dve' if k % 2 else 'pool'))
    m0 = P.sb_off
    load_weight_bf16(P, wp, Dm[f'wp{l}'], npre, 8, NP_ROWS)
    load_weight_bf16(P, wt, Dm[f'wt{l}'], npre, 8, NT_COLS, blk=NT_COLS)
    P.barrier()
    P.sb_off = m0
    xts = [P.sb([128, 8, TT], F32, "xt") for _ in range(2)]
    sq = P.sb([128, 8, TT], F32, "sq")
    hTs = [P.sb([128, 8, TT], BF16, "hT") for _ in range(2)]
    rstd = P.sb([128, TT], F32, "rstd")
    ostg = [P.sb([128, 4, TT], F32, "ostg") for _ in range(2)]
    tstg = [P.sb([128, NT_COLS], F32, "tstg") for _ in range(2)]
    xsrc = xT.r("(k p) t -> p k t", p=128)
    cdst = Dm['colsT'].r("(c p) t -> p c t", p=128)
    ctok = Dm['colsTok']
    for tt in range(NTT):
        t0 = tt * TT
        xt, hT = xts[tt % 2], hTs[tt % 2]
        P.dma(xt, xsrc[:, :, t0:t0 + TT])
        rms_tile(P, xt, hT, sq, rstd, ones, 8, D, TT)
        for k in range(8):
            P.tt(sq[:, k, :], xt[:, k, :], rstd, ALU.mult, eng='pool')
        for c in range(42):
            ps = P.ps()
            for k in range(8):
                if C_IQ <= c <= C_IK:
                    P.mm(ps, wf[:, k, (c - C_IQ) * 128:(c - C_IQ + 1) * 128], sq[:, k, :], start=(k == 0), stop=(k == 7))
                else:
                    P.mm(ps, wp[:, k, c * 128:(c + 1) * 128], hT[:, k, :], start=(k == 0), stop=(k == 7))
            stg = ostg[(c // 4) % 2]
            if c % 3 == 2:
                P.copy(stg[:, c % 4, :], ps, eng='dve')
            else:
                P.act(stg[:, c % 4, :], ps, AF.Copy)
            if c % 4 == 3 or c == 41:
                c0 = c - c % 4
                P.dma(cdst[:, c0:c + 1, t0:t0 + TT], stg[:, 0:c % 4 + 1, :])
        for s in range(4):
            ps = P.ps()
            ps2 = P.ps()
            for k in range(8):
                P.mm(ps, hT[:, k, s * 128:(s + 1) * 128], wt[:, k, 0:512], start=(k == 0), stop=(k == 7))
            for k in range(8):
                P.mm(ps2[:, 0:4], sq[:, k, s * 128:(s + 1) * 128], wf[:, k, 640:644], start=(k == 0), stop=(k == 7))
            ts_ = tstg[s % 2]
            P.act(ts_[:, 0:512], ps, AF.Copy)
            P.copy(ts_[:, 512:516], ps2[:, 0:4], eng='dve')
            P.dma(ctok[t0 + s * 128:t0 + (s + 1) * 128, :], ts_)
    P.barrier()


BR_NAMES = ['rwkv', 'dsa', 'ret', 's5', 'xatt']


def stage_M(P, l, Dm, xT, xT_out):
    P.sb_off = SB_BASE
    npre = P.sb([128, 8], F32, "npre")
    npost = P.sb([128, 8], F32, "npost")
    P.dma(npre, Dm[f'npre{l}'])
    P.dma(npost, Dm[f'npost{l}'])
    ones = P.sb([128, 128], F32, "ones")
    P.memset(ones, 1.0)
    wg = P.sb([128, 8, 5120], BF16, "wg")
    wbr = P.sb([128, 10, 1024], BF16, "wbr")
    wout = P.sb([128, 8, 1024], BF16, "wout")
    m0 = P.sb_off
    load_weight_bf16(P, wg, Dm[f'wg{l}'], npre, 8, 5120, blk=1280)
    load_weight_bf16(P, wbr, Dm[f'wbr{l}'], None, 10, 1024, blk=1024)
    load_weight_bf16(P, wout, Dm[f'wout{l}'], None, 8, 1024, blk=1024)
    P.barrier()
    P.sb_off = m0
    xt = P.sb([128, 8, TT], F32, "xt")
    sq = P.sb([128, 8, TT], F32, "sq")
    hT = P.sb([128, 8, TT], BF16, "hT")
    rstd = P.sb([128, TT], F32, "rstd")
    yts = [P.sb([128, 2, TT], BF16, f"y{i}") for i in range(5)]
    sg = [P.sb([128, TT], F32, "sg") for _ in range(2)]
    term = [P.sb([128, TT], F32, "term") for _ in range(2)]
    macc = P.sb([128, TT], F32, "macc")
    mT = P.sb([128, 8, TT], BF16, "mT")
    osb = sq
    osq = P.sb([128, TT], F32, "osq")
    xsrc = xT.r("(k p) t -> p k t", p=128)
    xdst = xT_out.r("(k p) t -> p k t", p=128)
    for tt in range(NTT):
        t0 = tt * TT
        P.dma(xt, xsrc[:, :, t0:t0 + TT])
        for i in range(5):
            P.dma(yts[i], Dm[f'yT_{BR_NAMES[i]}'].r("(c p) t -> p c t", p=128)[:, :, t0:t0 + TT])
        rms_tile(P, xt, hT, sq, rstd, ones, 8, D, TT)
        j = 0
        for dc in range(8):
            for i in range(5):
                psg = P.ps()
                for k in range(8):
                    P.mm(psg, wg[:, k, i * 1024 + dc * 128:i * 1024 + (dc + 1) * 128], hT[:, k, :],
                         start=(k == 0), stop=(k == 7))
                psb = P.ps()
                for kk in range(2):
                    P.mm(psb, wbr[:, i * 2 + kk, dc * 128:(dc + 1) * 128], yts[i][:, kk, :],
                         start=(kk == 0), stop=(kk == 1))
                s_, t_ = sg[j % 2], term[j % 2]
                j += 1
                P.act(s_, psg, AF.Sigmoid)
                if i == 0:
                    P.tt(macc, s_, psb, ALU.mult)
                elif i < 4:
                    P.tt(t_, s_, psb, ALU.mult)
                    P.tt(macc, macc, t_, ALU.add, eng='pool')
                else:
                    P.tt(t_, s_, psb, ALU.mult)
                    P.tt(mT[:, dc, :], macc, t_, ALU.add, eng='pool')
        pss = P.ps_acc()
        for ec in range(8):
            ps = P.ps()
            for k in range(8):
                P.mm(ps, wout[:, k, ec * 128:(ec + 1) * 128], mT[:, k, :], start=(k == 0), stop=(k == 7))
            P.act(osb[:, ec, :], ps, AF.Copy)
            P.act(osq, ps, AF.Square)
            P.mm(pss, ones, osq, start=(ec == 0), stop=(ec == 7))
        rsqrt_ps(P, rstd, pss, 1.0 / D, 1e-6)
        for ec in range(8):
            P.stt(osb[:, ec, :], osb[:, ec, :], npost[:, ec:ec + 1], rstd, ALU.mult, ALU.mult)
            P.tt(xt[:, ec, :], xt[:, ec, :], osb[:, ec, :], ALU.add, eng='pool')
        P.dma(xdst[:, :, t0:t0 + TT], xt)
    P.barrier()


def stage_X(P, l, Dm):
    P.sb_off = SB_BASE
    nmem = P.sb([128, 8], F32, "nmem")
    P.dma(nmem, Dm[f'nmem{l}'])
    ones = P.sb([128, 128], F32, "ones")
    P.memset(ones, 1.0)
    wm = P.sb([128, 8, 512], BF16, "wm")
    m0 = P.sb_off
    load_weight_bf16(P, wm, Dm[f'wmem{l}'], nmem, 8, 512, blk=512)
    P.barrier()
    P.sb_off = m0
    mt = P.sb([128, 8, 256], F32, "mt")
    msq = P.sb([128, 8, 256], F32, "msq")
    mh = P.sb([128, 8, 256], BF16, "mh")
    mr = P.sb([128, 256], F32, "mr")
    P.dma(mt, Dm['memT'].r("(k p) m -> p k m", p=128))
    rms_tile(P, mt, mh, msq, mr, ones, 8, D, 256)
    kmT = [P.sb([128, 256], BF16, "kmT") for _ in range(2)]
    for c in range(2):
        ps = P.ps()
        for k in range(8):
            P.mm(ps[:, 0:256], wm[:, k, c * 128:(c + 1) * 128], mh[:, k, :], start=(k == 0), stop=(k == 7))
        P.copy(kmT[c], ps[:, 0:256])
    vpad = [[P.sb([128, 128], BF16, "vpad") for _ in range(4)] for _ in range(2)]
    opad = [P.sb([128, 128], BF16, "opad") for _ in range(2)]
    for hh in range(2):
        P.memset(opad[hh], 0.0)
        P.memset(opad[hh][:, hh * 64:(hh + 1) * 64], 1.0)
    for mc in range(2):
        ps = P.ps()
        for k in range(8):
            P.mm(ps[:, 0:256], mh[:, k, mc * 128:(mc + 1) * 128], wm[:, k, 256:512], start=(k == 0), stop=(k == 7))
        for h in range(4):
            hh = h % 2
            P.memset(vpad[mc][h], 0.0)
            P.copy(vpad[mc][h][:, hh * 64:(hh + 1) * 64], ps[:, h * 64:(h + 1) * 64])
    qf = P.sb([128, 2, TT], F32, "qf")
    gf = P.sb([128, 2, TT], F32, "gf")
    qb = P.sb([128, 2, TT], BF16, "qb")
    E = [[P.sb([128, TT], BF16, "E") for _ in range(2)] for _ in range(2)]
    rs = P.sb([128, TT], F32, "rs")
    o = P.sb([128, TT], F32, "o")
    sgl = P.sb([128, TT], F32, "sgl")
    yst = P.sb([128, 2, TT], BF16, "yst")
    csrc = Dm['colsT'].r("(c p) t -> p c t", p=128)
    ydst = Dm['yT_xatt'].r("(c p) t -> p c t", p=128)
    for tt in range(NTT):
        t0 = tt * TT
        P.dma(qf, csrc[:, C_XQ:C_XQ + 2, t0:t0 + TT])
        P.dma(gf, csrc[:, C_XG:C_XG + 2, t0:t0 + TT])
        P.copy(qb, qf)
        for p in range(2):
            for hh in range(2):
                for mc in range(2):
                    ps = P.ps()
                    P.mm(ps, kmT[p][hh * 64:(hh + 1) * 64, mc * 128:(mc + 1) * 128],
                         qb[hh * 64:(hh + 1) * 64, p, :])
                    P.act(E[hh][mc], ps, AF.Exp, scale=0.125)
            pso = P.ps()
            pss = P.ps()
            n = 0
            for hh in range(2):
                for mc in range(2):
                    P.mm(pso, vpad[mc][2 * p + hh], E[hh][mc], start=(n == 0), stop=(n == 3))
                    n += 1
            n = 0
            for hh in range(2):
                for mc in range(2):
                    P.mm(pss, opad[hh], E[hh][mc], start=(n == 0), stop=(n == 3))
                    n += 1
            P.recip(rs, pss)
            P.tt(o, pso, rs, ALU.mult)
            P.act(sgl, gf[:, p, :], AF.Silu)
            P.tt(yst[:, p, :], o, sgl, ALU.mult)
        P.dma(ydst[:, :, t0:t0 + TT], yst)
    P.barrier()


def dram_specs():
    sp = {
        'xT': ([D, S], F32, 'in'), 'memT': ([D, 256], F32, 'in'), 'pos': ([1, S], I32, 'in'),
        'colsT': ([NP_ROWS, S], F32, 'scratch'), 'colsTok': ([S, NT_COLS], F32, 'scratch'),
        'xT1': ([D, S], F32, 'scratch'),
    }
    for n in BR_NAMES:
        sp[f'yT_{n}'] = ([256, S], BF16, 'scratch')
    sp['iqR'] = ([256, S], F32, 'scratch')
    sp['ropeC'] = ([64, S], F32, 'scratch')
    sp['ropeS'] = ([64, S], F32, 'scratch')
    sp['ropeconst'] = ([64, 2], F32, 'in')
    sp['ident'] = ([128, 128], F32, 'in')
    sp['ret_idT'] = ([128, 4, 128], F32, 'in')
    sp['ret_qd'] = ([64, 4, 128], F32, 'in')
    sp['ret_kd'] = ([128, 4], F32, 'in')
    sp['ret_cd'] = ([64, 256], F32, 'in')
    sp['s5mask'] = ([128, 8, 8], F32, 'in')
    sp['rw_masks'] = ([64, 3, 64], F32, 'in')
    sp['dsa_cb'] = ([128, 128], F32, 'in')
    sp['dsa_pw'] = ([128, 32], F32, 'in')
    for l in range(2):
        sp[f'rwprm{l}'] = ([64, 8, 4], F32, 'in')
        sp[f'rwmu{l}'] = ([64, 18], F32, 'in')
        sp[f'rww2{l}'] = ([64, 256], F32, 'in')
        sp[f'rwa2{l}'] = ([64, 256], F32, 'in')
    sp['s5tau'] = ([128, 512], F32, 'in')
    for l in range(2):
        sp[f'retgn{l}'] = ([64, 4], F32, 'in')
        sp[f's5lam{l}'] = ([128, 8, 3], F32, 'in')
        sp[f's5b{l}'] = ([128, 8, 2, 16], F32, 'in')
        sp[f's5c{l}'] = ([128, 8, 2, 16], F32, 'in')
        sp[f's5d{l}'] = ([128, 2], F32, 'in')
        sp[f's5wglu{l}'] = ([256, 256], F32, 'in')
    for l in range(2):
        sp[f'wp{l}'] = ([D, NP_ROWS], F32, 'in')
        sp[f'wt{l}'] = ([D, NT_COLS], F32, 'in')
        sp[f'wpf{l}'] = ([D, 644], F32, 'in')
        sp[f'wg{l}'] = ([D, 5120], F32, 'in')
        sp[f'wbr{l}'] = ([1280, D], F32, 'in')
        sp[f'wout{l}'] = ([D, D], F32, 'in')
        sp[f'wmem{l}'] = ([D, 512], F32, 'in')
        for n in ['npre', 'npost', 'nmem']:
            sp[f'{n}{l}'] = ([128, 8], F32, 'in')
    return sp


def host_inputs(inputs, b):
    f_idx, t_idx = proj_col_indices()
    d = {}
    d['xT'] = np.ascontiguousarray(inputs['x'][b].T)
    d['memT'] = np.ascontiguousarray(inputs['mem'][b].T)
    d['pos'] = np.ascontiguousarray(inputs['positions'][b][None, :]).astype(np.int32)
    pk = lambda v: np.ascontiguousarray(v.reshape(8, 128).T)
    jj = np.arange(64)
    inv = (10000.0 ** (-(np.arange(32, dtype=np.float32)) / 32)).astype(np.float32)
    d['ropeconst'] = np.stack([inv[jj % 32], np.where(jj < 32, -1.0, 1.0)], 1).astype(np.float32)
    d['ident'] = np.eye(128, dtype=np.float32)
    d['ret_idT'], d['ret_qd'], d['ret_kd'], d['ret_cd'] = ret_consts()
    ii = np.arange(64)
    rm = np.zeros((64, 3, 64), np.float32)
    rm[:, 0, :] = (ii[None, :] > ii[:, None])
    rm[:, 1, :] = (ii[None, :] >= ii[:, None])
    rm[:, 2, :] = (ii[None, :] < ii[:, None])
    d['rw_masks'] = rm
    i128 = np.arange(128)
    d['dsa_pw'] = np.ascontiguousarray(np.broadcast_to((0.5 ** np.arange(1, 33, dtype=np.float64)).astype(np.float32)[None, :], (128, 32)))
    d['dsa_cb'] = np.where(i128[None, :] <= i128[:, None], 0.0, -1e30).astype(np.float32)
    for l in range(2):
        hd = lambda v: np.ascontiguousarray(v.reshape(4, 64).T)
        z = np.zeros((64, 4), np.float32)
        d[f'rwprm{l}'] = np.ascontiguousarray(np.stack([hd(inputs['rwkv_w0'][l]), hd(inputs['rwkv_a0'][l]), hd(inputs['rwkv_k_k'][l]),
                                   hd(inputs['rwkv_k_a'][l]), hd(inputs['rwkv_r_k'][l].reshape(256)), hd(inputs['rwkv_lnx_w'][l]),
                                   hd(inputs['rwkv_lnx_b'][l]), z], 1).astype(np.float32))
        d[f'rwmu{l}'] = np.ascontiguousarray(inputs['rwkv_mu'][l].reshape(18, 64).T)
        d[f'rww2{l}'] = np.ascontiguousarray(inputs['rwkv_w2'][l])
        d[f'rwa2{l}'] = np.ascontiguousarray(inputs['rwkv_a2'][l])
    sidx = np.arange(128)
    mk = np.zeros((128, 8, 8), np.float32)
    for j in range(8):
        mk[sidx, j, (2 * j + sidx // 64) % 8] = 1.0
    d['s5mask'] = mk
    d['s5tau'] = np.ascontiguousarray(np.broadcast_to(np.arange(1, 513, dtype=np.float32)[None, :], (128, 512)))
    sj = lambda a: np.ascontiguousarray(a.reshape((8, 128) + a.shape[1:]).swapaxes(0, 1))
    for l in range(2):
        d[f'retgn{l}'] = np.ascontiguousarray(inputs['ret_gn_w'][l].reshape(4, 64).T)
        lam3 = np.stack([inputs['s5_lam_re'][l].reshape(1024), inputs['s5_lam_im'][l].reshape(1024),
                         np.repeat(inputs['s5_log_dt'][l], 64)], 1).astype(np.float32)
        d[f's5lam{l}'] = sj(lam3)
        d[f's5b{l}'] = sj(np.stack([inputs['s5_b_re'][l].reshape(1024, 16), inputs['s5_b_im'][l].reshape(1024, 16)], 1))
        ct = lambda c: np.ascontiguousarray(c.transpose(0, 2, 1)).reshape(1024, 16)
        d[f's5c{l}'] = sj(np.stack([ct(inputs['s5_c_re'][l]), ct(inputs['s5_c_im'][l])], 1))
        d[f's5d{l}'] = np.ascontiguousarray(inputs['s5_d'][l].reshape(2, 128).T)
        d[f's5wglu{l}'] = np.ascontiguousarray(inputs['s5_w_glu'][l])
    for l in range(2):
        w = inputs['w_in'][l]
        d[f'wp{l}'] = np.ascontiguousarray(w[:, f_idx])
        d[f'wt{l}'] = np.ascontiguousarray(w[:, t_idx])
        d[f'wpf{l}'] = np.ascontiguousarray(w[:, np.concatenate([f_idx[C_IQ * 128:(C_IK + 1) * 128], t_idx[512:516]])])
        d[f'wg{l}'] = np.ascontiguousarray(w[:, B_GATE:B_GATE + 5120])
        d[f'wbr{l}'] = np.ascontiguousarray(inputs['w_branch'][l].reshape(1280, D))
        d[f'wout{l}'] = np.ascontiguousarray(inputs['w_out'][l])
        d[f'wmem{l}'] = np.ascontiguousarray(inputs['w_mem_kv'][l])
        d[f'npre{l}'] = pk(inputs['norm_pre'][l])
        d[f'npost{l}'] = pk(inputs['norm_post'][l])
        d[f'nmem{l}'] = pk(inputs['norm_mem'][l])
    return d


STAGE_FNS = {}


def build(plan, ext_in=(), ext_out=()):
    nc = bass.Bass("TRN2", target_bir_lowering=False)
    P = Prog(nc)
    Dm = {}
    used_in = []
    for name, (shape, dtype, role) in dram_specs().items():
        if role == 'in' or name in ext_in:
            kind = "ExternalInput"
            used_in.append(name)
        elif name in ext_out:
            kind = "ExternalOutput"
        else:
            kind = "Internal"
        Dm[name] = P.dram(name, shape, dtype, kind=kind)
    Dm['outT'] = P.dram('outT', [D, S], F32, kind="ExternalOutput")
    for st, l in plan:
        xin = Dm['xT'] if l == 0 else Dm['xT1']
        xout = Dm['xT1'] if l == 0 else Dm['outT']
        if st == 'P':
            stage_P(P, l, Dm, xin)
        elif st == 'M':
            stage_M(P, l, Dm, xin, xout)
        elif st == 'X':
            stage_X(P, l, Dm)
        else:
            STAGE_FNS[st](P, l, Dm)
    P.finish()
    return nc, P, used_in


def sin_reduced(P, out, ang, kq, ki, m1):
    P.ts(kq, ang, 1.0 / (2 * math.pi), ALU.mult)
    P.copy(ki, kq)
    P.copy(kq, ki)
    P.stt(ang, kq, -2 * math.pi, ang, ALU.mult, ALU.add)
    P.ts(m1, ang, math.pi, ALU.is_gt, -2 * math.pi, ALU.mult)
    P.tt(ang, ang, m1, ALU.add)
    P.ts(m1, ang, -math.pi, ALU.is_lt, 2 * math.pi, ALU.mult)
    P.tt(ang, ang, m1, ALU.add)
    P.act(out, ang, AF.Sin)


def stage_R(P, l, Dm):
    P.sb_off = SB_BASE
    W = 2048
    rc = P.sb([64, 2], F32, "rc")
    P.dma(rc, Dm['ropeconst'])
    posi = P.sb([64, W], I32, "posi")
    posf = P.sb([64, W], F32, "posf")
    ang = P.sb([64, W], F32, "ang")
    kq = P.sb([64, W], F32, "kq")
    ki = P.sb([64, W], I32, "ki")
    m1 = P.sb([64, W], F32, "m1")
    o = P.sb([64, W], F32, "o")
    for half in range(S // W):
        sl = slice(half * W, (half + 1) * W)
        P.dma(posi, Dm['pos'][:, sl].m(lambda x: x.to_broadcast([64, W])))
        P.copy(posf, posi)
        P.ts(ang, posf, rc[:, 0:1], ALU.mult)
        sin_reduced(P, o, ang, kq, ki, m1)
        P.ts(o, o, rc[:, 1:2], ALU.mult)
        P.dma(Dm['ropeS'][:, sl], o)
        P.ts(ang, posf, rc[:, 0:1], ALU.mult, math.pi / 2, ALU.add)
        sin_reduced(P, o, ang, kq, ki, m1)
        P.dma(Dm['ropeC'][:, sl], o)
    P.barrier()


def rope_heads(P, dst, Dm, c_base, c_swap, ropeC, ropeS, nheads=4, scale=None, dram_dst=None):
    a = P.sb([64, nheads, TT], F32, "ra")
    b = P.sb([64, nheads, TT], F32, "rb")
    if dram_dst is not None:
        ro = [P.sb([64, nheads, TT], F32, "ro") for _ in range(2)]
    src = Dm['colsT']
    for tt in range(NTT):
        sl = slice(tt * TT, (tt + 1) * TT)
        rb_ = c_base * 128 if isinstance(c_base, int) else c_base[0]
        rs_ = c_swap * 128 if isinstance(c_swap, int) else c_swap[0]
        P.dma(a, src[rb_:rb_ + nheads * 64, sl].r("(h d) t -> d h t", d=64))
        P.dma(b, src[rs_:rs_ + nheads * 64, sl].r("(h d) t -> d h t", d=64))
        cb = ropeC[:, sl].m(lambda x: x.unsqueeze(1).to_broadcast([64, nheads, TT]))
        sb_ = ropeS[:, sl].m(lambda x: x.unsqueeze(1).to_broadcast([64, nheads, TT]))
        P.tt(a, a, cb, ALU.mult)
        P.tt(b, b, sb_, ALU.mult, eng='pool')
        if dram_dst is None:
            P.tt(dst[:, :, sl], a, b, ALU.add)
        else:
            o_ = ro[tt % 2]
            P.tt(o_, a, b, ALU.add)
            P.dma(dram_dst.r("(h d) t -> d h t", d=64)[:, :, sl], o_)


RET_LOGG = [math.log(1.0 - math.exp(v)) for v in np.linspace(math.log(1.0 / 32), math.log(1.0 / 512), 4)]


def ret_consts():
    j = np.arange(128, dtype=np.float64)
    idT = np.zeros((128, 4, 128), np.float32)
    qd = np.zeros((64, 4, 128), np.float32)
    kd = np.zeros((128, 4), np.float32)
    cd = np.zeros((64, 256), np.float32)
    for h in range(4):
        lg = RET_LOGG[h]
        rel = j[None, :] - j[:, None]
        idT[:, h, :] = np.where(rel >= 0, np.exp(lg * np.maximum(rel, 0.0)), 0.0) * 0.125
        qd[:, h, :] = np.exp(lg * (j + 1.0))[None, :]
        kd[:, h] = np.exp(lg * (127.0 - j)) * 0.125
        cd[:, h * 64:(h + 1) * 64] = math.exp(lg * 128)
    return idT, qd, kd, cd


def stage_RET(P, l, Dm):
    P.sb_off = SB_BASE
    ropeC = P.sb([64, S], F32, "ropeC")
    ropeS = P.sb([64, S], F32, "ropeS")
    P.dma(ropeC, Dm['ropeC'])
    P.dma(ropeS, Dm['ropeS'])
    idT = P.sb([128, 4, 128], F32, "idT")
    qd = P.sb([64, 4, 128], F32, "qd")
    kd = P.sb([128, 4], F32, "kd")
    cd = P.sb([64, 256], F32, "cd")
    gn = P.sb([64, 4], F32, "gn")
    identb = P.sb([128, 128], BF16, "identb")
    identf = P.sb([128, 128], F32, "identf")
    ones64 = P.sb([64, 64], F32, "ones64")
    P.dma(idT, Dm['ret_idT'])
    P.dma(qd, Dm['ret_qd'])
    P.dma(kd, Dm['ret_kd'])
    P.dma(cd, Dm['ret_cd'])
    P.dma(gn, Dm[f'retgn{l}'])
    P.dma(identf, Dm['ident'])
    P.copy(identb, identf)
    P.memset(ones64, 1.0 / 64)
    qT = P.sb([64, 4, S], BF16, "qT")
    kT = P.sb([64, 4, S], BF16, "kT")
    qdT = P.sb([64, 4, S], BF16, "qdT")
    m0 = P.sb_off
    rope_heads(P, qT, Dm, C_RQ, C_RQS, ropeC, ropeS)
    rope_heads(P, kT, Dm, C_RK, C_RKS, ropeC, ropeS)
    for c in range(32):
        cs = slice(c * 128, (c + 1) * 128)
        P.tt(qdT[:, :, cs], qT[:, :, cs], qd, ALU.mult, eng=('dve' if c % 2 else 'pool'))
    P.barrier()
    P.sb_off = m0
    Vt = P.sb([128, 32, 256], BF16, "Vt")
    Kd = P.sb([128, 32, 256], BF16, "Kd")
    vst = [P.sb([128, 4, 256], F32, "vst") for _ in range(2)]
    vsrc = Dm['colsTok'].r("(c p) n -> p c n", p=128)
    for i in range(8):
        v_ = vst[i % 2]
        P.dma(v_, vsrc[:, i * 4:(i + 1) * 4, 256:512])
        P.copy(Vt[:, i * 4:(i + 1) * 4, :], v_, eng=('dve' if i % 2 else 'pool'))
    for c in range(32):
        cs = slice(c * 128, (c + 1) * 128)
        ps = P.ps()
        psb = ps.bitcast(BF16)
        for h in range(4):
            P.transpose(psb[:, h * 64:(h + 1) * 64], kT[:, h, cs], identb[0:64, 0:64])
        P.tt(Kd[:, c, :].r("p (h d) -> p h d", h=4), psb[:, 0:256].r("p (h d) -> p h d", h=4),
             kd.m(lambda x: x.unsqueeze(2).to_broadcast([128, 4, 64])), ALU.mult)
    R = P.sb([64, 256], F32, "R")
    Rb = P.sb([64, 256], BF16, "Rb")
    P.memset(R, 0.0)
    P.memset(Rb, 0.0)
    AT = [P.sb([128, 4, 128], BF16, "AT") for _ in range(2)]
    Osb = P.sb([64, 512], F32, "Osb")
    dd = P.sb([64, 512], F32, "dd")
    dsq = P.sb([64, 512], F32, "dsq")
    rstd = P.sb([64, 512], F32, "rstd")
    gt = [P.sb([64, 4, 128], F32, "gt") for _ in range(2)]
    sg = P.sb([64, 4, 128], F32, "sg")
    yo = [P.sb([64, 4, 128], BF16, "yo") for _ in range(2)]
    gsrc = Dm['colsT'][C_RG * 128:C_RG * 128 + 256, :].r("(h d) t -> d h t", d=64)
    ydst = Dm['yT_ret'].r("(h d) t -> d h t", d=64)
    for c in range(32):
        cs = slice(c * 128, (c + 1) * 128)
        g_ = gt[c % 2]
        P.dma(g_, gsrc[:, :, cs])
        psA = P.ps()
        for h in range(4):
            P.mm(psA[:, h * 128:(h + 1) * 128], kT[:, h, cs], qT[:, h, cs])
        at = AT[c % 2]
        P.tt(at, psA.r("p (h q) -> p h q", h=4), idT, ALU.mult)
        psO = P.ps()
        for h in range(4):
            P.mm(psO[0:64, h * 128:(h + 1) * 128], Vt[:, c, h * 64:(h + 1) * 64], at[:, h, :], start=True, stop=False, inc=False)
            P.mm(psO[0:64, h * 128:(h + 1) * 128], Rb[:, h * 64:(h + 1) * 64], qdT[:, h, cs], start=False, stop=True)
        psKV = P.ps()
        for h in range(4):
            P.mm(psKV[0:64, h * 64:(h + 1) * 64], Kd[:, c, h * 64:(h + 1) * 64], Vt[:, c, h * 64:(h + 1) * 64])
        P.tt(R, R, cd, ALU.mult)
        P.tt(R, R, psKV[0:64, 0:256], ALU.add)
        P.copy(Rb, R, eng='pool')
        P.act(Osb, psO[0:64, :], AF.Copy)
        psM = P.ps()
        P.mm(psM[0:64, :], ones64, Osb)
        P.tt(dd, Osb, psM[0:64, :], ALU.subtract)
        P.act(dsq, dd, AF.Square)
        psV = P.ps()
        P.mm(psV[0:64, :], ones64, dsq)
        P.ts(rstd, psV[0:64, :], 1e-6, ALU.add)
        P.act(rstd, rstd, AF.Sqrt)
        P.recip(rstd, rstd)
        P.tt(dd, dd, rstd, ALU.mult)
        P.tt(dd.r("p (h q) -> p h q", h=4), dd.r("p (h q) -> p h q", h=4),
             gn.m(lambda x: x.unsqueeze(2).to_broadcast([64, 4, 128])), ALU.mult)
        P.act(sg, g_, AF.Silu)
        y_ = yo[c % 2]
        P.tt(y_, dd.r("p (h q) -> p h q", h=4), sg, ALU.mult)
        P.dma(ydst[:, :, cs], y_)
    P.barrier()


STAGE_FNS['R'] = stage_R
STAGE_FNS['RET'] = stage_RET


def stage_S5(P, l, Dm):
    P.sb_off = SB_BASE
    W = TT
    lam = P.sb([128, 8, 3], F32, "lam")
    bsb = P.sb([128, 8, 2, 16], F32, "bsb")
    csb = P.sb([128, 8, 2, 16], F32, "csb")
    msk = P.sb([128, 8, 8], F32, "msk")
    tau = P.sb([128, W], F32, "tau")
    dsk = P.sb([128, 2], F32, "dsk")
    identf = P.sb([128, 128], F32, "identf")
    P.dma(lam, Dm[f's5lam{l}'])
    P.dma(bsb, Dm[f's5b{l}'])
    P.dma(csb, Dm[f's5c{l}'])
    P.dma(msk, Dm['s5mask'])
    P.dma(tau, Dm['s5tau'])
    P.dma(dsk, Dm[f's5d{l}'])
    P.dma(identf, Dm['ident'])
    wglu = P.sb([128, 2, 256], BF16, "wglu")
    cosT = P.sb([128, 8, W], F32, "cosT")
    sinT = P.sb([128, 8, W], F32, "sinT")
    mag = P.sb([128, 8], F32, "mag")
    BT = P.sb([128, 8, 2, 128], BF16, "BT")
    CX = P.sb([128, 8, 2, 128], BF16, "CX")
    m0 = P.sb_off
    load_weight_bf16(P, wglu, Dm[f's5wglu{l}'], None, 2, 256, blk=256)
    lr = P.sb([128, 8], F32, "lr")
    li = P.sb([128, 8], F32, "li")
    dt = P.sb([128, 8], F32, "dt")
    th = P.sb([128, 8], F32, "th")
    P.ts(lr, lam[:, :, 0], -1e-4, ALU.min)
    P.copy(li, lam[:, :, 1])
    P.act(dt, lam[:, :, 2], AF.Exp)
    P.tt(th, li, dt, ALU.mult)
    P.tt(mag, lr, dt, ALU.mult)
    P.act(mag, mag, AF.Exp)
    ang = P.sb([128, W], F32, "ang")
    kq = P.sb([128, W], F32, "kq")
    ki = P.sb([128, W], I32, "ki")
    m1 = P.sb([128, W], F32, "m1")
    for j in range(8):
        P.ts(ang, tau, th[:, j:j + 1], ALU.mult)
        sin_reduced(P, sinT[:, j, :], ang, kq, ki, m1)
        P.ts(ang, tau, th[:, j:j + 1], ALU.mult, math.pi / 2, ALU.add)
        sin_reduced(P, cosT[:, j, :], ang, kq, ki, m1)
    abr = P.sb([128, 8], F32, "abr")
    abi = P.sb([128, 8], F32, "abi")
    den = P.sb([128, 8], F32, "den")
    t8 = P.sb([128, 8], F32, "t8")
    fre = P.sb([128, 8], F32, "fre")
    fim = P.sb([128, 8], F32, "fim")
    P.tt(abr, mag, cosT[:, :, 0], ALU.mult)
    P.tt(abi, mag, sinT[:, :, 0], ALU.mult)
    P.ts(abr, abr, -1.0, ALU.add)
    P.tt(den, lr, lr, ALU.mult)
    P.tt(t8, li, li, ALU.mult)
    P.tt(den, den, t8, ALU.add)
    P.recip(den, den)
    P.tt(fre, abr, lr, ALU.mult)
    P.tt(t8, abi, li, ALU.mult)
    P.tt(fre, fre, t8, ALU.add)
    P.tt(fre, fre, den, ALU.mult)
    P.tt(fim, abi, lr, ALU.mult)
    P.tt(t8, abr, li, ALU.mult)
    P.tt(fim, fim, t8, ALU.subtract)
    P.tt(fim, fim, den, ALU.mult)
    bb = P.sb([128, 8, 2, 16], F32, "bb")
    tb = P.sb([128, 8, 16], F32, "tb")
    bc16 = lambda v: v.m(lambda x: x.unsqueeze(2).to_broadcast([128, 8, 16]))
    P.tt(bb[:, :, 0, :], bsb[:, :, 0, :], bc16(fre), ALU.mult)
    P.tt(tb, bsb[:, :, 1, :], bc16(fim), ALU.mult)
    P.tt(bb[:, :, 0, :], bb[:, :, 0, :], tb, ALU.subtract)
    P.tt(bb[:, :, 1, :], bsb[:, :, 1, :], bc16(fre), ALU.mult)
    P.tt(tb, bsb[:, :, 0, :], bc16(fim), ALU.mult)
    P.tt(bb[:, :, 1, :], bb[:, :, 1, :], tb, ALU.add)
    P.ts(csb[:, :, 1, :], csb[:, :, 1, :], -1.0, ALU.mult)
    bx = P.sb([128, 8, 16], F32, "bx")
    for j in range(8):
        mj = msk[:, j, :].m(lambda x: x.unsqueeze(2).to_broadcast([128, 8, 16]))
        for ri in range(2):
            P.tt(bx, bb[:, j, ri, :].m(lambda x: x.unsqueeze(1).to_broadcast([128, 8, 16])), mj, ALU.mult)
            ps = P.ps()
            P.transpose(ps[:, 0:128], bx.r("p a b -> p (a b)"), identf)
            P.copy(BT[:, j, ri, :], ps[:, 0:128])
            P.tt(CX[:, j, ri, :].r("p (a b) -> p a b", a=8),
                 csb[:, j, ri, :].m(lambda x: x.unsqueeze(1).to_broadcast([128, 8, 16])), mj, ALU.mult)
    P.barrier()
    P.sb_off = m0
    A = P.sb([128, 8, W], F32, "A")
    B = P.sb([128, 8, W], F32, "B")
    t1 = P.sb([128, 8, W], F32, "t1")
    t2 = P.sb([128, 8, W], F32, "t2")
    wre = P.sb([128, 8, W], F32, "wre")
    wim = P.sb([128, 8, W], F32, "wim")
    xre = P.sb([128, 8, W], BF16, "xre")
    xim = P.sb([128, 8, W], BF16, "xim")
    cre = P.sb([128, 8], F32, "cre")
    cim = P.sb([128, 8], F32, "cim")
    P.memset(cre, 0.0)
    P.memset(cim, 0.0)
    uf = P.sb([128, 2, W], F32, "uf")
    ub = P.sb([128, 2, W], BF16, "ub")
    gf = P.sb([128, 2, W], F32, "gf")
    y = P.sb([128, 2, W], F32, "y")
    y2 = P.sb([128, 2, W], F32, "y2")
    glb = P.sb([128, 2, W], BF16, "glb")
    yo = P.sb([128, 2, W], BF16, "yo")
    csrc = Dm['colsT'].r("(c p) t -> p c t", p=128)
    ydst = Dm['yT_s5'].r("(c p) t -> p c t", p=128)
    for tt in range(NTT):
        sl = slice(tt * W, (tt + 1) * W)
        P.dma(uf, csrc[:, C_SU:C_SU + 2, sl])
        P.dma(gf, csrc[:, C_SG:C_SG + 2, sl])
        P.copy(ub, uf, eng='pool')
        for j in range(8):
            for ri, dst in ((0, A), (1, B)):
                ps = P.ps()
                P.mm(ps, BT[:, j, ri, :], ub[:, j // 4, :])
                P.act(dst[:, j, :], ps, AF.Copy)
        P.tt(t1, A, cosT, ALU.mult)
        P.tt(t2, B, sinT, ALU.mult, eng='pool')
        P.tt(t1, t1, t2, ALU.add)
        P.tt(t2, A, sinT, ALU.mult, eng='pool')
        P.tt(B, B, cosT, ALU.mult)
        P.tt(t2, B, t2, ALU.subtract, eng='pool')
        for j in range(8):
            mb = mag[:, j:j + 1].bc([128, W])
            P.scan(wre[:, j, :], mb, t1[:, j, :], cre[:, j:j + 1], ALU.mult, ALU.add)
            P.scan(wim[:, j, :], mb, t2[:, j, :], cim[:, j:j + 1], ALU.mult, ALU.add)
        P.tt(t1, wre, cosT, ALU.mult)
        P.tt(A, wim, sinT, ALU.mult, eng='pool')
        P.tt(xre, t1, A, ALU.subtract)
        P.tt(cre, t1[:, :, W - 1], A[:, :, W - 1], ALU.subtract)
        P.tt(t2, wre, sinT, ALU.mult, eng='pool')
        P.tt(B, wim, cosT, ALU.mult)
        P.tt(xim, t2, B, ALU.add, eng='pool')
        P.tt(cim, t2[:, :, W - 1], B[:, :, W - 1], ALU.add)
        for jc in range(2):
            ps = P.ps()
            n = 0
            for j in range(4 * jc, 4 * jc + 4):
                for ri, xx in ((0, xre), (1, xim)):
                    P.mm(ps, CX[:, j, ri, :], xx[:, j, :], start=(n == 0), stop=(n == 7))
                    n += 1
            P.stt(y[:, jc, :], uf[:, jc, :], dsk[:, jc:jc + 1], ps, ALU.mult, ALU.add)
        P.tt(y2, y, y, ALU.mult)
        P.ts(y2, y2, 0.044715, ALU.mult, 1.0, ALU.add)
        P.tt(y2, y2, y, ALU.mult)
        P.act(y2, y2, AF.Sigmoid, scale=1.5957691216057308)
        P.tt(y, y, y2, ALU.mult)
        P.copy(glb, y, eng='pool')
        for oc in range(2):
            ps = P.ps()
            for kc in range(2):
                P.mm(ps, wglu[:, kc, oc * 128:(oc + 1) * 128], glb[:, kc, :], start=(kc == 0), stop=(kc == 1))
            P.act(y2[:, oc, :], ps, AF.Sigmoid)
        P.tt(y, y, y2, ALU.mult)
        P.act(y2, gf, AF.Silu)
        P.tt(yo, y, y2, ALU.mult)
        P.dma(ydst[:, :, sl], yo)
    P.barrier()


STAGE_FNS['S5'] = stage_S5


import os
RW_DEBUG = int(os.environ.get('RW_DEBUG', '3'))


def stage_RWKV(P, l, Dm):
    P.sb_off = SB_BASE
    W = 256
    H4 = 4
    NCH = W // 64
    HC = H4 * NCH
    prm = P.sb([64, 8, 4], F32, "prm")
    mu = P.sb([64, 18], F32, "mu")
    w2 = P.sb([64, 256], F32, "w2")
    a2 = P.sb([64, 256], F32, "a2")
    msks = P.sb([64, 3, 64], F32, "msks")
    identf = P.sb([128, 128], F32, "identf")
    ones64 = P.sb([64, 64], F32, "ones64")
    onesw = P.sb([64, 1], F32, "onesw")
    P.dma(prm, Dm[f'rwprm{l}'])
    P.dma(mu, Dm[f'rwmu{l}'])
    P.dma(w2, Dm[f'rww2{l}'])
    P.dma(a2, Dm[f'rwa2{l}'])
    P.dma(msks, Dm['rw_masks'])
    P.dma(identf, Dm['ident'])
    P.memset(ones64, 1.0)
    P.memset(onesw, 1.0)
    id64 = identf[0:64, 0:64]
    hb = lambda v, n=W: v.m(lambda x: x.unsqueeze(2).to_broadcast([64, H4, n]))
    mb = lambda k: msks[:, k, :].m(lambda x: x.unsqueeze(1).to_broadcast([64, H4, 64]))
    idb = id64.m(lambda x: x.unsqueeze(1).to_broadcast([64, H4, 64]))
    cin = P.sb([64, 18, W + 1], F32, "cin")
    cs = P.sb([64, 18, W], F32, "cs")
    f = lambda nm: P.sb([64, H4, W], F32, nm)
    twl = P.sb([64, W], F32, "twl")
    sgz, av, kx, t0, kp, beta = f("sgz"), f("av"), f("kx"), f("t0"), f("kp"), f("beta")
    kkn, lw, cw, e1, e2 = f("kkn"), f("lw"), f("cw"), f("e1"), f("e2")
    rt, at, bt, kt, Bh, Kh = f("rt"), f("at"), f("bt"), f("kt"), f("Bh"), f("Kh")
    bonus, Yt = f("bonus"), f("Yt")
    base = P.sb([64, HC], F32, "base")
    cwC = P.sb([64, HC], F32, "cwC")
    gC = P.sb([64, HC], F32, "gC")
    S0 = P.sb([64, H4, 64], F32, "S0")
    P.memset(S0, 0.0)
    g4 = lambda nm: P.sb([64, H4, 64], F32, nm)
    X, XT, PaT, AakT, ArbT, ArkT = g4("X"), g4("XT"), g4("PaT"), g4("AakT"), g4("ArbT"), g4("ArkT")
    Vt, BhT, KhT, Wsb, Usb = g4("Vt"), g4("BhT"), g4("KhT"), g4("Wsb"), g4("Usb")
    yo = P.sb([64, H4, W], BF16, "yo")
    src = Dm['colsT'][0:1152, :].r("(g d) t -> d g t", d=64)
    ydst = Dm['yT_rwkv'].r("(h d) t -> d h t", d=64)
    ps4 = lambda ps: ps[0:64, 0:256].r("p (h x) -> p h x", h=H4)
    for tt in range(S // W):
        t_0 = tt * W
        if tt == 0:
            P.dma(cin[:, :, 1:W + 1], src[:, :, t_0:t_0 + W])
            P.memset(cin[:, :, 0:1], 0.0)
        else:
            P.dma(cin, src[:, :, t_0 - 1:t_0 + W])
        P.tt(cs, cin[:, :, 0:W], cin[:, :, 1:W + 1], ALU.subtract)
        P.tt(cs, cs, mu.m(lambda x: x.unsqueeze(2).to_broadcast([64, 18, W])), ALU.mult)
        P.tt(cs, cs, cin[:, :, 1:W + 1], ALU.add)
        Rr, Kk, Vv, G = cs[:, 0:4, :], cs[:, 4:8, :], cs[:, 8:12, :], cs[:, 14:18, :]
        P.act(twl, cs[:, 12, :], AF.Tanh)
        for h in range(H4):
            ps = P.ps()
            P.mm(ps[0:64, 0:W], w2[:, h * 64:(h + 1) * 64], twl)
            P.act(sgz[:, h, :], ps[0:64, 0:W], AF.Sigmoid, bias=prm[:, 0, h:h + 1])
            ps = P.ps()
            P.mm(ps[0:64, 0:W], a2[:, h * 64:(h + 1) * 64], cs[:, 13, :])
            P.act(av[:, h, :], ps[0:64, 0:W], AF.Sigmoid, bias=prm[:, 1, h:h + 1])
        P.tt(kx, Kk, hb(prm[:, 2, :]), ALU.mult)
        P.tt(t0, kx, kx, ALU.mult, eng='pool')
        for h in range(H4):
            ps = P.ps()
            P.mm(ps[0:64, 0:W], ones64, t0[:, h, :])
            P.ts(kkn[:, h, :], ps[0:64, 0:W], 1e-24, ALU.add)
        P.act(kkn, kkn, AF.Sqrt)
        P.recip(kkn, kkn)
        P.tt(kkn, kkn, kx, ALU.mult)
        P.ts(t0, av, -1.0, ALU.add)
        P.tt(t0, t0, hb(prm[:, 3, :]), ALU.mult)
        P.stt(kp, t0, 1.0, Kk, ALU.add, ALU.mult)
        P.tt(beta, kkn, av, ALU.mult, eng='pool')
        P.tt(t0, Rr, kp, ALU.mult)
        P.tt(t0, t0, hb(prm[:, 4, :]), ALU.mult)
        for h in range(H4):
            ps = P.ps()
            P.mm(ps[0:64, 0:W], ones64, t0[:, h, :])
            P.tt(bonus[:, h, :], ps[0:64, 0:W], Vv[:, h, :], ALU.mult)
        P.ts(lw, sgz, -math.exp(-0.5), ALU.mult)
        for h in range(H4):
            P.scan(cw[:, h, :], onesw[:, 0:1].bc([64, W]), lw[:, h, :], 0.0, ALU.mult, ALU.add)
        cw3 = cw.r("p h (c i) -> p (h c) i", i=64)
        P.memset(base, 0.0)
        P.copy(base.r("p (h c) -> p h c", h=H4)[:, :, 1:NCH], cw.r("p h (c i) -> p h c i", i=64)[:, :, 0:NCH - 1, 63])
        P.tt(cw3, cw3, base.m(lambda x: x.unsqueeze(2).to_broadcast([64, HC, 64])), ALU.subtract)
        P.copy(cwC, cw3[:, :, 63])
        P.act(gC, cwC, AF.Exp)
        P.act(e1, cw, AF.Exp)
        P.tt(rt, Rr, e1, ALU.mult)
        P.act(e1, cw, AF.Exp, scale=-1.0)
        P.tt(bt, beta, e1, ALU.mult)
        P.tt(kt, kp, e1, ALU.mult, eng='pool')
        P.tt(e2, cw, lw, ALU.subtract)
        P.act(e2, e2, AF.Exp)
        P.stt(at, kkn, -1.0, e2, ALU.mult, ALU.mult)
        e13 = e1.r("p h (c i) -> p (h c) i", i=64)
        P.tt(e13, cw3, cwC.m(lambda x: x.unsqueeze(2).to_broadcast([64, HC, 64])), ALU.subtract)
        P.act(e1, e1, AF.Exp, scale=-1.0)
        P.tt(Bh, beta, e1, ALU.mult)
        P.tt(Kh, kp, e1, ALU.mult, eng='pool')
        for c in range(NCH if RW_DEBUG >= 1 else 0):
            sl = slice(c * 64, (c + 1) * 64)
            def mm4(lh, rh):
                ps = P.ps()
                for h in range(H4):
                    P.mm(ps[0:64, h * 64:(h + 1) * 64], lh[:, h, sl], rh[:, h, sl], inc=(h == 3))
                return ps4(ps)
            P.tt(X, mm4(at, bt), mb(2), ALU.mult)
            P.tt(XT, mm4(bt, at), mb(0), ALU.mult)
            P.tt(AakT, mm4(kt, at), mb(0), ALU.mult)
            P.tt(ArbT, mm4(bt, rt), mb(1), ALU.mult)
            P.tt(ArkT, mm4(kt, rt), mb(1), ALU.mult)
            P.tt(PaT, XT, idb, ALU.add, eng='pool')
            for srcT, dstT, eng in ((Vv, Vt, 'act'), (Bh, BhT, 'dve'), (Kh, KhT, 'act')):
                ps = P.ps()
                for h in range(H4):
                    P.transpose(ps[0:64, h * 64:(h + 1) * 64], srcT[:, h, sl], id64)
                if eng == 'act':
                    P.act(dstT, ps4(ps), AF.Copy)
                else:
                    P.copy(dstT, ps4(ps))
            for it in range(5 if RW_DEBUG >= 2 else 0):
                psx = P.ps()
                psxt = P.ps()
                for h in range(H4):
                    P.mm(psx[0:64, h * 64:(h + 1) * 64], XT[:, h, :], X[:, h, :], inc=(h == 3))
                for h in range(H4):
                    P.mm(psxt[0:64, h * 64:(h + 1) * 64], X[:, h, :], XT[:, h, :], inc=(h == 3))
                P.act(X, ps4(psx), AF.Copy)
                P.copy(XT, ps4(psxt))
                psp = P.ps()
                for h in range(H4):
                    P.mm(psp[0:64, h * 64:(h + 1) * 64], X[:, h, :], PaT[:, h, :], inc=(h == 3))
                P.tt(PaT, PaT, ps4(psp), ALU.add)
            if RW_DEBUG < 3:
                continue
            psw = P.ps()
            for h in range(H4):
                o_ = psw[0:64, h * 64:(h + 1) * 64]
                P.mm(o_, at[:, h, sl], S0[:, h, :], start=True, stop=False, inc=False)
                P.mm(o_, AakT[:, h, :], Vt[:, h, :], start=False, stop=True, inc=(h == 3))
            P.act(Wsb, ps4(psw), AF.Copy)
            psu = P.ps()
            for h in range(H4):
                P.mm(psu[0:64, h * 64:(h + 1) * 64], PaT[:, h, :], Wsb[:, h, :], inc=(h == 3))
            P.copy(Usb, ps4(psu))
            psy = P.ps()
            for h in range(H4):
                o_ = psy[0:64, h * 64:(h + 1) * 64]
                P.mm(o_, S0[:, h, :], rt[:, h, sl], start=True, stop=False, inc=False)
                P.mm(o_, Usb[:, h, :], ArbT[:, h, :], start=False, stop=False, inc=False)
                P.mm(o_, Vt[:, h, :], ArkT[:, h, :], start=False, stop=True, inc=(h == 3))
            P.act(Yt[:, :, sl], ps4(psy), AF.Copy)
            pss = P.ps()
            for h in range(H4):
                o_ = pss[0:64, h * 64:(h + 1) * 64]
                P.mm(o_, BhT[:, h, :], Usb[:, h, :], start=True, stop=False, inc=False)
                P.mm(o_, KhT[:, h, :], Vt[:, h, :], start=False, stop=True, inc=(h == 3))
            gcb = gC.r("p (h c) -> p h c", h=H4)[:, :, c].m(lambda x: x.unsqueeze(2).to_broadcast([64, H4, 64]))
            P.tt(S0, S0, gcb, ALU.mult)
            P.tt(S0, S0, ps4(pss), ALU.add)
        for h in range(H4):
            ps = P.ps()
            P.mm(ps[0:64, 0:W], ones64, Yt[:, h, :])
            P.stt(e1[:, h, :], ps[0:64, 0:W], -1.0 / 64, Yt[:, h, :], ALU.mult, ALU.add)
        P.tt(e2, e1, e1, ALU.mult, eng='pool')
        for h in range(H4):
            ps = P.ps()
            P.mm(ps[0:64, 0:W], ones64, e2[:, h, :])
            P.ts(t0[:, h, :], ps[0:64, 0:W], 1.0 / 64, ALU.mult, 64e-5, ALU.add)
        P.act(t0, t0, AF.Sqrt)
        P.recip(t0, t0)
        P.tt(e1, e1, t0, ALU.mult)
        P.tt(e1, e1, hb(prm[:, 5, :]), ALU.mult)
        P.tt(e1, e1, hb(prm[:, 6, :]), ALU.add)
        P.tt(e1, e1, bonus, ALU.add)
        P.act(e2, G, AF.Silu)
        P.tt(yo, e1, e2, ALU.mult)
        P.dma(ydst[:, :, t_0:t_0 + W], yo)
    P.barrier()


STAGE_FNS['RWKV'] = stage_RWKV


N_BISECT = 20


def stage_DSA(P, l, Dm):
    P.sb_off = SB_BASE
    qT = P.sb([64, 4, S], BF16, "qT")
    kT = P.sb([64, 4, S], BF16, "kT")
    ikT = P.sb([64, 1, S], F32, "ikT")
    m0 = P.sb_off
    ropeC = P.sb([64, S], F32, "ropeC")
    ropeS = P.sb([64, S], F32, "ropeS")
    P.dma(ropeC, Dm['ropeC'])
    P.dma(ropeS, Dm['ropeS'])
    rope_heads(P, qT, Dm, C_DQ, C_DQS, ropeC, ropeS)
    rope_heads(P, kT, Dm, C_DK, C_DKS, ropeC, ropeS)
    rope_heads(P, None, Dm, C_IQ, C_IQS, ropeC, ropeS, dram_dst=Dm['iqR'])
    rope_heads(P, ikT, Dm, (C_IK * 128,), (C_IK * 128 + 64,), ropeC, ropeS, nheads=1)
    P.barrier()
    P.sb_off = m0
    identf = P.sb([128, 128], F32, "identf")
    identb = P.sb([128, 128], BF16, "identb")
    cb = P.sb([128, 128], F32, "cb")
    P.dma(identf, Dm['ident'])
    P.copy(identb, identf)
    P.dma(cb, Dm['dsa_cb'])
    Vaug = P.sb([128, 32, 4, 65], BF16, "Vaug")
    iwt = P.sb([128, 32, 4], F32, "iwt")
    vst = [P.sb([128, 4, 256], F32, "vst") for _ in range(2)]
    tsrc = Dm['colsTok'].r("(c p) n -> p c n", p=128)
    P.memset(Vaug[:, :, :, 64:65], 1.0)
    for i in range(8):
        v_ = vst[i % 2]
        P.dma(v_, tsrc[:, i * 4:(i + 1) * 4, 0:256])
        P.copy(Vaug[:, i * 4:(i + 1) * 4, :, 0:64], v_.r("p c (h d) -> p c h d", h=4), eng=('dve' if i % 2 else 'pool'))
    P.dma(iwt, tsrc[:, :, 512:516])
    P.ts(iwt, iwt, 1.0 / 16, ALU.mult)
    score = P.sb([128, S], F32, "score")
    junk = P.sb([128, S], BF16, "junk")
    mask01 = P.sb([128, S], BF16, "mask01")
    maskT = P.sb([128, 32, 128], BF16, "maskT")
    rl = [P.sb([128, 512], F32, "rl") for _ in range(4)]
    E = [P.sb([128, 512], BF16, "E") for _ in range(2)]
    lo = P.sb([128, 1], F32, "lo")
    hi = P.sb([128, 1], F32, "hi")
    mid = P.sb([128, 1], F32, "mid")
    cnt = P.sb([128, 1], F32, "cnt")
    sel = P.sb([128, 1], F32, "sel")
    dlt = P.sb([128, 1], F32, "dlt")
    stp = P.sb([128, 32], F32, "stp")
    pw = P.sb([128, 32], F32, "pw")
    P.dma(pw, Dm['dsa_pw'])
    zt = P.sb([128, S], F32, "zt")
    cz = P.sb([128, S], F32, "cz")
    nz = P.sb([128, 1], F32, "nz")
    npos = P.sb([128, 1], F32, "npos")
    flag = P.sb([128, 1], F32, "flag")
    f2 = P.sb([128, 1], F32, "f2")
    rr = P.sb([128, 1], F32, "rr")
    onesw = P.sb([128, 1], F32, "onesw")
    P.memset(onesw, 1.0)
    osb = P.sb([128, 4, 64], F32, "osb")
    rs = P.sb([128, 4, 1], F32, "rs")
    gt = [P.sb([128, 2, 128], F32, "gt") for _ in range(2)]
    sg = P.sb([128, 2, 128], F32, "sg")
    yo = [P.sb([128, 2, 128], BF16, "yo") for _ in range(2)]
    iqt = [P.sb([64, 4, 128], F32, "iqt") for _ in range(2)]
    iqsrc = Dm['iqR'].r("(h d) t -> d h t", d=64)
    gsrc = Dm['colsT'].r("(c p) t -> p c t", p=128)
    ydst = Dm['yT_dsa'].r("(c p) t -> p c t", p=128)
    ne = 0
    for i in range(32):
        qs = slice(i * 128, (i + 1) * 128)
        Nk = 128 * (i + 1)
        g_ = gt[i % 2]
        P.dma(g_, gsrc[:, C_DG:C_DG + 2, qs])
        iq_ = iqt[i % 2]
        P.dma(iq_, iqsrc[:, :, qs])
        for k0 in range(0, Nk, 512):
            kw = min(512, Nk - k0)
            pss = []
            for h in range(4):
                ps = P.ps()
                P.mm(ps[:, 0:kw], iq_[:, h, :], ikT[:, 0, k0:k0 + kw], inc=(h == 3))
                pss.append(ps)
            for h in range(4):
                P.act(rl[h][:, 0:kw], pss[h][:, 0:kw], AF.Relu)
            P.ts(score[:, k0:k0 + kw], rl[0][:, 0:kw], iwt[:, i, 0:1], ALU.mult)
            for h in range(1, 4):
                P.stt(score[:, k0:k0 + kw], rl[h][:, 0:kw], iwt[:, i, h:h + 1], score[:, k0:k0 + kw], ALU.mult, ALU.add)
        P.tt(score[:, i * 128:Nk], score[:, i * 128:Nk], cb, ALU.add)
        if Nk > 256:
            P.reduce(hi, score[:, 0:Nk], ALU.max)
            P.reduce(lo, score[:, 0:i * 128], ALU.min)
            P.tt(dlt, hi, lo, ALU.subtract)
            P.ts(dlt, dlt, 2.0, ALU.add)
            P.ts(stp, pw, dlt[:, 0:1], ALU.mult)
            P.stt(mid, dlt, 0.5, lo, ALU.mult, ALU.add)
            P.ts(mid, mid, -1.0, ALU.add)
            for it in range(N_BISECT):
                P.ts(junk[:, 0:Nk], score[:, 0:Nk], mid[:, 0:1], ALU.is_ge, 0.0, ALU.add, accum=cnt)
                if it < N_BISECT - 1:
                    P.ts(sel, cnt, 255.5, ALU.is_ge, 0.5, ALU.subtract)
                    P.stt(mid, sel, stp[:, it:it + 1], mid, ALU.mult, ALU.add)
                else:
                    P.ts(sel, cnt, 255.5, ALU.is_ge, 1.0, ALU.subtract)
                    P.stt(lo, sel, stp[:, it:it + 1], mid, ALU.mult, ALU.add)
        else:
            P.memset(lo, -1e29)
        P.ts(zt[:, 0:Nk], score[:, 0:Nk], 0.0, ALU.is_equal, 0.0, ALU.add, accum=nz)
        P.ts(junk[:, 0:Nk], score[:, 0:Nk], 0.0, ALU.is_gt, 0.0, ALU.add, accum=npos)
        P.ts(flag, npos, 255.5, ALU.is_lt)
        P.tt(f2, npos, nz, ALU.add)
        P.ts(f2, f2, 255.5, ALU.is_ge)
        P.tt(flag, flag, f2, ALU.mult)
        P.ts(rr, npos, -1.0, ALU.mult, 256.0, ALU.add)
        P.scan(cz[:, 0:Nk], onesw[:, 0:1].bc([128, Nk]), zt[:, 0:Nk], 0.0, ALU.mult, ALU.add)
        P.ts(cz[:, 0:Nk], cz[:, 0:Nk], rr[:, 0:1], ALU.is_le, flag[:, 0:1], ALU.mult)
        P.tt(zt[:, 0:Nk], zt[:, 0:Nk], cz[:, 0:Nk], ALU.mult)
        P.ts(f2, flag, -1.0, ALU.mult, 1.0, ALU.add)
        P.tt(lo, lo, f2, ALU.mult)
        P.stt(lo, flag, 1e-30, lo, ALU.mult, ALU.add)
        P.ts(mask01[:, 0:Nk], score[:, 0:Nk], lo[:, 0:1], ALU.is_ge)
        P.tt(mask01[:, 0:Nk], mask01[:, 0:Nk], zt[:, 0:Nk], ALU.add)
        for c0 in range(0, i + 1, 4):
            nc_ = min(4, i + 1 - c0)
            ps = P.ps()
            psb = ps.bitcast(BF16)
            for cl in range(nc_):
                c = c0 + cl
                P.transpose(psb[:, cl * 128:(cl + 1) * 128], mask01[:, c * 128:(c + 1) * 128], identb, inc=(cl == nc_ - 1))
            P.act(maskT[:, c0:c0 + nc_, :], psb[:, 0:nc_ * 128].r("p (c q) -> p c q", q=128), AF.Copy)
        psO = P.ps_acc()
        for h in range(4):
            for c0 in range(0, i + 1, 4):
                nc_ = min(4, i + 1 - c0)
                ps = P.ps()
                for cl in range(nc_):
                    c = c0 + cl
                    P.mm(ps[:, cl * 128:(cl + 1) * 128], kT[:, h, c * 128:(c + 1) * 128], qT[:, h, qs], inc=(cl == nc_ - 1))
                e_ = E[ne % 2]
                ne += 1
                P.act(e_[:, 0:nc_ * 128], ps[:, 0:nc_ * 128], AF.Exp, scale=0.125)
                P.tt(e_[:, 0:nc_ * 128].r("p (c q) -> p c q", q=128), e_[:, 0:nc_ * 128].r("p (c q) -> p c q", q=128),
                     maskT[:, c0:c0 + nc_, :], ALU.mult, eng=('dve' if ne % 2 else 'pool'))
                for cl in range(nc_):
                    c = c0 + cl
                    P.mm(psO[:, h * 65:(h + 1) * 65], e_[:, cl * 128:(cl + 1) * 128], Vaug[:, c, h, :],
                         start=(c == 0), stop=(c == i), inc=(cl == nc_ - 1))
        pv = psO[:, 0:260].r("p (h x) -> p h x", h=4)
        P.recip(rs, pv[:, :, 64:65])
        P.tt(osb, pv[:, :, 0:64], rs.m(lambda x: x.to_broadcast([128, 4, 64])), ALU.mult)
        P.act(sg, g_, AF.Silu)
        y_ = yo[i % 2]
        for p in range(2):
            ps = P.ps()
            P.transpose(ps[:, 0:128], osb[:, 2 * p:2 * p + 2, :].r("p a b -> p (a b)"), identf)
            P.tt(y_[:, p, :], ps[:, 0:128], sg[:, p, :], ALU.mult)
        P.dma(ydst[:, :, qs], y_)
    P.barrier()


STAGE_FNS['DSA'] = stage_DSA


FULL_PLAN = [('R', 0)] + [(st, l) for l in range(2) for st in ('P', 'X', 'RET', 'S5', 'RWKV', 'DSA', 'M')]


def kernel(**inputs):
    inputs = {k: np.asarray(v) for k, v in inputs.items()}
    nb = inputs['x'].shape[0]
    nc, P, used_in = build(FULL_PLAN)
    in_maps = []
    for b in range(nb):
        d = host_inputs(inputs, b)
        in_maps.append({k: v for k, v in d.items() if k in used_in})
    res = run_bass_kernel_spmd(nc, in_maps, core_ids=list(range(nb)))
    out = np.stack([np.ascontiguousarray(np.asarray(r['outT']).T) for r in res.results], 0)
    return out.astype(np.float32)
```

```python
from contextlib import ExitStack
import math
import numpy as np
import ml_dtypes
import concourse.bass as bass
import concourse.mybir as mybir
from concourse.bass_utils import run_bass_kernel_spmd

F32 = mybir.dt.float32
BF16 = mybir.dt.bfloat16
I32 = mybir.dt.int32
AF = mybir.ActivationFunctionType
ALU = mybir.AluOpType
AX = mybir.AxisListType

ENGS = ['sp', 'act', 'dve', 'pool', 'pe']
EPOCH = 16000
NDMASEM = 24
SB_BASE = 16640
SBUF_BYTES = 229000


class Buf:
    __slots__ = ('name', 'wev', 'rev', 'tracked')

    def __init__(self, name, tracked=True):
        self.name = name
        self.wev = {}
        self.rev = {}
        self.tracked = tracked


class V:
    __slots__ = ('buf', 'ap')

    def __init__(self, buf, ap):
        self.buf = buf
        self.ap = ap

    def __getitem__(self, k):
        return V(self.buf, self.ap[k])

    def m(self, fn):
        return V(self.buf, fn(self.ap))

    def r(self, s, **kw):
        return V(self.buf, self.ap.rearrange(s, **kw))

    def bc(self, shape):
        return V(self.buf, self.ap.to_broadcast(list(shape)))

    def bitcast(self, dt):
        return V(self.buf, self.ap.bitcast(dt))

    @property
    def shape(self):
        return tuple(self.ap.shape)


def _ap(x):
    return x.ap if isinstance(x, V) else x


class Prog:
    def __init__(self, nc):
        self.nc = nc
        self.q = {e: [] for e in ENGS}
        self.cnt = {e: 0 for e in ENGS}
        self.noinc = {e: False for e in ENGS}
        self.known = {e: {} for e in ENGS}
        self.dma_n = {e: 0 for e in ENGS}
        self.nbar = 0
        self.bufs = []
        self.sb_off = SB_BASE
        self.sb_id = 0
        self.sb_mark = 0
        self.psum = []
        for i in range(8):
            h = nc.alloc_psum_tensor(f"ps{i}", [128, 512], F32)
            self.psum.append(V(self._newbuf(f"ps{i}"), h[:]))
        self.ps_rr = 0

    def _newbuf(self, name, tracked=True):
        b = Buf(name, tracked)
        if tracked:
            self.bufs.append(b)
        return b

    def sb(self, shape, dtype=F32, name="t"):
        esz = {F32: 4, BF16: 2, I32: 4}[dtype]
        per = esz * int(np.prod(shape[1:]))
        per = (per + 63) // 64 * 64
        off = self.sb_off
        assert off + per <= SBUF_BYTES, f"SBUF overflow {name} {off}+{per}"
        self.sb_off += per
        self.sb_id += 1
        nm = f"{name}_{self.sb_id}"
        h = self.nc.alloc_sbuf_tensor_at(nm, list(shape), dtype, offset=off)
        return V(self._newbuf(nm), h[:])

    def mark(self):
        self.sb_mark = self.sb_off

    def release(self):
        self.sb_off = self.sb_mark

    def ps(self):
        v = self.psum[self.ps_rr % 7]
        self.ps_rr += 1
        return v

    def ps_acc(self):
        return self.psum[7]

    def dram(self, name, shape, dtype=F32, kind="Internal"):
        h = self.nc.dram_tensor(name, list(shape), dtype, kind=kind)
        return V(self._newbuf(name, tracked=False), h.ap())

    def _collect(self, eng, reads, writes, extra=None):
        waits = {}

        def need(evs, skip_own):
            for sk, v in evs.items():
                if skip_own and sk == eng:
                    continue
                if waits.get(sk, 0) < v:
                    waits[sk] = v
        for x in reads:
            if x.buf.tracked:
                need(x.buf.wev, False)
        for x in writes:
            if x.buf.tracked:
                need(x.buf.wev, True)
                need(x.buf.rev, True)
        if extra:
            need(extra, False)
        kn = self.known[eng]
        wl = []
        for sk, v in waits.items():
            if kn.get(sk, 0) < v:
                kn[sk] = v
                wl.append((sk, v))
        return wl

    def emit(self, eng, fn, reads=(), writes=(), inc=True):
        reads = [x for x in reads if isinstance(x, V)]
        writes = [x for x in writes if isinstance(x, V)]
        wl = self._collect(eng, reads, writes)
        idx = self.cnt[eng] + 1
        self.cnt[eng] = idx
        self.q[eng].append((wl, fn, ('c', idx)))
        for x in reads:
            b = x.buf
            if b.tracked and b.rev.get(eng, 0) < idx:
                b.rev[eng] = idx
        for x in writes:
            b = x.buf
            if b.tracked:
                b.wev = {eng: idx}
                b.rev = {}

    def dma(self, out, in_, eng='sp'):
        n = self.dma_n[eng]
        self.dma_n[eng] = n + 1
        slot, k = n % NDMASEM, n // NDMASEM
        sk = ('dma', eng, slot)
        val = 16 * (k + 1)
        extra = {sk: 16 * k} if k > 0 else None
        wl = self._collect(eng, [in_], [out], extra)
        oa, ia = out.ap, in_.ap
        self.q[eng].append((wl, lambda e: e.dma_start(out=oa, in_=ia), ('d', sk)))
        b = in_.buf
        if b.tracked:
            b.rev[sk] = val
        b = out.buf
        if b.tracked:
            b.wev = {sk: val}
            b.rev = {}

    def barrier(self):
        waits = {}
        for e in ENGS:
            if e != 'sp' and self.cnt[e] > 0:
                waits[e] = self.cnt[e]
            n = self.dma_n[e]
            for slot in range(min(n, NDMASEM)):
                k = (n - 1 - slot) // NDMASEM
                waits[('dma', e, slot)] = 16 * (k + 1)
        kn = self.known['sp']
        wl = []
        for sk, v in waits.items():
            if kn.get(sk, 0) < v:
                kn[sk] = v
                wl.append((sk, v))
        self.nbar += 1
        nb = self.nbar
        bk = ('bar', 0)
        self.q['sp'].append((wl, None, ('b', bk)))
        for e in ENGS:
            if e != 'sp':
                self.q[e].append(([(bk, nb)], None, None))
                for sk, v in waits.items():
                    if self.known[e].get(sk, 0) < v:
                        self.known[e][sk] = v
        for b in self.bufs:
            b.wev = {}
            b.rev = {}

    def finish(self):
        self.barrier()
        nc = self.nc
        targets = {e: set() for e in ENGS}
        for e in ENGS:
            for wl, fn, tag in self.q[e]:
                for sk, v in wl:
                    if isinstance(sk, str):
                        targets[sk].add(v)
        rank = {e: {v: r + 1 for r, v in enumerate(sorted(targets[e]))} for e in ENGS}
        self.n_inc = {e: len(rank[e]) for e in ENGS}
        keys = set()

        def semkey(sk, v):
            if isinstance(sk, str):
                r = rank[sk][v]
                return ((sk, (r - 1) // EPOCH), (r - 1) % EPOCH + 1)
            return (sk, v)
        prog = {e: [] for e in ENGS}
        for e in ENGS:
            for wl, fn, tag in self.q[e]:
                w2 = [semkey(sk, v) for sk, v in wl]
                inc = None
                if tag is not None:
                    if tag[0] == 'c':
                        if tag[1] in rank[e]:
                            r = rank[e][tag[1]]
                            inc = ((e, (r - 1) // EPOCH), 1)
                    elif tag[0] == 'd':
                        inc = (tag[1], 16)
                    elif tag[0] == 'b':
                        inc = (tag[1], 1)
                for k_, _ in w2:
                    keys.add(k_)
                if inc is not None:
                    keys.add(inc[0])
                prog[e].append((w2, fn, inc, tag))
        stack = ExitStack()
        sems = {}
        for i, sk in enumerate(sorted(keys, key=str)):
            sems[sk] = stack.enter_context(nc.semaphore(f"s{i}"))
        self.nsem = len(sems)

        def mk(en):
            def body(e):
                for wl, fn, inc, tag in prog[en]:
                    for sk, v in wl:
                        e.wait_ge(sems[sk], v)
                    if fn is None:
                        if tag is not None and tag[0] == 'b':
                            e.sem_inc(sems[inc[0]], inc[1])
                        continue
                    ins = fn(e)
                    if inc is not None:
                        ins.then_inc(sems[inc[0]], inc[1])
            return body
        with stack:
            with nc.Block() as block:
                block.sync(mk('sp'))
                block.scalar(mk('act'))
                block.vector(mk('dve'))
                block.gpsimd(mk('pool'))
                block.tensor(mk('pe'))

    def act(self, out, in_, func, bias=None, scale=1.0, accum=None):
        o, i, b, s, a = _ap(out), _ap(in_), _ap(bias), _ap(scale), _ap(accum)
        kw = {}
        if b is not None:
            kw['bias'] = b
        if a is not None:
            kw['accum_out'] = a
        self.emit('act', lambda e: e.activation(out=o, in_=i, func=func, scale=s, **kw),
                  [in_, bias, scale], [out, accum])

    def ts(self, out, in0, s1, op0, s2=None, op1=None, accum=None, eng='dve'):
        o, i, a1, a2, ac = _ap(out), _ap(in0), _ap(s1), _ap(s2), _ap(accum)
        kw = {}
        if op1 is not None:
            kw['op1'] = op1
        if ac is not None:
            kw['accum_out'] = ac
        self.emit(eng, lambda e: e.tensor_scalar(out=o, in0=i, scalar1=a1, scalar2=a2, op0=op0, **kw),
                  [in0, s1, s2], [out, accum])

    def tt(self, out, in0, in1, op, eng='dve'):
        o, a, b = _ap(out), _ap(in0), _ap(in1)
        self.emit(eng, lambda e: e.tensor_tensor(out=o, in0=a, in1=b, op=op), [in0, in1], [out])

    def stt(self, out, in0, scalar, in1, op0, op1, eng='dve'):
        o, a, s, b = _ap(out), _ap(in0), _ap(scalar), _ap(in1)
        self.emit(eng, lambda e: e.scalar_tensor_tensor(out=o, in0=a, scalar=s, in1=b, op0=op0, op1=op1),
                  [in0, scalar, in1], [out])

    def copy(self, out, in_, eng='dve'):
        o, i = _ap(out), _ap(in_)
        if eng == 'act':
            self.emit('act', lambda e: e.copy(out=o, in_=i), [in_], [out])
        else:
            self.emit(eng, lambda e: e.tensor_copy(out=o, in_=i), [in_], [out])

    def memset(self, out, val, eng='dve'):
        o = _ap(out)
        self.emit(eng, lambda e: e.memset(o, val), [], [out])

    def recip(self, out, in_):
        o, i = _ap(out), _ap(in_)
        self.emit('dve', lambda e: e.reciprocal(out=o, in_=i), [in_], [out])

    def reduce(self, out, in_, op, axis=AX.X):
        o, i = _ap(out), _ap(in_)
        self.emit('dve', lambda e: e.tensor_reduce(out=o, in_=i, axis=axis, op=op), [in_], [out])

    def scan(self, out, d0, d1, initial, op0, op1):
        o, a, b, ini = _ap(out), _ap(d0), _ap(d1), _ap(initial)
        self.emit('dve', lambda e: e.tensor_tensor_scan(out=o, data0=a, data1=b, initial=ini, op0=op0, op1=op1),
                  [d0, d1, initial], [out])

    def mm(self, out, lhsT, rhs, start=True, stop=True, inc=None):
        o, l, r = _ap(out), _ap(lhsT), _ap(rhs)
        if inc is None:
            inc = stop
        self.emit('pe', lambda e: e.matmul(o, l, r, start=start, stop=stop), [lhsT, rhs], [out], inc=inc)

    def transpose(self, out, in_, ident, inc=True):
        o, i, d = _ap(out), _ap(in_), _ap(ident)
        self.emit('pe', lambda e: e.transpose(o, i, d), [in_, ident], [out], inc=inc)


S = 4096
D = 1024
TT = 512
NTT = S // TT
NP_ROWS = 5376
NT_COLS = 516
B_RWKV, B_DSA, B_RET, B_S5, B_X, B_GATE = 0, 1152, 2500, 3524, 4036, 4548
C_RWKV = 0
C_DQ, C_DQS, C_DK, C_DKS, C_IQ, C_IQS, C_IK, C_DG = 9, 11, 13, 15, 17, 19, 21, 22
C_RQ, C_RQS, C_RK, C_RKS, C_RG = 24, 26, 28, 30, 32
C_SU, C_SG = 34, 36
C_XQ, C_XG = 38, 40


def _swap_idx(base, nheads):
    idx = []
    for h in range(nheads):
        for j in range(64):
            idx.append(base + h * 64 + (j + 32) % 64)
    return idx


def proj_col_indices():
    r = lambda a, n: list(range(a, a + n))
    f = []
    f += r(B_RWKV, 1152)
    f += r(B_DSA, 256) + _swap_idx(B_DSA, 4)
    f += r(B_DSA + 256, 256) + _swap_idx(B_DSA + 256, 4)
    f += r(B_DSA + 768, 256) + _swap_idx(B_DSA + 768, 4)
    f += r(B_DSA + 1024, 64) + _swap_idx(B_DSA + 1024, 1)
    f += r(B_DSA + 1092, 256)
    f += r(B_RET, 256) + _swap_idx(B_RET, 4)
    f += r(B_RET + 256, 256) + _swap_idx(B_RET + 256, 4)
    f += r(B_RET + 768, 256)
    f += r(B_S5, 512)
    f += r(B_X, 512)
    assert len(f) == NP_ROWS
    t = r(B_DSA + 512, 256) + r(B_RET + 512, 256) + r(B_DSA + 1088, 4)
    assert len(t) == NT_COLS
    return np.array(f), np.array(t)


def load_weight_bf16(P, dst, src_dram, gcol, nk, ncols, blk=1344):
    src = src_dram.r("(k p) n -> p k n", p=128)
    stg = [P.sb([128, blk], F32, "wstg") for _ in range(2)]
    i = 0
    for k in range(nk):
        for c0 in range(0, ncols, blk):
            c1 = min(ncols, c0 + blk)
            s = stg[i % 2]
            P.dma(s[:, 0:c1 - c0], src[:, k, c0:c1])
            eng = 'dve' if i % 2 == 0 else 'pool'
            if gcol is not None:
                P.ts(dst[:, k, c0:c1], s[:, 0:c1 - c0], gcol[:, k:k + 1], ALU.mult, eng=eng)
            else:
                P.copy(dst[:, k, c0:c1], s[:, 0:c1 - c0], eng=eng)
            i += 1


def rsqrt_ps(P, out, src, scale, eps):
    P.ts(out, src, scale, ALU.mult, eps, ALU.add)
    P.act(out, out, AF.Sqrt)
    P.recip(out, out)


def rms_tile(P, xt, hT, sq, rstd, ones, nk, n, width):
    P.act(sq, xt, AF.Square)
    ps = P.ps()
    for k in range(nk):
        P.mm(ps[:, 0:width], ones, sq[:, k, :], start=(k == 0), stop=(k == nk - 1))
    rsqrt_ps(P, rstd, ps[:, 0:width], 1.0 / n, 1e-6)
    for k in range(nk):
        P.tt(hT[:, k, :], xt[:, k, :], rstd, ALU.mult)


def stage_P(P, l, Dm, xT):
    P.sb_off = SB_BASE
    npre = P.sb([128, 8], F32, "npre")
    P.dma(npre, Dm[f'npre{l}'])
    ones = P.sb([128, 128], F32, "ones")
    P.memset(ones, 1.0)
    wp = P.sb([128, 8, NP_ROWS], BF16, "wp")
    wt = P.sb([128, 8, NT_COLS], BF16, "wt")
    wf = P.sb([128, 8, 644], F32, "wf")
    wfsrc = Dm[f'wpf{l}'].r("(k p) n -> p k n", p=128)
    for k in range(8):
        P.dma(wf[:, k, :], wfsrc[:, k, :])
    for k in range(8):
        P.ts(wf[:, k, :], wf[:, k, :], npre[:, k:k + 1], ALU.mult, eng=('dve' if k % 2 else 'pool'))
    m0 = P.sb_off
    load_weight_bf16(P, wp, Dm[f'wp{l}'], npre, 8, NP_ROWS)
    load_weight_bf16(P, wt, Dm[f'wt{l}'], npre, 8, NT_COLS, blk=NT_COLS)
    P.barrier()
    P.sb_off = m0
    xts = [P.sb([128, 8, TT], F32, "xt") for _ in range(2)]
    sq = P.sb([128, 8, TT], F32, "sq")
    hTs = [P.sb([128, 8, TT], BF16, "hT") for _ in range(2)]
    rstd = P.sb([128, TT], F32, "rstd")
    ostg = [P.sb([128, 4, TT], F32, "ostg") for _ in range(2)]
    tstg = [P.sb([128, NT_COLS], F32, "tstg") for _ in range(2)]
    xsrc = xT.r("(k p) t -> p k t", p=128)
    cdst = Dm['colsT'].r("(c p) t -> p c t", p=128)
    ctok = Dm['colsTok']
    for tt in range(NTT):
        t0 = tt * TT
        xt, hT = xts[tt % 2], hTs[tt % 2]
        P.dma(xt, xsrc[:, :, t0:t0 + TT])
        rms_tile(P, xt, hT, sq, rstd, ones, 8, D, TT)
        for k in range(8):
            P.tt(sq[:, k, :], xt[:, k, :], rstd, ALU.mult, eng='pool')
        for c in range(42):
            ps = P.ps()
            for k in range(8):
                if C_IQ <= c <= C_IK:
                    P.mm(ps, wf[:, k, (c - C_IQ) * 128:(c - C_IQ + 1) * 128], sq[:, k, :], start=(k == 0), stop=(k == 7))
                else:
                    P.mm(ps, wp[:, k, c * 128:(c + 1) * 128], hT[:, k, :], start=(k == 0), stop=(k == 7))
            stg = ostg[(c // 4) % 2]
            if c % 3 == 2:
                P.copy(stg[:, c % 4, :], ps, eng='dve')
            else:
                P.act(stg[:, c % 4, :], ps, AF.Copy)
            if c % 4 == 3 or c == 41:
                c0 = c - c % 4
                P.dma(cdst[:, c0:c + 1, t0:t0 + TT], stg[:, 0:c % 4 + 1, :])
        for s in range(4):
            ps = P.ps()
            ps2 = P.ps()
            for k in range(8):
                P.mm(ps, hT[:, k, s * 128:(s + 1) * 128], wt[:, k, 0:512], start=(k == 0), stop=(k == 7))
            for k in range(8):
                P.mm(ps2[:, 0:4], sq[:, k, s * 128:(s + 1) * 128], wf[:, k, 640:644], start=(k == 0), stop=(k == 7))
            ts_ = tstg[s % 2]
            P.act(ts_[:, 0:512], ps, AF.Copy)
            P.copy(ts_[:, 512:516], ps2[:, 0:4], eng='dve')
            P.dma(ctok[t0 + s * 128:t0 + (s + 1) * 128, :], ts_)
    P.barrier()


BR_NAMES = ['rwkv', 'dsa', 'ret', 's5', 'xatt']


def stage_M(P, l, Dm, xT, xT_out):
    P.sb_off = SB_BASE
    npre = P.sb([128, 8], F32, "npre")
    npost = P.sb([128, 8], F32, "npost")
    P.dma(npre, Dm[f'npre{l}'])
    P.dma(npost, Dm[f'npost{l}'])
    ones = P.sb([128, 128], F32, "ones")
    P.memset(ones, 1.0)
    wg = P.sb([128, 8, 5120], BF16, "wg")
    wbr = P.sb([128, 10, 1024], BF16, "wbr")
    wout = P.sb([128, 8, 1024], BF16, "wout")
    m0 = P.sb_off
    load_weight_bf16(P, wg, Dm[f'wg{l}'], npre, 8, 5120, blk=1280)
    load_weight_bf16(P, wbr, Dm[f'wbr{l}'], None, 10, 1024, blk=1024)
    load_weight_bf16(P, wout, Dm[f'wout{l}'], None, 8, 1024, blk=1024)
    P.barrier()
    P.sb_off = m0
    xt = P.sb([128, 8, TT], F32, "xt")
    sq = P.sb([128, 8, TT], F32, "sq")
    hT = P.sb([128, 8, TT], BF16, "hT")
    rstd = P.sb([128, TT], F32, "rstd")
    yts = [P.sb([128, 2, TT], BF16, f"y{i}") for i in range(5)]
    sg = [P.sb([128, TT], F32, "sg") for _ in range(2)]
    term = [P.sb([128, TT], F32, "term") for _ in range(2)]
    macc = P.sb([128, TT], F32, "macc")
    mT = P.sb([128, 8, TT], BF16, "mT")
    osb = sq
    osq = P.sb([128, TT], F32, "osq")
    xsrc = xT.r("(k p) t -> p k t", p=128)
    xdst = xT_out.r("(k p) t -> p k t", p=128)
    for tt in range(NTT):
        t0 = tt * TT
        P.dma(xt, xsrc[:, :, t0:t0 + TT])
        for i in range(5):
            P.dma(yts[i], Dm[f'yT_{BR_NAMES[i]}'].r("(c p) t -> p c t", p=128)[:, :, t0:t0 + TT])
        rms_tile(P, xt, hT, sq, rstd, ones, 8, D, TT)
        j = 0
        for dc in range(8):
            for i in range(5):
                psg = P.ps()
                for k in range(8):
                    P.mm(psg, wg[:, k, i * 1024 + dc * 128:i * 1024 + (dc + 1) * 128], hT[:, k, :],
                         start=(k == 0), stop=(k == 7))
                psb = P.ps()
                for kk in range(2):
                    P.mm(psb, wbr[:, i * 2 + kk, dc * 128:(dc + 1) * 128], yts[i][:, kk, :],
                         start=(kk == 0), stop=(kk == 1))
                s_, t_ = sg[j % 2], term[j % 2]
                j += 1
                P.act(s_, psg, AF.Sigmoid)
                if i == 0:
                    P.tt(macc, s_, psb, ALU.mult)
                elif i < 4:
                    P.tt(t_, s_, psb, ALU.mult)
                    P.tt(macc, macc, t_, ALU.add, eng='pool')
                else:
                    P.tt(t_, s_, psb, ALU.mult)
                    P.tt(mT[:, dc, :], macc, t_, ALU.add, eng='pool')
        pss = P.ps_acc()
        for ec in range(8):
            ps = P.ps()
            for k in range(8):
                P.mm(ps, wout[:, k, ec * 128:(ec + 1) * 128], mT[:, k, :], start=(k == 0), stop=(k == 7))
            P.act(osb[:, ec, :], ps, AF.Copy)
            P.act(osq, ps, AF.Square)
            P.mm(pss, ones, osq, start=(ec == 0), stop=(ec == 7))
        rsqrt_ps(P, rstd, pss, 1.0 / D, 1e-6)
        for ec in range(8):
            P.stt(osb[:, ec, :], osb[:, ec, :], npost[:, ec:ec + 1], rstd, ALU.mult, ALU.mult)
            P.tt(xt[:, ec, :], xt[:, ec, :], osb[:, ec, :], ALU.add, eng='pool')
        P.dma(xdst[:, :, t0:t0 + TT], xt)
    P.barrier()


def stage_X(P, l, Dm):
    P.sb_off = SB_BASE
    nmem = P.sb([128, 8], F32, "nmem")
    P.dma(nmem, Dm[f'nmem{l}'])
    ones = P.sb([128, 128], F32, "ones")
    P.memset(ones, 1.0)
    wm = P.sb([128, 8, 512], BF16, "wm")
    m0 = P.sb_off
    load_weight_bf16(P, wm, Dm[f'wmem{l}'], nmem, 8, 512, blk=512)
    P.barrier()
    P.sb_off = m0
    mt = P.sb([128, 8, 256], F32, "mt")
    msq = P.sb([128, 8, 256], F32, "msq")
    mh = P.sb([128, 8, 256], BF16, "mh")
    mr = P.sb([128, 256], F32, "mr")
    P.dma(mt, Dm['memT'].r("(k p) m -> p k m", p=128))
    rms_tile(P, mt, mh, msq, mr, ones, 8, D, 256)
    kmT = [P.sb([128, 256], BF16, "kmT") for _ in range(2)]
    for c in range(2):
        ps = P.ps()
        for k in range(8):
            P.mm(ps[:, 0:256], wm[:, k, c * 128:(c + 1) * 128], mh[:, k, :], start=(k == 0), stop=(k == 7))
        P.copy(kmT[c], ps[:, 0:256])
    vpad = [[P.sb([128, 128], BF16, "vpad") for _ in range(4)] for _ in range(2)]
    opad = [P.sb([128, 128], BF16, "opad") for _ in range(2)]
    for hh in range(2):
        P.memset(opad[hh], 0.0)
        P.memset(opad[hh][:, hh * 64:(hh + 1) * 64], 1.0)
    for mc in range(2):
        ps = P.ps()
        for k in range(8):
            P.mm(ps[:, 0:256], mh[:, k, mc * 128:(mc + 1) * 128], wm[:, k, 256:512], start=(k == 0), stop=(k == 7))
        for h in range(4):
            hh = h % 2
            P.memset(vpad[mc][h], 0.0)
            P.copy(vpad[mc][h][:, hh * 64:(hh + 1) * 64], ps[:, h * 64:(h + 1) * 64])
    qf = P.sb([128, 2, TT], F32, "qf")
    gf = P.sb([128, 2, TT], F32, "gf")
    qb = P.sb([128, 2, TT], BF16, "qb")
    E = [[P.sb([128, TT], BF16, "E") for _ in range(2)] for _ in range(2)]
    rs = P.sb([128, TT], F32, "rs")
    o = P.sb([128, TT], F32, "o")
    sgl = P.sb([128, TT], F32, "sgl")
    yst = P.sb([128, 2, TT], BF16, "yst")
    csrc = Dm['colsT'].r("(c p) t -> p c t", p=128)
    ydst = Dm['yT_xatt'].r("(c p) t -> p c t", p=128)
    for tt in range(NTT):
        t0 = tt * TT
        P.dma(qf, csrc[:, C_XQ:C_XQ + 2, t0:t0 + TT])
        P.dma(gf, csrc[:, C_XG:C_XG + 2, t0:t0 + TT])
        P.copy(qb, qf)
        for p in range(2):
            for hh in range(2):
                for mc in range(2):
                    ps = P.ps()
                    P.mm(ps, kmT[p][hh * 64:(hh + 1) * 64, mc * 128:(mc + 1) * 128],
                         qb[hh * 64:(hh + 1) * 64, p, :])
                    P.act(E[hh][mc], ps, AF.Exp, scale=0.125)
            pso = P.ps()
            pss = P.ps()
            n = 0
            for hh in range(2):
                for mc in range(2):
                    P.mm(pso, vpad[mc][2 * p + hh], E[hh][mc], start=(n == 0), stop=(n == 3))
                    n += 1
            n = 0
            for hh in range(2):
                for mc in range(2):
                    P.mm(pss, opad[hh], E[hh][mc], start=(n == 0), stop=(n == 3))
                    n += 1
            P.recip(rs, pss)
            P.tt(o, pso, rs, ALU.mult)
            P.act(sgl, gf[:, p, :], AF.Silu)
            P.tt(yst[:, p, :], o, sgl, ALU.mult)
        P.dma(ydst[:, :, t0:t0 + TT], yst)
    P.barrier()


def dram_specs():
    sp = {
        'xT': ([D, S], F32, 'in'), 'memT': ([D, 256], F32, 'in'), 'pos': ([1, S], I32, 'in'),
        'colsT': ([NP_ROWS, S], F32, 'scratch'), 'colsTok': ([S, NT_COLS], F32, 'scratch'),
        'xT1': ([D, S], F32, 'scratch'),
    }
    for n in BR_NAMES:
        sp[f'yT_{n}'] = ([256, S], BF16, 'scratch')
    sp['iqR'] = ([256, S], F32, 'scratch')
    sp['ropeC'] = ([64, S], F32, 'scratch')
    sp['ropeS'] = ([64, S], F32, 'scratch')
    sp['ropeconst'] = ([64, 2], F32, 'in')
    sp['ident'] = ([128, 128], F32, 'in')
    sp['ret_idT'] = ([128, 4, 128], F32, 'in')
    sp['ret_qd'] = ([64, 4, 128], F32, 'in')
    sp['ret_kd'] = ([128, 4], F32, 'in')
    sp['ret_cd'] = ([64, 256], F32, 'in')
    sp['s5mask'] = ([128, 8, 8], F32, 'in')
    sp['rw_masks'] = ([64, 3, 64], F32, 'in')
    sp['dsa_cb'] = ([128, 128], F32, 'in')
    sp['dsa_pw'] = ([128, 32], F32, 'in')
    for l in range(2):
        sp[f'rwprm{l}'] = ([64, 8, 4], F32, 'in')
        sp[f'rwmu{l}'] = ([64, 18], F32, 'in')
        sp[f'rww2{l}'] = ([64, 256], F32, 'in')
        sp[f'rwa2{l}'] = ([64, 256], F32, 'in')
    sp['s5tau'] = ([128, 512], F32, 'in')
    for l in range(2):
        sp[f'retgn{l}'] = ([64, 4], F32, 'in')
        sp[f's5lam{l}'] = ([128, 8, 3], F32, 'in')
        sp[f's5b{l}'] = ([128, 8, 2, 16], F32, 'in')
        sp[f's5c{l}'] = ([128, 8, 2, 16], F32, 'in')
        sp[f's5d{l}'] = ([128, 2], F32, 'in')
        sp[f's5wglu{l}'] = ([256, 256], F32, 'in')
    for l in range(2):
        sp[f'wp{l}'] = ([D, NP_ROWS], F32, 'in')
        sp[f'wt{l}'] = ([D, NT_COLS], F32, 'in')
        sp[f'wpf{l}'] = ([D, 644], F32, 'in')
        sp[f'wg{l}'] = ([D, 5120], F32, 'in')
        sp[f'wbr{l}'] = ([1280, D], F32, 'in')
        sp[f'wout{l}'] = ([D, D], F32, 'in')
        sp[f'wmem{l}'] = ([D, 512], F32, 'in')
        for n in ['npre', 'npost', 'nmem']:
            sp[f'{n}{l}'] = ([128, 8], F32, 'in')
    return sp


def host_inputs(inputs, b):
    f_idx, t_idx = proj_col_indices()
    d = {}
    d['xT'] = np.ascontiguousarray(inputs['x'][b].T)
    d['memT'] = np.ascontiguousarray(inputs['mem'][b].T)
    d['pos'] = np.ascontiguousarray(inputs['positions'][b][None, :]).astype(np.int32)
    pk = lambda v: np.ascontiguousarray(v.reshape(8, 128).T)
    jj = np.arange(64)
    inv = (10000.0 ** (-(np.arange(32, dtype=np.float32)) / 32)).astype(np.float32)
    d['ropeconst'] = np.stack([inv[jj % 32], np.where(jj < 32, -1.0, 1.0)], 1).astype(np.float32)
    d['ident'] = np.eye(128, dtype=np.float32)
    d['ret_idT'], d['ret_qd'], d['ret_kd'], d['ret_cd'] = ret_consts()
    ii = np.arange(64)
    rm = np.zeros((64, 3, 64), np.float32)
    rm[:, 0, :] = (ii[None, :] > ii[:, None])
    rm[:, 1, :] = (ii[None, :] >= ii[:, None])
    rm[:, 2, :] = (ii[None, :] < ii[:, None])
    d['rw_masks'] = rm
    i128 = np.arange(128)
    d['dsa_pw'] = np.ascontiguousarray(np.broadcast_to((0.5 ** np.arange(1, 33, dtype=np.float64)).astype(np.float32)[None, :], (128, 32)))
    d['dsa_cb'] = np.where(i128[None, :] <= i128[:, None], 0.0, -1e30).astype(np.float32)
    for l in range(2):
        hd = lambda v: np.ascontiguousarray(v.reshape(4, 64).T)
        z = np.zeros((64, 4), np.float32)
        d[f'rwprm{l}'] = np.ascontiguousarray(np.stack([hd(inputs['rwkv_w0'][l]), hd(inputs['rwkv_a0'][l]), hd(inputs['rwkv_k_k'][l]),
                                   hd(inputs['rwkv_k_a'][l]), hd(inputs['rwkv_r_k'][l].reshape(256)), hd(inputs['rwkv_lnx_w'][l]),
                                   hd(inputs['rwkv_lnx_b'][l]), z], 1).astype(np.float32))
        d[f'rwmu{l}'] = np.ascontiguousarray(inputs['rwkv_mu'][l].reshape(18, 64).T)
        d[f'rww2{l}'] = np.ascontiguousarray(inputs['rwkv_w2'][l])
        d[f'rwa2{l}'] = np.ascontiguousarray(inputs['rwkv_a2'][l])
    sidx = np.arange(128)
    mk = np.zeros((128, 8, 8), np.float32)
    for j in range(8):
        mk[sidx, j, (2 * j + sidx // 64) % 8] = 1.0
    d['s5mask'] = mk
    d['s5tau'] = np.ascontiguousarray(np.broadcast_to(np.arange(1, 513, dtype=np.float32)[None, :], (128, 512)))
    sj = lambda a: np.ascontiguousarray(a.reshape((8, 128) + a.shape[1:]).swapaxes(0, 1))
    for l in range(2):
        d[f'retgn{l}'] = np.ascontiguousarray(inputs['ret_gn_w'][l].reshape(4, 64).T)
        lam3 = np.stack([inputs['s5_lam_re'][l].reshape(1024), inputs['s5_lam_im'][l].reshape(1024),
                         np.repeat(inputs['s5_log_dt'][l], 64)], 1).astype(np.float32)
        d[f's5lam{l}'] = sj(lam3)
        d[f's5b{l}'] = sj(np.stack([inputs['s5_b_re'][l].reshape(1024, 16), inputs['s5_b_im'][l].reshape(1024, 16)], 1))
        ct = lambda c: np.ascontiguousarray(c.transpose(0, 2, 1)).reshape(1024, 16)
        d[f's5c{l}'] = sj(np.stack([ct(inputs['s5_c_re'][l]), ct(inputs['s5_c_im'][l])], 1))
        d[f's5d{l}'] = np.ascontiguousarray(inputs['s5_d'][l].reshape(2, 128).T)
        d[f's5wglu{l}'] = np.ascontiguousarray(inputs['s5_w_glu'][l])
    for l in range(2):
        w = inputs['w_in'][l]
        d[f'wp{l}'] = np.ascontiguousarray(w[:, f_idx])
        d[f'wt{l}'] = np.ascontiguousarray(w[:, t_idx])
        d[f'wpf{l}'] = np.ascontiguousarray(w[:, np.concatenate([f_idx[C_IQ * 128:(C_IK + 1) * 128], t_idx[512:516]])])
        d[f'wg{l}'] = np.ascontiguousarray(w[:, B_GATE:B_GATE + 5120])
        d[f'wbr{l}'] = np.ascontiguousarray(inputs['w_branch'][l].reshape(1280, D))
        d[f'wout{l}'] = np.ascontiguousarray(inputs['w_out'][l])
        d[f'wmem{l}'] = np.ascontiguousarray(inputs['w_mem_kv'][l])
        d[f'npre{l}'] = pk(inputs['norm_pre'][l])
        d[f'npost{l}'] = pk(inputs['norm_post'][l])
        d[f'nmem{l}'] = pk(inputs['norm_mem'][l])
    return d


STAGE_FNS = {}


def build(plan, ext_in=(), ext_out=()):
    nc = bass.Bass("TRN2", target_bir_lowering=False)
    P = Prog(nc)
    Dm = {}
    used_in = []
    for name, (shape, dtype, role) in dram_specs().items():
        if role == 'in' or name in ext_in:
            kind = "ExternalInput"
            used_in.append(name)
        elif name in ext_out:
            kind = "ExternalOutput"
        else:
            kind = "Internal"
        Dm[name] = P.dram(name, shape, dtype, kind=kind)
    Dm['outT'] = P.dram('outT', [D, S], F32, kind="ExternalOutput")
    for st, l in plan:
        xin = Dm['xT'] if l == 0 else Dm['xT1']
        xout = Dm['xT1'] if l == 0 else Dm['outT']
        if st == 'P':
            stage_P(P, l, Dm, xin)
        elif st == 'M':
            stage_M(P, l, Dm, xin, xout)
        elif st == 'X':
            stage_X(P, l, Dm)
        else:
            STAGE_FNS[st](P, l, Dm)
    P.finish()
    return nc, P, used_in


def sin_reduced(P, out, ang, kq, ki, m1):
    P.ts(kq, ang, 1.0 / (2 * math.pi), ALU.mult)
    P.copy(ki, kq)
    P.copy(kq, ki)
    P.stt(ang, kq, -2 * math.pi, ang, ALU.mult, ALU.add)
    P.ts(m1, ang, math.pi, ALU.is_gt, -2 * math.pi, ALU.mult)
    P.tt(ang, ang, m1, ALU.add)
    P.ts(m1, ang, -math.pi, ALU.is_lt, 2 * math.pi, ALU.mult)
    P.tt(ang, ang, m1, ALU.add)
    P.act(out, ang, AF.Sin)


def stage_R(P, l, Dm):
    P.sb_off = SB_BASE
    W = 2048
    rc = P.sb([64, 2], F32, "rc")
    P.dma(rc, Dm['ropeconst'])
    posi = P.sb([64, W], I32, "posi")
    posf = P.sb([64, W], F32, "posf")
    ang = P.sb([64, W], F32, "ang")
    kq = P.sb([64, W], F32, "kq")
    ki = P.sb([64, W], I32, "ki")
    m1 = P.sb([64, W], F32, "m1")
    o = P.sb([64, W], F32, "o")
    for half in range(S // W):
        sl = slice(half * W, (half + 1) * W)
        P.dma(posi, Dm['pos'][:, sl].m(lambda x: x.to_broadcast([64, W])))
        P.copy(posf, posi)
        P.ts(ang, posf, rc[:, 0:1], ALU.mult)
        sin_reduced(P, o, ang, kq, ki, m1)
        P.ts(o, o, rc[:, 1:2], ALU.mult)
        P.dma(Dm['ropeS'][:, sl], o)
        P.ts(ang, posf, rc[:, 0:1], ALU.mult, math.pi / 2, ALU.add)
        sin_reduced(P, o, ang, kq, ki, m1)
        P.dma(Dm['ropeC'][:, sl], o)
    P.barrier()


def rope_heads(P, dst, Dm, c_base, c_swap, ropeC, ropeS, nheads=4, scale=None, dram_dst=None):
    a = P.sb([64, nheads, TT], F32, "ra")
    b = P.sb([64, nheads, TT], F32, "rb")
    if dram_dst is not None:
        ro = [P.sb([64, nheads, TT], F32, "ro") for _ in range(2)]
    src = Dm['colsT']
    for tt in range(NTT):
        sl = slice(tt * TT, (tt + 1) * TT)
        rb_ = c_base * 128 if isinstance(c_base, int) else c_base[0]
        rs_ = c_swap * 128 if isinstance(c_swap, int) else c_swap[0]
        P.dma(a, src[rb_:rb_ + nheads * 64, sl].r("(h d) t -> d h t", d=64))
        P.dma(b, src[rs_:rs_ + nheads * 64, sl].r("(h d) t -> d h t", d=64))
        cb = ropeC[:, sl].m(lambda x: x.unsqueeze(1).to_broadcast([64, nheads, TT]))
        sb_ = ropeS[:, sl].m(lambda x: x.unsqueeze(1).to_broadcast([64, nheads, TT]))
        P.tt(a, a, cb, ALU.mult)
        P.tt(b, b, sb_, ALU.mult, eng='pool')
        if dram_dst is None:
            P.tt(dst[:, :, sl], a, b, ALU.add)
        else:
            o_ = ro[tt % 2]
            P.tt(o_, a, b, ALU.add)
            P.dma(dram_dst.r("(h d) t -> d h t", d=64)[:, :, sl], o_)


RET_LOGG = [math.log(1.0 - math.exp(v)) for v in np.linspace(math.log(1.0 / 32), math.log(1.0 / 512), 4)]


def ret_consts():
    j = np.arange(128, dtype=np.float64)
    idT = np.zeros((128, 4, 128), np.float32)
    qd = np.zeros((64, 4, 128), np.float32)
    kd = np.zeros((128, 4), np.float32)
    cd = np.zeros((64, 256), np.float32)
    for h in range(4):
        lg = RET_LOGG[h]
        rel = j[None, :] - j[:, None]
        idT[:, h, :] = np.where(rel >= 0, np.exp(lg * np.maximum(rel, 0.0)), 0.0) * 0.125
        qd[:, h, :] = np.exp(lg * (j + 1.0))[None, :]
        kd[:, h] = np.exp(lg * (127.0 - j)) * 0.125
        cd[:, h * 64:(h + 1) * 64] = math.exp(lg * 128)
    return idT, qd, kd, cd


def stage_RET(P, l, Dm):
    P.sb_off = SB_BASE
    ropeC = P.sb([64, S], F32, "ropeC")
    ropeS = P.sb([64, S], F32, "ropeS")
    P.dma(ropeC, Dm['ropeC'])
    P.dma(ropeS, Dm['ropeS'])
    idT = P.sb([128, 4, 128], F32, "idT")
    qd = P.sb([64, 4, 128], F32, "qd")
    kd = P.sb([128, 4], F32, "kd")
    cd = P.sb([64, 256], F32, "cd")
    gn = P.sb([64, 4], F32, "gn")
    identb = P.sb([128, 128], BF16, "identb")
    identf = P.sb([128, 128], F32, "identf")
    ones64 = P.sb([64, 64], F32, "ones64")
    P.dma(idT, Dm['ret_idT'])
    P.dma(qd, Dm['ret_qd'])
    P.dma(kd, Dm['ret_kd'])
    P.dma(cd, Dm['ret_cd'])
    P.dma(gn, Dm[f'retgn{l}'])
    P.dma(identf, Dm['ident'])
    P.copy(identb, identf)
    P.memset(ones64, 1.0 / 64)
    qT = P.sb([64, 4, S], BF16, "qT")
    kT = P.sb([64, 4, S], BF16, "kT")
    qdT = P.sb([64, 4, S], BF16, "qdT")
    m0 = P.sb_off
    rope_heads(P, qT, Dm, C_RQ, C_RQS, ropeC, ropeS)
    rope_heads(P, kT, Dm, C_RK, C_RKS, ropeC, ropeS)
    for c in range(32):
        cs = slice(c * 128, (c + 1) * 128)
        P.tt(qdT[:, :, cs], qT[:, :, cs], qd, ALU.mult, eng=('dve' if c % 2 else 'pool'))
    P.barrier()
    P.sb_off = m0
    Vt = P.sb([128, 32, 256], BF16, "Vt")
    Kd = P.sb([128, 32, 256], BF16, "Kd")
    vst = [P.sb([128, 4, 256], F32, "vst") for _ in range(2)]
    vsrc = Dm['colsTok'].r("(c p) n -> p c n", p=128)
    for i in range(8):
        v_ = vst[i % 2]
        P.dma(v_, vsrc[:, i * 4:(i + 1) * 4, 256:512])
        P.copy(Vt[:, i * 4:(i + 1) * 4, :], v_, eng=('dve' if i % 2 else 'pool'))
    for c in range(32):
        cs = slice(c * 128, (c + 1) * 128)
        ps = P.ps()
        psb = ps.bitcast(BF16)
        for h in range(4):
            P.transpose(psb[:, h * 64:(h + 1) * 64], kT[:, h, cs], identb[0:64, 0:64])
        P.tt(Kd[:, c, :].r("p (h d) -> p h d", h=4), psb[:, 0:256].r("p (h d) -> p h d", h=4),
             kd.m(lambda x: x.unsqueeze(2).to_broadcast([128, 4, 64])), ALU.mult)
    R = P.sb([64, 256], F32, "R")
    Rb = P.sb([64, 256], BF16, "Rb")
    P.memset(R, 0.0)
    P.memset(Rb, 0.0)
    AT = [P.sb([128, 4, 128], BF16, "AT") for _ in range(2)]
    Osb = P.sb([64, 512], F32, "Osb")
    dd = P.sb([64, 512], F32, "dd")
    dsq = P.sb([64, 512], F32, "dsq")
    rstd = P.sb([64, 512], F32, "rstd")
    gt = [P.sb([64, 4, 128], F32, "gt") for _ in range(2)]
    sg = P.sb([64, 4, 128], F32, "sg")
    yo = [P.sb([64, 4, 128], BF16, "yo") for _ in range(2)]
    gsrc = Dm['colsT'][C_RG * 128:C_RG * 128 + 256, :].r("(h d) t -> d h t", d=64)
    ydst = Dm['yT_ret'].r("(h d) t -> d h t", d=64)
    for c in range(32):
        cs = slice(c * 128, (c + 1) * 128)
        g_ = gt[c % 2]
        P.dma(g_, gsrc[:, :, cs])
        psA = P.ps()
        for h in range(4):
            P.mm(psA[:, h * 128:(h + 1) * 128], kT[:, h, cs], qT[:, h, cs])
        at = AT[c % 2]
        P.tt(at, psA.r("p (h q) -> p h q", h=4), idT, ALU.mult)
        psO = P.ps()
        for h in range(4):
            P.mm(psO[0:64, h * 128:(h + 1) * 128], Vt[:, c, h * 64:(h + 1) * 64], at[:, h, :], start=True, stop=False, inc=False)
            P.mm(psO[0:64, h * 128:(h + 1) * 128], Rb[:, h * 64:(h + 1) * 64], qdT[:, h, cs], start=False, stop=True)
        psKV = P.ps()
        for h in range(4):
            P.mm(psKV[0:64, h * 64:(h + 1) * 64], Kd[:, c, h * 64:(h + 1) * 64], Vt[:, c, h * 64:(h + 1) * 64])
        P.tt(R, R, cd, ALU.mult)
        P.tt(R, R, psKV[0:64, 0:256], ALU.add)
        P.copy(Rb, R, eng='pool')
        P.act(Osb, psO[0:64, :], AF.Copy)
        psM = P.ps()
        P.mm(psM[0:64, :], ones64, Osb)
        P.tt(dd, Osb, psM[0:64, :], ALU.subtract)
        P.act(dsq, dd, AF.Square)
        psV = P.ps()
        P.mm(psV[0:64, :], ones64, dsq)
        P.ts(rstd, psV[0:64, :], 1e-6, ALU.add)
        P.act(rstd, rstd, AF.Sqrt)
        P.recip(rstd, rstd)
        P.tt(dd, dd, rstd, ALU.mult)
        P.tt(dd.r("p (h q) -> p h q", h=4), dd.r("p (h q) -> p h q", h=4),
             gn.m(lambda x: x.unsqueeze(2).to_broadcast([64, 4, 128])), ALU.mult)
        P.act(sg, g_, AF.Silu)
        y_ = yo[c % 2]
        P.tt(y_, dd.r("p (h q) -> p h q", h=4), sg, ALU.mult)
        P.dma(ydst[:, :, cs], y_)
    P.barrier()


STAGE_FNS['R'] = stage_R
STAGE_FNS['RET'] = stage_RET


def stage_S5(P, l, Dm):
    P.sb_off = SB_BASE
    W = TT
    lam = P.sb([128, 8, 3], F32, "lam")
    bsb = P.sb([128, 8, 2, 16], F32, "bsb")
    csb = P.sb([128, 8, 2, 16], F32, "csb")
    msk = P.sb([128, 8, 8], F32, "msk")
    tau = P.sb([128, W], F32, "tau")
    dsk = P.sb([128, 2], F32, "dsk")
    identf = P.sb([128, 128], F32, "identf")
    P.dma(lam, Dm[f's5lam{l}'])
    P.dma(bsb, Dm[f's5b{l}'])
    P.dma(csb, Dm[f's5c{l}'])
    P.dma(msk, Dm['s5mask'])
    P.dma(tau, Dm['s5tau'])
    P.dma(dsk, Dm[f's5d{l}'])
    P.dma(identf, Dm['ident'])
    wglu = P.sb([128, 2, 256], BF16, "wglu")
    cosT = P.sb([128, 8, W], F32, "cosT")
    sinT = P.sb([128, 8, W], F32, "sinT")
    mag = P.sb([128, 8], F32, "mag")
    BT = P.sb([128, 8, 2, 128], BF16, "BT")
    CX = P.sb([128, 8, 2, 128], BF16, "CX")
    m0 = P.sb_off
    load_weight_bf16(P, wglu, Dm[f's5wglu{l}'], None, 2, 256, blk=256)
    lr = P.sb([128, 8], F32, "lr")
    li = P.sb([128, 8], F32, "li")
    dt = P.sb([128, 8], F32, "dt")
    th = P.sb([128, 8], F32, "th")
    P.ts(lr, lam[:, :, 0], -1e-4, ALU.min)
    P.copy(li, lam[:, :, 1])
    P.act(dt, lam[:, :, 2], AF.Exp)
    P.tt(th, li, dt, ALU.mult)
    P.tt(mag, lr, dt, ALU.mult)
    P.act(mag, mag, AF.Exp)
    ang = P.sb([128, W], F32, "ang")
    kq = P.sb([128, W], F32, "kq")
    ki = P.sb([128, W], I32, "ki")
    m1 = P.sb([128, W], F32, "m1")
    for j in range(8):
        P.ts(ang, tau, th[:, j:j + 1], ALU.mult)
        sin_reduced(P, sinT[:, j, :], ang, kq, ki, m1)
        P.ts(ang, tau, th[:, j:j + 1], ALU.mult, math.pi / 2, ALU.add)
        sin_reduced(P, cosT[:, j, :], ang, kq, ki, m1)
    abr = P.sb([128, 8], F32, "abr")
    abi = P.sb([128, 8], F32, "abi")
    den = P.sb([128, 8], F32, "den")
    t8 = P.sb([128, 8], F32, "t8")
    fre = P.sb([128, 8], F32, "fre")
    fim = P.sb([128, 8], F32, "fim")
    P.tt(abr, mag, cosT[:, :, 0], ALU.mult)
    P.tt(abi, mag, sinT[:, :, 0], ALU.mult)
    P.ts(abr, abr, -1.0, ALU.add)
    P.tt(den, lr, lr, ALU.mult)
    P.tt(t8, li, li, ALU.mult)
    P.tt(den, den, t8, ALU.add)
    P.recip(den, den)
    P.tt(fre, abr, lr, ALU.mult)
    P.tt(t8, abi, li, ALU.mult)
    P.tt(fre, fre, t8, ALU.add)
    P.tt(fre, fre, den, ALU.mult)
    P.tt(fim, abi, lr, ALU.mult)
    P.tt(t8, abr, li, ALU.mult)
    P.tt(fim, fim, t8, ALU.subtract)
    P.tt(fim, fim, den, ALU.mult)
    bb = P.sb([128, 8, 2, 16], F32, "bb")
    tb = P.sb([128, 8, 16], F32, "tb")
    bc16 = lambda v: v.m(lambda x: x.unsqueeze(2).to_broadcast([128, 8, 16]))
    P.tt(bb[:, :, 0, :], bsb[:, :, 0, :], bc16(fre), ALU.mult)
    P.tt(tb, bsb[:, :, 1, :], bc16(fim), ALU.mult)
    P.tt(bb[:, :, 0, :], bb[:, :, 0, :], tb, ALU.subtract)
    P.tt(bb[:, :, 1, :], bsb[:, :, 1, :], bc16(fre), ALU.mult)
    P.tt(tb, bsb[:, :, 0, :], bc16(fim), ALU.mult)
    P.tt(bb[:, :, 1, :], bb[:, :, 1, :], tb, ALU.add)
    P.ts(csb[:, :, 1, :], csb[:, :, 1, :], -1.0, ALU.mult)
    bx = P.sb([128, 8, 16], F32, "bx")
    for j in range(8):
        mj = msk[:, j, :].m(lambda x: x.unsqueeze(2).to_broadcast([128, 8, 16]))
        for ri in range(2):
            P.tt(bx, bb[:, j, ri, :].m(lambda x: x.unsqueeze(1).to_broadcast([128, 8, 16])), mj, ALU.mult)
            ps = P.ps()
            P.transpose(ps[:, 0:128], bx.r("p a b -> p (a b)"), identf)
            P.copy(BT[:, j, ri, :], ps[:, 0:128])
            P.tt(CX[:, j, ri, :].r("p (a b) -> p a b", a=8),
                 csb[:, j, ri, :].m(lambda x: x.unsqueeze(1).to_broadcast([128, 8, 16])), mj, ALU.mult)
    P.barrier()
    P.sb_off = m0
    A = P.sb([128, 8, W], F32, "A")
    B = P.sb([128, 8, W], F32, "B")
    t1 = P.sb([128, 8, W], F32, "t1")
    t2 = P.sb([128, 8, W], F32, "t2")
    wre = P.sb([128, 8, W], F32, "wre")
    wim = P.sb([128, 8, W], F32, "wim")
    xre = P.sb([128, 8, W], BF16, "xre")
    xim = P.sb([128, 8, W], BF16, "xim")
    cre = P.sb([128, 8], F32, "cre")
    cim = P.sb([128, 8], F32, "cim")
    P.memset(cre, 0.0)
    P.memset(cim, 0.0)
    uf = P.sb([128, 2, W], F32, "uf")
    ub = P.sb([128, 2, W], BF16, "ub")
    gf = P.sb([128, 2, W], F32, "gf")
    y = P.sb([128, 2, W], F32, "y")
    y2 = P.sb([128, 2, W], F32, "y2")
    glb = P.sb([128, 2, W], BF16, "glb")
    yo = P.sb([128, 2, W], BF16, "yo")
    csrc = Dm['colsT'].r("(c p) t -> p c t", p=128)
    ydst = Dm['yT_s5'].r("(c p) t -> p c t", p=128)
    for tt in range(NTT):
        sl = slice(tt * W, (tt + 1) * W)
        P.dma(uf, csrc[:, C_SU:C_SU + 2, sl])
        P.dma(gf, csrc[:, C_SG:C_SG + 2, sl])
        P.copy(ub, uf, eng='pool')
        for j in range(8):
            for ri, dst in ((0, A), (1, B)):
                ps = P.ps()
                P.mm(ps, BT[:, j, ri, :], ub[:, j // 4, :])
                P.act(dst[:, j, :], ps, AF.Copy)
        P.tt(t1, A, cosT, ALU.mult)
        P.tt(t2, B, sinT, ALU.mult, eng='pool')
        P.tt(t1, t1, t2, ALU.add)
        P.tt(t2, A, sinT, ALU.mult, eng='pool')
        P.tt(B, B, cosT, ALU.mult)
        P.tt(t2, B, t2, ALU.subtract, eng='pool')
        for j in range(8):
            mb = mag[:, j:j + 1].bc([128, W])
            P.scan(wre[:, j, :], mb, t1[:, j, :], cre[:, j:j + 1], ALU.mult, ALU.add)
            P.scan(wim[:, j, :], mb, t2[:, j, :], cim[:, j:j + 1], ALU.mult, ALU.add)
        P.tt(t1, wre, cosT, ALU.mult)
        P.tt(A, wim, sinT, ALU.mult, eng='pool')
        P.tt(xre, t1, A, ALU.subtract)
        P.tt(cre, t1[:, :, W - 1], A[:, :, W - 1], ALU.subtract)
        P.tt(t2, wre, sinT, ALU.mult, eng='pool')
        P.tt(B, wim, cosT, ALU.mult)
        P.tt(xim, t2, B, ALU.add, eng='pool')
        P.tt(cim, t2[:, :, W - 1], B[:, :, W - 1], ALU.add)
        for jc in range(2):
            ps = P.ps()
            n = 0
            for j in range(4 * jc, 4 * jc + 4):
                for ri, xx in ((0, xre), (1, xim)):
                    P.mm(ps, CX[:, j, ri, :], xx[:, j, :], start=(n == 0), stop=(n == 7))
                    n += 1
            P.stt(y[:, jc, :], uf[:, jc, :], dsk[:, jc:jc + 1], ps, ALU.mult, ALU.add)
        P.tt(y2, y, y, ALU.mult)
        P.ts(y2, y2, 0.044715, ALU.mult, 1.0, ALU.add)
        P.tt(y2, y2, y, ALU.mult)
        P.act(y2, y2, AF.Sigmoid, scale=1.5957691216057308)
        P.tt(y, y, y2, ALU.mult)
        P.copy(glb, y, eng='pool')
        for oc in range(2):
            ps = P.ps()
            for kc in range(2):
                P.mm(ps, wglu[:, kc, oc * 128:(oc + 1) * 128], glb[:, kc, :], start=(kc == 0), stop=(kc == 1))
            P.act(y2[:, oc, :], ps, AF.Sigmoid)
        P.tt(y, y, y2, ALU.mult)
        P.act(y2, gf, AF.Silu)
        P.tt(yo, y, y2, ALU.mult)
        P.dma(ydst[:, :, sl], yo)
    P.barrier()


STAGE_FNS['S5'] = stage_S5


import os
RW_DEBUG = int(os.environ.get('RW_DEBUG', '3'))


def stage_RWKV(P, l, Dm):
    P.sb_off = SB_BASE
    W = 256
    H4 = 4
    NCH = W // 64
    HC = H4 * NCH
    prm = P.sb([64, 8, 4], F32, "prm")
    mu = P.sb([64, 18], F32, "mu")
    w2 = P.sb([64, 256], F32, "w2")
    a2 = P.sb([64, 256], F32, "a2")
    msks = P.sb([64, 3, 64], F32, "msks")
    identf = P.sb([128, 128], F32, "identf")
    ones64 = P.sb([64, 64], F32, "ones64")
    onesw = P.sb([64, 1], F32, "onesw")
    P.dma(prm, Dm[f'rwprm{l}'])
    P.dma(mu, Dm[f'rwmu{l}'])
    P.dma(w2, Dm[f'rww2{l}'])
    P.dma(a2, Dm[f'rwa2{l}'])
    P.dma(msks, Dm['rw_masks'])
    P.dma(identf, Dm['ident'])
    P.memset(ones64, 1.0)
    P.memset(onesw, 1.0)
    id64 = identf[0:64, 0:64]
    hb = lambda v, n=W: v.m(lambda x: x.unsqueeze(2).to_broadcast([64, H4, n]))
    mb = lambda k: msks[:, k, :].m(lambda x: x.unsqueeze(1).to_broadcast([64, H4, 64]))
    idb = id64.m(lambda x: x.unsqueeze(1).to_broadcast([64, H4, 64]))
    cin = P.sb([64, 18, W + 1], F32, "cin")
    cs = P.sb([64, 18, W], F32, "cs")
    f = lambda nm: P.sb([64, H4, W], F32, nm)
    twl = P.sb([64, W], F32, "twl")
    sgz, av, kx, t0, kp, beta = f("sgz"), f("av"), f("kx"), f("t0"), f("kp"), f("beta")
    kkn, lw, cw, e1, e2 = f("kkn"), f("lw"), f("cw"), f("e1"), f("e2")
    rt, at, bt, kt, Bh, Kh = f("rt"), f("at"), f("bt"), f("kt"), f("Bh"), f("Kh")
    bonus, Yt = f("bonus"), f("Yt")
    base = P.sb([64, HC], F32, "base")
    cwC = P.sb([64, HC], F32, "cwC")
    gC = P.sb([64, HC], F32, "gC")
    S0 = P.sb([64, H4, 64], F32, "S0")
    P.memset(S0, 0.0)
    g4 = lambda nm: P.sb([64, H4, 64], F32, nm)
    X, XT, PaT, AakT, ArbT, ArkT = g4("X"), g4("XT"), g4("PaT"), g4("AakT"), g4("ArbT"), g4("ArkT")
    Vt, BhT, KhT, Wsb, Usb = g4("Vt"), g4("BhT"), g4("KhT"), g4("Wsb"), g4("Usb")
    yo = P.sb([64, H4, W], BF16, "yo")
    src = Dm['colsT'][0:1152, :].r("(g d) t -> d g t", d=64)
    ydst = Dm['yT_rwkv'].r("(h d) t -> d h t", d=64)
    ps4 = lambda ps: ps[0:64, 0:256].r("p (h x) -> p h x", h=H4)
    for tt in range(S // W):
        t_0 = tt * W
        if tt == 0:
            P.dma(cin[:, :, 1:W + 1], src[:, :, t_0:t_0 + W])
            P.memset(cin[:, :, 0:1], 0.0)
        else:
            P.dma(cin, src[:, :, t_0 - 1:t_0 + W])
        P.tt(cs, cin[:, :, 0:W], cin[:, :, 1:W + 1], ALU.subtract)
        P.tt(cs, cs, mu.m(lambda x: x.unsqueeze(2).to_broadcast([64, 18, W])), ALU.mult)
        P.tt(cs, cs, cin[:, :, 1:W + 1], ALU.add)
        Rr, Kk, Vv, G = cs[:, 0:4, :], cs[:, 4:8, :], cs[:, 8:12, :], cs[:, 14:18, :]
        P.act(twl, cs[:, 12, :], AF.Tanh)
        for h in range(H4):
            ps = P.ps()
            P.mm(ps[0:64, 0:W], w2[:, h * 64:(h + 1) * 64], twl)
            P.act(sgz[:, h, :], ps[0:64, 0:W], AF.Sigmoid, bias=prm[:, 0, h:h + 1])
            ps = P.ps()
            P.mm(ps[0:64, 0:W], a2[:, h * 64:(h + 1) * 64], cs[:, 13, :])
            P.act(av[:, h, :], ps[0:64, 0:W], AF.Sigmoid, bias=prm[:, 1, h:h + 1])
        P.tt(kx, Kk, hb(prm[:, 2, :]), ALU.mult)
        P.tt(t0, kx, kx, ALU.mult, eng='pool')
        for h in range(H4):
            ps = P.ps()
            P.mm(ps[0:64, 0:W], ones64, t0[:, h, :])
            P.ts(kkn[:, h, :], ps[0:64, 0:W], 1e-24, ALU.add)
        P.act(kkn, kkn, AF.Sqrt)
        P.recip(kkn, kkn)
        P.tt(kkn, kkn, kx, ALU.mult)
        P.ts(t0, av, -1.0, ALU.add)
        P.tt(t0, t0, hb(prm[:, 3, :]), ALU.mult)
        P.stt(kp, t0, 1.0, Kk, ALU.add, ALU.mult)
        P.tt(beta, kkn, av, ALU.mult, eng='pool')
        P.tt(t0, Rr, kp, ALU.mult)
        P.tt(t0, t0, hb(prm[:, 4, :]), ALU.mult)
        for h in range(H4):
            ps = P.ps()
            P.mm(ps[0:64, 0:W], ones64, t0[:, h, :])
            P.tt(bonus[:, h, :], ps[0:64, 0:W], Vv[:, h, :], ALU.mult)
        P.ts(lw, sgz, -math.exp(-0.5), ALU.mult)
        for h in range(H4):
            P.scan(cw[:, h, :], onesw[:, 0:1].bc([64, W]), lw[:, h, :], 0.0, ALU.mult, ALU.add)
        cw3 = cw.r("p h (c i) -> p (h c) i", i=64)
        P.memset(base, 0.0)
        P.copy(base.r("p (h c) -> p h c", h=H4)[:, :, 1:NCH], cw.r("p h (c i) -> p h c i", i=64)[:, :, 0:NCH - 1, 63])
        P.tt(cw3, cw3, base.m(lambda x: x.unsqueeze(2).to_broadcast([64, HC, 64])), ALU.subtract)
        P.copy(cwC, cw3[:, :, 63])
        P.act(gC, cwC, AF.Exp)
        P.act(e1, cw, AF.Exp)
        P.tt(rt, Rr, e1, ALU.mult)
        P.act(e1, cw, AF.Exp, scale=-1.0)
        P.tt(bt, beta, e1, ALU.mult)
        P.tt(kt, kp, e1, ALU.mult, eng='pool')
        P.tt(e2, cw, lw, ALU.subtract)
        P.act(e2, e2, AF.Exp)
        P.stt(at, kkn, -1.0, e2, ALU.mult, ALU.mult)
        e13 = e1.r("p h (c i) -> p (h c) i", i=64)
        P.tt(e13, cw3, cwC.m(lambda x: x.unsqueeze(2).to_broadcast([64, HC, 64])), ALU.subtract)
        P.act(e1, e1, AF.Exp, scale=-1.0)
        P.tt(Bh, beta, e1, ALU.mult)
        P.tt(Kh, kp, e1, ALU.mult, eng='pool')
        for c in range(NCH if RW_DEBUG >= 1 else 0):
            sl = slice(c * 64, (c + 1) * 64)
            def mm4(lh, rh):
                ps = P.ps()
                for h in range(H4):
                    P.mm(ps[0:64, h * 64:(h + 1) * 64], lh[:, h, sl], rh[:, h, sl], inc=(h == 3))
                return ps4(ps)
            P.tt(X, mm4(at, bt), mb(2), ALU.mult)
            P.tt(XT, mm4(bt, at), mb(0), ALU.mult)
            P.tt(AakT, mm4(kt, at), mb(0), ALU.mult)
            P.tt(ArbT, mm4(bt, rt), mb(1), ALU.mult)
            P.tt(ArkT, mm4(kt, rt), mb(1), ALU.mult)
            P.tt(PaT, XT, idb, ALU.add, eng='pool')
            for srcT, dstT, eng in ((Vv, Vt, 'act'), (Bh, BhT, 'dve'), (Kh, KhT, 'act')):
                ps = P.ps()
                for h in range(H4):
                    P.transpose(ps[0:64, h * 64:(h + 1) * 64], srcT[:, h, sl], id64)
                if eng == 'act':
                    P.act(dstT, ps4(ps), AF.Copy)
                else:
                    P.copy(dstT, ps4(ps))
            for it in range(5 if RW_DEBUG >= 2 else 0):
                psx = P.ps()
                psxt = P.ps()
                for h in range(H4):
                    P.mm(psx[0:64, h * 64:(h + 1) * 64], XT[:, h, :], X[:, h, :], inc=(h == 3))
                for h in range(H4):
                    P.mm(psxt[0:64, h * 64:(h + 1) * 64], X[:, h, :], XT[:, h, :], inc=(h == 3))
                P.act(X, ps4(psx), AF.Copy)
                P.copy(XT, ps4(psxt))
                psp = P.ps()
                for h in range(H4):
                    P.mm(psp[0:64, h * 64:(h + 1) * 64], X[:, h, :], PaT[:, h, :], inc=(h == 3))
                P.tt(PaT, PaT, ps4(psp), ALU.add)
            if RW_DEBUG < 3:
                continue
            psw = P.ps()
            for h in range(H4):
                o_ = psw[0:64, h * 64:(h + 1) * 64]
                P.mm(o_, at[:, h, sl], S0[:, h, :], start=True, stop=False, inc=False)
                P.mm(o_, AakT[:, h, :], Vt[:, h, :], start=False, stop=True, inc=(h == 3))
            P.act(Wsb, ps4(psw), AF.Copy)
            psu = P.ps()
            for h in range(H4):
                P.mm(psu[0:64, h * 64:(h + 1) * 64], PaT[:, h, :], Wsb[:, h, :], inc=(h == 3))
            P.copy(Usb, ps4(psu))
            psy = P.ps()
            for h in range(H4):
                o_ = psy[0:64, h * 64:(h + 1) * 64]
                P.mm(o_, S0[:, h, :], rt[:, h, sl], start=True, stop=False, inc=False)
                P.mm(o_, Usb[:, h, :], ArbT[:, h, :], start=False, stop=False, inc=False)
                P.mm(o_, Vt[:, h, :], ArkT[:, h, :], start=False, stop=True, inc=(h == 3))
            P.act(Yt[:, :, sl], ps4(psy), AF.Copy)
            pss = P.ps()
            for h in range(H4):
                o_ = pss[0:64, h * 64:(h + 1) * 64]
                P.mm(o_, BhT[:, h, :], Usb[:, h, :], start=True, stop=False, inc=False)
                P.mm(o_, KhT[:, h, :], Vt[:, h, :], start=False, stop=True, inc=(h == 3))
            gcb = gC.r("p (h c) -> p h c", h=H4)[:, :, c].m(lambda x: x.unsqueeze(2).to_broadcast([64, H4, 64]))
            P.tt(S0, S0, gcb, ALU.mult)
            P.tt(S0, S0, ps4(pss), ALU.add)
        for h in range(H4):
            ps = P.ps()
            P.mm(ps[0:64, 0:W], ones64, Yt[:, h, :])
            P.stt(e1[:, h, :], ps[0:64, 0:W], -1.0 / 64, Yt[:, h, :], ALU.mult, ALU.add)
        P.tt(e2, e1, e1, ALU.mult, eng='pool')
        for h in range(H4):
            ps = P.ps()
            P.mm(ps[0:64, 0:W], ones64, e2[:, h, :])
            P.ts(t0[:, h, :], ps[0:64, 0:W], 1.0 / 64, ALU.mult, 64e-5, ALU.add)
        P.act(t0, t0, AF.Sqrt)
        P.recip(t0, t0)
        P.tt(e1, e1, t0, ALU.mult)
        P.tt(e1, e1, hb(prm[:, 5, :]), ALU.mult)
        P.tt(e1, e1, hb(prm[:, 6, :]), ALU.add)
        P.tt(e1, e1, bonus, ALU.add)
        P.act(e2, G, AF.Silu)
        P.tt(yo, e1, e2, ALU.mult)
        P.dma(ydst[:, :, t_0:t_0 + W], yo)
    P.barrier()


STAGE_FNS['RWKV'] = stage_RWKV


N_BISECT = 20


def stage_DSA(P, l, Dm):
    P.sb_off = SB_BASE
    qT = P.sb([64, 4, S], BF16, "qT")
    kT = P.sb([64, 4, S], BF16, "kT")
    ikT = P.sb([64, 1, S], F32, "ikT")
    m0 = P.sb_off
    ropeC = P.sb([64, S], F32, "ropeC")
    ropeS = P.sb([64, S], F32, "ropeS")
    P.dma(ropeC, Dm['ropeC'])
    P.dma(ropeS, Dm['ropeS'])
    rope_heads(P, qT, Dm, C_DQ, C_DQS, ropeC, ropeS)
    rope_heads(P, kT, Dm, C_DK, C_DKS, ropeC, ropeS)
    rope_heads(P, None, Dm, C_IQ, C_IQS, ropeC, ropeS, dram_dst=Dm['iqR'])
    rope_heads(P, ikT, Dm, (C_IK * 128,), (C_IK * 128 + 64,), ropeC, ropeS, nheads=1)
    P.barrier()
    P.sb_off = m0
    identf = P.sb([128, 128], F32, "identf")
    identb = P.sb([128, 128], BF16, "identb")
    cb = P.sb([128, 128], F32, "cb")
    P.dma(identf, Dm['ident'])
    P.copy(identb, identf)
    P.dma(cb, Dm['dsa_cb'])
    Vaug = P.sb([128, 32, 4, 65], BF16, "Vaug")
    iwt = P.sb([128, 32, 4], F32, "iwt")
    vst = [P.sb([128, 4, 256], F32, "vst") for _ in range(2)]
    tsrc = Dm['colsTok'].r("(c p) n -> p c n", p=128)
    P.memset(Vaug[:, :, :, 64:65], 1.0)
    for i in range(8):
        v_ = vst[i % 2]
        P.dma(v_, tsrc[:, i * 4:(i + 1) * 4, 0:256])
        P.copy(Vaug[:, i * 4:(i + 1) * 4, :, 0:64], v_.r("p c (h d) -> p c h d", h=4), eng=('dve' if i % 2 else 'pool'))
    P.dma(iwt, tsrc[:, :, 512:516])
    P.ts(iwt, iwt, 1.0 / 16, ALU.mult)
    score = P.sb([128, S], F32, "score")
    junk = P.sb([128, S], BF16, "junk")
    mask01 = P.sb([128, S], BF16, "mask01")
    maskT = P.sb([128, 32, 128], BF16, "maskT")
    rl = [P.sb([128, 512], F32, "rl") for _ in range(4)]
    E = [P.sb([128, 512], BF16, "E") for _ in range(2)]
    lo = P.sb([128, 1], F32, "lo")
    hi = P.sb([128, 1], F32, "hi")
    mid = P.sb([128, 1], F32, "mid")
    cnt = P.sb([128, 1], F32, "cnt")
    sel = P.sb([128, 1], F32, "sel")
    dlt = P.sb([128, 1], F32, "dlt")
    stp = P.sb([128, 32], F32, "stp")
    pw = P.sb([128, 32], F32, "pw")
    P.dma(pw, Dm['dsa_pw'])
    zt = P.sb([128, S], F32, "zt")
    cz = P.sb([128, S], F32, "cz")
    nz = P.sb([128, 1], F32, "nz")
    npos = P.sb([128, 1], F32, "npos")
    flag = P.sb([128, 1], F32, "flag")
    f2 = P.sb([128, 1], F32, "f2")
    rr = P.sb([128, 1], F32, "rr")
    onesw = P.sb([128, 1], F32, "onesw")
    P.memset(onesw, 1.0)
    osb = P.sb([128, 4, 64], F32, "osb")
    rs = P.sb([128, 4, 1], F32, "rs")
    gt = [P.sb([128, 2, 128], F32, "gt") for _ in range(2)]
    sg = P.sb([128, 2, 128], F32, "sg")
    yo = [P.sb([128, 2, 128], BF16, "yo") for _ in range(2)]
    iqt = [P.sb([64, 4, 128], F32, "iqt") for _ in range(2)]
    iqsrc = Dm['iqR'].r("(h d) t -> d h t", d=64)
    gsrc = Dm['colsT'].r("(c p) t -> p c t", p=128)
    ydst = Dm['yT_dsa'].r("(c p) t -> p c t", p=128)
    ne = 0
    for i in range(32):
        qs = slice(i * 128, (i + 1) * 128)
        Nk = 128 * (i + 1)
        g_ = gt[i % 2]
        P.dma(g_, gsrc[:, C_DG:C_DG + 2, qs])
        iq_ = iqt[i % 2]
        P.dma(iq_, iqsrc[:, :, qs])
        for k0 in range(0, Nk, 512):
            kw = min(512, Nk - k0)
            pss = []
            for h in range(4):
                ps = P.ps()
                P.mm(ps[:, 0:kw], iq_[:, h, :], ikT[:, 0, k0:k0 + kw], inc=(h == 3))
                pss.append(ps)
            for h in range(4):
                P.act(rl[h][:, 0:kw], pss[h][:, 0:kw], AF.Relu)
            P.ts(score[:, k0:k0 + kw], rl[0][:, 0:kw], iwt[:, i, 0:1], ALU.mult)
            for h in range(1, 4):
                P.stt(score[:, k0:k0 + kw], rl[h][:, 0:kw], iwt[:, i, h:h + 1], score[:, k0:k0 + kw], ALU.mult, ALU.add)
        P.tt(score[:, i * 128:Nk], score[:, i * 128:Nk], cb, ALU.add)
        if Nk > 256:
            P.reduce(hi, score[:, 0:Nk], ALU.max)
            P.reduce(lo, score[:, 0:i * 128], ALU.min)
            P.tt(dlt, hi, lo, ALU.subtract)
            P.ts(dlt, dlt, 2.0, ALU.add)
            P.ts(stp, pw, dlt[:, 0:1], ALU.mult)
            P.stt(mid, dlt, 0.5, lo, ALU.mult, ALU.add)
            P.ts(mid, mid, -1.0, ALU.add)
            for it in range(N_BISECT):
                P.ts(junk[:, 0:Nk], score[:, 0:Nk], mid[:, 0:1], ALU.is_ge, 0.0, ALU.add, accum=cnt)
                if it < N_BISECT - 1:
                    P.ts(sel, cnt, 255.5, ALU.is_ge, 0.5, ALU.subtract)
                    P.stt(mid, sel, stp[:, it:it + 1], mid, ALU.mult, ALU.add)
                else:
                    P.ts(sel, cnt, 255.5, ALU.is_ge, 1.0, ALU.subtract)
                    P.stt(lo, sel, stp[:, it:it + 1], mid, ALU.mult, ALU.add)
        else:
            P.memset(lo, -1e29)
        P.ts(zt[:, 0:Nk], score[:, 0:Nk], 0.0, ALU.is_equal, 0.0, ALU.add, accum=nz)
        P.ts(junk[:, 0:Nk], score[:, 0:Nk], 0.0, ALU.is_gt, 0.0, ALU.add, accum=npos)
        P.ts(flag, npos, 255.5, ALU.is_lt)
        P.tt(f2, npos, nz, ALU.add)
        P.ts(f2, f2, 255.5, ALU.is_ge)
        P.tt(flag, flag, f2, ALU.mult)
        P.ts(rr, npos, -1.0, ALU.mult, 256.0, ALU.add)
        P.scan(cz[:, 0:Nk], onesw[:, 0:1].bc([128, Nk]), zt[:, 0:Nk], 0.0, ALU.mult, ALU.add)
        P.ts(cz[:, 0:Nk], cz[:, 0:Nk], rr[:, 0:1], ALU.is_le, flag[:, 0:1], ALU.mult)
        P.tt(zt[:, 0:Nk], zt[:, 0:Nk], cz[:, 0:Nk], ALU.mult)
        P.ts(f2, flag, -1.0, ALU.mult, 1.0, ALU.add)
        P.tt(lo, lo, f2, ALU.mult)
        P.stt(lo, flag, 1e-30, lo, ALU.mult, ALU.add)
        P.ts(mask01[:, 0:Nk], score[:, 0:Nk], lo[:, 0:1], ALU.is_ge)
        P.tt(mask01[:, 0:Nk], mask01[:, 0:Nk], zt[:, 0:Nk], ALU.add)
        for c0 in range(0, i + 1, 4):
            nc_ = min(4, i + 1 - c0)
            ps = P.ps()
            psb = ps.bitcast(BF16)
            for cl in range(nc_):
                c = c0 + cl
                P.transpose(psb[:, cl * 128:(cl + 1) * 128], mask01[:, c * 128:(c + 1) * 128], identb, inc=(cl == nc_ - 1))
            P.act(maskT[:, c0:c0 + nc_, :], psb[:, 0:nc_ * 128].r("p (c q) -> p c q", q=128), AF.Copy)
        psO = P.ps_acc()
        for h in range(4):
            for c0 in range(0, i + 1, 4):
                nc_ = min(4, i + 1 - c0)
                ps = P.ps()
                for cl in range(nc_):
                    c = c0 + cl
                    P.mm(ps[:, cl * 128:(cl + 1) * 128], kT[:, h, c * 128:(c + 1) * 128], qT[:, h, qs], inc=(cl == nc_ - 1))
                e_ = E[ne % 2]
                ne += 1
                P.act(e_[:, 0:nc_ * 128], ps[:, 0:nc_ * 128], AF.Exp, scale=0.125)
                P.tt(e_[:, 0:nc_ * 128].r("p (c q) -> p c q", q=128), e_[:, 0:nc_ * 128].r("p (c q) -> p c q", q=128),
                     maskT[:, c0:c0 + nc_, :], ALU.mult, eng=('dve' if ne % 2 else 'pool'))
                for cl in range(nc_):
                    c = c0 + cl
                    P.mm(psO[:, h * 65:(h + 1) * 65], e_[:, cl * 128:(cl + 1) * 128], Vaug[:, c, h, :],
                         start=(c == 0), stop=(c == i), inc=(cl == nc_ - 1))
        pv = psO[:, 0:260].r("p (h x) -> p h x", h=4)
        P.recip(rs, pv[:, :, 64:65])
        P.tt(osb, pv[:, :, 0:64], rs.m(lambda x: x.to_broadcast([128, 4, 64])), ALU.mult)
        P.act(sg, g_, AF.Silu)
        y_ = yo[i % 2]
        for p in range(2):
            ps = P.ps()
            P.transpose(ps[:, 0:128], osb[:, 2 * p:2 * p + 2, :].r("p a b -> p (a b)"), identf)
            P.tt(y_[:, p, :], ps[:, 0:128], sg[:, p, :], ALU.mult)
        P.dma(ydst[:, :, qs], y_)
    P.barrier()


STAGE_FNS['DSA'] = stage_DSA


FULL_PLAN = [('R', 0)] + [(st, l) for l in range(2) for st in ('P', 'X', 'RET', 'S5', 'RWKV', 'DSA', 'M')]


def kernel(**inputs):
    inputs = {k: np.asarray(v) for k, v in inputs.items()}
    nb = inputs['x'].shape[0]
    nc, P, used_in = build(FULL_PLAN)
    in_maps = []
    for b in range(nb):
        d = host_inputs(inputs, b)
        in_maps.append({k: v for k, v in d.items() if k in used_in})
    res = run_bass_kernel_spmd(nc, in_maps, core_ids=list(range(nb)))
    out = np.stack([np.ascontiguousarray(np.asarray(r['outT']).T) for r in res.results], 0)
    return out.astype(np.float32)
```

```python
from contextlib import ExitStack
import math
import numpy as np
import ml_dtypes
import concourse.bass as bass
import concourse.mybir as mybir
from concourse.bass_utils import run_bass_kernel_spmd

F32 = mybir.dt.float32
BF16 = mybir.dt.bfloat16
I32 = mybir.dt.int32
AF = mybir.ActivationFunctionType
ALU = mybir.AluOpType
AX = mybir.AxisListType

ENGS = ['sp', 'act', 'dve', 'pool', 'pe']
EPOCH = 16000
NDMASEM = 24
SB_BASE = 16640
SBUF_BYTES = 229000


class Buf:
    __slots__ = ('name', 'wev', 'rev', 'tracked')

    def __init__(self, name, tracked=True):
        self.name = name
        self.wev = {}
        self.rev = {}
        self.tracked = tracked


class V:
    __slots__ = ('buf', 'ap')

    def __init__(self, buf, ap):
        self.buf = buf
        self.ap = ap

    def __getitem__(self, k):
        return V(self.buf, self.ap[k])

    def m(self, fn):
        return V(self.buf, fn(self.ap))

    def r(self, s, **kw):
        return V(self.buf, self.ap.rearrange(s, **kw))

    def bc(self, shape):
        return V(self.buf, self.ap.to_broadcast(list(shape)))

    def bitcast(self, dt):
        return V(self.buf, self.ap.bitcast(dt))

    @property
    def shape(self):
        return tuple(self.ap.shape)


def _ap(x):
    return x.ap if isinstance(x, V) else x


class Prog:
    def __init__(self, nc):
        self.nc = nc
        self.q = {e: [] for e in ENGS}
        self.cnt = {e: 0 for e in ENGS}
        self.noinc = {e: False for e in ENGS}
        self.known = {e: {} for e in ENGS}
        self.dma_n = {e: 0 for e in ENGS}
        self.nbar = 0
        self.bufs = []
        self.sb_off = SB_BASE
        self.sb_id = 0
        self.sb_mark = 0
        self.psum = []
        for i in range(8):
            h = nc.alloc_psum_tensor(f"ps{i}", [128, 512], F32)
            self.psum.append(V(self._newbuf(f"ps{i}"), h[:]))
        self.ps_rr = 0

    def _newbuf(self, name, tracked=True):
        b = Buf(name, tracked)
        if tracked:
            self.bufs.append(b)
        return b

    def sb(self, shape, dtype=F32, name="t"):
        esz = {F32: 4, BF16: 2, I32: 4}[dtype]
        per = esz * int(np.prod(shape[1:]))
        per = (per + 63) // 64 * 64
        off = self.sb_off
        assert off + per <= SBUF_BYTES, f"SBUF overflow {name} {off}+{per}"
        self.sb_off += per
        self.sb_id += 1
        nm = f"{name}_{self.sb_id}"
        h = self.nc.alloc_sbuf_tensor_at(nm, list(shape), dtype, offset=off)
        return V(self._newbuf(nm), h[:])

    def mark(self):
        self.sb_mark = self.sb_off

    def release(self):
        self.sb_off = self.sb_mark

    def ps(self):
        v = self.psum[self.ps_rr % 7]
        self.ps_rr += 1
        return v

    def ps_acc(self):
        return self.psum[7]

    def dram(self, name, shape, dtype=F32, kind="Internal"):
        h = self.nc.dram_tensor(name, list(shape), dtype, kind=kind)
        return V(self._newbuf(name, tracked=False), h.ap())

    def _collect(self, eng, reads, writes, extra=None):
        waits = {}

        def need(evs, skip_own):
            for sk, v in evs.items():
                if skip_own and sk == eng:
                    continue
                if waits.get(sk, 0) < v:
                    waits[sk] = v
        for x in reads:
            if x.buf.tracked:
                need(x.buf.wev, False)
        for x in writes:
            if x.buf.tracked:
                need(x.buf.wev, True)
                need(x.buf.rev, True)
        if extra:
            need(extra, False)
        kn = self.known[eng]
        wl = []
        for sk, v in waits.items():
            if kn.get(sk, 0) < v:
                kn[sk] = v
                wl.append((sk, v))
        return wl

    def emit(self, eng, fn, reads=(), writes=(), inc=True):
        reads = [x for x in reads if isinstance(x, V)]
        writes = [x for x in writes if isinstance(x, V)]
        wl = self._collect(eng, reads, writes)
        idx = self.cnt[eng] + 1
        self.cnt[eng] = idx
        self.q[eng].append((wl, fn, ('c', idx)))
        for x in reads:
            b = x.buf
            if b.tracked and b.rev.get(eng, 0) < idx:
                b.rev[eng] = idx
        for x in writes:
            b = x.buf
            if b.tracked:
                b.wev = {eng: idx}
                b.rev = {}

    def dma(self, out, in_, eng='sp'):
        n = self.dma_n[eng]
        self.dma_n[eng] = n + 1
        slot, k = n % NDMASEM, n // NDMASEM
        sk = ('dma', eng, slot)
        val = 16 * (k + 1)
        extra = {sk: 16 * k} if k > 0 else None
        wl = self._collect(eng, [in_], [out], extra)
        oa, ia = out.ap, in_.ap
        self.q[eng].append((wl, lambda e: e.dma_start(out=oa, in_=ia), ('d', sk)))
        b = in_.buf
        if b.tracked:
            b.rev[sk] = val
        b = out.buf
        if b.tracked:
            b.wev = {sk: val}
            b.rev = {}

    def barrier(self):
        waits = {}
        for e in ENGS:
            if e != 'sp' and self.cnt[e] > 0:
                waits[e] = self.cnt[e]
            n = self.dma_n[e]
            for slot in range(min(n, NDMASEM)):
                k = (n - 1 - slot) // NDMASEM
                waits[('dma', e, slot)] = 16 * (k + 1)
        kn = self.known['sp']
        wl = []
        for sk, v in waits.items():
            if kn.get(sk, 0) < v:
                kn[sk] = v
                wl.append((sk, v))
        self.nbar += 1
        nb = self.nbar
        bk = ('bar', 0)
        self.q['sp'].append((wl, None, ('b', bk)))
        for e in ENGS:
            if e != 'sp':
                self.q[e].append(([(bk, nb)], None, None))
                for sk, v in waits.items():
                    if self.known[e].get(sk, 0) < v:
                        self.known[e][sk] = v
        for b in self.bufs:
            b.wev = {}
            b.rev = {}

    def finish(self):
        self.barrier()
        nc = self.nc
        targets = {e: set() for e in ENGS}
        for e in ENGS:
            for wl, fn, tag in self.q[e]:
                for sk, v in wl:
                    if isinstance(sk, str):
                        targets[sk].add(v)
        rank = {e: {v: r + 1 for r, v in enumerate(sorted(targets[e]))} for e in ENGS}
        self.n_inc = {e: len(rank[e]) for e in ENGS}
        keys = set()

        def semkey(sk, v):
            if isinstance(sk, str):
                r = rank[sk][v]
                return ((sk, (r - 1) // EPOCH), (r - 1) % EPOCH + 1)
            return (sk, v)
        prog = {e: [] for e in ENGS}
        for e in ENGS:
            for wl, fn, tag in self.q[e]:
                w2 = [semkey(sk, v) for sk, v in wl]
                inc = None
                if tag is not None:
                    if tag[0] == 'c':
                        if tag[1] in rank[e]:
                            r = rank[e][tag[1]]
                            inc = ((e, (r - 1) // EPOCH), 1)
                    elif tag[0] == 'd':
                        inc = (tag[1], 16)
                    elif tag[0] == 'b':
                        inc = (tag[1], 1)
                for k_, _ in w2:
                    keys.add(k_)
                if inc is not None:
                    keys.add(inc[0])
                prog[e].append((w2, fn, inc, tag))
        stack = ExitStack()
        sems = {}
        for i, sk in enumerate(sorted(keys, key=str)):
            sems[sk] = stack.enter_context(nc.semaphore(f"s{i}"))
        self.nsem = len(sems)

        def mk(en):
            def body(e):
                for wl, fn, inc, tag in prog[en]:
                    for sk, v in wl:
                        e.wait_ge(sems[sk], v)
                    if fn is None:
                        if tag is not None and tag[0] == 'b':
                            e.sem_inc(sems[inc[0]], inc[1])
                        continue
                    ins = fn(e)
                    if inc is not None:
                        ins.then_inc(sems[inc[0]], inc[1])
            return body
        with stack:
            with nc.Block() as block:
                block.sync(mk('sp'))
                block.scalar(mk('act'))
                block.vector(mk('dve'))
                block.gpsimd(mk('pool'))
                block.tensor(mk('pe'))

    def act(self, out, in_, func, bias=None, scale=1.0, accum=None):
        o, i, b, s, a = _ap(out), _ap(in_), _ap(bias), _ap(scale), _ap(accum)
        kw = {}
        if b is not None:
            kw['bias'] = b
        if a is not None:
            kw['accum_out'] = a
        self.emit('act', lambda e: e.activation(out=o, in_=i, func=func, scale=s, **kw),
                  [in_, bias, scale], [out, accum])

    def ts(self, out, in0, s1, op0, s2=None, op1=None, accum=None, eng='dve'):
        o, i, a1, a2, ac = _ap(out), _ap(in0), _ap(s1), _ap(s2), _ap(accum)
        kw = {}
        if op1 is not None:
            kw['op1'] = op1
        if ac is not None:
            kw['accum_out'] = ac
        self.emit(eng, lambda e: e.tensor_scalar(out=o, in0=i, scalar1=a1, scalar2=a2, op0=op0, **kw),
                  [in0, s1, s2], [out, accum])

    def tt(self, out, in0, in1, op, eng='dve'):
        o, a, b = _ap(out), _ap(in0), _ap(in1)
        self.emit(eng, lambda e: e.tensor_tensor(out=o, in0=a, in1=b, op=op), [in0, in1], [out])

    def stt(self, out, in0, scalar, in1, op0, op1, eng='dve'):
        o, a, s, b = _ap(out), _ap(in0), _ap(scalar), _ap(in1)
        self.emit(eng, lambda e: e.scalar_tensor_tensor(out=o, in0=a, scalar=s, in1=b, op0=op0, op1=op1),
                  [in0, scalar, in1], [out])

    def copy(self, out, in_, eng='dve'):
        o, i = _ap(out), _ap(in_)
        if eng == 'act':
            self.emit('act', lambda e: e.copy(out=o, in_=i), [in_], [out])
        else:
            self.emit(eng, lambda e: e.tensor_copy(out=o, in_=i), [in_], [out])

    def memset(self, out, val, eng='dve'):
        o = _ap(out)
        self.emit(eng, lambda e: e.memset(o, val), [], [out])

    def recip(self, out, in_):
        o, i = _ap(out), _ap(in_)
        self.emit('dve', lambda e: e.reciprocal(out=o, in_=i), [in_], [out])

    def reduce(self, out, in_, op, axis=AX.X):
        o, i = _ap(out), _ap(in_)
        self.emit('dve', lambda e: e.tensor_reduce(out=o, in_=i, axis=axis, op=op), [in_], [out])

    def scan(self, out, d0, d1, initial, op0, op1):
        o, a, b, ini = _ap(out), _ap(d0), _ap(d1), _ap(initial)
        self.emit('dve', lambda e: e.tensor_tensor_scan(out=o, data0=a, data1=b, initial=ini, op0=op0, op1=op1),
                  [d0, d1, initial], [out])

    def mm(self, out, lhsT, rhs, start=True, stop=True, inc=None):
        o, l, r = _ap(out), _ap(lhsT), _ap(rhs)
        if inc is None:
            inc = stop
        self.emit('pe', lambda e: e.matmul(o, l, r, start=start, stop=stop), [lhsT, rhs], [out], inc=inc)

    def transpose(self, out, in_, ident, inc=True):
        o, i, d = _ap(out), _ap(in_), _ap(ident)
        self.emit('pe', lambda e: e.transpose(o, i, d), [in_, ident], [out], inc=inc)


S = 4096
D = 1024
TT = 512
NTT = S // TT
NP_ROWS = 5376
NT_COLS = 516
B_RWKV, B_DSA, B_RET, B_S5, B_X, B_GATE = 0, 1152, 2500, 3524, 4036, 4548
C_RWKV = 0
C_DQ, C_DQS, C_DK, C_DKS, C_IQ, C_IQS, C_IK, C_DG = 9, 11, 13, 15, 17, 19, 21, 22
C_RQ, C_RQS, C_RK, C_RKS, C_RG = 24, 26, 28, 30, 32
C_SU, C_SG = 34, 36
C_XQ, C_XG = 38, 40


def _swap_idx(base, nheads):
    idx = []
    for h in range(nheads):
        for j in range(64):
            idx.append(base + h * 64 + (j + 32) % 64)
    return idx


def proj_col_indices():
    r = lambda a, n: list(range(a, a + n))
    f = []
    f += r(B_RWKV, 1152)
    f += r(B_DSA, 256) + _swap_idx(B_DSA, 4)
    f += r(B_DSA + 256, 256) + _swap_idx(B_DSA + 256, 4)
    f += r(B_DSA + 768, 256) + _swap_idx(B_DSA + 768, 4)
    f += r(B_DSA + 1024, 64) + _swap_idx(B_DSA + 1024, 1)
    f += r(B_DSA + 1092, 256)
    f += r(B_RET, 256) + _swap_idx(B_RET, 4)
    f += r(B_RET + 256, 256) + _swap_idx(B_RET + 256, 4)
    f += r(B_RET + 768, 256)
    f += r(B_S5, 512)
    f += r(B_X, 512)
    assert len(f) == NP_ROWS
    t = r(B_DSA + 512, 256) + r(B_RET + 512, 256) + r(B_DSA + 1088, 4)
    assert len(t) == NT_COLS
    return np.array(f), np.array(t)


def load_weight_bf16(P, dst, src_dram, gcol, nk, ncols, blk=1344):
    src = src_dram.r("(k p) n -> p k n", p=128)
    stg = [P.sb([128, blk], F32, "wstg") for _ in range(2)]
    i = 0
    for k in range(nk):
        for c0 in range(0, ncols, blk):
            c1 = min(ncols, c0 + blk)
            s = stg[i % 2]
            P.dma(s[:, 0:c1 - c0], src[:, k, c0:c1])
            eng = 'dve' if i % 2 == 0 else 'pool'
            if gcol is not None:
                P.ts(dst[:, k, c0:c1], s[:, 0:c1 - c0], gcol[:, k:k + 1], ALU.mult, eng=eng)
            else:
                P.copy(dst[:, k, c0:c1], s[:, 0:c1 - c0], eng=eng)
            i += 1


def rsqrt_ps(P, out, src, scale, eps):
    P.ts(out, src, scale, ALU.mult, eps, ALU.add)
    P.act(out, out, AF.Sqrt)
    P.recip(out, out)


def rms_tile(P, xt, hT, sq, rstd, ones, nk, n, width):
    P.act(sq, xt, AF.Square)
    ps = P.ps()
    for k in range(nk):
        P.mm(ps[:, 0:width], ones, sq[:, k, :], start=(k == 0), stop=(k == nk - 1))
    rsqrt_ps(P, rstd, ps[:, 0:width], 1.0 / n, 1e-6)
    for k in range(nk):
        P.tt(hT[:, k, :], xt[:, k, :], rstd, ALU.mult)


def stage_P(P, l, Dm, xT):
    P.sb_off = SB_BASE
    npre = P.sb([128, 8], F32, "npre")
    P.dma(npre, Dm[f'npre{l}'])
    ones = P.sb([128, 128], F32, "ones")
    P.memset(ones, 1.0)
    wp = P.sb([128, 8, NP_ROWS], BF16, "wp")
    wt = P.sb([128, 8, NT_COLS], BF16, "wt")
    wf = P.sb([128, 8, 644], F32, "wf")
    wfsrc = Dm[f'wpf{l}'].r("(k p) n -> p k n", p=128)
    for k in range(8):
        P.dma(wf[:, k, :], wfsrc[:, k, :])
    for k in range(8):
        P.ts(wf[:, k, :], wf[:, k, :], npre[:, k:k + 1], ALU.mult, eng=('dve' if k % 2 else 'pool'))
    m0 = P.sb_off
    load_weight_bf16(P, wp, Dm[f'wp{l}'], npre, 8, NP_ROWS)
    load_weight_bf16(P, wt, Dm[f'wt{l}'], npre, 8, NT_COLS, blk=NT_COLS)
    P.barrier()
    P.sb_off = m0
    xts = [P.sb([128, 8, TT], F32, "xt") for _ in range(2)]
    sq = P.sb([128, 8, TT], F32, "sq")
    hTs = [P.sb([128, 8, TT], BF16, "hT") for _ in range(2)]
    rstd = P.sb([128, TT], F32, "rstd")
    ostg = [P.sb([128, 4, TT], F32, "ostg") for _ in range(2)]
    tstg = [P.sb([128, NT_COLS], F32, "tstg") for _ in range(2)]
    xsrc = xT.r("(k p) t -> p k t", p=128)
    cdst = Dm['colsT'].r("(c p) t -> p c t", p=128)
    ctok = Dm['colsTok']
    for tt in range(NTT):
        t0 = tt * TT
        xt, hT = xts[tt % 2], hTs[tt % 2]
        P.dma(xt, xsrc[:, :, t0:t0 + TT])
        rms_tile(P, xt, hT, sq, rstd, ones, 8, D, TT)
        for k in range(8):
            P.tt(sq[:, k, :], xt[:, k, :], rstd, ALU.mult, eng='pool')
        for c in range(42):
            ps = P.ps()
            for k in range(8):
                if C_IQ <= c <= C_IK:
                    P.mm(ps, wf[:, k, (c - C_IQ) * 128:(c - C_IQ + 1) * 128], sq[:, k, :], start=(k == 0), stop=(k == 7))
                else:
                    P.mm(ps, wp[:, k, c * 128:(c + 1) * 128], hT[:, k, :], start=(k == 0), stop=(k == 7))
            stg = ostg[(c // 4) % 2]
            if c % 3 == 2:
                P.copy(stg[:, c % 4, :], ps, eng='dve')
            else:
                P.act(stg[:, c % 4, :], ps, AF.Copy)
            if c % 4 == 3 or c == 41:
                c0 = c - c % 4
                P.dma(cdst[:, c0:c + 1, t0:t0 + TT], stg[:, 0:c % 4 + 1, :])
        for s in range(4):
            ps = P.ps()
            ps2 = P.ps()
            for k in range(8):
                P.mm(ps, hT[:, k, s * 128:(s + 1) * 128], wt[:, k, 0:512], start=(k == 0), stop=(k == 7))
            for k in range(8):
                P.mm(ps2[:, 0:4], sq[:, k, s * 128:(s + 1) * 128], wf[:, k, 640:644], start=(k == 0), stop=(k == 7))
            ts_ = tstg[s % 2]
            P.act(ts_[:, 0:512], ps, AF.Copy)
            P.copy(ts_[:, 512:516], ps2[:, 0:4], eng='dve')
            P.dma(ctok[t0 + s * 128:t0 + (s + 1) * 128, :], ts_)
    P.barrier()


BR_NAMES = ['rwkv', 'dsa', 'ret', 's5', 'xatt']


def stage_M(P, l, Dm, xT, xT_out):
    P.sb_off = SB_BASE
    npre = P.sb([128, 8], F32, "npre")
    npost = P.sb([128, 8], F32, "npost")
    P.dma(npre, Dm[f'npre{l}'])
    P.dma(npost, Dm[f'npost{l}'])
    ones = P.sb([128, 128], F32, "ones")
    P.memset(ones, 1.0)
    wg = P.sb([128, 8, 5120], BF16, "wg")
    wbr = P.sb([128, 10, 1024], BF16, "wbr")
    wout = P.sb([128, 8, 1024], BF16, "wout")
    m0 = P.sb_off
    load_weight_bf16(P, wg, Dm[f'wg{l}'], npre, 8, 5120, blk=1280)
    load_weight_bf16(P, wbr, Dm[f'wbr{l}'], None, 10, 1024, blk=1024)
    load_weight_bf16(P, wout, Dm[f'wout{l}'], None, 8, 1024, blk=1024)
    P.barrier()
    P.sb_off = m0
    xt = P.sb([128, 8, TT], F32, "xt")
    sq = P.sb([128, 8, TT], F32, "sq")
    hT = P.sb([128, 8, TT], BF16, "hT")
    rstd = P.sb([128, TT], F32, "rstd")
    yts = [P.sb([128, 2, TT], BF16, f"y{i}") for i in range(5)]
    sg = [P.sb([128, TT], F32, "sg") for _ in range(2)]
    term = [P.sb([128, TT], F32, "term") for _ in range(2)]
    macc = P.sb([128, TT], F32, "macc")
    mT = P.sb([128, 8, TT], BF16, "mT")
    osb = sq
    osq = P.sb([128, TT], F32, "osq")
    xsrc = xT.r("(k p) t -> p k t", p=128)
    xdst = xT_out.r("(k p) t -> p k t", p=128)
    for tt in range(NTT):
        t0 = tt * TT
        P.dma(xt, xsrc[:, :, t0:t0 + TT])
        for i in range(5):
            P.dma(yts[i], Dm[f'yT_{BR_NAMES[i]}'].r("(c p) t -> p c t", p=128)[:, :, t0:t0 + TT])
        rms_tile(P, xt, hT, sq, rstd, ones, 8, D, TT)
        j = 0
        for dc in range(8):
            for i in range(5):
                psg = P.ps()
                for k in range(8):
                    P.mm(psg, wg[:, k, i * 1024 + dc * 128:i * 1024 + (dc + 1) * 128], hT[:, k, :],
                         start=(k == 0), stop=(k == 7))
                psb = P.ps()
                for kk in range(2):
                    P.mm(psb, wbr[:, i * 2 + kk, dc * 128:(dc + 1) * 128], yts[i][:, kk, :],
                         start=(kk == 0), stop=(kk == 1))
                s_, t_ = sg[j % 2], term[j % 2]
                j += 1
                P.act(s_, psg, AF.Sigmoid)
                if i == 0:
                    P.tt(macc, s_, psb, ALU.mult)
                elif i < 4:
                    P.tt(t_, s_, psb, ALU.mult)
                    P.tt(macc, macc, t_, ALU.add, eng='pool')
                else:
                    P.tt(t_, s_, psb, ALU.mult)
                    P.tt(mT[:, dc, :], macc, t_, ALU.add, eng='pool')
        pss = P.ps_acc()
        for ec in range(8):
            ps = P.ps()
            for k in range(8):
                P.mm(ps, wout[:, k, ec * 128:(ec + 1) * 128], mT[:, k, :], start=(k == 0), stop=(k == 7))
            P.act(osb[:, ec, :], ps, AF.Copy)
            P.act(osq, ps, AF.Square)
            P.mm(pss, ones, osq, start=(ec == 0), stop=(ec == 7))
        rsqrt_ps(P, rstd, pss, 1.0 / D, 1e-6)
        for ec in range(8):
            P.stt(osb[:, ec, :], osb[:, ec, :], npost[:, ec:ec + 1], rstd, ALU.mult, ALU.mult)
            P.tt(xt[:, ec, :], xt[:, ec, :], osb[:, ec, :], ALU.add, eng='pool')
        P.dma(xdst[:, :, t0:t0 + TT], xt)
    P.barrier()


def stage_X(P, l, Dm):
    P.sb_off = SB_BASE
    nmem = P.sb([128, 8], F32, "nmem")
    P.dma(nmem, Dm[f'nmem{l}'])
    ones = P.sb([128, 128], F32, "ones")
    P.memset(ones, 1.0)
    wm = P.sb([128, 8, 512], BF16, "wm")
    m0 = P.sb_off
    load_weight_bf16(P, wm, Dm[f'wmem{l}'], nmem, 8, 512, blk=512)
    P.barrier()
    P.sb_off = m0
    mt = P.sb([128, 8, 256], F32, "mt")
    msq = P.sb([128, 8, 256], F32, "msq")
    mh = P.sb([128, 8, 256], BF16, "mh")
    mr = P.sb([128, 256], F32, "mr")
    P.dma(mt, Dm['memT'].r("(k p) m -> p k m", p=128))
    rms_tile(P, mt, mh, msq, mr, ones, 8, D, 256)
    kmT = [P.sb([128, 256], BF16, "kmT") for _ in range(2)]
    for c in range(2):
        ps = P.ps()
        for k in range(8):
            P.mm(ps[:, 0:256], wm[:, k, c * 128:(c + 1) * 128], mh[:, k, :], start=(k == 0), stop=(k == 7))
        P.copy(kmT[c], ps[:, 0:256])
    vpad = [[P.sb([128, 128], BF16, "vpad") for _ in range(4)] for _ in range(2)]
    opad = [P.sb([128, 128], BF16, "opad") for _ in range(2)]
    for hh in range(2):
        P.memset(opad[hh], 0.0)
        P.memset(opad[hh][:, hh * 64:(hh + 1) * 64], 1.0)
    for mc in range(2):
        ps = P.ps()
        for k in range(8):
            P.mm(ps[:, 0:256], mh[:, k, mc * 128:(mc + 1) * 128], wm[:, k, 256:512], start=(k == 0), stop=(k == 7))
        for h in range(4):
            hh = h % 2
            P.memset(vpad[mc][h], 0.0)
            P.copy(vpad[mc][h][:, hh * 64:(hh + 1) * 64], ps[:, h * 64:(h + 1) * 64])
    qf = P.sb([128, 2, TT], F32, "qf")
    gf = P.sb([128, 2, TT], F32, "gf")
    qb = P.sb([128, 2, TT], BF16, "qb")
    E = [[P.sb([128, TT], BF16, "E") for _ in range(2)] for _ in range(2)]
    rs = P.sb([128, TT], F32, "rs")
    o = P.sb([128, TT], F32, "o")
    sgl = P.sb([128, TT], F32, "sgl")
    yst = P.sb([128, 2, TT], BF16, "yst")
    csrc = Dm['colsT'].r("(c p) t -> p c t", p=128)
    ydst = Dm['yT_xatt'].r("(c p) t -> p c t", p=128)
    for tt in range(NTT):
        t0 = tt * TT
        P.dma(qf, csrc[:, C_XQ:C_XQ + 2, t0:t0 + TT])
        P.dma(gf, csrc[:, C_XG:C_XG + 2, t0:t0 + TT])
        P.copy(qb, qf)
        for p in range(2):
            for hh in range(2):
                for mc in range(2):
                    ps = P.ps()
                    P.mm(ps, kmT[p][hh * 64:(hh + 1) * 64, mc * 128:(mc + 1) * 128],
                         qb[hh * 64:(hh + 1) * 64, p, :])
                    P.act(E[hh][mc], ps, AF.Exp, scale=0.125)
            pso = P.ps()
            pss = P.ps()
            n = 0
            for hh in range(2):
                for mc in range(2):
                    P.mm(pso, vpad[mc][2 * p + hh], E[hh][mc], start=(n == 0), stop=(n == 3))
                    n += 1
            n = 0
            for hh in range(2):
                for mc in range(2):
                    P.mm(pss, opad[hh], E[hh][mc], start=(n == 0), stop=(n == 3))
                    n += 1
            P.recip(rs, pss)
            P.tt(o, pso, rs, ALU.mult)
            P.act(sgl, gf[:, p, :], AF.Silu)
            P.tt(yst[:, p, :], o, sgl, ALU.mult)
        P.dma(ydst[:, :, t0:t0 + TT], yst)
    P.barrier()


def dram_specs():
    sp = {
        'xT': ([D, S], F32, 'in'), 'memT': ([D, 256], F32, 'in'), 'pos': ([1, S], I32, 'in'),
        'colsT': ([NP_ROWS, S], F32, 'scratch'), 'colsTok': ([S, NT_COLS], F32, 'scratch'),
        'xT1': ([D, S], F32, 'scratch'),
    }
    for n in BR_NAMES:
        sp[f'yT_{n}'] = ([256, S], BF16, 'scratch')
    sp['iqR'] = ([256, S], F32, 'scratch')
    sp['ropeC'] = ([64, S], F32, 'scratch')
    sp['ropeS'] = ([64, S], F32, 'scratch')
    sp['ropeconst'] = ([64, 2], F32, 'in')
    sp['ident'] = ([128, 128], F32, 'in')
    sp['ret_idT'] = ([128, 4, 128], F32, 'in')
    sp['ret_qd'] = ([64, 4, 128], F32, 'in')
    sp['ret_kd'] = ([128, 4], F32, 'in')
    sp['ret_cd'] = ([64, 256], F32, 'in')
    sp['s5mask'] = ([128, 8, 8], F32, 'in')
    sp['rw_masks'] = ([64, 3, 64], F32, 'in')
    sp['dsa_cb'] = ([128, 128], F32, 'in')
    sp['dsa_pw'] = ([128, 32], F32, 'in')
    for l in range(2):
        sp[f'rwprm{l}'] = ([64, 8, 4], F32, 'in')
        sp[f'rwmu{l}'] = ([64, 18], F32, 'in')
        sp[f'rww2{l}'] = ([64, 256], F32, 'in')
        sp[f'rwa2{l}'] = ([64, 256], F32, 'in')
    sp['s5tau'] = ([128, 512], F32, 'in')
    for l in range(2):
        sp[f'retgn{l}'] = ([64, 4], F32, 'in')
        sp[f's5lam{l}'] = ([128, 8, 3], F32, 'in')
        sp[f's5b{l}'] = ([128, 8, 2, 16], F32, 'in')
        sp[f's5c{l}'] = ([128, 8, 2, 16], F32, 'in')
        sp[f's5d{l}'] = ([128, 2], F32, 'in')
        sp[f's5wglu{l}'] = ([256, 256], F32, 'in')
    for l in range(2):
        sp[f'wp{l}'] = ([D, NP_ROWS], F32, 'in')
        sp[f'wt{l}'] = ([D, NT_COLS], F32, 'in')
        sp[f'wpf{l}'] = ([D, 644], F32, 'in')
        sp[f'wg{l}'] = ([D, 5120], F32, 'in')
        sp[f'wbr{l}'] = ([1280, D], F32, 'in')
        sp[f'wout{l}'] = ([D, D], F32, 'in')
        sp[f'wmem{l}'] = ([D, 512], F32, 'in')
        for n in ['npre', 'npost', 'nmem']:
            sp[f'{n}{l}'] = ([128, 8], F32, 'in')
    return sp


def host_inputs(inputs, b):
    f_idx, t_idx = proj_col_indices()
    d = {}
    d['xT'] = np.ascontiguousarray(inputs['x'][b].T)
    d['memT'] = np.ascontiguousarray(inputs['mem'][b].T)
    d['pos'] = np.ascontiguousarray(inputs['positions'][b][None, :]).astype(np.int32)
    pk = lambda v: np.ascontiguousarray(v.reshape(8, 128).T)
    jj = np.arange(64)
    inv = (10000.0 ** (-(np.arange(32, dtype=np.float32)) / 32)).astype(np.float32)
    d['ropeconst'] = np.stack([inv[jj % 32], np.where(jj < 32, -1.0, 1.0)], 1).astype(np.float32)
    d['ident'] = np.eye(128, dtype=np.float32)
    d['ret_idT'], d['ret_qd'], d['ret_kd'], d['ret_cd'] = ret_consts()
    ii = np.arange(64)
    rm = np.zeros((64, 3, 64), np.float32)
    rm[:, 0, :] = (ii[None, :] > ii[:, None])
    rm[:, 1, :] = (ii[None, :] >= ii[:, None])
    rm[:, 2, :] = (ii[None, :] < ii[:, None])
    d['rw_masks'] = rm
    i128 = np.arange(128)
    d['dsa_pw'] = np.ascontiguousarray(np.broadcast_to((0.5 ** np.arange(1, 33, dtype=np.float64)).astype(np.float32)[None, :], (128, 32)))
    d['dsa_cb'] = np.where(i128[None, :] <= i128[:, None], 0.0, -1e30).astype(np.float32)
    for l in range(2):
        hd = lambda v: np.ascontiguousarray(v.reshape(4, 64).T)
        z = np.zeros((64, 4), np.float32)
        d[f'rwprm{l}'] = np.ascontiguousarray(np.stack([hd(inputs['rwkv_w0'][l]), hd(inputs['rwkv_a0'][l]), hd(inputs['rwkv_k_k'][l]),
                                   hd(inputs['rwkv_k_a'][l]), hd(inputs['rwkv_r_k'][l].reshape(256)), hd(inputs['rwkv_lnx_w'][l]),
                                   hd(inputs['rwkv_lnx_b'][l]), z], 1).astype(np.float32))
        d[f'rwmu{l}'] = np.ascontiguousarray(inputs['rwkv_mu'][l].reshape(18, 64).T)
        d[f'rww2{l}'] = np.ascontiguousarray(inputs['rwkv_w2'][l])
        d[f'rwa2{l}'] = np.ascontiguousarray(inputs['rwkv_a2'][l])
    sidx = np.arange(128)
    mk = np.zeros((128, 8, 8), np.float32)
    for j in range(8):
        mk[sidx, j, (2 * j + sidx // 64) % 8] = 1.0
    d['s5mask'] = mk
    d['s5tau'] = np.ascontiguousarray(np.broadcast_to(np.arange(1, 513, dtype=np.float32)[None, :], (128, 512)))
    sj = lambda a: np.ascontiguousarray(a.reshape((8, 128) + a.shape[1:]).swapaxes(0, 1))
    for l in range(2):
        d[f'retgn{l}'] = np.ascontiguousarray(inputs['ret_gn_w'][l].reshape(4, 64).T)
        lam3 = np.stack([inputs['s5_lam_re'][l].reshape(1024), inputs['s5_lam_im'][l].reshape(1024),
                         np.repeat(inputs['s5_log_dt'][l], 64)], 1).astype(np.float32)
        d[f's5lam{l}'] = sj(lam3)
        d[f's5b{l}'] = sj(np.stack([inputs['s5_b_re'][l].reshape(1024, 16), inputs['s5_b_im'][l].reshape(1024, 16)], 1))
        ct = lambda c: np.ascontiguousarray(c.transpose(0, 2, 1)).reshape(1024, 16)
        d[f's5c{l}'] = sj(np.stack([ct(inputs['s5_c_re'][l]), ct(inputs['s5_c_im'][l])], 1))
        d[f's5d{l}'] = np.ascontiguousarray(inputs['s5_d'][l].reshape(2, 128).T)
        d[f's5wglu{l}'] = np.ascontiguousarray(inputs['s5_w_glu'][l])
    for l in range(2):
        w = inputs['w_in'][l]
        d[f'wp{l}'] = np.ascontiguousarray(w[:, f_idx])
        d[f'wt{l}'] = np.ascontiguousarray(w[:, t_idx])
        d[f'wpf{l}'] = np.ascontiguousarray(w[:, np.concatenate([f_idx[C_IQ * 128:(C_IK + 1) * 128], t_idx[512:516]])])
        d[f'wg{l}'] = np.ascontiguousarray(w[:, B_GATE:B_GATE + 5120])
        d[f'wbr{l}'] = np.ascontiguousarray(inputs['w_branch'][l].reshape(1280, D))
        d[f'wout{l}'] = np.ascontiguousarray(inputs['w_out'][l])
        d[f'wmem{l}'] = np.ascontiguousarray(inputs['w_mem_kv'][l])
        d[f'npre{l}'] = pk(inputs['norm_pre'][l])
        d[f'npost{l}'] = pk(inputs['norm_post'][l])
        d[f'nmem{l}'] = pk(inputs['norm_mem'][l])
    return d


STAGE_FNS = {}


def build(plan, ext_in=(), ext_out=()):
    nc = bass.Bass("TRN2", target_bir_lowering=False)
    P = Prog(nc)
    Dm = {}
    used_in = []
    for name, (shape, dtype, role) in dram_specs().items():
        if role == 'in' or name in ext_in:
            kind = "ExternalInput"
            used_in.append(name)
        elif name in ext_out:
            kind = "ExternalOutput"
        else:
            kind = "Internal"
        Dm[name] = P.dram(name, shape, dtype, kind=kind)
    Dm['outT'] = P.dram('outT', [D, S], F32, kind="ExternalOutput")
    for st, l in plan:
        xin = Dm['xT'] if l == 0 else Dm['xT1']
        xout = Dm['xT1'] if l == 0 else Dm['outT']
        if st == 'P':
            stage_P(P, l, Dm, xin)
        elif st == 'M':
            stage_M(P, l, Dm, xin, xout)
        elif st == 'X':
            stage_X(P, l, Dm)
        else:
            STAGE_FNS[st](P, l, Dm)
    P.finish()
    return nc, P, used_in


def sin_reduced(P, out, ang, kq, ki, m1):
    P.ts(kq, ang, 1.0 / (2 * math.pi), ALU.mult)
    P.copy(ki, kq)
    P.copy(kq, ki)
    P.stt(ang, kq, -2 * math.pi, ang, ALU.mult, ALU.add)
    P.ts(m1, ang, math.pi, ALU.is_gt, -2 * math.pi, ALU.mult)
    P.tt(ang, ang, m1, ALU.add)
    P.ts(m1, ang, -math.pi, ALU.is_lt, 2 * math.pi, ALU.mult)
    P.tt(ang, ang, m1, ALU.add)
    P.act(out, ang, AF.Sin)


def stage_R(P, l, Dm):
    P.sb_off = SB_BASE
    W = 2048
    rc = P.sb([64, 2], F32, "rc")
    P.dma(rc, Dm['ropeconst'])
    posi = P.sb([64, W], I32, "posi")
    posf = P.sb([64, W], F32, "posf")
    ang = P.sb([64, W], F32, "ang")
    kq = P.sb([64, W], F32, "kq")
    ki = P.sb([64, W], I32, "ki")
    m1 = P.sb([64, W], F32, "m1")
    o = P.sb([64, W], F32, "o")
    for half in range(S // W):
        sl = slice(half * W, (half + 1) * W)
        P.dma(posi, Dm['pos'][:, sl].m(lambda x: x.to_broadcast([64, W])))
        P.copy(posf, posi)
        P.ts(ang, posf, rc[:, 0:1], ALU.mult)
        sin_reduced(P, o, ang, kq, ki, m1)
        P.ts(o, o, rc[:, 1:2], ALU.mult)
        P.dma(Dm['ropeS'][:, sl], o)
        P.ts(ang, posf, rc[:, 0:1], ALU.mult, math.pi / 2, ALU.add)
        sin_reduced(P, o, ang, kq, ki, m1)
        P.dma(Dm['ropeC'][:, sl], o)
    P.barrier()


def rope_heads(P, dst, Dm, c_base, c_swap, ropeC, ropeS, nheads=4, scale=None, dram_dst=None):
    a = P.sb([64, nheads, TT], F32, "ra")
    b = P.sb([64, nheads, TT], F32, "rb")
    if dram_dst is not None:
        ro = [P.sb([64, nheads, TT], F32, "ro") for _ in range(2)]
    src = Dm['colsT']
    for tt in range(NTT):
        sl = slice(tt * TT, (tt + 1) * TT)
        rb_ = c_base * 128 if isinstance(c_base, int) else c_base[0]
        rs_ = c_swap * 128 if isinstance(c_swap, int) else c_swap[0]
        P.dma(a, src[rb_:rb_ + nheads * 64, sl].r("(h d) t -> d h t", d=64))
        P.dma(b, src[rs_:rs_ + nheads * 64, sl].r("(h d) t -> d h t", d=64))
        cb = ropeC[:, sl].m(lambda x: x.unsqueeze(1).to_broadcast([64, nheads, TT]))
        sb_ = ropeS[:, sl].m(lambda x: x.unsqueeze(1).to_broadcast([64, nheads, TT]))
        P.tt(a, a, cb, ALU.mult)
        P.tt(b, b, sb_, ALU.mult, eng='pool')
        if dram_dst is None:
            P.tt(dst[:, :, sl], a, b, ALU.add)
        else:
            o_ = ro[tt % 2]
            P.tt(o_, a, b, ALU.add)
            P.dma(dram_dst.r("(h d) t -> d h t", d=64)[:, :, sl], o_)


RET_LOGG = [math.log(1.0 - math.exp(v)) for v in np.linspace(math.log(1.0 / 32), math.log(1.0 / 512), 4)]


def ret_consts():
    j = np.arange(128, dtype=np.float64)
    idT = np.zeros((128, 4, 128), np.float32)
    qd = np.zeros((64, 4, 128), np.float32)
    kd = np.zeros((128, 4), np.float32)
    cd = np.zeros((64, 256), np.float32)
    for h in range(4):
        lg = RET_LOGG[h]
        rel = j[None, :] - j[:, None]
        idT[:, h, :] = np.where(rel >= 0, np.exp(lg * np.maximum(rel, 0.0)), 0.0) * 0.125
        qd[:, h, :] = np.exp(lg * (j + 1.0))[None, :]
        kd[:, h] = np.exp(lg * (127.0 - j)) * 0.125
        cd[:, h * 64:(h + 1) * 64] = math.exp(lg * 128)
    return idT, qd, kd, cd


def stage_RET(P, l, Dm):
    P.sb_off = SB_BASE
    ropeC = P.sb([64, S], F32, "ropeC")
    ropeS = P.sb([64, S], F32, "ropeS")
    P.dma(ropeC, Dm['ropeC'])
    P.dma(ropeS, Dm['ropeS'])
    idT = P.sb([128, 4, 128], F32, "idT")
    qd = P.sb([64, 4, 128], F32, "qd")
    kd = P.sb([128, 4], F32, "kd")
    cd = P.sb([64, 256], F32, "cd")
    gn = P.sb([64, 4], F32, "gn")
    identb = P.sb([128, 128], BF16, "identb")
    identf = P.sb([128, 128], F32, "identf")
    ones64 = P.sb([64, 64], F32, "ones64")
    P.dma(idT, Dm['ret_idT'])
    P.dma(qd, Dm['ret_qd'])
    P.dma(kd, Dm['ret_kd'])
    P.dma(cd, Dm['ret_cd'])
    P.dma(gn, Dm[f'retgn{l}'])
    P.dma(identf, Dm['ident'])
    P.copy(identb, identf)
    P.memset(ones64, 1.0 / 64)
    qT = P.sb([64, 4, S], BF16, "qT")
    kT = P.sb([64, 4, S], BF16, "kT")
    qdT = P.sb([64, 4, S], BF16, "qdT")
    m0 = P.sb_off
    rope_heads(P, qT, Dm, C_RQ, C_RQS, ropeC, ropeS)
    rope_heads(P, kT, Dm, C_RK, C_RKS, ropeC, ropeS)
    for c in range(32):
        cs = slice(c * 128, (c + 1) * 128)
        P.tt(qdT[:, :, cs], qT[:, :, cs], qd, ALU.mult, eng=('dve' if c % 2 else 'pool'))
    P.barrier()
    P.sb_off = m0
    Vt = P.sb([128, 32, 256], BF16, "Vt")
    Kd = P.sb([128, 32, 256], BF16, "Kd")
    vst = [P.sb([128, 4, 256], F32, "vst") for _ in range(2)]
    vsrc = Dm['colsTok'].r("(c p) n -> p c n", p=128)
    for i in range(8):
        v_ = vst[i % 2]
        P.dma(v_, vsrc[:, i * 4:(i + 1) * 4, 256:512])
        P.copy(Vt[:, i * 4:(i + 1) * 4, :], v_, eng=('dve' if i % 2 else 'pool'))
    for c in range(32):
        cs = slice(c * 128, (c + 1) * 128)
        ps = P.ps()
        psb = ps.bitcast(BF16)
        for h in range(4):
            P.transpose(psb[:, h * 64:(h + 1) * 64], kT[:, h, cs], identb[0:64, 0:64])
        P.tt(Kd[:, c, :].r("p (h d) -> p h d", h=4), psb[:, 0:256].r("p (h d) -> p h d", h=4),
             kd.m(lambda x: x.unsqueeze(2).to_broadcast([128, 4, 64])), ALU.mult)
    R = P.sb([64, 256], F32, "R")
    Rb = P.sb([64, 256], BF16, "Rb")
    P.memset(R, 0.0)
    P.memset(Rb, 0.0)
    AT = [P.sb([128, 4, 128], BF16, "AT") for _ in range(2)]
    Osb = P.sb([64, 512], F32, "Osb")
    dd = P.sb([64, 512], F32, "dd")
    dsq = P.sb([64, 512], F32, "dsq")
    rstd = P.sb([64, 512], F32, "rstd")
    gt = [P.sb([64, 4, 128], F32, "gt") for _ in range(2)]
    sg = P.sb([64, 4, 128], F32, "sg")
    yo = [P.sb([64, 4, 128], BF16, "yo") for _ in range(2)]
    gsrc = Dm['colsT'][C_RG * 128:C_RG * 128 + 256, :].r("(h d) t -> d h t", d=64)
    ydst = Dm['yT_ret'].r("(h d) t -> d h t", d=64)
    for c in range(32):
        cs = slice(c * 128, (c + 1) * 128)
        g_ = gt[c % 2]
        P.dma(g_, gsrc[:, :, cs])
        psA = P.ps()
        for h in range(4):
            P.mm(psA[:, h * 128:(h + 1) * 128], kT[:, h, cs], qT[:, h, cs])
        at = AT[c % 2]
        P.tt(at, psA.r("p (h q) -> p h q", h=4), idT, ALU.mult)
        psO = P.ps()
        for h in range(4):
            P.mm(psO[0:64, h * 128:(h + 1) * 128], Vt[:, c, h * 64:(h + 1) * 64], at[:, h, :], start=True, stop=False, inc=False)
            P.mm(psO[0:64, h * 128:(h + 1) * 128], Rb[:, h * 64:(h + 1) * 64], qdT[:, h, cs], start=False, stop=True)
        psKV = P.ps()
        for h in range(4):
            P.mm(psKV[0:64, h * 64:(h + 1) * 64], Kd[:, c, h * 64:(h + 1) * 64], Vt[:, c, h * 64:(h + 1) * 64])
        P.tt(R, R, cd, ALU.mult)
        P.tt(R, R, psKV[0:64, 0:256], ALU.add)
        P.copy(Rb, R, eng='pool')
        P.act(Osb, psO[0:64, :], AF.Copy)
        psM = P.ps()
        P.mm(psM[0:64, :], ones64, Osb)
        P.tt(dd, Osb, psM[0:64, :], ALU.subtract)
        P.act(dsq, dd, AF.Square)
        psV = P.ps()
        P.mm(psV[0:64, :], ones64, dsq)
        P.ts(rstd, psV[0:64, :], 1e-6, ALU.add)
        P.act(rstd, rstd, AF.Sqrt)
        P.recip(rstd, rstd)
        P.tt(dd, dd, rstd, ALU.mult)
        P.tt(dd.r("p (h q) -> p h q", h=4), dd.r("p (h q) -> p h q", h=4),
             gn.m(lambda x: x.unsqueeze(2).to_broadcast([64, 4, 128])), ALU.mult)
        P.act(sg, g_, AF.Silu)
        y_ = yo[c % 2]
        P.tt(y_, dd.r("p (h q) -> p h q", h=4), sg, ALU.mult)
        P.dma(ydst[:, :, cs], y_)
    P.barrier()


STAGE_FNS['R'] = stage_R
STAGE_FNS['RET'] = stage_RET


def stage_S5(P, l, Dm):
    P.sb_off = SB_BASE
    W = TT
    lam = P.sb([128, 8, 3], F32, "lam")
    bsb = P.sb([128, 8, 2, 16], F32, "bsb")
    csb = P.sb([128, 8, 2, 16], F32, "csb")
    msk = P.sb([128, 8, 8], F32, "msk")
    tau = P.sb([128, W], F32, "tau")
    dsk = P.sb([128, 2], F32, "dsk")
    identf = P.sb([128, 128], F32, "identf")
    P.dma(lam, Dm[f's5lam{l}'])
    P.dma(bsb, Dm[f's5b{l}'])
    P.dma(csb, Dm[f's5c{l}'])
    P.dma(msk, Dm['s5mask'])
    P.dma(tau, Dm['s5tau'])
    P.dma(dsk, Dm[f's5d{l}'])
    P.dma(identf, Dm['ident'])
    wglu = P.sb([128, 2, 256], BF16, "wglu")
    cosT = P.sb([128, 8, W], F32, "cosT")
    sinT = P.sb([128, 8, W], F32, "sinT")
    mag = P.sb([128, 8], F32, "mag")
    BT = P.sb([128, 8, 2, 128], BF16, "BT")
    CX = P.sb([128, 8, 2, 128], BF16, "CX")
    m0 = P.sb_off
    load_weight_bf16(P, wglu, Dm[f's5wglu{l}'], None, 2, 256, blk=256)
    lr = P.sb([128, 8], F32, "lr")
    li = P.sb([128, 8], F32, "li")
    dt = P.sb([128, 8], F32, "dt")
    th = P.sb([128, 8], F32, "th")
    P.ts(lr, lam[:, :, 0], -1e-4, ALU.min)
    P.copy(li, lam[:, :, 1])
    P.act(dt, lam[:, :, 2], AF.Exp)
    P.tt(th, li, dt, ALU.mult)
    P.tt(mag, lr, dt, ALU.mult)
    P.act(mag, mag, AF.Exp)
    ang = P.sb([128, W], F32, "ang")
    kq = P.sb([128, W], F32, "kq")
    ki = P.sb([128, W], I32, "ki")
    m1 = P.sb([128, W], F32, "m1")
    for j in range(8):
        P.ts(ang, tau, th[:, j:j + 1], ALU.mult)
        sin_reduced(P, sinT[:, j, :], ang, kq, ki, m1)
        P.ts(ang, tau, th[:, j:j + 1], ALU.mult, math.pi / 2, ALU.add)
        sin_reduced(P, cosT[:, j, :], ang, kq, ki, m1)
    abr = P.sb([128, 8], F32, "abr")
    abi = P.sb([128, 8], F32, "abi")
    den = P.sb([128, 8], F32, "den")
    t8 = P.sb([128, 8], F32, "t8")
    fre = P.sb([128, 8], F32, "fre")
    fim = P.sb([128, 8], F32, "fim")
    P.tt(abr, mag, cosT[:, :, 0], ALU.mult)
    P.tt(abi, mag, sinT[:, :, 0], ALU.mult)
    P.ts(abr, abr, -1.0, ALU.add)
    P.tt(den, lr, lr, ALU.mult)
    P.tt(t8, li, li, ALU.mult)
    P.tt(den, den, t8, ALU.add)
    P.recip(den, den)
    P.tt(fre, abr, lr, ALU.mult)
    P.tt(t8, abi, li, ALU.mult)
    P.tt(fre, fre, t8, ALU.add)
    P.tt(fre, fre, den, ALU.mult)
    P.tt(fim, abi, lr, ALU.mult)
    P.tt(t8, abr, li, ALU.mult)
    P.tt(fim, fim, t8, ALU.subtract)
    P.tt(fim, fim, den, ALU.mult)
    bb = P.sb([128, 8, 2, 16], F32, "bb")
    tb = P.sb([128, 8, 16], F32, "tb")
    bc16 = lambda v: v.m(lambda x: x.unsqueeze(2).to_broadcast([128, 8, 16]))
    P.tt(bb[:, :, 0, :], bsb[:, :, 0, :], bc16(fre), ALU.mult)
    P.tt(tb, bsb[:, :, 1, :], bc16(fim), ALU.mult)
    P.tt(bb[:, :, 0, :], bb[:, :, 0, :], tb, ALU.subtract)
    P.tt(bb[:, :, 1, :], bsb[:, :, 1, :], bc16(fre), ALU.mult)
    P.tt(tb, bsb[:, :, 0, :], bc16(fim), ALU.mult)
    P.tt(bb[:, :, 1, :], bb[:, :, 1, :], tb, ALU.add)
    P.ts(csb[:, :, 1, :], csb[:, :, 1, :], -1.0, ALU.mult)
    bx = P.sb([128, 8, 16], F32, "bx")
    for j in range(8):
        mj = msk[:, j, :].m(lambda x: x.unsqueeze(2).to_broadcast([128, 8, 16]))
        for ri in range(2):
            P.tt(bx, bb[:, j, ri, :].m(lambda x: x.unsqueeze(1).to_broadcast([128, 8, 16])), mj, ALU.mult)
            ps = P.ps()
            P.transpose(ps[:, 0:128], bx.r("p a b -> p (a b)"), identf)
            P.copy(BT[:, j, ri, :], ps[:, 0:128])
            P.tt(CX[:, j, ri, :].r("p (a b) -> p a b", a=8),
                 csb[:, j, ri, :].m(lambda x: x.unsqueeze(1).to_broadcast([128, 8, 16])), mj, ALU.mult)
    P.barrier()
    P.sb_off = m0
    A = P.sb([128, 8, W], F32, "A")
    B = P.sb([128, 8, W], F32, "B")
    t1 = P.sb([128, 8, W], F32, "t1")
    t2 = P.sb([128, 8, W], F32, "t2")
    wre = P.sb([128, 8, W], F32, "wre")
    wim = P.sb([128, 8, W], F32, "wim")
    xre = P.sb([128, 8, W], BF16, "xre")
    xim = P.sb([128, 8, W], BF16, "xim")
    cre = P.sb([128, 8], F32, "cre")
    cim = P.sb([128, 8], F32, "cim")
    P.memset(cre, 0.0)
    P.memset(cim, 0.0)
    uf = P.sb([128, 2, W], F32, "uf")
    ub = P.sb([128, 2, W], BF16, "ub")
    gf = P.sb([128, 2, W], F32, "gf")
    y = P.sb([128, 2, W], F32, "y")
    y2 = P.sb([128, 2, W], F32, "y2")
    glb = P.sb([128, 2, W], BF16, "glb")
    yo = P.sb([128, 2, W], BF16, "yo")
    csrc = Dm['colsT'].r("(c p) t -> p c t", p=128)
    ydst = Dm['yT_s5'].r("(c p) t -> p c t", p=128)
    for tt in range(NTT):
        sl = slice(tt * W, (tt + 1) * W)
        P.dma(uf, csrc[:, C_SU:C_SU + 2, sl])
        P.dma(gf, csrc[:, C_SG:C_SG + 2, sl])
        P.copy(ub, uf, eng='pool')
        for j in range(8):
            for ri, dst in ((0, A), (1, B)):
                ps = P.ps()
                P.mm(ps, BT[:, j, ri, :], ub[:, j // 4, :])
                P.act(dst[:, j, :], ps, AF.Copy)
        P.tt(t1, A, cosT, ALU.mult)
        P.tt(t2, B, sinT, ALU.mult, eng='pool')
        P.tt(t1, t1, t2, ALU.add)
        P.tt(t2, A, sinT, ALU.mult, eng='pool')
        P.tt(B, B, cosT, ALU.mult)
        P.tt(t2, B, t2, ALU.subtract, eng='pool')
        for j in range(8):
            mb = mag[:, j:j + 1].bc([128, W])
            P.scan(wre[:, j, :], mb, t1[:, j, :], cre[:, j:j + 1], ALU.mult, ALU.add)
            P.scan(wim[:, j, :], mb, t2[:, j, :], cim[:, j:j + 1], ALU.mult, ALU.add)
        P.tt(t1, wre, cosT, ALU.mult)
        P.tt(A, wim, sinT, ALU.mult, eng='pool')
        P.tt(xre, t1, A, ALU.subtract)
        P.tt(cre, t1[:, :, W - 1], A[:, :, W - 1], ALU.subtract)
        P.tt(t2, wre, sinT, ALU.mult, eng='pool')
        P.tt(B, wim, cosT, ALU.mult)
        P.tt(xim, t2, B, ALU.add, eng='pool')
        P.tt(cim, t2[:, :, W - 1], B[:, :, W - 1], ALU.add)
        for jc in range(2):
            ps = P.ps()
            n = 0
            for j in range(4 * jc, 4 * jc + 4):
                for ri, xx in ((0, xre), (1, xim)):
                    P.mm(ps, CX[:, j, ri, :], xx[:, j, :], start=(n == 0), stop=(n == 7))
                    n += 1
            P.stt(y[:, jc, :], uf[:, jc, :], dsk[:, jc:jc + 1], ps, ALU.mult, ALU.add)
        P.tt(y2, y, y, ALU.mult)
        P.ts(y2, y2, 0.044715, ALU.mult, 1.0, ALU.add)
        P.tt(y2, y2, y, ALU.mult)
        P.act(y2, y2, AF.Sigmoid, scale=1.5957691216057308)
        P.tt(y, y, y2, ALU.mult)
        P.copy(glb, y, eng='pool')
        for oc in range(2):
            ps = P.ps()
            for kc in range(2):
                P.mm(ps, wglu[:, kc, oc * 128:(oc + 1) * 128], glb[:, kc, :], start=(kc == 0), stop=(kc == 1))
            P.act(y2[:, oc, :], ps, AF.Sigmoid)
        P.tt(y, y, y2, ALU.mult)
        P.act(y2, gf, AF.Silu)
        P.tt(yo, y, y2, ALU.mult)
        P.dma(ydst[:, :, sl], yo)
    P.barrier()


STAGE_FNS['S5'] = stage_S5


import os
RW_DEBUG = int(os.environ.get('RW_DEBUG', '3'))


def stage_RWKV(P, l, Dm):
    P.sb_off = SB_BASE
    W = 256
    H4 = 4
    NCH = W // 64
    HC = H4 * NCH
    prm = P.sb([64, 8, 4], F32, "prm")
    mu = P.sb([64, 18], F32, "mu")
    w2 = P.sb([64, 256], F32, "w2")
    a2 = P.sb([64, 256], F32, "a2")
    msks = P.sb([64, 3, 64], F32, "msks")
    identf = P.sb([128, 128], F32, "identf")
    ones64 = P.sb([64, 64], F32, "ones64")
    onesw = P.sb([64, 1], F32, "onesw")
    P.dma(prm, Dm[f'rwprm{l}'])
    P.dma(mu, Dm[f'rwmu{l}'])
    P.dma(w2, Dm[f'rww2{l}'])
    P.dma(a2, Dm[f'rwa2{l}'])
    P.dma(msks, Dm['rw_masks'])
    P.dma(identf, Dm['ident'])
    P.memset(ones64, 1.0)
    P.memset(onesw, 1.0)
    id64 = identf[0:64, 0:64]
    hb = lambda v, n=W: v.m(lambda x: x.unsqueeze(2).to_broadcast([64, H4, n]))
    mb = lambda k: msks[:, k, :].m(lambda x: x.unsqueeze(1).to_broadcast([64, H4, 64]))
    idb = id64.m(lambda x: x.unsqueeze(1).to_broadcast([64, H4, 64]))
    cin = P.sb([64, 18, W + 1], F32, "cin")
    cs = P.sb([64, 18, W], F32, "cs")
    f = lambda nm: P.sb([64, H4, W], F32, nm)
    twl = P.sb([64, W], F32, "twl")
    sgz, av, kx, t0, kp, beta = f("sgz"), f("av"), f("kx"), f("t0"), f("kp"), f("beta")
    kkn, lw, cw, e1, e2 = f("kkn"), f("lw"), f("cw"), f("e1"), f("e2")
    rt, at, bt, kt, Bh, Kh = f("rt"), f("at"), f("bt"), f("kt"), f("Bh"), f("Kh")
    bonus, Yt = f("bonus"), f("Yt")
    base = P.sb([64, HC], F32, "base")
    cwC = P.sb([64, HC], F32, "cwC")
    gC = P.sb([64, HC], F32, "gC")
    S0 = P.sb([64, H4, 64], F32, "S0")
    P.memset(S0, 0.0)
    NP2 = NCH // 2
    g8 = lambda nm: [P.sb([64, 2, H4, 64], F32, nm) for _ in range(NP2)]
    X, XT, PaT, AakT, ArbT, ArkT = g8("X"), g8("XT"), g8("PaT"), g8("AakT"), g8("ArbT"), g8("ArkT")
    Vt, BhT, KhT, atT, W2, M2, M1T, KV, Gd = (g8("Vt"), g8("BhT"), g8("KhT"), g8("atT"), g8("W2"), g8("M2"),
                                              g8("M1T"), g8("KV"), g8("Gd"))
    Usb = [P.sb([64, H4, 64], F32, "Usb") for _ in range(2)]
    mb8 = lambda k: msks[:, k, :].m(lambda x: x.unsqueeze(1).unsqueeze(1).to_broadcast([64, 2, H4, 64]))
    id8 = id64.m(lambda x: x.unsqueeze(1).unsqueeze(1).to_broadcast([64, 2, H4, 64]))
    ps8 = lambda ps: ps[0:64, 0:512].r("p (c h x) -> p c h x", c=2, h=H4)
    yo = P.sb([64, H4, W], BF16, "yo")
    src = Dm['colsT'][0:1152, :].r("(g d) t -> d g t", d=64)
    ydst = Dm['yT_rwkv'].r("(h d) t -> d h t", d=64)
    ps4 = lambda ps: ps[0:64, 0:256].r("p (h x) -> p h x", h=H4)
    for tt in range(S // W):
        t_0 = tt * W
        if tt == 0:
            P.dma(cin[:, :, 1:W + 1], src[:, :, t_0:t_0 + W])
            P.memset(cin[:, :, 0:1], 0.0)
        else:
            P.dma(cin, src[:, :, t_0 - 1:t_0 + W])
        P.tt(cs, cin[:, :, 0:W], cin[:, :, 1:W + 1], ALU.subtract)
        P.tt(cs, cs, mu.m(lambda x: x.unsqueeze(2).to_broadcast([64, 18, W])), ALU.mult)
        P.tt(cs, cs, cin[:, :, 1:W + 1], ALU.add)
        Rr, Kk, Vv, G = cs[:, 0:4, :], cs[:, 4:8, :], cs[:, 8:12, :], cs[:, 14:18, :]
        P.act(twl, cs[:, 12, :], AF.Tanh)
        for h in range(H4):
            ps = P.ps()
            P.mm(ps[0:64, 0:W], w2[:, h * 64:(h + 1) * 64], twl)
            P.act(sgz[:, h, :], ps[0:64, 0:W], AF.Sigmoid, bias=prm[:, 0, h:h + 1])
            ps = P.ps()
            P.mm(ps[0:64, 0:W], a2[:, h * 64:(h + 1) * 64], cs[:, 13, :])
            P.act(av[:, h, :], ps[0:64, 0:W], AF.Sigmoid, bias=prm[:, 1, h:h + 1])
        P.tt(kx, Kk, hb(prm[:, 2, :]), ALU.mult)
        P.tt(t0, kx, kx, ALU.mult, eng='pool')
        for h in range(H4):
            ps = P.ps()
            P.mm(ps[0:64, 0:W], ones64, t0[:, h, :])
            P.ts(kkn[:, h, :], ps[0:64, 0:W], 1e-24, ALU.add)
        P.act(kkn, kkn, AF.Sqrt)
        P.recip(kkn, kkn)
        P.tt(kkn, kkn, kx, ALU.mult)
        P.ts(t0, av, -1.0, ALU.add)
        P.tt(t0, t0, hb(prm[:, 3, :]), ALU.mult)
        P.stt(kp, t0, 1.0, Kk, ALU.add, ALU.mult)
        P.tt(beta, kkn, av, ALU.mult, eng='pool')
        P.tt(t0, Rr, kp, ALU.mult)
        P.tt(t0, t0, hb(prm[:, 4, :]), ALU.mult)
        for h in range(H4):
            ps = P.ps()
            P.mm(ps[0:64, 0:W], ones64, t0[:, h, :])
            P.tt(bonus[:, h, :], ps[0:64, 0:W], Vv[:, h, :], ALU.mult)
        P.ts(lw, sgz, -math.exp(-0.5), ALU.mult)
        for h in range(H4):
            P.scan(cw[:, h, :], onesw[:, 0:1].bc([64, W]), lw[:, h, :], 0.0, ALU.mult, ALU.add)
        cw3 = cw.r("p h (c i) -> p (h c) i", i=64)
        P.memset(base, 0.0)
        P.copy(base.r("p (h c) -> p h c", h=H4)[:, :, 1:NCH], cw.r("p h (c i) -> p h c i", i=64)[:, :, 0:NCH - 1, 63])
        P.tt(cw3, cw3, base.m(lambda x: x.unsqueeze(2).to_broadcast([64, HC, 64])), ALU.subtract)
        P.copy(cwC, cw3[:, :, 63])
        P.act(gC, cwC, AF.Exp)
        P.act(e1, cw, AF.Exp)
        P.tt(rt, Rr, e1, ALU.mult)
        P.act(e1, cw, AF.Exp, scale=-1.0)
        P.tt(bt, beta, e1, ALU.mult)
        P.tt(kt, kp, e1, ALU.mult, eng='pool')
        P.tt(e2, cw, lw, ALU.subtract)
        P.act(e2, e2, AF.Exp)
        P.stt(at, kkn, -1.0, e2, ALU.mult, ALU.mult)
        e13 = e1.r("p h (c i) -> p (h c) i", i=64)
        P.tt(e13, cw3, cwC.m(lambda x: x.unsqueeze(2).to_broadcast([64, HC, 64])), ALU.subtract)
        P.act(e1, e1, AF.Exp, scale=-1.0)
        P.tt(Bh, beta, e1, ALU.mult)
        P.tt(Kh, kp, e1, ALU.mult, eng='pool')
        def mm8(p, lhf, rhf):
            ps = P.ps()
            for cl in range(2):
                for h in range(H4):
                    o_ = ps[0:64, (cl * H4 + h) * 64:(cl * H4 + h + 1) * 64]
                    P.mm(o_, lhf(p, cl, h), rhf(p, cl, h))
            return ps8(ps)
        csl = lambda p, cl: slice((2 * p + cl) * 64, (2 * p + cl + 1) * 64)
        tok = lambda t_: (lambda p, cl, h: t_[:, h, csl(p, cl)])
        blk = lambda t_: (lambda p, cl, h: t_[p][:, cl, h, :])
        for p in range(NP2):
            P.tt(X[p], mm8(p, tok(at), tok(bt)), mb8(2), ALU.mult)
            P.tt(XT[p], mm8(p, tok(bt), tok(at)), mb8(0), ALU.mult)
            P.tt(AakT[p], mm8(p, tok(kt), tok(at)), mb8(0), ALU.mult)
            P.tt(ArbT[p], mm8(p, tok(bt), tok(rt)), mb8(1), ALU.mult)
            P.tt(ArkT[p], mm8(p, tok(kt), tok(rt)), mb8(1), ALU.mult)
            P.tt(PaT[p], XT[p], id8, ALU.add)
        for srcT, dstT, eng in ((Vv, Vt, 'act'), (Bh, BhT, 'dve'), (Kh, KhT, 'act'), (at, atT, 'dve')):
            for p in range(NP2):
                ps = P.ps()
                for cl in range(2):
                    for h in range(H4):
                        P.transpose(ps[0:64, (cl * H4 + h) * 64:(cl * H4 + h + 1) * 64], srcT[:, h, csl(p, cl)], id64)
                if eng == 'act':
                    P.act(dstT[p], ps8(ps), AF.Copy)
                else:
                    P.copy(dstT[p], ps8(ps))
        for it in range(5):
            pxs = [(mm8(p, blk(XT), blk(X)), mm8(p, blk(X), blk(XT))) for p in range(NP2)]
            for p in range(NP2):
                P.act(X[p], pxs[p][0], AF.Copy)
                P.copy(XT[p], pxs[p][1])
            pps = [mm8(p, blk(X), blk(PaT)) for p in range(NP2)]
            for p in range(NP2):
                P.tt(PaT[p], PaT[p], pps[p], ALU.add)
        for p in range(NP2):
            P.act(KV[p], mm8(p, blk(KhT), blk(Vt)), AF.Copy)
            P.act(W2[p], mm8(p, blk(AakT), blk(Vt)), AF.Copy)
            P.copy(M1T[p], mm8(p, blk(atT), blk(PaT)))
            for cl in range(2):
                c = 2 * p + cl
                gcb = gC.r("p (h c) -> p h c", h=H4)[:, :, c].m(lambda x: x.unsqueeze(2).to_broadcast([64, H4, 64]))
                P.tt(Gd[p][:, cl], idb, gcb, ALU.mult)
        for p in range(NP2):
            P.act(M2[p], mm8(p, blk(PaT), blk(W2)), AF.Copy)
        for c in range(NCH):
            p, cl = c // 2, c % 2
            sl = slice(c * 64, (c + 1) * 64)
            us = Usb[c % 2]
            psu = P.ps()
            for h in range(H4):
                P.mm(psu[0:64, h * 64:(h + 1) * 64], M1T[p][:, cl, h, :], S0[:, h, :])
            psy = P.ps()
            for h in range(H4):
                o_ = psy[0:64, h * 64:(h + 1) * 64]
                P.mm(o_, S0[:, h, :], rt[:, h, sl], start=True, stop=False)
                P.mm(o_, Vt[p][:, cl, h, :], ArkT[p][:, cl, h, :], start=False, stop=True)
            P.tt(us, ps4(psu), M2[p][:, cl], ALU.add)
            pss = P.ps()
            for h in range(H4):
                o_ = pss[0:64, h * 64:(h + 1) * 64]
                P.mm(o_, Gd[p][:, cl, h, :], S0[:, h, :], start=True, stop=False)
                P.mm(o_, BhT[p][:, cl, h, :], us[:, h, :], start=False, stop=True)
            psy2 = P.ps()
            for h in range(H4):
                P.mm(psy2[0:64, h * 64:(h + 1) * 64], us[:, h, :], ArbT[p][:, cl, h, :])
            P.tt(S0, ps4(pss), KV[p][:, cl], ALU.add)
            P.act(Yt[:, :, sl], ps4(psy), AF.Copy)
            P.tt(Yt[:, :, sl], Yt[:, :, sl], ps4(psy2), ALU.add)
        for h in range(H4):
            ps = P.ps()
            P.mm(ps[0:64, 0:W], ones64, Yt[:, h, :])
            P.stt(e1[:, h, :], ps[0:64, 0:W], -1.0 / 64, Yt[:, h, :], ALU.mult, ALU.add)
        P.tt(e2, e1, e1, ALU.mult, eng='pool')
        for h in range(H4):
            ps = P.ps()
            P.mm(ps[0:64, 0:W], ones64, e2[:, h, :])
            P.ts(t0[:, h, :], ps[0:64, 0:W], 1.0 / 64, ALU.mult, 64e-5, ALU.add)
        P.act(t0, t0, AF.Sqrt)
        P.recip(t0, t0)
        P.tt(e1, e1, t0, ALU.mult)
        P.tt(e1, e1, hb(prm[:, 5, :]), ALU.mult)
        P.tt(e1, e1, hb(prm[:, 6, :]), ALU.add)
        P.tt(e1, e1, bonus, ALU.add)
        P.act(e2, G, AF.Silu)
        P.tt(yo, e1, e2, ALU.mult)
        P.dma(ydst[:, :, t_0:t_0 + W], yo)
    P.barrier()


STAGE_FNS['RWKV'] = stage_RWKV


N_BISECT = 20


def stage_DSA(P, l, Dm):
    P.sb_off = SB_BASE
    qT = P.sb([64, 4, S], BF16, "qT")
    kT = P.sb([64, 4, S], BF16, "kT")
    ikT = P.sb([64, 1, S], F32, "ikT")
    m0 = P.sb_off
    ropeC = P.sb([64, S], F32, "ropeC")
    ropeS = P.sb([64, S], F32, "ropeS")
    P.dma(ropeC, Dm['ropeC'])
    P.dma(ropeS, Dm['ropeS'])
    rope_heads(P, qT, Dm, C_DQ, C_DQS, ropeC, ropeS)
    rope_heads(P, kT, Dm, C_DK, C_DKS, ropeC, ropeS)
    rope_heads(P, None, Dm, C_IQ, C_IQS, ropeC, ropeS, dram_dst=Dm['iqR'])
    rope_heads(P, ikT, Dm, (C_IK * 128,), (C_IK * 128 + 64,), ropeC, ropeS, nheads=1)
    P.barrier()
    P.sb_off = m0
    identf = P.sb([128, 128], F32, "identf")
    identb = P.sb([128, 128], BF16, "identb")
    cb = P.sb([128, 128], F32, "cb")
    P.dma(identf, Dm['ident'])
    P.copy(identb, identf)
    P.dma(cb, Dm['dsa_cb'])
    Vaug = P.sb([128, 32, 4, 65], BF16, "Vaug")
    iwt = P.sb([128, 32, 4], F32, "iwt")
    vst = [P.sb([128, 4, 256], F32, "vst") for _ in range(2)]
    tsrc = Dm['colsTok'].r("(c p) n -> p c n", p=128)
    P.memset(Vaug[:, :, :, 64:65], 1.0)
    for i in range(8):
        v_ = vst[i % 2]
        P.dma(v_, tsrc[:, i * 4:(i + 1) * 4, 0:256])
        P.copy(Vaug[:, i * 4:(i + 1) * 4, :, 0:64], v_.r("p c (h d) -> p c h d", h=4), eng=('dve' if i % 2 else 'pool'))
    P.dma(iwt, tsrc[:, :, 512:516])
    P.ts(iwt, iwt, 1.0 / 16, ALU.mult)
    score = P.sb([128, S], F32, "score")
    junk = P.sb([128, S], BF16, "junk")
    mask01 = P.sb([128, S], BF16, "mask01")
    maskT = P.sb([128, 32, 128], BF16, "maskT")
    rl = [P.sb([128, 512], F32, "rl") for _ in range(4)]
    E = [P.sb([128, 512], BF16, "E") for _ in range(2)]
    lo = P.sb([128, 1], F32, "lo")
    hi = P.sb([128, 1], F32, "hi")
    mid = P.sb([128, 1], F32, "mid")
    cnt = P.sb([128, 1], F32, "cnt")
    sel = P.sb([128, 1], F32, "sel")
    dlt = P.sb([128, 1], F32, "dlt")
    stp = P.sb([128, 32], F32, "stp")
    pw = P.sb([128, 32], F32, "pw")
    P.dma(pw, Dm['dsa_pw'])
    zt = P.sb([128, S], F32, "zt")
    cz = P.sb([128, S], F32, "cz")
    nz = P.sb([128, 1], F32, "nz")
    npos = P.sb([128, 1], F32, "npos")
    flag = P.sb([128, 1], F32, "flag")
    f2 = P.sb([128, 1], F32, "f2")
    rr = P.sb([128, 1], F32, "rr")
    onesw = P.sb([128, 1], F32, "onesw")
    P.memset(onesw, 1.0)
    osb = P.sb([128, 4, 64], F32, "osb")
    rs = P.sb([128, 4, 1], F32, "rs")
    gt = [P.sb([128, 2, 128], F32, "gt") for _ in range(2)]
    sg = P.sb([128, 2, 128], F32, "sg")
    yo = [P.sb([128, 2, 128], BF16, "yo") for _ in range(2)]
    iqt = [P.sb([64, 4, 128], F32, "iqt") for _ in range(2)]
    iqsrc = Dm['iqR'].r("(h d) t -> d h t", d=64)
    gsrc = Dm['colsT'].r("(c p) t -> p c t", p=128)
    ydst = Dm['yT_dsa'].r("(c p) t -> p c t", p=128)
    ne = 0
    for i in range(32):
        qs = slice(i * 128, (i + 1) * 128)
        Nk = 128 * (i + 1)
        g_ = gt[i % 2]
        P.dma(g_, gsrc[:, C_DG:C_DG + 2, qs])
        iq_ = iqt[i % 2]
        P.dma(iq_, iqsrc[:, :, qs])
        for k0 in range(0, Nk, 512):
            kw = min(512, Nk - k0)
            pss = []
            for h in range(4):
                ps = P.ps()
                P.mm(ps[:, 0:kw], iq_[:, h, :], ikT[:, 0, k0:k0 + kw], inc=(h == 3))
                pss.append(ps)
            for h in range(4):
                P.act(rl[h][:, 0:kw], pss[h][:, 0:kw], AF.Relu)
            P.ts(score[:, k0:k0 + kw], rl[0][:, 0:kw], iwt[:, i, 0:1], ALU.mult)
            for h in range(1, 4):
                P.stt(score[:, k0:k0 + kw], rl[h][:, 0:kw], iwt[:, i, h:h + 1], score[:, k0:k0 + kw], ALU.mult, ALU.add)
        P.tt(score[:, i * 128:Nk], score[:, i * 128:Nk], cb, ALU.add)
        if Nk > 256:
            P.reduce(hi, score[:, 0:Nk], ALU.max)
            P.reduce(lo, score[:, 0:i * 128], ALU.min)
            P.tt(dlt, hi, lo, ALU.subtract)
            P.ts(dlt, dlt, 2.0, ALU.add)
            P.ts(stp, pw, dlt[:, 0:1], ALU.mult)
            P.stt(mid, dlt, 0.5, lo, ALU.mult, ALU.add)
            P.ts(mid, mid, -1.0, ALU.add)
            for it in range(N_BISECT):
                P.ts(junk[:, 0:Nk], score[:, 0:Nk], mid[:, 0:1], ALU.is_ge, 0.0, ALU.add, accum=cnt)
                if it < N_BISECT - 1:
                    P.ts(sel, cnt, 255.5, ALU.is_ge, 0.5, ALU.subtract)
                    P.stt(mid, sel, stp[:, it:it + 1], mid, ALU.mult, ALU.add)
                else:
                    P.ts(sel, cnt, 255.5, ALU.is_ge, 1.0, ALU.subtract)
                    P.stt(lo, sel, stp[:, it:it + 1], mid, ALU.mult, ALU.add)
        else:
            P.memset(lo, -1e29)
        P.ts(zt[:, 0:Nk], score[:, 0:Nk], 0.0, ALU.is_equal, 0.0, ALU.add, accum=nz)
        P.ts(junk[:, 0:Nk], score[:, 0:Nk], 0.0, ALU.is_gt, 0.0, ALU.add, accum=npos)
        P.ts(flag, npos, 255.5, ALU.is_lt)
        P.tt(f2, npos, nz, ALU.add)
        P.ts(f2, f2, 255.5, ALU.is_ge)
        P.tt(flag, flag, f2, ALU.mult)
        P.ts(rr, npos, -1.0, ALU.mult, 256.0, ALU.add)
        P.scan(cz[:, 0:Nk], onesw[:, 0:1].bc([128, Nk]), zt[:, 0:Nk], 0.0, ALU.mult, ALU.add)
        P.ts(cz[:, 0:Nk], cz[:, 0:Nk], rr[:, 0:1], ALU.is_le, flag[:, 0:1], ALU.mult)
        P.tt(zt[:, 0:Nk], zt[:, 0:Nk], cz[:, 0:Nk], ALU.mult)
        P.ts(f2, flag, -1.0, ALU.mult, 1.0, ALU.add)
        P.tt(lo, lo, f2, ALU.mult)
        P.stt(lo, flag, 1e-30, lo, ALU.mult, ALU.add)
        P.ts(mask01[:, 0:Nk], score[:, 0:Nk], lo[:, 0:1], ALU.is_ge)
        P.tt(mask01[:, 0:Nk], mask01[:, 0:Nk], zt[:, 0:Nk], ALU.add)
        for c0 in range(0, i + 1, 4):
            nc_ = min(4, i + 1 - c0)
            ps = P.ps()
            psb = ps.bitcast(BF16)
            for cl in range(nc_):
                c = c0 + cl
                P.transpose(psb[:, cl * 128:(cl + 1) * 128], mask01[:, c * 128:(c + 1) * 128], identb, inc=(cl == nc_ - 1))
            P.act(maskT[:, c0:c0 + nc_, :], psb[:, 0:nc_ * 128].r("p (c q) -> p c q", q=128), AF.Copy)
        psO = P.ps_acc()
        for h in range(4):
            for c0 in range(0, i + 1, 4):
                nc_ = min(4, i + 1 - c0)
                ps = P.ps()
                for cl in range(nc_):
                    c = c0 + cl
                    P.mm(ps[:, cl * 128:(cl + 1) * 128], kT[:, h, c * 128:(c + 1) * 128], qT[:, h, qs], inc=(cl == nc_ - 1))
                e_ = E[ne % 2]
                ne += 1
                P.act(e_[:, 0:nc_ * 128], ps[:, 0:nc_ * 128], AF.Exp, scale=0.125)
                P.tt(e_[:, 0:nc_ * 128].r("p (c q) -> p c q", q=128), e_[:, 0:nc_ * 128].r("p (c q) -> p c q", q=128),
                     maskT[:, c0:c0 + nc_, :], ALU.mult, eng=('dve' if ne % 2 else 'pool'))
                for cl in range(nc_):
                    c = c0 + cl
                    P.mm(psO[:, h * 65:(h + 1) * 65], e_[:, cl * 128:(cl + 1) * 128], Vaug[:, c, h, :],
                         start=(c == 0), stop=(c == i), inc=(cl == nc_ - 1))
        pv = psO[:, 0:260].r("p (h x) -> p h x", h=4)
        P.recip(rs, pv[:, :, 64:65])
        P.tt(osb, pv[:, :, 0:64], rs.m(lambda x: x.to_broadcast([128, 4, 64])), ALU.mult)
        P.act(sg, g_, AF.Silu)
        y_ = yo[i % 2]
        for p in range(2):
            ps = P.ps()
            P.transpose(ps[:, 0:128], osb[:, 2 * p:2 * p + 2, :].r("p a b -> p (a b)"), identf)
            P.tt(y_[:, p, :], ps[:, 0:128], sg[:, p, :], ALU.mult)
        P.dma(ydst[:, :, qs], y_)
    P.barrier()


STAGE_FNS['DSA'] = stage_DSA


FULL_PLAN = [('R', 0)] + [(st, l) for l in range(2) for st in ('P', 'X', 'RET', 'S5', 'RWKV', 'DSA', 'M')]


def kernel(**inputs):
    inputs = {k: np.asarray(v) for k, v in inputs.items()}
    nb = inputs['x'].shape[0]
    nc, P, used_in = build(FULL_PLAN)
    in_maps = []
    for b in range(nb):
        d = host_inputs(inputs, b)
        in_maps.append({k: v for k, v in d.items() if k in used_in})
    res = run_bass_kernel_spmd(nc, in_maps, core_ids=list(range(nb)))
    out = np.stack([np.ascontiguousarray(np.asarray(r['outT']).T) for r in res.results], 0)
    return out.astype(np.float32)
```

```python
from contextlib import ExitStack
import math
import numpy as np
import ml_dtypes
import concourse.bass as bass
import concourse.mybir as mybir
from concourse.bass_utils import run_bass_kernel_spmd

F32 = mybir.dt.float32
BF16 = mybir.dt.bfloat16
I32 = mybir.dt.int32
AF = mybir.ActivationFunctionType
ALU = mybir.AluOpType
AX = mybir.AxisListType

ENGS = ['sp', 'act', 'dve', 'pool', 'pe']
EPOCH = 16000
NDMASEM = 24
SB_BASE = 16640
SBUF_BYTES = 229000


class Buf:
    __slots__ = ('name', 'wev', 'rev', 'tracked')

    def __init__(self, name, tracked=True):
        self.name = name
        self.wev = {}
        self.rev = {}
        self.tracked = tracked


class V:
    __slots__ = ('buf', 'ap')

    def __init__(self, buf, ap):
        self.buf = buf
        self.ap = ap

    def __getitem__(self, k):
        return V(self.buf, self.ap[k])

    def m(self, fn):
        return V(self.buf, fn(self.ap))

    def r(self, s, **kw):
        return V(self.buf, self.ap.rearrange(s, **kw))

    def bc(self, shape):
        return V(self.buf, self.ap.to_broadcast(list(shape)))

    def bitcast(self, dt):
        return V(self.buf, self.ap.bitcast(dt))

    @property
    def shape(self):
        return tuple(self.ap.shape)


def _ap(x):
    return x.ap if isinstance(x, V) else x


class Prog:
    def __init__(self, nc):
        self.nc = nc
        self.q = {e: [] for e in ENGS}
        self.cnt = {e: 0 for e in ENGS}
        self.noinc = {e: False for e in ENGS}
        self.known = {e: {} for e in ENGS}
        self.dma_n = {e: 0 for e in ENGS}
        self.nbar = 0
        self.bufs = []
        self.sb_off = SB_BASE
        self.sb_id = 0
        self.sb_mark = 0
        self.psum = []
        for i in range(8):
            h = nc.alloc_psum_tensor(f"ps{i}", [128, 512], F32)
            self.psum.append(V(self._newbuf(f"ps{i}"), h[:]))
        self.ps_rr = 0

    def _newbuf(self, name, tracked=True):
        b = Buf(name, tracked)
        if tracked:
            self.bufs.append(b)
        return b

    def sb(self, shape, dtype=F32, name="t"):
        esz = {F32: 4, BF16: 2, I32: 4}[dtype]
        per = esz * int(np.prod(shape[1:]))
        per = (per + 63) // 64 * 64
        off = self.sb_off
        assert off + per <= SBUF_BYTES, f"SBUF overflow {name} {off}+{per}"
        self.sb_off += per
        self.sb_id += 1
        nm = f"{name}_{self.sb_id}"
        h = self.nc.alloc_sbuf_tensor_at(nm, list(shape), dtype, offset=off)
        return V(self._newbuf(nm), h[:])

    def mark(self):
        self.sb_mark = self.sb_off

    def release(self):
        self.sb_off = self.sb_mark

    def ps(self):
        v = self.psum[self.ps_rr % 7]
        self.ps_rr += 1
        return v

    def ps_acc(self):
        return self.psum[7]

    def dram(self, name, shape, dtype=F32, kind="Internal"):
        h = self.nc.dram_tensor(name, list(shape), dtype, kind=kind)
        return V(self._newbuf(name, tracked=False), h.ap())

    def _collect(self, eng, reads, writes, extra=None):
        waits = {}

        def need(evs, skip_own):
            for sk, v in evs.items():
                if skip_own and sk == eng:
                    continue
                if waits.get(sk, 0) < v:
                    waits[sk] = v
        for x in reads:
            if x.buf.tracked:
                need(x.buf.wev, False)
        for x in writes:
            if x.buf.tracked:
                need(x.buf.wev, True)
                need(x.buf.rev, True)
        if extra:
            need(extra, False)
        kn = self.known[eng]
        wl = []
        for sk, v in waits.items():
            if kn.get(sk, 0) < v:
                kn[sk] = v
                wl.append((sk, v))
        return wl

    def emit(self, eng, fn, reads=(), writes=(), inc=True):
        reads = [x for x in reads if isinstance(x, V)]
        writes = [x for x in writes if isinstance(x, V)]
        wl = self._collect(eng, reads, writes)
        idx = self.cnt[eng] + 1
        self.cnt[eng] = idx
        self.q[eng].append((wl, fn, ('c', idx)))
        for x in reads:
            b = x.buf
            if b.tracked and b.rev.get(eng, 0) < idx:
                b.rev[eng] = idx
        for x in writes:
            b = x.buf
            if b.tracked:
                b.wev = {eng: idx}
                b.rev = {}

    def dma(self, out, in_, eng='sp'):
        n = self.dma_n[eng]
        self.dma_n[eng] = n + 1
        slot, k = n % NDMASEM, n // NDMASEM
        sk = ('dma', eng, slot)
        val = 16 * (k + 1)
        extra = {sk: 16 * k} if k > 0 else None
        wl = self._collect(eng, [in_], [out], extra)
        oa, ia = out.ap, in_.ap
        self.q[eng].append((wl, lambda e: e.dma_start(out=oa, in_=ia), ('d', sk)))
        b = in_.buf
        if b.tracked:
            b.rev[sk] = val
        b = out.buf
        if b.tracked:
            b.wev = {sk: val}
            b.rev = {}

    def barrier(self):
        waits = {}
        for e in ENGS:
            if e != 'sp' and self.cnt[e] > 0:
                waits[e] = self.cnt[e]
            n = self.dma_n[e]
            for slot in range(min(n, NDMASEM)):
                k = (n - 1 - slot) // NDMASEM
                waits[('dma', e, slot)] = 16 * (k + 1)
        kn = self.known['sp']
        wl = []
        for sk, v in waits.items():
            if kn.get(sk, 0) < v:
                kn[sk] = v
                wl.append((sk, v))
        self.nbar += 1
        nb = self.nbar
        bk = ('bar', 0)
        self.q['sp'].append((wl, None, ('b', bk)))
        for e in ENGS:
            if e != 'sp':
                self.q[e].append(([(bk, nb)], None, None))
                for sk, v in waits.items():
                    if self.known[e].get(sk, 0) < v:
                        self.known[e][sk] = v
        for b in self.bufs:
            b.wev = {}
            b.rev = {}

    def finish(self):
        self.barrier()
        nc = self.nc
        targets = {e: set() for e in ENGS}
        for e in ENGS:
            for wl, fn, tag in self.q[e]:
                for sk, v in wl:
                    if isinstance(sk, str):
                        targets[sk].add(v)
        rank = {e: {v: r + 1 for r, v in enumerate(sorted(targets[e]))} for e in ENGS}
        self.n_inc = {e: len(rank[e]) for e in ENGS}
        keys = set()

        def semkey(sk, v):
            if isinstance(sk, str):
                r = rank[sk][v]
                return ((sk, (r - 1) // EPOCH), (r - 1) % EPOCH + 1)
            return (sk, v)
        prog = {e: [] for e in ENGS}
        for e in ENGS:
            for wl, fn, tag in self.q[e]:
                w2 = [semkey(sk, v) for sk, v in wl]
                inc = None
                if tag is not None:
                    if tag[0] == 'c':
                        if tag[1] in rank[e]:
                            r = rank[e][tag[1]]
                            inc = ((e, (r - 1) // EPOCH), 1)
                    elif tag[0] == 'd':
                        inc = (tag[1], 16)
                    elif tag[0] == 'b':
                        inc = (tag[1], 1)
                for k_, _ in w2:
                    keys.add(k_)
                if inc is not None:
                    keys.add(inc[0])
                prog[e].append((w2, fn, inc, tag))
        stack = ExitStack()
        sems = {}
        for i, sk in enumerate(sorted(keys, key=str)):
            sems[sk] = stack.enter_context(nc.semaphore(f"s{i}"))
        self.nsem = len(sems)

        def mk(en):
            def body(e):
                for wl, fn, inc, tag in prog[en]:
                    for sk, v in wl:
                        e.wait_ge(sems[sk], v)
                    if fn is None:
                        if tag is not None and tag[0] == 'b':
                            e.sem_inc(sems[inc[0]], inc[1])
                        continue
                    ins = fn(e)
                    if inc is not None:
                        ins.then_inc(sems[inc[0]], inc[1])
            return body
        with stack:
            with nc.Block() as block:
                block.sync(mk('sp'))
                block.scalar(mk('act'))
                block.vector(mk('dve'))
                block.gpsimd(mk('pool'))
                block.tensor(mk('pe'))

    def act(self, out, in_, func, bias=None, scale=1.0, accum=None):
        o, i, b, s, a = _ap(out), _ap(in_), _ap(bias), _ap(scale), _ap(accum)
        kw = {}
        if b is not None:
            kw['bias'] = b
        if a is not None:
            kw['accum_out'] = a
        self.emit('act', lambda e: e.activation(out=o, in_=i, func=func, scale=s, **kw),
                  [in_, bias, scale], [out, accum])

    def ts(self, out, in0, s1, op0, s2=None, op1=None, accum=None, eng='dve'):
        o, i, a1, a2, ac = _ap(out), _ap(in0), _ap(s1), _ap(s2), _ap(accum)
        kw = {}
        if op1 is not None:
            kw['op1'] = op1
        if ac is not None:
            kw['accum_out'] = ac
        self.emit(eng, lambda e: e.tensor_scalar(out=o, in0=i, scalar1=a1, scalar2=a2, op0=op0, **kw),
                  [in0, s1, s2], [out, accum])

    def tt(self, out, in0, in1, op, eng='dve'):
        o, a, b = _ap(out), _ap(in0), _ap(in1)
        self.emit(eng, lambda e: e.tensor_tensor(out=o, in0=a, in1=b, op=op), [in0, in1], [out])

    def stt(self, out, in0, scalar, in1, op0, op1, eng='dve'):
        o, a, s, b = _ap(out), _ap(in0), _ap(scalar), _ap(in1)
        self.emit(eng, lambda e: e.scalar_tensor_tensor(out=o, in0=a, scalar=s, in1=b, op0=op0, op1=op1),
                  [in0, scalar, in1], [out])

    def copy(self, out, in_, eng='dve'):
        o, i = _ap(out), _ap(in_)
        if eng == 'act':
            self.emit('act', lambda e: e.copy(out=o, in_=i), [in_], [out])
        else:
            self.emit(eng, lambda e: e.tensor_copy(out=o, in_=i), [in_], [out])

    def memset(self, out, val, eng='dve'):
        o = _ap(out)
        self.emit(eng, lambda e: e.memset(o, val), [], [out])

    def recip(self, out, in_):
        o, i = _ap(out), _ap(in_)
        self.emit('dve', lambda e: e.reciprocal(out=o, in_=i), [in_], [out])

    def reduce(self, out, in_, op, axis=AX.X):
        o, i = _ap(out), _ap(in_)
        self.emit('dve', lambda e: e.tensor_reduce(out=o, in_=i, axis=axis, op=op), [in_], [out])

    def scan(self, out, d0, d1, initial, op0, op1):
        o, a, b, ini = _ap(out), _ap(d0), _ap(d1), _ap(initial)
        self.emit('dve', lambda e: e.tensor_tensor_scan(out=o, data0=a, data1=b, initial=ini, op0=op0, op1=op1),
                  [d0, d1, initial], [out])

    def mm(self, out, lhsT, rhs, start=True, stop=True, inc=None):
        o, l, r = _ap(out), _ap(lhsT), _ap(rhs)
        if inc is None:
            inc = stop
        self.emit('pe', lambda e: e.matmul(o, l, r, start=start, stop=stop), [lhsT, rhs], [out], inc=inc)

    def transpose(self, out, in_, ident, inc=True):
        o, i, d = _ap(out), _ap(in_), _ap(ident)
        self.emit('pe', lambda e: e.transpose(o, i, d), [in_, ident], [out], inc=inc)


S = 4096
D = 1024
TT = 512
NTT = S // TT
NP_ROWS = 5376
NT_COLS = 516
B_RWKV, B_DSA, B_RET, B_S5, B_X, B_GATE = 0, 1152, 2500, 3524, 4036, 4548
C_RWKV = 0
C_DQ, C_DQS, C_DK, C_DKS, C_IQ, C_IQS, C_IK, C_DG = 9, 11, 13, 15, 17, 19, 21, 22
C_RQ, C_RQS, C_RK, C_RKS, C_RG = 24, 26, 28, 30, 32
C_SU, C_SG = 34, 36
C_XQ, C_XG = 38, 40


def _swap_idx(base, nheads):
    idx = []
    for h in range(nheads):
        for j in range(64):
            idx.append(base + h * 64 + (j + 32) % 64)
    return idx


def proj_col_indices():
    r = lambda a, n: list(range(a, a + n))
    f = []
    f += r(B_RWKV, 1152)
    f += r(B_DSA, 256) + _swap_idx(B_DSA, 4)
    f += r(B_DSA + 256, 256) + _swap_idx(B_DSA + 256, 4)
    f += r(B_DSA + 768, 256) + _swap_idx(B_DSA + 768, 4)
    f += r(B_DSA + 1024, 64) + _swap_idx(B_DSA + 1024, 1)
    f += r(B_DSA + 1092, 256)
    f += r(B_RET, 256) + _swap_idx(B_RET, 4)
    f += r(B_RET + 256, 256) + _swap_idx(B_RET + 256, 4)
    f += r(B_RET + 768, 256)
    f += r(B_S5, 512)
    f += r(B_X, 512)
    assert len(f) == NP_ROWS
    t = r(B_DSA + 512, 256) + r(B_RET + 512, 256) + r(B_DSA + 1088, 4)
    assert len(t) == NT_COLS
    return np.array(f), np.array(t)


def load_weight_bf16(P, dst, src_dram, gcol, nk, ncols, blk=1344):
    src = src_dram.r("(k p) n -> p k n", p=128)
    stg = [P.sb([128, blk], F32, "wstg") for _ in range(2)]
    i = 0
    for k in range(nk):
        for c0 in range(0, ncols, blk):
            c1 = min(ncols, c0 + blk)
            s = stg[i % 2]
            P.dma(s[:, 0:c1 - c0], src[:, k, c0:c1])
            eng = 'dve' if i % 2 == 0 else 'pool'
            if gcol is not None:
                P.ts(dst[:, k, c0:c1], s[:, 0:c1 - c0], gcol[:, k:k + 1], ALU.mult, eng=eng)
            else:
                P.copy(dst[:, k, c0:c1], s[:, 0:c1 - c0], eng=eng)
            i += 1


def rsqrt_ps(P, out, src, scale, eps):
    P.ts(out, src, scale, ALU.mult, eps, ALU.add)
    P.act(out, out, AF.Sqrt)
    P.recip(out, out)


def rms_tile(P, xt, hT, sq, rstd, ones, nk, n, width):
    P.act(sq, xt, AF.Square)
    ps = P.ps()
    for k in range(nk):
        P.mm(ps[:, 0:width], ones, sq[:, k, :], start=(k == 0), stop=(k == nk - 1))
    rsqrt_ps(P, rstd, ps[:, 0:width], 1.0 / n, 1e-6)
    for k in range(nk):
        P.tt(hT[:, k, :], xt[:, k, :], rstd, ALU.mult)


def stage_P(P, l, Dm, xT):
    P.sb_off = SB_BASE
    npre = P.sb([128, 8], F32, "npre")
    P.dma(npre, Dm[f'npre{l}'])
    ones = P.sb([128, 128], F32, "ones")
    P.memset(ones, 1.0)
    wp = P.sb([128, 8, NP_ROWS], BF16, "wp")
    wt = P.sb([128, 8, NT_COLS], BF16, "wt")
    wf = P.sb([128, 8, 644], F32, "wf")
    wfsrc = Dm[f'wpf{l}'].r("(k p) n -> p k n", p=128)
    for k in range(8):
        P.dma(wf[:, k, :], wfsrc[:, k, :])
    for k in range(8):
        P.ts(wf[:, k, :], wf[:, k, :], npre[:, k:k + 1], ALU.mult, eng=('dve' if k % 2 else 'pool'))
    m0 = P.sb_off
    load_weight_bf16(P, wp, Dm[f'wp{l}'], npre, 8, NP_ROWS)
    load_weight_bf16(P, wt, Dm[f'wt{l}'], npre, 8, NT_COLS, blk=NT_COLS)
    P.barrier()
    P.sb_off = m0
    xts = [P.sb([128, 8, TT], F32, "xt") for _ in range(2)]
    sq = P.sb([128, 8, TT], F32, "sq")
    hTs = [P.sb([128, 8, TT], BF16, "hT") for _ in range(2)]
    rstd = P.sb([128, TT], F32, "rstd")
    ostg = [P.sb([128, 4, TT], F32, "ostg") for _ in range(2)]
    tstg = [P.sb([128, NT_COLS], F32, "tstg") for _ in range(2)]
    xsrc = xT.r("(k p) t -> p k t", p=128)
    cdst = Dm['colsT'].r("(c p) t -> p c t", p=128)
    ctok = Dm['colsTok']
    for tt in range(NTT):
        t0 = tt * TT
        xt, hT = xts[tt % 2], hTs[tt % 2]
        P.dma(xt, xsrc[:, :, t0:t0 + TT])
        rms_tile(P, xt, hT, sq, rstd, ones, 8, D, TT)
        for k in range(8):
            P.tt(sq[:, k, :], xt[:, k, :], rstd, ALU.mult, eng='pool')
        for c in range(42):
            ps = P.ps()
            for k in range(8):
                if C_IQ <= c <= C_IK:
                    P.mm(ps, wf[:, k, (c - C_IQ) * 128:(c - C_IQ + 1) * 128], sq[:, k, :], start=(k == 0), stop=(k == 7))
                else:
                    P.mm(ps, wp[:, k, c * 128:(c + 1) * 128], hT[:, k, :], start=(k == 0), stop=(k == 7))
            stg = ostg[(c // 4) % 2]
            if c % 3 == 2:
                P.copy(stg[:, c % 4, :], ps, eng='dve')
            else:
                P.act(stg[:, c % 4, :], ps, AF.Copy)
            if c % 4 == 3 or c == 41:
                c0 = c - c % 4
                P.dma(cdst[:, c0:c + 1, t0:t0 + TT], stg[:, 0:c % 4 + 1, :])
        for s in range(4):
            ps = P.ps()
            ps2 = P.ps()
            for k in range(8):
                P.mm(ps, hT[:, k, s * 128:(s + 1) * 128], wt[:, k, 0:512], start=(k == 0), stop=(k == 7))
            for k in range(8):
                P.mm(ps2[:, 0:4], sq[:, k, s * 128:(s + 1) * 128], wf[:, k, 640:644], start=(k == 0), stop=(k == 7))
            ts_ = tstg[s % 2]
            P.act(ts_[:, 0:512], ps, AF.Copy)
            P.copy(ts_[:, 512:516], ps2[:, 0:4], eng='dve')
            P.dma(ctok[t0 + s * 128:t0 + (s + 1) * 128, :], ts_)
    P.barrier()


BR_NAMES = ['rwkv', 'dsa', 'ret', 's5', 'xatt']


def stage_M(P, l, Dm, xT, xT_out):
    P.sb_off = SB_BASE
    npre = P.sb([128, 8], F32, "npre")
    npost = P.sb([128, 8], F32, "npost")
    P.dma(npre, Dm[f'npre{l}'])
    P.dma(npost, Dm[f'npost{l}'])
    ones = P.sb([128, 128], F32, "ones")
    P.memset(ones, 1.0)
    wg = P.sb([128, 8, 5120], BF16, "wg")
    wbr = P.sb([128, 10, 1024], BF16, "wbr")
    wout = P.sb([128, 8, 1024], BF16, "wout")
    m0 = P.sb_off
    load_weight_bf16(P, wg, Dm[f'wg{l}'], npre, 8, 5120, blk=1280)
    load_weight_bf16(P, wbr, Dm[f'wbr{l}'], None, 10, 1024, blk=1024)
    load_weight_bf16(P, wout, Dm[f'wout{l}'], None, 8, 1024, blk=1024)
    P.barrier()
    P.sb_off = m0
    xt = P.sb([128, 8, TT], F32, "xt")
    sq = P.sb([128, 8, TT], F32, "sq")
    hT = P.sb([128, 8, TT], BF16, "hT")
    rstd = P.sb([128, TT], F32, "rstd")
    yts = [P.sb([128, 2, TT], BF16, f"y{i}") for i in range(5)]
    sg = [P.sb([128, TT], F32, "sg") for _ in range(2)]
    term = [P.sb([128, TT], F32, "term") for _ in range(2)]
    macc = P.sb([128, TT], F32, "macc")
    mT = P.sb([128, 8, TT], BF16, "mT")
    osb = sq
    osq = P.sb([128, TT], F32, "osq")
    xsrc = xT.r("(k p) t -> p k t", p=128)
    xdst = xT_out.r("(k p) t -> p k t", p=128)
    for tt in range(NTT):
        t0 = tt * TT
        P.dma(xt, xsrc[:, :, t0:t0 + TT])
        for i in range(5):
            P.dma(yts[i], Dm[f'yT_{BR_NAMES[i]}'].r("(c p) t -> p c t", p=128)[:, :, t0:t0 + TT])
        rms_tile(P, xt, hT, sq, rstd, ones, 8, D, TT)
        j = 0
        for dc in range(8):
            for i in range(5):
                psg = P.ps()
                for k in range(8):
                    P.mm(psg, wg[:, k, i * 1024 + dc * 128:i * 1024 + (dc + 1) * 128], hT[:, k, :],
                         start=(k == 0), stop=(k == 7))
                psb = P.ps()
                for kk in range(2):
                    P.mm(psb, wbr[:, i * 2 + kk, dc * 128:(dc + 1) * 128], yts[i][:, kk, :],
                         start=(kk == 0), stop=(kk == 1))
                s_, t_ = sg[j % 2], term[j % 2]
                j += 1
                P.act(s_, psg, AF.Sigmoid)
                if i == 0:
                    P.tt(macc, s_, psb, ALU.mult)
                elif i < 4:
                    P.tt(t_, s_, psb, ALU.mult)
                    P.tt(macc, macc, t_, ALU.add, eng='pool')
                else:
                    P.tt(t_, s_, psb, ALU.mult)
                    P.tt(mT[:, dc, :], macc, t_, ALU.add, eng='pool')
        pss = P.ps_acc()
        for ec in range(8):
            ps = P.ps()
            for k in range(8):
                P.mm(ps, wout[:, k, ec * 128:(ec + 1) * 128], mT[:, k, :], start=(k == 0), stop=(k == 7))
            P.act(osb[:, ec, :], ps, AF.Copy)
            P.act(osq, ps, AF.Square)
            P.mm(pss, ones, osq, start=(ec == 0), stop=(ec == 7))
        rsqrt_ps(P, rstd, pss, 1.0 / D, 1e-6)
        for ec in range(8):
            P.stt(osb[:, ec, :], osb[:, ec, :], npost[:, ec:ec + 1], rstd, ALU.mult, ALU.mult)
            P.tt(xt[:, ec, :], xt[:, ec, :], osb[:, ec, :], ALU.add, eng='pool')
        P.dma(xdst[:, :, t0:t0 + TT], xt)
    P.barrier()


def stage_X(P, l, Dm):
    P.sb_off = SB_BASE
    nmem = P.sb([128, 8], F32, "nmem")
    P.dma(nmem, Dm[f'nmem{l}'])
    ones = P.sb([128, 128], F32, "ones")
    P.memset(ones, 1.0)
    wm = P.sb([128, 8, 512], BF16, "wm")
    m0 = P.sb_off
    load_weight_bf16(P, wm, Dm[f'wmem{l}'], nmem, 8, 512, blk=512)
    P.barrier()
    P.sb_off = m0
    mt = P.sb([128, 8, 256], F32, "mt")
    msq = P.sb([128, 8, 256], F32, "msq")
    mh = P.sb([128, 8, 256], BF16, "mh")
    mr = P.sb([128, 256], F32, "mr")
    P.dma(mt, Dm['memT'].r("(k p) m -> p k m", p=128))
    rms_tile(P, mt, mh, msq, mr, ones, 8, D, 256)
    kmT = [P.sb([128, 256], BF16, "kmT") for _ in range(2)]
    for c in range(2):
        ps = P.ps()
        for k in range(8):
            P.mm(ps[:, 0:256], wm[:, k, c * 128:(c + 1) * 128], mh[:, k, :], start=(k == 0), stop=(k == 7))
        P.copy(kmT[c], ps[:, 0:256])
    vpad = [[P.sb([128, 128], BF16, "vpad") for _ in range(4)] for _ in range(2)]
    opad = [P.sb([128, 128], BF16, "opad") for _ in range(2)]
    for hh in range(2):
        P.memset(opad[hh], 0.0)
        P.memset(opad[hh][:, hh * 64:(hh + 1) * 64], 1.0)
    for mc in range(2):
        ps = P.ps()
        for k in range(8):
            P.mm(ps[:, 0:256], mh[:, k, mc * 128:(mc + 1) * 128], wm[:, k, 256:512], start=(k == 0), stop=(k == 7))
        for h in range(4):
            hh = h % 2
            P.memset(vpad[mc][h], 0.0)
            P.copy(vpad[mc][h][:, hh * 64:(hh + 1) * 64], ps[:, h * 64:(h + 1) * 64])
    qf = P.sb([128, 2, TT], F32, "qf")
    gf = P.sb([128, 2, TT], F32, "gf")
    qb = P.sb([128, 2, TT], BF16, "qb")
    E = [[P.sb([128, TT], BF16, "E") for _ in range(2)] for _ in range(2)]
    rs = P.sb([128, TT], F32, "rs")
    o = P.sb([128, TT], F32, "o")
    sgl = P.sb([128, TT], F32, "sgl")
    yst = P.sb([128, 2, TT], BF16, "yst")
    csrc = Dm['colsT'].r("(c p) t -> p c t", p=128)
    ydst = Dm['yT_xatt'].r("(c p) t -> p c t", p=128)
    for tt in range(NTT):
        t0 = tt * TT
        P.dma(qf, csrc[:, C_XQ:C_XQ + 2, t0:t0 + TT])
        P.dma(gf, csrc[:, C_XG:C_XG + 2, t0:t0 + TT])
        P.copy(qb, qf)
        for p in range(2):
            for hh in range(2):
                for mc in range(2):
                    ps = P.ps()
                    P.mm(ps, kmT[p][hh * 64:(hh + 1) * 64, mc * 128:(mc + 1) * 128],
                         qb[hh * 64:(hh + 1) * 64, p, :])
                    P.act(E[hh][mc], ps, AF.Exp, scale=0.125)
            pso = P.ps()
            pss = P.ps()
            n = 0
            for hh in range(2):
                for mc in range(2):
                    P.mm(pso, vpad[mc][2 * p + hh], E[hh][mc], start=(n == 0), stop=(n == 3))
                    n += 1
            n = 0
            for hh in range(2):
                for mc in range(2):
                    P.mm(pss, opad[hh], E[hh][mc], start=(n == 0), stop=(n == 3))
                    n += 1
            P.recip(rs, pss)
            P.tt(o, pso, rs, ALU.mult)
            P.act(sgl, gf[:, p, :], AF.Silu)
            P.tt(yst[:, p, :], o, sgl, ALU.mult)
        P.dma(ydst[:, :, t0:t0 + TT], yst)
    P.barrier()


def dram_specs():
    sp = {
        'xT': ([D, S], F32, 'in'), 'memT': ([D, 256], F32, 'in'), 'pos': ([1, S], I32, 'in'),
        'colsT': ([NP_ROWS, S], F32, 'scratch'), 'colsTok': ([S, NT_COLS], F32, 'scratch'),
        'xT1': ([D, S], F32, 'scratch'),
    }
    for n in BR_NAMES:
        sp[f'yT_{n}'] = ([256, S], BF16, 'scratch')
    sp['iqR'] = ([256, S], F32, 'scratch')
    sp['qR'] = ([256, S], F32, 'scratch')
    sp['ropeC'] = ([64, S], F32, 'scratch')
    sp['ropeS'] = ([64, S], F32, 'scratch')
    sp['ropeconst'] = ([64, 2], F32, 'in')
    sp['ident'] = ([128, 128], F32, 'in')
    sp['ret_idT'] = ([128, 4, 128], F32, 'in')
    sp['ret_qd'] = ([64, 4, 128], F32, 'in')
    sp['ret_kd'] = ([128, 4], F32, 'in')
    sp['ret_cd'] = ([64, 256], F32, 'in')
    sp['s5mask'] = ([128, 8, 8], F32, 'in')
    sp['rw_masks'] = ([64, 3, 64], F32, 'in')
    sp['dsa_cb'] = ([128, 128], F32, 'in')
    sp['dsa_pw'] = ([128, 32], F32, 'in')
    for l in range(2):
        sp[f'rwprm{l}'] = ([64, 8, 4], F32, 'in')
        sp[f'rwmu{l}'] = ([64, 18], F32, 'in')
        sp[f'rww2{l}'] = ([64, 256], F32, 'in')
        sp[f'rwa2{l}'] = ([64, 256], F32, 'in')
    sp['s5tau'] = ([128, 512], F32, 'in')
    for l in range(2):
        sp[f'retgn{l}'] = ([64, 4], F32, 'in')
        sp[f's5lam{l}'] = ([128, 8, 3], F32, 'in')
        sp[f's5b{l}'] = ([128, 8, 2, 16], F32, 'in')
        sp[f's5c{l}'] = ([128, 8, 2, 16], F32, 'in')
        sp[f's5d{l}'] = ([128, 2], F32, 'in')
        sp[f's5wglu{l}'] = ([256, 256], F32, 'in')
    for l in range(2):
        sp[f'wp{l}'] = ([D, NP_ROWS], F32, 'in')
        sp[f'wt{l}'] = ([D, NT_COLS], F32, 'in')
        sp[f'wpf{l}'] = ([D, 644], F32, 'in')
        sp[f'wg{l}'] = ([D, 5120], F32, 'in')
        sp[f'wbr{l}'] = ([1280, D], F32, 'in')
        sp[f'wout{l}'] = ([D, D], F32, 'in')
        sp[f'wmem{l}'] = ([D, 512], F32, 'in')
        for n in ['npre', 'npost', 'nmem']:
            sp[f'{n}{l}'] = ([128, 8], F32, 'in')
    return sp


def host_inputs(inputs, b):
    f_idx, t_idx = proj_col_indices()
    d = {}
    d['xT'] = np.ascontiguousarray(inputs['x'][b].T)
    d['memT'] = np.ascontiguousarray(inputs['mem'][b].T)
    d['pos'] = np.ascontiguousarray(inputs['positions'][b][None, :]).astype(np.int32)
    pk = lambda v: np.ascontiguousarray(v.reshape(8, 128).T)
    jj = np.arange(64)
    inv = (10000.0 ** (-(np.arange(32, dtype=np.float32)) / 32)).astype(np.float32)
    d['ropeconst'] = np.stack([inv[jj % 32], np.where(jj < 32, -1.0, 1.0)], 1).astype(np.float32)
    d['ident'] = np.eye(128, dtype=np.float32)
    d['ret_idT'], d['ret_qd'], d['ret_kd'], d['ret_cd'] = ret_consts()
    ii = np.arange(64)
    rm = np.zeros((64, 3, 64), np.float32)
    rm[:, 0, :] = (ii[None, :] > ii[:, None])
    rm[:, 1, :] = (ii[None, :] >= ii[:, None])
    rm[:, 2, :] = (ii[None, :] < ii[:, None])
    d['rw_masks'] = rm
    i128 = np.arange(128)
    d['dsa_pw'] = np.ascontiguousarray(np.broadcast_to((0.5 ** np.arange(1, 33, dtype=np.float64)).astype(np.float32)[None, :], (128, 32)))
    d['dsa_cb'] = np.where(i128[None, :] <= i128[:, None], 0.0, -1e30).astype(np.float32)
    for l in range(2):
        hd = lambda v: np.ascontiguousarray(v.reshape(4, 64).T)
        z = np.zeros((64, 4), np.float32)
        d[f'rwprm{l}'] = np.ascontiguousarray(np.stack([hd(inputs['rwkv_w0'][l]), hd(inputs['rwkv_a0'][l]), hd(inputs['rwkv_k_k'][l]),
                                   hd(inputs['rwkv_k_a'][l]), hd(inputs['rwkv_r_k'][l].reshape(256)), hd(inputs['rwkv_lnx_w'][l]),
                                   hd(inputs['rwkv_lnx_b'][l]), z], 1).astype(np.float32))
        d[f'rwmu{l}'] = np.ascontiguousarray(inputs['rwkv_mu'][l].reshape(18, 64).T)
        d[f'rww2{l}'] = np.ascontiguousarray(inputs['rwkv_w2'][l])
        d[f'rwa2{l}'] = np.ascontiguousarray(inputs['rwkv_a2'][l])
    sidx = np.arange(128)
    mk = np.zeros((128, 8, 8), np.float32)
    for j in range(8):
        mk[sidx, j, (2 * j + sidx // 64) % 8] = 1.0
    d['s5mask'] = mk
    d['s5tau'] = np.ascontiguousarray(np.broadcast_to(np.arange(1, 513, dtype=np.float32)[None, :], (128, 512)))
    sj = lambda a: np.ascontiguousarray(a.reshape((8, 128) + a.shape[1:]).swapaxes(0, 1))
    for l in range(2):
        d[f'retgn{l}'] = np.ascontiguousarray(inputs['ret_gn_w'][l].reshape(4, 64).T)
        lam3 = np.stack([inputs['s5_lam_re'][l].reshape(1024), inputs['s5_lam_im'][l].reshape(1024),
                         np.repeat(inputs['s5_log_dt'][l], 64)], 1).astype(np.float32)
        d[f's5lam{l}'] = sj(lam3)
        d[f's5b{l}'] = sj(np.stack([inputs['s5_b_re'][l].reshape(1024, 16), inputs['s5_b_im'][l].reshape(1024, 16)], 1))
        ct = lambda c: np.ascontiguousarray(c.transpose(0, 2, 1)).reshape(1024, 16)
        d[f's5c{l}'] = sj(np.stack([ct(inputs['s5_c_re'][l]), ct(inputs['s5_c_im'][l])], 1))
        d[f's5d{l}'] = np.ascontiguousarray(inputs['s5_d'][l].reshape(2, 128).T)
        d[f's5wglu{l}'] = np.ascontiguousarray(inputs['s5_w_glu'][l])
    for l in range(2):
        w = inputs['w_in'][l]
        d[f'wp{l}'] = np.ascontiguousarray(w[:, f_idx])
        d[f'wt{l}'] = np.ascontiguousarray(w[:, t_idx])
        d[f'wpf{l}'] = np.ascontiguousarray(w[:, np.concatenate([f_idx[C_IQ * 128:(C_IK + 1) * 128], t_idx[512:516]])])
        d[f'wg{l}'] = np.ascontiguousarray(w[:, B_GATE:B_GATE + 5120])
        d[f'wbr{l}'] = np.ascontiguousarray(inputs['w_branch'][l].reshape(1280, D))
        d[f'wout{l}'] = np.ascontiguousarray(inputs['w_out'][l])
        d[f'wmem{l}'] = np.ascontiguousarray(inputs['w_mem_kv'][l])
        d[f'npre{l}'] = pk(inputs['norm_pre'][l])
        d[f'npost{l}'] = pk(inputs['norm_post'][l])
        d[f'nmem{l}'] = pk(inputs['norm_mem'][l])
    return d


STAGE_FNS = {}


def build(plan, ext_in=(), ext_out=()):
    nc = bass.Bass("TRN2", target_bir_lowering=False)
    P = Prog(nc)
    Dm = {}
    used_in = []
    for name, (shape, dtype, role) in dram_specs().items():
        if role == 'in' or name in ext_in:
            kind = "ExternalInput"
            used_in.append(name)
        elif name in ext_out:
            kind = "ExternalOutput"
        else:
            kind = "Internal"
        Dm[name] = P.dram(name, shape, dtype, kind=kind)
    Dm['outT'] = P.dram('outT', [D, S], F32, kind="ExternalOutput")
    for st, l in plan:
        xin = Dm['xT'] if l == 0 else Dm['xT1']
        xout = Dm['xT1'] if l == 0 else Dm['outT']
        if st == 'P':
            stage_P(P, l, Dm, xin)
        elif st == 'M':
            stage_M(P, l, Dm, xin, xout)
        elif st == 'X':
            stage_X(P, l, Dm)
        else:
            STAGE_FNS[st](P, l, Dm)
    P.finish()
    return nc, P, used_in


def sin_reduced(P, out, ang, kq, ki, m1):
    P.ts(kq, ang, 1.0 / (2 * math.pi), ALU.mult)
    P.copy(ki, kq)
    P.copy(kq, ki)
    P.stt(ang, kq, -2 * math.pi, ang, ALU.mult, ALU.add)
    P.ts(m1, ang, math.pi, ALU.is_gt, -2 * math.pi, ALU.mult)
    P.tt(ang, ang, m1, ALU.add)
    P.ts(m1, ang, -math.pi, ALU.is_lt, 2 * math.pi, ALU.mult)
    P.tt(ang, ang, m1, ALU.add)
    P.act(out, ang, AF.Sin)


def stage_R(P, l, Dm):
    P.sb_off = SB_BASE
    W = 2048
    rc = P.sb([64, 2], F32, "rc")
    P.dma(rc, Dm['ropeconst'])
    posi = P.sb([64, W], I32, "posi")
    posf = P.sb([64, W], F32, "posf")
    ang = P.sb([64, W], F32, "ang")
    kq = P.sb([64, W], F32, "kq")
    ki = P.sb([64, W], I32, "ki")
    m1 = P.sb([64, W], F32, "m1")
    o = P.sb([64, W], F32, "o")
    for half in range(S // W):
        sl = slice(half * W, (half + 1) * W)
        P.dma(posi, Dm['pos'][:, sl].m(lambda x: x.to_broadcast([64, W])))
        P.copy(posf, posi)
        P.ts(ang, posf, rc[:, 0:1], ALU.mult)
        sin_reduced(P, o, ang, kq, ki, m1)
        P.ts(o, o, rc[:, 1:2], ALU.mult)
        P.dma(Dm['ropeS'][:, sl], o)
        P.ts(ang, posf, rc[:, 0:1], ALU.mult, math.pi / 2, ALU.add)
        sin_reduced(P, o, ang, kq, ki, m1)
        P.dma(Dm['ropeC'][:, sl], o)
    P.barrier()


def rope_heads(P, dst, Dm, c_base, c_swap, ropeC, ropeS, nheads=4, scale=None, dram_dst=None):
    a = P.sb([64, nheads, TT], F32, "ra")
    b = P.sb([64, nheads, TT], F32, "rb")
    if dram_dst is not None:
        ro = [P.sb([64, nheads, TT], F32, "ro") for _ in range(2)]
    src = Dm['colsT']
    for tt in range(NTT):
        sl = slice(tt * TT, (tt + 1) * TT)
        rb_ = c_base * 128 if isinstance(c_base, int) else c_base[0]
        rs_ = c_swap * 128 if isinstance(c_swap, int) else c_swap[0]
        P.dma(a, src[rb_:rb_ + nheads * 64, sl].r("(h d) t -> d h t", d=64))
        P.dma(b, src[rs_:rs_ + nheads * 64, sl].r("(h d) t -> d h t", d=64))
        cb = ropeC[:, sl].m(lambda x: x.unsqueeze(1).to_broadcast([64, nheads, TT]))
        sb_ = ropeS[:, sl].m(lambda x: x.unsqueeze(1).to_broadcast([64, nheads, TT]))
        P.tt(a, a, cb, ALU.mult)
        P.tt(b, b, sb_, ALU.mult, eng='pool')
        if dram_dst is None:
            P.tt(dst[:, :, sl], a, b, ALU.add)
        else:
            o_ = ro[tt % 2]
            P.tt(o_, a, b, ALU.add)
            P.dma(dram_dst.r("(h d) t -> d h t", d=64)[:, :, sl], o_)


RET_LOGG = [math.log(1.0 - math.exp(v)) for v in np.linspace(math.log(1.0 / 32), math.log(1.0 / 512), 4)]


def ret_consts():
    j = np.arange(128, dtype=np.float64)
    idT = np.zeros((128, 4, 128), np.float32)
    qd = np.zeros((64, 4, 128), np.float32)
    kd = np.zeros((128, 4), np.float32)
    cd = np.zeros((64, 256), np.float32)
    for h in range(4):
        lg = RET_LOGG[h]
        rel = j[None, :] - j[:, None]
        idT[:, h, :] = np.where(rel >= 0, np.exp(lg * np.maximum(rel, 0.0)), 0.0) * 0.125
        qd[:, h, :] = np.exp(lg * (j + 1.0))[None, :]
        kd[:, h] = np.exp(lg * (127.0 - j)) * 0.125
        cd[:, h * 64:(h + 1) * 64] = math.exp(lg * 128)
    return idT, qd, kd, cd


def stage_RET(P, l, Dm):
    P.sb_off = SB_BASE
    ropeC = P.sb([64, S], F32, "ropeC")
    ropeS = P.sb([64, S], F32, "ropeS")
    P.dma(ropeC, Dm['ropeC'])
    P.dma(ropeS, Dm['ropeS'])
    idT = P.sb([128, 4, 128], F32, "idT")
    qd = P.sb([64, 4, 128], F32, "qd")
    kd = P.sb([128, 4], F32, "kd")
    cd = P.sb([64, 256], F32, "cd")
    gn = P.sb([64, 4], F32, "gn")
    identb = P.sb([128, 128], BF16, "identb")
    identf = P.sb([128, 128], F32, "identf")
    ones64 = P.sb([64, 64], F32, "ones64")
    P.dma(idT, Dm['ret_idT'])
    P.dma(qd, Dm['ret_qd'])
    P.dma(kd, Dm['ret_kd'])
    P.dma(cd, Dm['ret_cd'])
    P.dma(gn, Dm[f'retgn{l}'])
    P.dma(identf, Dm['ident'])
    P.copy(identb, identf)
    P.memset(ones64, 1.0 / 64)
    qT = P.sb([64, 4, S], BF16, "qT")
    kT = P.sb([64, 4, S], BF16, "kT")
    qdT = P.sb([64, 4, S], BF16, "qdT")
    m0 = P.sb_off
    rope_heads(P, qT, Dm, C_RQ, C_RQS, ropeC, ropeS)
    rope_heads(P, kT, Dm, C_RK, C_RKS, ropeC, ropeS)
    for c in range(32):
        cs = slice(c * 128, (c + 1) * 128)
        P.tt(qdT[:, :, cs], qT[:, :, cs], qd, ALU.mult, eng=('dve' if c % 2 else 'pool'))
    P.barrier()
    P.sb_off = m0
    Vt = P.sb([128, 32, 256], BF16, "Vt")
    Kd = P.sb([128, 32, 256], BF16, "Kd")
    vst = [P.sb([128, 4, 256], F32, "vst") for _ in range(2)]
    vsrc = Dm['colsTok'].r("(c p) n -> p c n", p=128)
    for i in range(8):
        v_ = vst[i % 2]
        P.dma(v_, vsrc[:, i * 4:(i + 1) * 4, 256:512])
        P.copy(Vt[:, i * 4:(i + 1) * 4, :], v_, eng=('dve' if i % 2 else 'pool'))
    for c in range(32):
        cs = slice(c * 128, (c + 1) * 128)
        ps = P.ps()
        psb = ps.bitcast(BF16)
        for h in range(4):
            P.transpose(psb[:, h * 64:(h + 1) * 64], kT[:, h, cs], identb[0:64, 0:64])
        P.tt(Kd[:, c, :].r("p (h d) -> p h d", h=4), psb[:, 0:256].r("p (h d) -> p h d", h=4),
             kd.m(lambda x: x.unsqueeze(2).to_broadcast([128, 4, 64])), ALU.mult)
    R = P.sb([64, 256], F32, "R")
    Rb = P.sb([64, 256], BF16, "Rb")
    P.memset(R, 0.0)
    P.memset(Rb, 0.0)
    AT = [P.sb([128, 4, 128], BF16, "AT") for _ in range(2)]
    Osb = P.sb([64, 512], F32, "Osb")
    dd = P.sb([64, 512], F32, "dd")
    dsq = P.sb([64, 512], F32, "dsq")
    rstd = P.sb([64, 512], F32, "rstd")
    gt = [P.sb([64, 4, 128], F32, "gt") for _ in range(2)]
    sg = P.sb([64, 4, 128], F32, "sg")
    yo = [P.sb([64, 4, 128], BF16, "yo") for _ in range(2)]
    gsrc = Dm['colsT'][C_RG * 128:C_RG * 128 + 256, :].r("(h d) t -> d h t", d=64)
    ydst = Dm['yT_ret'].r("(h d) t -> d h t", d=64)
    for c in range(32):
        cs = slice(c * 128, (c + 1) * 128)
        g_ = gt[c % 2]
        P.dma(g_, gsrc[:, :, cs])
        psA = P.ps()
        for h in range(4):
            P.mm(psA[:, h * 128:(h + 1) * 128], kT[:, h, cs], qT[:, h, cs])
        at = AT[c % 2]
        P.tt(at, psA.r("p (h q) -> p h q", h=4), idT, ALU.mult)
        psO = P.ps()
        for h in range(4):
            P.mm(psO[0:64, h * 128:(h + 1) * 128], Vt[:, c, h * 64:(h + 1) * 64], at[:, h, :], start=True, stop=False, inc=False)
            P.mm(psO[0:64, h * 128:(h + 1) * 128], Rb[:, h * 64:(h + 1) * 64], qdT[:, h, cs], start=False, stop=True)
        psKV = P.ps()
        for h in range(4):
            P.mm(psKV[0:64, h * 64:(h + 1) * 64], Kd[:, c, h * 64:(h + 1) * 64], Vt[:, c, h * 64:(h + 1) * 64])
        P.tt(R, R, cd, ALU.mult)
        P.tt(R, R, psKV[0:64, 0:256], ALU.add)
        P.copy(Rb, R, eng='pool')
        P.act(Osb, psO[0:64, :], AF.Copy)
        psM = P.ps()
        P.mm(psM[0:64, :], ones64, Osb)
        P.tt(dd, Osb, psM[0:64, :], ALU.subtract)
        P.act(dsq, dd, AF.Square)
        psV = P.ps()
        P.mm(psV[0:64, :], ones64, dsq)
        P.ts(rstd, psV[0:64, :], 1e-6, ALU.add)
        P.act(rstd, rstd, AF.Sqrt)
        P.recip(rstd, rstd)
        P.tt(dd, dd, rstd, ALU.mult)
        P.tt(dd.r("p (h q) -> p h q", h=4), dd.r("p (h q) -> p h q", h=4),
             gn.m(lambda x: x.unsqueeze(2).to_broadcast([64, 4, 128])), ALU.mult)
        P.act(sg, g_, AF.Silu)
        y_ = yo[c % 2]
        P.tt(y_, dd.r("p (h q) -> p h q", h=4), sg, ALU.mult)
        P.dma(ydst[:, :, cs], y_)
    P.barrier()


STAGE_FNS['R'] = stage_R
STAGE_FNS['RET'] = stage_RET


def stage_S5(P, l, Dm):
    P.sb_off = SB_BASE
    W = TT
    lam = P.sb([128, 8, 3], F32, "lam")
    bsb = P.sb([128, 8, 2, 16], F32, "bsb")
    csb = P.sb([128, 8, 2, 16], F32, "csb")
    msk = P.sb([128, 8, 8], F32, "msk")
    tau = P.sb([128, W], F32, "tau")
    dsk = P.sb([128, 2], F32, "dsk")
    identf = P.sb([128, 128], F32, "identf")
    P.dma(lam, Dm[f's5lam{l}'])
    P.dma(bsb, Dm[f's5b{l}'])
    P.dma(csb, Dm[f's5c{l}'])
    P.dma(msk, Dm['s5mask'])
    P.dma(tau, Dm['s5tau'])
    P.dma(dsk, Dm[f's5d{l}'])
    P.dma(identf, Dm['ident'])
    wglu = P.sb([128, 2, 256], BF16, "wglu")
    cosT = P.sb([128, 8, W], F32, "cosT")
    sinT = P.sb([128, 8, W], F32, "sinT")
    mag = P.sb([128, 8], F32, "mag")
    BT = P.sb([128, 8, 2, 128], BF16, "BT")
    CX = P.sb([128, 8, 2, 128], BF16, "CX")
    m0 = P.sb_off
    load_weight_bf16(P, wglu, Dm[f's5wglu{l}'], None, 2, 256, blk=256)
    lr = P.sb([128, 8], F32, "lr")
    li = P.sb([128, 8], F32, "li")
    dt = P.sb([128, 8], F32, "dt")
    th = P.sb([128, 8], F32, "th")
    P.ts(lr, lam[:, :, 0], -1e-4, ALU.min)
    P.copy(li, lam[:, :, 1])
    P.act(dt, lam[:, :, 2], AF.Exp)
    P.tt(th, li, dt, ALU.mult)
    P.tt(mag, lr, dt, ALU.mult)
    P.act(mag, mag, AF.Exp)
    ang = P.sb([128, W], F32, "ang")
    kq = P.sb([128, W], F32, "kq")
    ki = P.sb([128, W], I32, "ki")
    m1 = P.sb([128, W], F32, "m1")
    for j in range(8):
        P.ts(ang, tau, th[:, j:j + 1], ALU.mult)
        sin_reduced(P, sinT[:, j, :], ang, kq, ki, m1)
        P.ts(ang, tau, th[:, j:j + 1], ALU.mult, math.pi / 2, ALU.add)
        sin_reduced(P, cosT[:, j, :], ang, kq, ki, m1)
    abr = P.sb([128, 8], F32, "abr")
    abi = P.sb([128, 8], F32, "abi")
    den = P.sb([128, 8], F32, "den")
    t8 = P.sb([128, 8], F32, "t8")
    fre = P.sb([128, 8], F32, "fre")
    fim = P.sb([128, 8], F32, "fim")
    P.tt(abr, mag, cosT[:, :, 0], ALU.mult)
    P.tt(abi, mag, sinT[:, :, 0], ALU.mult)
    P.ts(abr, abr, -1.0, ALU.add)
    P.tt(den, lr, lr, ALU.mult)
    P.tt(t8, li, li, ALU.mult)
    P.tt(den, den, t8, ALU.add)
    P.recip(den, den)
    P.tt(fre, abr, lr, ALU.mult)
    P.tt(t8, abi, li, ALU.mult)
    P.tt(fre, fre, t8, ALU.add)
    P.tt(fre, fre, den, ALU.mult)
    P.tt(fim, abi, lr, ALU.mult)
    P.tt(t8, abr, li, ALU.mult)
    P.tt(fim, fim, t8, ALU.subtract)
    P.tt(fim, fim, den, ALU.mult)
    bb = P.sb([128, 8, 2, 16], F32, "bb")
    tb = P.sb([128, 8, 16], F32, "tb")
    bc16 = lambda v: v.m(lambda x: x.unsqueeze(2).to_broadcast([128, 8, 16]))
    P.tt(bb[:, :, 0, :], bsb[:, :, 0, :], bc16(fre), ALU.mult)
    P.tt(tb, bsb[:, :, 1, :], bc16(fim), ALU.mult)
    P.tt(bb[:, :, 0, :], bb[:, :, 0, :], tb, ALU.subtract)
    P.tt(bb[:, :, 1, :], bsb[:, :, 1, :], bc16(fre), ALU.mult)
    P.tt(tb, bsb[:, :, 0, :], bc16(fim), ALU.mult)
    P.tt(bb[:, :, 1, :], bb[:, :, 1, :], tb, ALU.add)
    P.ts(csb[:, :, 1, :], csb[:, :, 1, :], -1.0, ALU.mult)
    bx = P.sb([128, 8, 16], F32, "bx")
    for j in range(8):
        mj = msk[:, j, :].m(lambda x: x.unsqueeze(2).to_broadcast([128, 8, 16]))
        for ri in range(2):
            P.tt(bx, bb[:, j, ri, :].m(lambda x: x.unsqueeze(1).to_broadcast([128, 8, 16])), mj, ALU.mult)
            ps = P.ps()
            P.transpose(ps[:, 0:128], bx.r("p a b -> p (a b)"), identf)
            P.copy(BT[:, j, ri, :], ps[:, 0:128])
            P.tt(CX[:, j, ri, :].r("p (a b) -> p a b", a=8),
                 csb[:, j, ri, :].m(lambda x: x.unsqueeze(1).to_broadcast([128, 8, 16])), mj, ALU.mult)
    P.barrier()
    P.sb_off = m0
    A = P.sb([128, 8, W], F32, "A")
    B = P.sb([128, 8, W], F32, "B")
    t1 = P.sb([128, 8, W], F32, "t1")
    t2 = P.sb([128, 8, W], F32, "t2")
    wre = P.sb([128, 8, W], F32, "wre")
    wim = P.sb([128, 8, W], F32, "wim")
    xre = P.sb([128, 8, W], BF16, "xre")
    xim = P.sb([128, 8, W], BF16, "xim")
    cre = P.sb([128, 8], F32, "cre")
    cim = P.sb([128, 8], F32, "cim")
    P.memset(cre, 0.0)
    P.memset(cim, 0.0)
    uf = P.sb([128, 2, W], F32, "uf")
    ub = P.sb([128, 2, W], BF16, "ub")
    gf = P.sb([128, 2, W], F32, "gf")
    y = P.sb([128, 2, W], F32, "y")
    y2 = P.sb([128, 2, W], F32, "y2")
    glb = P.sb([128, 2, W], BF16, "glb")
    yo = P.sb([128, 2, W], BF16, "yo")
    csrc = Dm['colsT'].r("(c p) t -> p c t", p=128)
    ydst = Dm['yT_s5'].r("(c p) t -> p c t", p=128)
    for tt in range(NTT):
        sl = slice(tt * W, (tt + 1) * W)
        P.dma(uf, csrc[:, C_SU:C_SU + 2, sl])
        P.dma(gf, csrc[:, C_SG:C_SG + 2, sl])
        P.copy(ub, uf, eng='pool')
        for j in range(8):
            for ri, dst in ((0, A), (1, B)):
                ps = P.ps()
                P.mm(ps, BT[:, j, ri, :], ub[:, j // 4, :])
                P.act(dst[:, j, :], ps, AF.Copy)
        P.tt(t1, A, cosT, ALU.mult)
        P.tt(t2, B, sinT, ALU.mult, eng='pool')
        P.tt(t1, t1, t2, ALU.add)
        P.tt(t2, A, sinT, ALU.mult, eng='pool')
        P.tt(B, B, cosT, ALU.mult)
        P.tt(t2, B, t2, ALU.subtract, eng='pool')
        for j in range(8):
            mb = mag[:, j:j + 1].bc([128, W])
            P.scan(wre[:, j, :], mb, t1[:, j, :], cre[:, j:j + 1], ALU.mult, ALU.add)
            P.scan(wim[:, j, :], mb, t2[:, j, :], cim[:, j:j + 1], ALU.mult, ALU.add)
        P.tt(t1, wre, cosT, ALU.mult)
        P.tt(A, wim, sinT, ALU.mult, eng='pool')
        P.tt(xre, t1, A, ALU.subtract)
        P.tt(cre, t1[:, :, W - 1], A[:, :, W - 1], ALU.subtract)
        P.tt(t2, wre, sinT, ALU.mult, eng='pool')
        P.tt(B, wim, cosT, ALU.mult)
        P.tt(xim, t2, B, ALU.add, eng='pool')
        P.tt(cim, t2[:, :, W - 1], B[:, :, W - 1], ALU.add)
        for jc in range(2):
            ps = P.ps()
            n = 0
            for j in range(4 * jc, 4 * jc + 4):
                for ri, xx in ((0, xre), (1, xim)):
                    P.mm(ps, CX[:, j, ri, :], xx[:, j, :], start=(n == 0), stop=(n == 7))
                    n += 1
            P.stt(y[:, jc, :], uf[:, jc, :], dsk[:, jc:jc + 1], ps, ALU.mult, ALU.add)
        P.tt(y2, y, y, ALU.mult)
        P.ts(y2, y2, 0.044715, ALU.mult, 1.0, ALU.add)
        P.tt(y2, y2, y, ALU.mult)
        P.act(y2, y2, AF.Sigmoid, scale=1.5957691216057308)
        P.tt(y, y, y2, ALU.mult)
        P.copy(glb, y, eng='pool')
        for oc in range(2):
            ps = P.ps()
            for kc in range(2):
                P.mm(ps, wglu[:, kc, oc * 128:(oc + 1) * 128], glb[:, kc, :], start=(kc == 0), stop=(kc == 1))
            P.act(y2[:, oc, :], ps, AF.Sigmoid)
        P.tt(y, y, y2, ALU.mult)
        P.act(y2, gf, AF.Silu)
        P.tt(yo, y, y2, ALU.mult)
        P.dma(ydst[:, :, sl], yo)
    P.barrier()


STAGE_FNS['S5'] = stage_S5


import os
RW_DEBUG = int(os.environ.get('RW_DEBUG', '3'))


def stage_RWKV(P, l, Dm):
    P.sb_off = SB_BASE
    W = 256
    H4 = 4
    NCH = W // 64
    HC = H4 * NCH
    prm = P.sb([64, 8, 4], F32, "prm")
    mu = P.sb([64, 18], F32, "mu")
    w2 = P.sb([64, 256], F32, "w2")
    a2 = P.sb([64, 256], F32, "a2")
    msks = P.sb([64, 3, 64], F32, "msks")
    identf = P.sb([128, 128], F32, "identf")
    ones64 = P.sb([64, 64], F32, "ones64")
    onesw = P.sb([64, 1], F32, "onesw")
    P.dma(prm, Dm[f'rwprm{l}'])
    P.dma(mu, Dm[f'rwmu{l}'])
    P.dma(w2, Dm[f'rww2{l}'])
    P.dma(a2, Dm[f'rwa2{l}'])
    P.dma(msks, Dm['rw_masks'])
    P.dma(identf, Dm['ident'])
    P.memset(ones64, 1.0)
    P.memset(onesw, 1.0)
    id64 = identf[0:64, 0:64]
    hb = lambda v, n=W: v.m(lambda x: x.unsqueeze(2).to_broadcast([64, H4, n]))
    mb = lambda k: msks[:, k, :].m(lambda x: x.unsqueeze(1).to_broadcast([64, H4, 64]))
    idb = id64.m(lambda x: x.unsqueeze(1).to_broadcast([64, H4, 64]))
    cin = P.sb([64, 18, W + 1], F32, "cin")
    cs = P.sb([64, 18, W], F32, "cs")
    f = lambda nm: P.sb([64, H4, W], F32, nm)
    twl = P.sb([64, W], F32, "twl")
    sgz, av, kx, t0, kp, beta = f("sgz"), f("av"), f("kx"), f("t0"), f("kp"), f("beta")
    kkn, lw, cw, e1, e2 = f("kkn"), f("lw"), f("cw"), f("e1"), f("e2")
    rt, at, bt, kt, Bh, Kh = f("rt"), f("at"), f("bt"), f("kt"), f("Bh"), f("Kh")
    bonus, Yt = f("bonus"), f("Yt")
    base = P.sb([64, HC], F32, "base")
    cwC = P.sb([64, HC], F32, "cwC")
    gC = P.sb([64, HC], F32, "gC")
    S0 = P.sb([64, H4, 64], F32, "S0")
    P.memset(S0, 0.0)
    NP2 = NCH // 2
    g8 = lambda nm, dt=F32: [P.sb([64, 2, H4, 64], dt, nm) for _ in range(NP2)]
    X, XT, PaT, AakT, ArbT, ArkT = g8("X", BF16), g8("XT", BF16), g8("PaT", BF16), g8("AakT"), g8("ArbT"), g8("ArkT")
    Vt, BhT, KhT, atT, W2, M2, M1T, KV, Gd = (g8("Vt"), g8("BhT"), g8("KhT"), g8("atT", BF16), g8("W2", BF16), g8("M2"),
                                              g8("M1T"), g8("KV"), g8("Gd"))
    Usb = [P.sb([64, H4, 64], F32, "Usb") for _ in range(2)]
    mb8 = lambda k: msks[:, k, :].m(lambda x: x.unsqueeze(1).unsqueeze(1).to_broadcast([64, 2, H4, 64]))
    id8 = id64.m(lambda x: x.unsqueeze(1).unsqueeze(1).to_broadcast([64, 2, H4, 64]))
    ps8 = lambda ps: ps[0:64, 0:512].r("p (c h x) -> p c h x", c=2, h=H4)
    yo = P.sb([64, H4, W], BF16, "yo")
    src = Dm['colsT'][0:1152, :].r("(g d) t -> d g t", d=64)
    ydst = Dm['yT_rwkv'].r("(h d) t -> d h t", d=64)
    ps4 = lambda ps: ps[0:64, 0:256].r("p (h x) -> p h x", h=H4)
    for tt in range(S // W):
        t_0 = tt * W
        if tt == 0:
            P.dma(cin[:, :, 1:W + 1], src[:, :, t_0:t_0 + W])
            P.memset(cin[:, :, 0:1], 0.0)
        else:
            P.dma(cin, src[:, :, t_0 - 1:t_0 + W])
        P.tt(cs, cin[:, :, 0:W], cin[:, :, 1:W + 1], ALU.subtract)
        P.tt(cs, cs, mu.m(lambda x: x.unsqueeze(2).to_broadcast([64, 18, W])), ALU.mult)
        P.tt(cs, cs, cin[:, :, 1:W + 1], ALU.add)
        Rr, Kk, Vv, G = cs[:, 0:4, :], cs[:, 4:8, :], cs[:, 8:12, :], cs[:, 14:18, :]
        P.act(twl, cs[:, 12, :], AF.Tanh)
        for h in range(H4):
            ps = P.ps()
            P.mm(ps[0:64, 0:W], w2[:, h * 64:(h + 1) * 64], twl)
            P.act(sgz[:, h, :], ps[0:64, 0:W], AF.Sigmoid, bias=prm[:, 0, h:h + 1])
            ps = P.ps()
            P.mm(ps[0:64, 0:W], a2[:, h * 64:(h + 1) * 64], cs[:, 13, :])
            P.act(av[:, h, :], ps[0:64, 0:W], AF.Sigmoid, bias=prm[:, 1, h:h + 1])
        P.tt(kx, Kk, hb(prm[:, 2, :]), ALU.mult)
        P.tt(t0, kx, kx, ALU.mult, eng='pool')
        for h in range(H4):
            ps = P.ps()
            P.mm(ps[0:64, 0:W], ones64, t0[:, h, :])
            P.ts(kkn[:, h, :], ps[0:64, 0:W], 1e-24, ALU.add)
        P.act(kkn, kkn, AF.Sqrt)
        P.recip(kkn, kkn)
        P.tt(kkn, kkn, kx, ALU.mult)
        P.ts(t0, av, -1.0, ALU.add)
        P.tt(t0, t0, hb(prm[:, 3, :]), ALU.mult)
        P.stt(kp, t0, 1.0, Kk, ALU.add, ALU.mult)
        P.tt(beta, kkn, av, ALU.mult, eng='pool')
        P.tt(t0, Rr, kp, ALU.mult)
        P.tt(t0, t0, hb(prm[:, 4, :]), ALU.mult)
        for h in range(H4):
            ps = P.ps()
            P.mm(ps[0:64, 0:W], ones64, t0[:, h, :])
            P.tt(bonus[:, h, :], ps[0:64, 0:W], Vv[:, h, :], ALU.mult)
        P.ts(lw, sgz, -math.exp(-0.5), ALU.mult)
        for h in range(H4):
            P.scan(cw[:, h, :], onesw[:, 0:1].bc([64, W]), lw[:, h, :], 0.0, ALU.mult, ALU.add)
        cw3 = cw.r("p h (c i) -> p (h c) i", i=64)
        P.memset(base, 0.0)
        P.copy(base.r("p (h c) -> p h c", h=H4)[:, :, 1:NCH], cw.r("p h (c i) -> p h c i", i=64)[:, :, 0:NCH - 1, 63])
        P.tt(cw3, cw3, base.m(lambda x: x.unsqueeze(2).to_broadcast([64, HC, 64])), ALU.subtract)
        P.copy(cwC, cw3[:, :, 63])
        P.act(gC, cwC, AF.Exp)
        P.act(e1, cw, AF.Exp)
        P.tt(rt, Rr, e1, ALU.mult)
        P.act(e1, cw, AF.Exp, scale=-1.0)
        P.tt(bt, beta, e1, ALU.mult)
        P.tt(kt, kp, e1, ALU.mult, eng='pool')
        P.tt(e2, cw, lw, ALU.subtract)
        P.act(e2, e2, AF.Exp)
        P.stt(at, kkn, -1.0, e2, ALU.mult, ALU.mult)
        e13 = e1.r("p h (c i) -> p (h c) i", i=64)
        P.tt(e13, cw3, cwC.m(lambda x: x.unsqueeze(2).to_broadcast([64, HC, 64])), ALU.subtract)
        P.act(e1, e1, AF.Exp, scale=-1.0)
        P.tt(Bh, beta, e1, ALU.mult)
        P.tt(Kh, kp, e1, ALU.mult, eng='pool')
        def mm8(p, lhf, rhf):
            ps = P.ps()
            for cl in range(2):
                for h in range(H4):
                    o_ = ps[0:64, (cl * H4 + h) * 64:(cl * H4 + h + 1) * 64]
                    P.mm(o_, lhf(p, cl, h), rhf(p, cl, h))
            return ps8(ps)
        csl = lambda p, cl: slice((2 * p + cl) * 64, (2 * p + cl + 1) * 64)
        tok = lambda t_: (lambda p, cl, h: t_[:, h, csl(p, cl)])
        blk = lambda t_: (lambda p, cl, h: t_[p][:, cl, h, :])
        for p in range(NP2):
            P.tt(X[p], mm8(p, tok(at), tok(bt)), mb8(2), ALU.mult)
            P.tt(XT[p], mm8(p, tok(bt), tok(at)), mb8(0), ALU.mult)
            P.tt(AakT[p], mm8(p, tok(kt), tok(at)), mb8(0), ALU.mult)
            P.tt(ArbT[p], mm8(p, tok(bt), tok(rt)), mb8(1), ALU.mult)
            P.tt(ArkT[p], mm8(p, tok(kt), tok(rt)), mb8(1), ALU.mult)
            P.tt(PaT[p], XT[p], id8, ALU.add)
        for srcT, dstT, eng in ((Vv, Vt, 'act'), (Bh, BhT, 'dve'), (Kh, KhT, 'act'), (at, atT, 'dve')):
            for p in range(NP2):
                ps = P.ps()
                for cl in range(2):
                    for h in range(H4):
                        P.transpose(ps[0:64, (cl * H4 + h) * 64:(cl * H4 + h + 1) * 64], srcT[:, h, csl(p, cl)], id64)
                if eng == 'act':
                    P.act(dstT[p], ps8(ps), AF.Copy)
                else:
                    P.copy(dstT[p], ps8(ps))
        for it in range(5):
            pxs = [(mm8(p, blk(XT), blk(X)), mm8(p, blk(X), blk(XT))) for p in range(NP2)]
            for p in range(NP2):
                P.act(X[p], pxs[p][0], AF.Copy)
                P.copy(XT[p], pxs[p][1])
            pps = [mm8(p, blk(X), blk(PaT)) for p in range(NP2)]
            for p in range(NP2):
                P.tt(PaT[p], PaT[p], pps[p], ALU.add)
        for p in range(NP2):
            P.act(KV[p], mm8(p, blk(KhT), blk(Vt)), AF.Copy)
            P.act(W2[p], mm8(p, blk(AakT), blk(Vt)), AF.Copy)
            P.copy(M1T[p], mm8(p, blk(atT), blk(PaT)))
            for cl in range(2):
                c = 2 * p + cl
                gcb = gC.r("p (h c) -> p h c", h=H4)[:, :, c].m(lambda x: x.unsqueeze(2).to_broadcast([64, H4, 64]))
                P.tt(Gd[p][:, cl], idb, gcb, ALU.mult)
        for p in range(NP2):
            P.act(M2[p], mm8(p, blk(PaT), blk(W2)), AF.Copy)
        for c in range(NCH):
            p, cl = c // 2, c % 2
            sl = slice(c * 64, (c + 1) * 64)
            us = Usb[c % 2]
            psu = P.ps()
            for h in range(H4):
                P.mm(psu[0:64, h * 64:(h + 1) * 64], M1T[p][:, cl, h, :], S0[:, h, :])
            psy = P.ps()
            for h in range(H4):
                o_ = psy[0:64, h * 64:(h + 1) * 64]
                P.mm(o_, S0[:, h, :], rt[:, h, sl], start=True, stop=False)
                P.mm(o_, Vt[p][:, cl, h, :], ArkT[p][:, cl, h, :], start=False, stop=True)
            P.tt(us, ps4(psu), M2[p][:, cl], ALU.add)
            pss = P.ps()
            for h in range(H4):
                o_ = pss[0:64, h * 64:(h + 1) * 64]
                P.mm(o_, Gd[p][:, cl, h, :], S0[:, h, :], start=True, stop=False)
                P.mm(o_, BhT[p][:, cl, h, :], us[:, h, :], start=False, stop=True)
            psy2 = P.ps()
            for h in range(H4):
                P.mm(psy2[0:64, h * 64:(h + 1) * 64], us[:, h, :], ArbT[p][:, cl, h, :])
            P.tt(S0, ps4(pss), KV[p][:, cl], ALU.add)
            P.act(Yt[:, :, sl], ps4(psy), AF.Copy)
            P.tt(Yt[:, :, sl], Yt[:, :, sl], ps4(psy2), ALU.add)
        for h in range(H4):
            ps = P.ps()
            P.mm(ps[0:64, 0:W], ones64, Yt[:, h, :])
            P.stt(e1[:, h, :], ps[0:64, 0:W], -1.0 / 64, Yt[:, h, :], ALU.mult, ALU.add)
        P.tt(e2, e1, e1, ALU.mult, eng='pool')
        for h in range(H4):
            ps = P.ps()
            P.mm(ps[0:64, 0:W], ones64, e2[:, h, :])
            P.ts(t0[:, h, :], ps[0:64, 0:W], 1.0 / 64, ALU.mult, 64e-5, ALU.add)
        P.act(t0, t0, AF.Sqrt)
        P.recip(t0, t0)
        P.tt(e1, e1, t0, ALU.mult)
        P.tt(e1, e1, hb(prm[:, 5, :]), ALU.mult)
        P.tt(e1, e1, hb(prm[:, 6, :]), ALU.add)
        P.tt(e1, e1, bonus, ALU.add)
        P.act(e2, G, AF.Silu)
        P.tt(yo, e1, e2, ALU.mult)
        P.dma(ydst[:, :, t_0:t_0 + W], yo)
    P.barrier()


STAGE_FNS['RWKV'] = stage_RWKV


N_BISECT = 20


def stage_DSA(P, l, Dm):
    P.sb_off = SB_BASE
    kT = P.sb([64, 4, S], BF16, "kT")
    ikT = P.sb([64, 1, S], F32, "ikT")
    m0 = P.sb_off
    ropeC = P.sb([64, S], F32, "ropeC")
    ropeS = P.sb([64, S], F32, "ropeS")
    P.dma(ropeC, Dm['ropeC'])
    P.dma(ropeS, Dm['ropeS'])
    rope_heads(P, None, Dm, C_DQ, C_DQS, ropeC, ropeS, dram_dst=Dm['qR'])
    rope_heads(P, kT, Dm, C_DK, C_DKS, ropeC, ropeS)
    rope_heads(P, None, Dm, C_IQ, C_IQS, ropeC, ropeS, dram_dst=Dm['iqR'])
    rope_heads(P, ikT, Dm, (C_IK * 128,), (C_IK * 128 + 64,), ropeC, ropeS, nheads=1)
    P.barrier()
    P.sb_off = m0
    identf = P.sb([128, 128], F32, "identf")
    identb = P.sb([128, 128], BF16, "identb")
    cb = P.sb([128, 128], F32, "cb")
    P.dma(identf, Dm['ident'])
    P.copy(identb, identf)
    P.dma(cb, Dm['dsa_cb'])
    Vaug = P.sb([128, 32, 4, 65], BF16, "Vaug")
    iwt = P.sb([128, 32, 4], F32, "iwt")
    vst = [P.sb([128, 4, 256], F32, "vst") for _ in range(2)]
    tsrc = Dm['colsTok'].r("(c p) n -> p c n", p=128)
    P.memset(Vaug[:, :, :, 64:65], 1.0)
    for i in range(8):
        v_ = vst[i % 2]
        P.dma(v_, tsrc[:, i * 4:(i + 1) * 4, 0:256])
        P.copy(Vaug[:, i * 4:(i + 1) * 4, :, 0:64], v_.r("p c (h d) -> p c h d", h=4), eng=('dve' if i % 2 else 'pool'))
    P.dma(iwt, tsrc[:, :, 512:516])
    P.ts(iwt, iwt, 1.0 / 16, ALU.mult)
    scores = [P.sb([128, S], F32, "score") for _ in range(2)]
    mask01s = [P.sb([128, S], BF16, "mask01") for _ in range(2)]
    maskTs = [P.sb([128, 32, 128], BF16, "maskT") for _ in range(2)]
    rl = [P.sb([128, 512], F32, "rl") for _ in range(4)]
    E = [P.sb([128, 512], BF16, "E") for _ in range(2)]
    lo = P.sb([128, 1], F32, "lo")
    hi = P.sb([128, 1], F32, "hi")
    mid = P.sb([128, 1], F32, "mid")
    cnt = P.sb([128, 1], F32, "cnt")
    sel = P.sb([128, 1], F32, "sel")
    dlt = P.sb([128, 1], F32, "dlt")
    stp = P.sb([128, 32], F32, "stp")
    pw = P.sb([128, 32], F32, "pw")
    P.dma(pw, Dm['dsa_pw'])
    zt = P.sb([128, S], BF16, "zt")
    cz = P.sb([128, S], F32, "cz")
    junk = cz
    nz = P.sb([128, 1], F32, "nz")
    npos = P.sb([128, 1], F32, "npos")
    flag = P.sb([128, 1], F32, "flag")
    f2 = P.sb([128, 1], F32, "f2")
    rr = P.sb([128, 1], F32, "rr")
    onesw = P.sb([128, 1], F32, "onesw")
    P.memset(onesw, 1.0)
    osb = P.sb([128, 4, 64], F32, "osb")
    rs = P.sb([128, 4, 1], F32, "rs")
    gt = [P.sb([128, 2, 128], F32, "gt") for _ in range(2)]
    sg = P.sb([128, 2, 128], F32, "sg")
    yo = [P.sb([128, 2, 128], BF16, "yo") for _ in range(2)]
    iqt = [P.sb([64, 4, 128], F32, "iqt") for _ in range(2)]
    iqsrc = Dm['iqR'].r("(h d) t -> d h t", d=64)
    qft = [P.sb([64, 4, 128], F32, "qft") for _ in range(2)]
    qbt = [P.sb([64, 4, 128], BF16, "qbt") for _ in range(2)]
    qsrc = Dm['qR'].r("(h d) t -> d h t", d=64)
    gsrc = Dm['colsT'].r("(c p) t -> p c t", p=128)
    ydst = Dm['yT_dsa'].r("(c p) t -> p c t", p=128)
    ne = 0
    for i in range(32):
        qs = slice(i * 128, (i + 1) * 128)
        Nk = 128 * (i + 1)
        g_ = gt[i % 2]
        P.dma(g_, gsrc[:, C_DG:C_DG + 2, qs])
        iq_ = iqt[i % 2]
        P.dma(iq_, iqsrc[:, :, qs])
        P.dma(qft[i % 2], qsrc[:, :, qs])
        qb_ = qbt[i % 2]
        P.copy(qb_, qft[i % 2], eng='pool')
        score = scores[i % 2]
        mask01 = mask01s[i % 2]
        maskT = maskTs[i % 2]
        for k0 in range(0, Nk, 512):
            kw = min(512, Nk - k0)
            pss = []
            for h in range(4):
                ps = P.ps()
                P.mm(ps[:, 0:kw], iq_[:, h, :], ikT[:, 0, k0:k0 + kw], inc=(h == 3))
                pss.append(ps)
            for h in range(4):
                P.act(rl[h][:, 0:kw], pss[h][:, 0:kw], AF.Relu)
            P.ts(score[:, k0:k0 + kw], rl[0][:, 0:kw], iwt[:, i, 0:1], ALU.mult)
            for h in range(1, 4):
                P.stt(score[:, k0:k0 + kw], rl[h][:, 0:kw], iwt[:, i, h:h + 1], score[:, k0:k0 + kw], ALU.mult, ALU.add)
        P.tt(score[:, i * 128:Nk], score[:, i * 128:Nk], cb, ALU.add)
        if Nk > 256:
            P.reduce(hi, score[:, 0:Nk], ALU.max)
            P.reduce(lo, score[:, 0:i * 128], ALU.min)
            P.tt(dlt, hi, lo, ALU.subtract)
            P.ts(dlt, dlt, 2.0, ALU.add)
            P.ts(stp, pw, dlt[:, 0:1], ALU.mult)
            P.stt(mid, dlt, 0.5, lo, ALU.mult, ALU.add)
            P.ts(mid, mid, -1.0, ALU.add)
            for it in range(N_BISECT):
                P.ts(junk[:, 0:Nk], score[:, 0:Nk], mid[:, 0:1], ALU.is_ge, 0.0, ALU.add, accum=cnt)
                if it < N_BISECT - 1:
                    P.ts(sel, cnt, 255.5, ALU.is_ge, 0.5, ALU.subtract)
                    P.stt(mid, sel, stp[:, it:it + 1], mid, ALU.mult, ALU.add)
                else:
                    P.ts(sel, cnt, 255.5, ALU.is_ge, 1.0, ALU.subtract)
                    P.stt(lo, sel, stp[:, it:it + 1], mid, ALU.mult, ALU.add)
        else:
            P.memset(lo, -1e29)
        P.ts(zt[:, 0:Nk], score[:, 0:Nk], 0.0, ALU.is_equal, 0.0, ALU.add, accum=nz)
        P.ts(junk[:, 0:Nk], score[:, 0:Nk], 0.0, ALU.is_gt, 0.0, ALU.add, accum=npos)
        P.ts(flag, npos, 255.5, ALU.is_lt)
        P.tt(f2, npos, nz, ALU.add)
        P.ts(f2, f2, 255.5, ALU.is_ge)
        P.tt(flag, flag, f2, ALU.mult)
        P.ts(rr, npos, -1.0, ALU.mult, 256.0, ALU.add)
        P.scan(cz[:, 0:Nk], onesw[:, 0:1].bc([128, Nk]), zt[:, 0:Nk], 0.0, ALU.mult, ALU.add)
        P.ts(cz[:, 0:Nk], cz[:, 0:Nk], rr[:, 0:1], ALU.is_le, flag[:, 0:1], ALU.mult)
        P.tt(zt[:, 0:Nk], zt[:, 0:Nk], cz[:, 0:Nk], ALU.mult)
        P.ts(f2, flag, -1.0, ALU.mult, 1.0, ALU.add)
        P.tt(lo, lo, f2, ALU.mult)
        P.stt(lo, flag, 1e-30, lo, ALU.mult, ALU.add)
        P.ts(mask01[:, 0:Nk], score[:, 0:Nk], lo[:, 0:1], ALU.is_ge)
        P.tt(mask01[:, 0:Nk], mask01[:, 0:Nk], zt[:, 0:Nk], ALU.add)
        for c0 in range(0, i + 1, 4):
            nc_ = min(4, i + 1 - c0)
            ps = P.ps()
            psb = ps.bitcast(BF16)
            for cl in range(nc_):
                c = c0 + cl
                P.transpose(psb[:, cl * 128:(cl + 1) * 128], mask01[:, c * 128:(c + 1) * 128], identb, inc=(cl == nc_ - 1))
            P.act(maskT[:, c0:c0 + nc_, :], psb[:, 0:nc_ * 128].r("p (c q) -> p c q", q=128), AF.Copy)
        psO = P.ps_acc()
        for h in range(4):
            for c0 in range(0, i + 1, 4):
                nc_ = min(4, i + 1 - c0)
                ps = P.ps()
                for cl in range(nc_):
                    c = c0 + cl
                    P.mm(ps[:, cl * 128:(cl + 1) * 128], kT[:, h, c * 128:(c + 1) * 128], qb_[:, h, :], inc=(cl == nc_ - 1))
                e_ = E[ne % 2]
                ne += 1
                P.act(e_[:, 0:nc_ * 128], ps[:, 0:nc_ * 128], AF.Exp, scale=0.125)
                P.tt(e_[:, 0:nc_ * 128].r("p (c q) -> p c q", q=128), e_[:, 0:nc_ * 128].r("p (c q) -> p c q", q=128),
                     maskT[:, c0:c0 + nc_, :], ALU.mult, eng=('dve' if ne % 2 else 'pool'))
                for cl in range(nc_):
                    c = c0 + cl
                    P.mm(psO[:, h * 65:(h + 1) * 65], e_[:, cl * 128:(cl + 1) * 128], Vaug[:, c, h, :],
                         start=(c == 0), stop=(c == i), inc=(cl == nc_ - 1))
        pv = psO[:, 0:260].r("p (h x) -> p h x", h=4)
        P.recip(rs, pv[:, :, 64:65])
        P.tt(osb, pv[:, :, 0:64], rs.m(lambda x: x.to_broadcast([128, 4, 64])), ALU.mult)
        P.act(sg, g_, AF.Silu)
        y_ = yo[i % 2]
        for p in range(2):
            ps = P.ps()
            P.transpose(ps[:, 0:128], osb[:, 2 * p:2 * p + 2, :].r("p a b -> p (a b)"), identf)
            P.tt(y_[:, p, :], ps[:, 0:128], sg[:, p, :], ALU.mult)
        P.dma(ydst[:, :, qs], y_)
    P.barrier()


STAGE_FNS['DSA'] = stage_DSA


FULL_PLAN = [('R', 0)] + [(st, l) for l in range(2) for st in ('P', 'X', 'RET', 'S5', 'RWKV', 'DSA', 'M')]


def kernel(**inputs):
    inputs = {k: np.asarray(v) for k, v in inputs.items()}
    nb = inputs['x'].shape[0]
    nc, P, used_in = build(FULL_PLAN)
    in_maps = []
    for b in range(nb):
        d = host_inputs(inputs, b)
        in_maps.append({k: v for k, v in d.items() if k in used_in})
    res = run_bass_kernel_spmd(nc, in_maps, core_ids=list(range(nb)))
    out = np.stack([np.ascontiguousarray(np.asarray(r['outT']).T) for r in res.results], 0)
    return out.astype(np.float32)
```

```python
from contextlib import ExitStack
import math
import numpy as np
import ml_dtypes
import concourse.bass as bass
import concourse.mybir as mybir
from concourse.bass_utils import run_bass_kernel_spmd

F32 = mybir.dt.float32
BF16 = mybir.dt.bfloat16
I32 = mybir.dt.int32
AF = mybir.ActivationFunctionType
ALU = mybir.AluOpType
AX = mybir.AxisListType

ENGS = ['sp', 'act', 'dve', 'pool', 'pe']
EPOCH = 16000
NDMASEM = 24
RELAX_SAME_ENGINE = True
SB_BASE = 16640
SBUF_BYTES = 229000


class Buf:
    __slots__ = ('name', 'wev', 'rev', 'tracked')

    def __init__(self, name, tracked=True):
        self.name = name
        self.wev = {}
        self.rev = {}
        self.tracked = tracked


class V:
    __slots__ = ('buf', 'ap')

    def __init__(self, buf, ap):
        self.buf = buf
        self.ap = ap

    def __getitem__(self, k):
        return V(self.buf, self.ap[k])

    def m(self, fn):
        return V(self.buf, fn(self.ap))

    def r(self, s, **kw):
        return V(self.buf, self.ap.rearrange(s, **kw))

    def bc(self, shape):
        return V(self.buf, self.ap.to_broadcast(list(shape)))

    def bitcast(self, dt):
        return V(self.buf, self.ap.bitcast(dt))

    @property
    def shape(self):
        return tuple(self.ap.shape)


def _ap(x):
    return x.ap if isinstance(x, V) else x


class Prog:
    def __init__(self, nc):
        self.nc = nc
        self.q = {e: [] for e in ENGS}
        self.cnt = {e: 0 for e in ENGS}
        self.noinc = {e: False for e in ENGS}
        self.known = {e: {} for e in ENGS}
        self.dma_n = {e: 0 for e in ENGS}
        self.nbar = 0
        self.bufs = []
        self.sb_off = SB_BASE
        self.sb_id = 0
        self.sb_mark = 0
        self.psum = []
        for i in range(8):
            h = nc.alloc_psum_tensor(f"ps{i}", [128, 512], F32)
            self.psum.append(V(self._newbuf(f"ps{i}"), h[:]))
        self.ps_rr = 0

    def _newbuf(self, name, tracked=True):
        b = Buf(name, tracked)
        if tracked:
            self.bufs.append(b)
        return b

    def sb(self, shape, dtype=F32, name="t"):
        esz = {F32: 4, BF16: 2, I32: 4}[dtype]
        per = esz * int(np.prod(shape[1:]))
        per = (per + 63) // 64 * 64
        off = self.sb_off
        assert off + per <= SBUF_BYTES, f"SBUF overflow {name} {off}+{per}"
        self.sb_off += per
        self.sb_id += 1
        nm = f"{name}_{self.sb_id}"
        h = self.nc.alloc_sbuf_tensor_at(nm, list(shape), dtype, offset=off)
        return V(self._newbuf(nm), h[:])

    def mark(self):
        self.sb_mark = self.sb_off

    def release(self):
        self.sb_off = self.sb_mark

    def ps(self):
        v = self.psum[self.ps_rr % 6]
        self.ps_rr += 1
        return v

    def ps_acc(self, i=1):
        return self.psum[6 + (i % 2)]

    def dram(self, name, shape, dtype=F32, kind="Internal"):
        h = self.nc.dram_tensor(name, list(shape), dtype, kind=kind)
        return V(self._newbuf(name, tracked=False), h.ap())

    def _collect(self, eng, reads, writes, extra=None):
        waits = {}

        last = self.cnt.get(eng, 0) if isinstance(eng, str) else 0

        def need(evs, skip_own):
            for sk, v in evs.items():
                if sk == eng:
                    if skip_own or (RELAX_SAME_ENGINE and v < last):
                        continue
                if waits.get(sk, 0) < v:
                    waits[sk] = v
        for x in reads:
            if x.buf.tracked:
                need(x.buf.wev, False)
        for x in writes:
            if x.buf.tracked:
                need(x.buf.wev, True)
                need(x.buf.rev, True)
        if extra:
            need(extra, False)
        kn = self.known[eng]
        wl = []
        for sk, v in waits.items():
            if kn.get(sk, 0) < v:
                kn[sk] = v
                wl.append((sk, v))
        return wl

    def emit(self, eng, fn, reads=(), writes=(), inc=True):
        reads = [x for x in reads if isinstance(x, V)]
        writes = [x for x in writes if isinstance(x, V)]
        wl = self._collect(eng, reads, writes)
        idx = self.cnt[eng] + 1
        self.cnt[eng] = idx
        self.q[eng].append((wl, fn, ('c', idx)))
        for x in reads:
            b = x.buf
            if b.tracked and b.rev.get(eng, 0) < idx:
                b.rev[eng] = idx
        for x in writes:
            b = x.buf
            if b.tracked:
                b.wev = {eng: idx}
                b.rev = {}

    def dma(self, out, in_, eng='sp'):
        n = self.dma_n[eng]
        self.dma_n[eng] = n + 1
        slot, k = n % NDMASEM, n // NDMASEM
        sk = ('dma', eng, slot)
        val = 16 * (k + 1)
        extra = {sk: 16 * k} if k > 0 else None
        wl = self._collect(eng, [in_], [out], extra)
        oa, ia = out.ap, in_.ap
        self.q[eng].append((wl, lambda e: e.dma_start(out=oa, in_=ia), ('d', sk)))
        b = in_.buf
        if b.tracked:
            b.rev[sk] = val
        b = out.buf
        if b.tracked:
            b.wev = {sk: val}
            b.rev = {}

    def barrier(self):
        waits = {}
        for e in ENGS:
            if e != 'sp' and self.cnt[e] > 0:
                waits[e] = self.cnt[e]
            n = self.dma_n[e]
            for slot in range(min(n, NDMASEM)):
                k = (n - 1 - slot) // NDMASEM
                waits[('dma', e, slot)] = 16 * (k + 1)
        kn = self.known['sp']
        wl = []
        for sk, v in waits.items():
            if kn.get(sk, 0) < v:
                kn[sk] = v
                wl.append((sk, v))
        self.nbar += 1
        nb = self.nbar
        bk = ('bar', 0)
        self.q['sp'].append((wl, None, ('b', bk)))
        for e in ENGS:
            if e != 'sp':
                self.q[e].append(([(bk, nb)], None, None))
                for sk, v in waits.items():
                    if self.known[e].get(sk, 0) < v:
                        self.known[e][sk] = v
        for b in self.bufs:
            b.wev = {}
            b.rev = {}

    def finish(self):
        self.barrier()
        nc = self.nc
        targets = {e: set() for e in ENGS}
        for e in ENGS:
            for wl, fn, tag in self.q[e]:
                for sk, v in wl:
                    if isinstance(sk, str):
                        targets[sk].add(v)
        rank = {e: {v: r + 1 for r, v in enumerate(sorted(targets[e]))} for e in ENGS}
        self.n_inc = {e: len(rank[e]) for e in ENGS}
        keys = set()

        def semkey(sk, v):
            if isinstance(sk, str):
                r = rank[sk][v]
                return ((sk, (r - 1) // EPOCH), (r - 1) % EPOCH + 1)
            return (sk, v)
        prog = {e: [] for e in ENGS}
        for e in ENGS:
            for wl, fn, tag in self.q[e]:
                w2 = [semkey(sk, v) for sk, v in wl]
                inc = None
                if tag is not None:
                    if tag[0] == 'c':
                        if tag[1] in rank[e]:
                            r = rank[e][tag[1]]
                            inc = ((e, (r - 1) // EPOCH), 1)
                    elif tag[0] == 'd':
                        inc = (tag[1], 16)
                    elif tag[0] == 'b':
                        inc = (tag[1], 1)
                for k_, _ in w2:
                    keys.add(k_)
                if inc is not None:
                    keys.add(inc[0])
                prog[e].append((w2, fn, inc, tag))
        stack = ExitStack()
        sems = {}
        for i, sk in enumerate(sorted(keys, key=str)):
            sems[sk] = stack.enter_context(nc.semaphore(f"s{i}"))
        self.nsem = len(sems)

        def mk(en):
            def body(e):
                for wl, fn, inc, tag in prog[en]:
                    for sk, v in wl:
                        e.wait_ge(sems[sk], v)
                    if fn is None:
                        if tag is not None and tag[0] == 'b':
                            e.sem_inc(sems[inc[0]], inc[1])
                        continue
                    ins = fn(e)
                    if inc is not None:
                        ins.then_inc(sems[inc[0]], inc[1])
            return body
        with stack:
            with nc.Block() as block:
                block.sync(mk('sp'))
                block.scalar(mk('act'))
                block.vector(mk('dve'))
                block.gpsimd(mk('pool'))
                block.tensor(mk('pe'))

    def act(self, out, in_, func, bias=None, scale=1.0, accum=None):
        o, i, b, s, a = _ap(out), _ap(in_), _ap(bias), _ap(scale), _ap(accum)
        kw = {}
        if b is not None:
            kw['bias'] = b
        if a is not None:
            kw['accum_out'] = a
        self.emit('act', lambda e: e.activation(out=o, in_=i, func=func, scale=s, **kw),
                  [in_, bias, scale], [out, accum])

    def ts(self, out, in0, s1, op0, s2=None, op1=None, accum=None, eng='dve'):
        o, i, a1, a2, ac = _ap(out), _ap(in0), _ap(s1), _ap(s2), _ap(accum)
        kw = {}
        if op1 is not None:
            kw['op1'] = op1
        if ac is not None:
            kw['accum_out'] = ac
        self.emit(eng, lambda e: e.tensor_scalar(out=o, in0=i, scalar1=a1, scalar2=a2, op0=op0, **kw),
                  [in0, s1, s2], [out, accum])

    def tt(self, out, in0, in1, op, eng='dve'):
        o, a, b = _ap(out), _ap(in0), _ap(in1)
        self.emit(eng, lambda e: e.tensor_tensor(out=o, in0=a, in1=b, op=op), [in0, in1], [out])

    def stt(self, out, in0, scalar, in1, op0, op1, eng='dve'):
        o, a, s, b = _ap(out), _ap(in0), _ap(scalar), _ap(in1)
        self.emit(eng, lambda e: e.scalar_tensor_tensor(out=o, in0=a, scalar=s, in1=b, op0=op0, op1=op1),
                  [in0, scalar, in1], [out])

    def copy(self, out, in_, eng='dve'):
        o, i = _ap(out), _ap(in_)
        if eng == 'act':
            self.emit('act', lambda e: e.copy(out=o, in_=i), [in_], [out])
        else:
            self.emit(eng, lambda e: e.tensor_copy(out=o, in_=i), [in_], [out])

    def memset(self, out, val, eng='dve'):
        o = _ap(out)
        self.emit(eng, lambda e: e.memset(o, val), [], [out])

    def recip(self, out, in_):
        o, i = _ap(out), _ap(in_)
        self.emit('dve', lambda e: e.reciprocal(out=o, in_=i), [in_], [out])

    def reduce(self, out, in_, op, axis=AX.X):
        o, i = _ap(out), _ap(in_)
        self.emit('dve', lambda e: e.tensor_reduce(out=o, in_=i, axis=axis, op=op), [in_], [out])

    def scan(self, out, d0, d1, initial, op0, op1):
        o, a, b, ini = _ap(out), _ap(d0), _ap(d1), _ap(initial)
        self.emit('dve', lambda e: e.tensor_tensor_scan(out=o, data0=a, data1=b, initial=ini, op0=op0, op1=op1),
                  [d0, d1, initial], [out])

    def mm(self, out, lhsT, rhs, start=True, stop=True, inc=None):
        o, l, r = _ap(out), _ap(lhsT), _ap(rhs)
        if inc is None:
            inc = stop
        self.emit('pe', lambda e: e.matmul(o, l, r, start=start, stop=stop), [lhsT, rhs], [out], inc=inc)

    def transpose(self, out, in_, ident, inc=True):
        o, i, d = _ap(out), _ap(in_), _ap(ident)
        self.emit('pe', lambda e: e.transpose(o, i, d), [in_, ident], [out], inc=inc)


S = 4096
D = 1024
TT = 512
NTT = S // TT
NP_ROWS = 5376
NT_COLS = 516
B_RWKV, B_DSA, B_RET, B_S5, B_X, B_GATE = 0, 1152, 2500, 3524, 4036, 4548
C_RWKV = 0
C_DQ, C_DQS, C_DK, C_DKS, C_IQ, C_IQS, C_IK, C_DG = 9, 11, 13, 15, 17, 19, 21, 22
C_RQ, C_RQS, C_RK, C_RKS, C_RG = 24, 26, 28, 30, 32
C_SU, C_SG = 34, 36
C_XQ, C_XG = 38, 40


def _swap_idx(base, nheads):
    idx = []
    for h in range(nheads):
        for j in range(64):
            idx.append(base + h * 64 + (j + 32) % 64)
    return idx


def proj_col_indices():
    r = lambda a, n: list(range(a, a + n))
    f = []
    f += r(B_RWKV, 1152)
    f += r(B_DSA, 256) + _swap_idx(B_DSA, 4)
    f += r(B_DSA + 256, 256) + _swap_idx(B_DSA + 256, 4)
    f += r(B_DSA + 768, 256) + _swap_idx(B_DSA + 768, 4)
    f += r(B_DSA + 1024, 64) + _swap_idx(B_DSA + 1024, 1)
    f += r(B_DSA + 1092, 256)
    f += r(B_RET, 256) + _swap_idx(B_RET, 4)
    f += r(B_RET + 256, 256) + _swap_idx(B_RET + 256, 4)
    f += r(B_RET + 768, 256)
    f += r(B_S5, 512)
    f += r(B_X, 512)
    assert len(f) == NP_ROWS
    t = r(B_DSA + 512, 256) + r(B_RET + 512, 256) + r(B_DSA + 1088, 4)
    assert len(t) == NT_COLS
    return np.array(f), np.array(t)


def load_weight_bf16(P, dst, src_dram, gcol, nk, ncols, blk=1344):
    src = src_dram.r("(k p) n -> p k n", p=128)
    stg = [P.sb([128, blk], F32, "wstg") for _ in range(2)]
    i = 0
    for k in range(nk):
        for c0 in range(0, ncols, blk):
            c1 = min(ncols, c0 + blk)
            s = stg[i % 2]
            P.dma(s[:, 0:c1 - c0], src[:, k, c0:c1])
            eng = 'dve' if i % 2 == 0 else 'pool'
            if gcol is not None:
                P.ts(dst[:, k, c0:c1], s[:, 0:c1 - c0], gcol[:, k:k + 1], ALU.mult, eng=eng)
            else:
                P.copy(dst[:, k, c0:c1], s[:, 0:c1 - c0], eng=eng)
            i += 1


def rsqrt_ps(P, out, src, scale, eps):
    P.ts(out, src, scale, ALU.mult, eps, ALU.add)
    P.act(out, out, AF.Sqrt)
    P.recip(out, out)


def rms_tile(P, xt, hT, sq, rstd, ones, nk, n, width):
    P.act(sq, xt, AF.Square)
    ps = P.ps()
    for k in range(nk):
        P.mm(ps[:, 0:width], ones, sq[:, k, :], start=(k == 0), stop=(k == nk - 1))
    rsqrt_ps(P, rstd, ps[:, 0:width], 1.0 / n, 1e-6)
    for k in range(nk):
        P.tt(hT[:, k, :], xt[:, k, :], rstd, ALU.mult)


def stage_P(P, l, Dm, xT):
    P.sb_off = SB_BASE
    npre = P.sb([128, 8], F32, "npre")
    P.dma(npre, Dm[f'npre{l}'])
    ones = P.sb([128, 128], F32, "ones")
    P.memset(ones, 1.0)
    wp = P.sb([128, 8, NP_ROWS], BF16, "wp")
    wt = P.sb([128, 8, NT_COLS], BF16, "wt")
    wf = P.sb([128, 8, 644], F32, "wf")
    wfsrc = Dm[f'wpf{l}'].r("(k p) n -> p k n", p=128)
    for k in range(8):
        P.dma(wf[:, k, :], wfsrc[:, k, :])
    for k in range(8):
        P.ts(wf[:, k, :], wf[:, k, :], npre[:, k:k + 1], ALU.mult, eng=('dve' if k % 2 else 'pool'))
    m0 = P.sb_off
    load_weight_bf16(P, wp, Dm[f'wp{l}'], npre, 8, NP_ROWS)
    load_weight_bf16(P, wt, Dm[f'wt{l}'], npre, 8, NT_COLS, blk=NT_COLS)
    P.barrier()
    P.sb_off = m0
    xts = [P.sb([128, 8, TT], F32, "xt") for _ in range(2)]
    sq = P.sb([128, 8, TT], F32, "sq")
    hTs = [P.sb([128, 8, TT], BF16, "hT") for _ in range(2)]
    rstd = P.sb([128, TT], F32, "rstd")
    ostg = [P.sb([128, 4, TT], F32, "ostg") for _ in range(2)]
    tstg = [P.sb([128, NT_COLS], F32, "tstg") for _ in range(2)]
    xsrc = xT.r("(k p) t -> p k t", p=128)
    cdst = Dm['colsT'].r("(c p) t -> p c t", p=128)
    ctok = Dm['colsTok']
    for tt in range(NTT):
        t0 = tt * TT
        xt, hT = xts[tt % 2], hTs[tt % 2]
        P.dma(xt, xsrc[:, :, t0:t0 + TT])
        rms_tile(P, xt, hT, sq, rstd, ones, 8, D, TT)
        for k in range(8):
            P.tt(sq[:, k, :], xt[:, k, :], rstd, ALU.mult, eng='pool')
        for c in range(42):
            ps = P.ps()
            for k in range(8):
                if C_IQ <= c <= C_IK:
                    P.mm(ps, wf[:, k, (c - C_IQ) * 128:(c - C_IQ + 1) * 128], sq[:, k, :], start=(k == 0), stop=(k == 7))
                else:
                    P.mm(ps, wp[:, k, c * 128:(c + 1) * 128], hT[:, k, :], start=(k == 0), stop=(k == 7))
            stg = ostg[(c // 4) % 2]
            if c % 3 == 2:
                P.copy(stg[:, c % 4, :], ps, eng='dve')
            else:
                P.act(stg[:, c % 4, :], ps, AF.Copy)
            if c % 4 == 3 or c == 41:
                c0 = c - c % 4
                P.dma(cdst[:, c0:c + 1, t0:t0 + TT], stg[:, 0:c % 4 + 1, :])
        for s in range(4):
            ps = P.ps()
            ps2 = P.ps()
            for k in range(8):
                P.mm(ps, hT[:, k, s * 128:(s + 1) * 128], wt[:, k, 0:512], start=(k == 0), stop=(k == 7))
            for k in range(8):
                P.mm(ps2[:, 0:4], sq[:, k, s * 128:(s + 1) * 128], wf[:, k, 640:644], start=(k == 0), stop=(k == 7))
            ts_ = tstg[s % 2]
            P.act(ts_[:, 0:512], ps, AF.Copy)
            P.copy(ts_[:, 512:516], ps2[:, 0:4], eng='dve')
            P.dma(ctok[t0 + s * 128:t0 + (s + 1) * 128, :], ts_)
    P.barrier()


BR_NAMES = ['rwkv', 'dsa', 'ret', 's5', 'xatt']


def stage_M(P, l, Dm, xT, xT_out):
    P.sb_off = SB_BASE
    npre = P.sb([128, 8], F32, "npre")
    npost = P.sb([128, 8], F32, "npost")
    P.dma(npre, Dm[f'npre{l}'])
    P.dma(npost, Dm[f'npost{l}'])
    ones = P.sb([128, 128], F32, "ones")
    P.memset(ones, 1.0)
    wg = P.sb([128, 8, 5120], BF16, "wg")
    wbr = P.sb([128, 10, 1024], BF16, "wbr")
    wout = P.sb([128, 8, 1024], BF16, "wout")
    m0 = P.sb_off
    load_weight_bf16(P, wg, Dm[f'wg{l}'], npre, 8, 5120, blk=1280)
    load_weight_bf16(P, wbr, Dm[f'wbr{l}'], None, 10, 1024, blk=1024)
    load_weight_bf16(P, wout, Dm[f'wout{l}'], None, 8, 1024, blk=1024)
    P.barrier()
    P.sb_off = m0
    xt = P.sb([128, 8, TT], F32, "xt")
    sq = P.sb([128, 8, TT], F32, "sq")
    hT = P.sb([128, 8, TT], BF16, "hT")
    rstd = P.sb([128, TT], F32, "rstd")
    yts = [P.sb([128, 2, TT], BF16, f"y{i}") for i in range(5)]
    sg = [P.sb([128, TT], F32, "sg") for _ in range(2)]
    term = [P.sb([128, TT], F32, "term") for _ in range(2)]
    macc = P.sb([128, TT], F32, "macc")
    mT = P.sb([128, 8, TT], BF16, "mT")
    osb = sq
    osq = P.sb([128, TT], F32, "osq")
    xsrc = xT.r("(k p) t -> p k t", p=128)
    xdst = xT_out.r("(k p) t -> p k t", p=128)
    for tt in range(NTT):
        t0 = tt * TT
        P.dma(xt, xsrc[:, :, t0:t0 + TT])
        for i in range(5):
            P.dma(yts[i], Dm[f'yT_{BR_NAMES[i]}'].r("(c p) t -> p c t", p=128)[:, :, t0:t0 + TT])
        rms_tile(P, xt, hT, sq, rstd, ones, 8, D, TT)
        j = 0
        for dc in range(8):
            for i in range(5):
                psg = P.ps()
                for k in range(8):
                    P.mm(psg, wg[:, k, i * 1024 + dc * 128:i * 1024 + (dc + 1) * 128], hT[:, k, :],
                         start=(k == 0), stop=(k == 7))
                psb = P.ps()
                for kk in range(2):
                    P.mm(psb, wbr[:, i * 2 + kk, dc * 128:(dc + 1) * 128], yts[i][:, kk, :],
                         start=(kk == 0), stop=(kk == 1))
                s_, t_ = sg[j % 2], term[j % 2]
                j += 1
                P.act(s_, psg, AF.Sigmoid)
                if i == 0:
                    P.tt(macc, s_, psb, ALU.mult)
                elif i < 4:
                    P.tt(t_, s_, psb, ALU.mult)
                    P.tt(macc, macc, t_, ALU.add, eng='pool')
                else:
                    P.tt(t_, s_, psb, ALU.mult)
                    P.tt(mT[:, dc, :], macc, t_, ALU.add, eng='pool')
        pss = P.ps_acc()
        for ec in range(8):
            ps = P.ps()
            for k in range(8):
                P.mm(ps, wout[:, k, ec * 128:(ec + 1) * 128], mT[:, k, :], start=(k == 0), stop=(k == 7))
            P.act(osb[:, ec, :], ps, AF.Copy)
            P.act(osq, ps, AF.Square)
            P.mm(pss, ones, osq, start=(ec == 0), stop=(ec == 7))
        rsqrt_ps(P, rstd, pss, 1.0 / D, 1e-6)
        for ec in range(8):
            P.stt(osb[:, ec, :], osb[:, ec, :], npost[:, ec:ec + 1], rstd, ALU.mult, ALU.mult)
            P.tt(xt[:, ec, :], xt[:, ec, :], osb[:, ec, :], ALU.add, eng='pool')
        P.dma(xdst[:, :, t0:t0 + TT], xt)
    P.barrier()


def stage_X(P, l, Dm):
    P.sb_off = SB_BASE
    nmem = P.sb([128, 8], F32, "nmem")
    P.dma(nmem, Dm[f'nmem{l}'])
    ones = P.sb([128, 128], F32, "ones")
    P.memset(ones, 1.0)
    wm = P.sb([128, 8, 512], BF16, "wm")
    m0 = P.sb_off
    load_weight_bf16(P, wm, Dm[f'wmem{l}'], nmem, 8, 512, blk=512)
    P.barrier()
    P.sb_off = m0
    mt = P.sb([128, 8, 256], F32, "mt")
    msq = P.sb([128, 8, 256], F32, "msq")
    mh = P.sb([128, 8, 256], BF16, "mh")
    mr = P.sb([128, 256], F32, "mr")
    P.dma(mt, Dm['memT'].r("(k p) m -> p k m", p=128))
    rms_tile(P, mt, mh, msq, mr, ones, 8, D, 256)
    kmT = [P.sb([128, 256], BF16, "kmT") for _ in range(2)]
    for c in range(2):
        ps = P.ps()
        for k in range(8):
            P.mm(ps[:, 0:256], wm[:, k, c * 128:(c + 1) * 128], mh[:, k, :], start=(k == 0), stop=(k == 7))
        P.copy(kmT[c], ps[:, 0:256])
    vpad = [[P.sb([128, 128], BF16, "vpad") for _ in range(4)] for _ in range(2)]
    opad = [P.sb([128, 128], BF16, "opad") for _ in range(2)]
    for hh in range(2):
        P.memset(opad[hh], 0.0)
        P.memset(opad[hh][:, hh * 64:(hh + 1) * 64], 1.0)
    for mc in range(2):
        ps = P.ps()
        for k in range(8):
            P.mm(ps[:, 0:256], mh[:, k, mc * 128:(mc + 1) * 128], wm[:, k, 256:512], start=(k == 0), stop=(k == 7))
        for h in range(4):
            hh = h % 2
            P.memset(vpad[mc][h], 0.0)
            P.copy(vpad[mc][h][:, hh * 64:(hh + 1) * 64], ps[:, h * 64:(h + 1) * 64])
    qf = P.sb([128, 2, TT], F32, "qf")
    gf = P.sb([128, 2, TT], F32, "gf")
    qb = P.sb([128, 2, TT], BF16, "qb")
    E = [[P.sb([128, TT], BF16, "E") for _ in range(2)] for _ in range(2)]
    rs = P.sb([128, TT], F32, "rs")
    o = P.sb([128, TT], F32, "o")
    sgl = P.sb([128, TT], F32, "sgl")
    yst = P.sb([128, 2, TT], BF16, "yst")
    csrc = Dm['colsT'].r("(c p) t -> p c t", p=128)
    ydst = Dm['yT_xatt'].r("(c p) t -> p c t", p=128)
    for tt in range(NTT):
        t0 = tt * TT
        P.dma(qf, csrc[:, C_XQ:C_XQ + 2, t0:t0 + TT])
        P.dma(gf, csrc[:, C_XG:C_XG + 2, t0:t0 + TT])
        P.copy(qb, qf)
        for p in range(2):
            for hh in range(2):
                for mc in range(2):
                    ps = P.ps()
                    P.mm(ps, kmT[p][hh * 64:(hh + 1) * 64, mc * 128:(mc + 1) * 128],
                         qb[hh * 64:(hh + 1) * 64, p, :])
                    P.act(E[hh][mc], ps, AF.Exp, scale=0.125)
            pso = P.ps()
            pss = P.ps()
            n = 0
            for hh in range(2):
                for mc in range(2):
                    P.mm(pso, vpad[mc][2 * p + hh], E[hh][mc], start=(n == 0), stop=(n == 3))
                    n += 1
            n = 0
            for hh in range(2):
                for mc in range(2):
                    P.mm(pss, opad[hh], E[hh][mc], start=(n == 0), stop=(n == 3))
                    n += 1
            P.recip(rs, pss)
            P.tt(o, pso, rs, ALU.mult)
            P.act(sgl, gf[:, p, :], AF.Silu)
            P.tt(yst[:, p, :], o, sgl, ALU.mult)
        P.dma(ydst[:, :, t0:t0 + TT], yst)
    P.barrier()


def dram_specs():
    sp = {
        'xT': ([D, S], F32, 'in'), 'memT': ([D, 256], F32, 'in'), 'pos': ([1, S], I32, 'in'),
        'colsT': ([NP_ROWS, S], F32, 'scratch'), 'colsTok': ([S, NT_COLS], F32, 'scratch'),
        'xT1': ([D, S], F32, 'scratch'),
    }
    for n in BR_NAMES:
        sp[f'yT_{n}'] = ([256, S], BF16, 'scratch')
    sp['iqR'] = ([256, S], F32, 'scratch')
    sp['qR'] = ([256, S], F32, 'scratch')
    sp['ropeC'] = ([64, S], F32, 'scratch')
    sp['ropeS'] = ([64, S], F32, 'scratch')
    sp['ropeconst'] = ([64, 2], F32, 'in')
    sp['ident'] = ([128, 128], F32, 'in')
    sp['ret_idT'] = ([128, 4, 128], F32, 'in')
    sp['ret_qd'] = ([64, 4, 128], F32, 'in')
    sp['ret_kd'] = ([128, 4], F32, 'in')
    sp['ret_cd'] = ([64, 256], F32, 'in')
    sp['s5mask'] = ([128, 8, 8], F32, 'in')
    sp['rw_masks'] = ([64, 3, 64], F32, 'in')
    sp['dsa_cb'] = ([128, 128], F32, 'in')
    sp['dsa_pw'] = ([128, 32], F32, 'in')
    for l in range(2):
        sp[f'rwprm{l}'] = ([64, 8, 4], F32, 'in')
        sp[f'rwmu{l}'] = ([64, 18], F32, 'in')
        sp[f'rww2{l}'] = ([64, 256], F32, 'in')
        sp[f'rwa2{l}'] = ([64, 256], F32, 'in')
    sp['s5tau'] = ([128, 512], F32, 'in')
    for l in range(2):
        sp[f'retgn{l}'] = ([64, 4], F32, 'in')
        sp[f's5lam{l}'] = ([128, 8, 3], F32, 'in')
        sp[f's5b{l}'] = ([128, 8, 2, 16], F32, 'in')
        sp[f's5c{l}'] = ([128, 8, 2, 16], F32, 'in')
        sp[f's5d{l}'] = ([128, 2], F32, 'in')
        sp[f's5wglu{l}'] = ([256, 256], F32, 'in')
    for l in range(2):
        sp[f'wp{l}'] = ([D, NP_ROWS], F32, 'in')
        sp[f'wt{l}'] = ([D, NT_COLS], F32, 'in')
        sp[f'wpf{l}'] = ([D, 644], F32, 'in')
        sp[f'wg{l}'] = ([D, 5120], F32, 'in')
        sp[f'wbr{l}'] = ([1280, D], F32, 'in')
        sp[f'wout{l}'] = ([D, D], F32, 'in')
        sp[f'wmem{l}'] = ([D, 512], F32, 'in')
        for n in ['npre', 'npost', 'nmem']:
            sp[f'{n}{l}'] = ([128, 8], F32, 'in')
    return sp


def host_inputs(inputs, b):
    f_idx, t_idx = proj_col_indices()
    d = {}
    d['xT'] = np.ascontiguousarray(inputs['x'][b].T)
    d['memT'] = np.ascontiguousarray(inputs['mem'][b].T)
    d['pos'] = np.ascontiguousarray(inputs['positions'][b][None, :]).astype(np.int32)
    pk = lambda v: np.ascontiguousarray(v.reshape(8, 128).T)
    jj = np.arange(64)
    inv = (10000.0 ** (-(np.arange(32, dtype=np.float32)) / 32)).astype(np.float32)
    d['ropeconst'] = np.stack([inv[jj % 32], np.where(jj < 32, -1.0, 1.0)], 1).astype(np.float32)
    d['ident'] = np.eye(128, dtype=np.float32)
    d['ret_idT'], d['ret_qd'], d['ret_kd'], d['ret_cd'] = ret_consts()
    ii = np.arange(64)
    rm = np.zeros((64, 3, 64), np.float32)
    rm[:, 0, :] = (ii[None, :] > ii[:, None])
    rm[:, 1, :] = (ii[None, :] >= ii[:, None])
    rm[:, 2, :] = (ii[None, :] < ii[:, None])
    d['rw_masks'] = rm
    i128 = np.arange(128)
    d['dsa_pw'] = np.ascontiguousarray(np.broadcast_to((0.5 ** np.arange(1, 33, dtype=np.float64)).astype(np.float32)[None, :], (128, 32)))
    d['dsa_cb'] = np.where(i128[None, :] <= i128[:, None], 0.0, -1e30).astype(np.float32)
    for l in range(2):
        hd = lambda v: np.ascontiguousarray(v.reshape(4, 64).T)
        z = np.zeros((64, 4), np.float32)
        d[f'rwprm{l}'] = np.ascontiguousarray(np.stack([hd(inputs['rwkv_w0'][l]), hd(inputs['rwkv_a0'][l]), hd(inputs['rwkv_k_k'][l]),
                                   hd(inputs['rwkv_k_a'][l]), hd(inputs['rwkv_r_k'][l].reshape(256)), hd(inputs['rwkv_lnx_w'][l]),
                                   hd(inputs['rwkv_lnx_b'][l]), z], 1).astype(np.float32))
        d[f'rwmu{l}'] = np.ascontiguousarray(inputs['rwkv_mu'][l].reshape(18, 64).T)
        d[f'rww2{l}'] = np.ascontiguousarray(inputs['rwkv_w2'][l])
        d[f'rwa2{l}'] = np.ascontiguousarray(inputs['rwkv_a2'][l])
    sidx = np.arange(128)
    mk = np.zeros((128, 8, 8), np.float32)
    for j in range(8):
        mk[sidx, j, (2 * j + sidx // 64) % 8] = 1.0
    d['s5mask'] = mk
    d['s5tau'] = np.ascontiguousarray(np.broadcast_to(np.arange(1, 513, dtype=np.float32)[None, :], (128, 512)))
    sj = lambda a: np.ascontiguousarray(a.reshape((8, 128) + a.shape[1:]).swapaxes(0, 1))
    for l in range(2):
        d[f'retgn{l}'] = np.ascontiguousarray(inputs['ret_gn_w'][l].reshape(4, 64).T)
        lam3 = np.stack([inputs['s5_lam_re'][l].reshape(1024), inputs['s5_lam_im'][l].reshape(1024),
                         np.repeat(inputs['s5_log_dt'][l], 64)], 1).astype(np.float32)
        d[f's5lam{l}'] = sj(lam3)
        d[f's5b{l}'] = sj(np.stack([inputs['s5_b_re'][l].reshape(1024, 16), inputs['s5_b_im'][l].reshape(1024, 16)], 1))
        ct = lambda c: np.ascontiguousarray(c.transpose(0, 2, 1)).reshape(1024, 16)
        d[f's5c{l}'] = sj(np.stack([ct(inputs['s5_c_re'][l]), ct(inputs['s5_c_im'][l])], 1))
        d[f's5d{l}'] = np.ascontiguousarray(inputs['s5_d'][l].reshape(2, 128).T)
        d[f's5wglu{l}'] = np.ascontiguousarray(inputs['s5_w_glu'][l])
    for l in range(2):
        w = inputs['w_in'][l]
        d[f'wp{l}'] = np.ascontiguousarray(w[:, f_idx])
        d[f'wt{l}'] = np.ascontiguousarray(w[:, t_idx])
        d[f'wpf{l}'] = np.ascontiguousarray(w[:, np.concatenate([f_idx[C_IQ * 128:(C_IK + 1) * 128], t_idx[512:516]])])
        d[f'wg{l}'] = np.ascontiguousarray(w[:, B_GATE:B_GATE + 5120])
        d[f'wbr{l}'] = np.ascontiguousarray(inputs['w_branch'][l].reshape(1280, D))
        d[f'wout{l}'] = np.ascontiguousarray(inputs['w_out'][l])
        d[f'wmem{l}'] = np.ascontiguousarray(inputs['w_mem_kv'][l])
        d[f'npre{l}'] = pk(inputs['norm_pre'][l])
        d[f'npost{l}'] = pk(inputs['norm_post'][l])
        d[f'nmem{l}'] = pk(inputs['norm_mem'][l])
    return d


STAGE_FNS = {}


def build(plan, ext_in=(), ext_out=()):
    nc = bass.Bass("TRN2", target_bir_lowering=False)
    P = Prog(nc)
    Dm = {}
    used_in = []
    for name, (shape, dtype, role) in dram_specs().items():
        if role == 'in' or name in ext_in:
            kind = "ExternalInput"
            used_in.append(name)
        elif name in ext_out:
            kind = "ExternalOutput"
        else:
            kind = "Internal"
        Dm[name] = P.dram(name, shape, dtype, kind=kind)
    Dm['outT'] = P.dram('outT', [D, S], F32, kind="ExternalOutput")
    for st, l in plan:
        xin = Dm['xT'] if l == 0 else Dm['xT1']
        xout = Dm['xT1'] if l == 0 else Dm['outT']
        if st == 'P':
            stage_P(P, l, Dm, xin)
        elif st == 'M':
            stage_M(P, l, Dm, xin, xout)
        elif st == 'X':
            stage_X(P, l, Dm)
        else:
            STAGE_FNS[st](P, l, Dm)
    P.finish()
    return nc, P, used_in


def sin_reduced(P, out, ang, kq, ki, m1):
    P.ts(kq, ang, 1.0 / (2 * math.pi), ALU.mult)
    P.copy(ki, kq)
    P.copy(kq, ki)
    P.stt(ang, kq, -2 * math.pi, ang, ALU.mult, ALU.add)
    P.ts(m1, ang, math.pi, ALU.is_gt, -2 * math.pi, ALU.mult)
    P.tt(ang, ang, m1, ALU.add)
    P.ts(m1, ang, -math.pi, ALU.is_lt, 2 * math.pi, ALU.mult)
    P.tt(ang, ang, m1, ALU.add)
    P.act(out, ang, AF.Sin)


def stage_R(P, l, Dm):
    P.sb_off = SB_BASE
    W = 2048
    rc = P.sb([64, 2], F32, "rc")
    P.dma(rc, Dm['ropeconst'])
    posi = P.sb([64, W], I32, "posi")
    posf = P.sb([64, W], F32, "posf")
    ang = P.sb([64, W], F32, "ang")
    kq = P.sb([64, W], F32, "kq")
    ki = P.sb([64, W], I32, "ki")
    m1 = P.sb([64, W], F32, "m1")
    o = P.sb([64, W], F32, "o")
    for half in range(S // W):
        sl = slice(half * W, (half + 1) * W)
        P.dma(posi, Dm['pos'][:, sl].m(lambda x: x.to_broadcast([64, W])))
        P.copy(posf, posi)
        P.ts(ang, posf, rc[:, 0:1], ALU.mult)
        sin_reduced(P, o, ang, kq, ki, m1)
        P.ts(o, o, rc[:, 1:2], ALU.mult)
        P.dma(Dm['ropeS'][:, sl], o)
        P.ts(ang, posf, rc[:, 0:1], ALU.mult, math.pi / 2, ALU.add)
        sin_reduced(P, o, ang, kq, ki, m1)
        P.dma(Dm['ropeC'][:, sl], o)
    P.barrier()


def rope_heads(P, dst, Dm, c_base, c_swap, ropeC, ropeS, nheads=4, scale=None, dram_dst=None):
    a = P.sb([64, nheads, TT], F32, "ra")
    b = P.sb([64, nheads, TT], F32, "rb")
    if dram_dst is not None:
        ro = [P.sb([64, nheads, TT], F32, "ro") for _ in range(2)]
    src = Dm['colsT']
    for tt in range(NTT):
        sl = slice(tt * TT, (tt + 1) * TT)
        rb_ = c_base * 128 if isinstance(c_base, int) else c_base[0]
        rs_ = c_swap * 128 if isinstance(c_swap, int) else c_swap[0]
        P.dma(a, src[rb_:rb_ + nheads * 64, sl].r("(h d) t -> d h t", d=64))
        P.dma(b, src[rs_:rs_ + nheads * 64, sl].r("(h d) t -> d h t", d=64))
        cb = ropeC[:, sl].m(lambda x: x.unsqueeze(1).to_broadcast([64, nheads, TT]))
        sb_ = ropeS[:, sl].m(lambda x: x.unsqueeze(1).to_broadcast([64, nheads, TT]))
        P.tt(a, a, cb, ALU.mult)
        P.tt(b, b, sb_, ALU.mult, eng='pool')
        if dram_dst is None:
            P.tt(dst[:, :, sl], a, b, ALU.add)
        else:
            o_ = ro[tt % 2]
            P.tt(o_, a, b, ALU.add)
            P.dma(dram_dst.r("(h d) t -> d h t", d=64)[:, :, sl], o_)


RET_LOGG = [math.log(1.0 - math.exp(v)) for v in np.linspace(math.log(1.0 / 32), math.log(1.0 / 512), 4)]


def ret_consts():
    j = np.arange(128, dtype=np.float64)
    idT = np.zeros((128, 4, 128), np.float32)
    qd = np.zeros((64, 4, 128), np.float32)
    kd = np.zeros((128, 4), np.float32)
    cd = np.zeros((64, 256), np.float32)
    for h in range(4):
        lg = RET_LOGG[h]
        rel = j[None, :] - j[:, None]
        idT[:, h, :] = np.where(rel >= 0, np.exp(lg * np.maximum(rel, 0.0)), 0.0) * 0.125
        qd[:, h, :] = np.exp(lg * (j + 1.0))[None, :]
        kd[:, h] = np.exp(lg * (127.0 - j)) * 0.125
        cd[:, h * 64:(h + 1) * 64] = math.exp(lg * 128)
    return idT, qd, kd, cd


def stage_RET(P, l, Dm):
    P.sb_off = SB_BASE
    ropeC = P.sb([64, S], F32, "ropeC")
    ropeS = P.sb([64, S], F32, "ropeS")
    P.dma(ropeC, Dm['ropeC'])
    P.dma(ropeS, Dm['ropeS'])
    idT = P.sb([128, 4, 128], F32, "idT")
    qd = P.sb([64, 4, 128], F32, "qd")
    kd = P.sb([128, 4], F32, "kd")
    cd = P.sb([64, 256], F32, "cd")
    gn = P.sb([64, 4], F32, "gn")
    identb = P.sb([128, 128], BF16, "identb")
    identf = P.sb([128, 128], F32, "identf")
    ones64 = P.sb([64, 64], F32, "ones64")
    P.dma(idT, Dm['ret_idT'])
    P.dma(qd, Dm['ret_qd'])
    P.dma(kd, Dm['ret_kd'])
    P.dma(cd, Dm['ret_cd'])
    P.dma(gn, Dm[f'retgn{l}'])
    P.dma(identf, Dm['ident'])
    P.copy(identb, identf)
    P.memset(ones64, 1.0 / 64)
    qT = P.sb([64, 4, S], BF16, "qT")
    kT = P.sb([64, 4, S], BF16, "kT")
    qdT = P.sb([64, 4, S], BF16, "qdT")
    m0 = P.sb_off
    rope_heads(P, qT, Dm, C_RQ, C_RQS, ropeC, ropeS)
    rope_heads(P, kT, Dm, C_RK, C_RKS, ropeC, ropeS)
    for c in range(32):
        cs = slice(c * 128, (c + 1) * 128)
        P.tt(qdT[:, :, cs], qT[:, :, cs], qd, ALU.mult, eng=('dve' if c % 2 else 'pool'))
    P.barrier()
    P.sb_off = m0
    Vt = P.sb([128, 32, 256], BF16, "Vt")
    Kd = P.sb([128, 32, 256], BF16, "Kd")
    vst = [P.sb([128, 4, 256], F32, "vst") for _ in range(2)]
    vsrc = Dm['colsTok'].r("(c p) n -> p c n", p=128)
    for i in range(8):
        v_ = vst[i % 2]
        P.dma(v_, vsrc[:, i * 4:(i + 1) * 4, 256:512])
        P.copy(Vt[:, i * 4:(i + 1) * 4, :], v_, eng=('dve' if i % 2 else 'pool'))
    for c in range(32):
        cs = slice(c * 128, (c + 1) * 128)
        ps = P.ps()
        psb = ps.bitcast(BF16)
        for h in range(4):
            P.transpose(psb[:, h * 64:(h + 1) * 64], kT[:, h, cs], identb[0:64, 0:64])
        P.tt(Kd[:, c, :].r("p (h d) -> p h d", h=4), psb[:, 0:256].r("p (h d) -> p h d", h=4),
             kd.m(lambda x: x.unsqueeze(2).to_broadcast([128, 4, 64])), ALU.mult)
    R = P.sb([64, 256], F32, "R")
    Rb = P.sb([64, 256], BF16, "Rb")
    P.memset(R, 0.0)
    P.memset(Rb, 0.0)
    AT = [P.sb([128, 4, 128], BF16, "AT") for _ in range(2)]
    Osb = P.sb([64, 512], F32, "Osb")
    dd = P.sb([64, 512], F32, "dd")
    dsq = P.sb([64, 512], F32, "dsq")
    rstd = P.sb([64, 512], F32, "rstd")
    gt = [P.sb([64, 4, 128], F32, "gt") for _ in range(2)]
    sg = P.sb([64, 4, 128], F32, "sg")
    yo = [P.sb([64, 4, 128], BF16, "yo") for _ in range(2)]
    gsrc = Dm['colsT'][C_RG * 128:C_RG * 128 + 256, :].r("(h d) t -> d h t", d=64)
    ydst = Dm['yT_ret'].r("(h d) t -> d h t", d=64)
    for c in range(32):
        cs = slice(c * 128, (c + 1) * 128)
        g_ = gt[c % 2]
        P.dma(g_, gsrc[:, :, cs])
        psA = P.ps()
        for h in range(4):
            P.mm(psA[:, h * 128:(h + 1) * 128], kT[:, h, cs], qT[:, h, cs])
        at = AT[c % 2]
        P.tt(at, psA.r("p (h q) -> p h q", h=4), idT, ALU.mult)
        psO = P.ps()
        for h in range(4):
            P.mm(psO[0:64, h * 128:(h + 1) * 128], Vt[:, c, h * 64:(h + 1) * 64], at[:, h, :], start=True, stop=False, inc=False)
            P.mm(psO[0:64, h * 128:(h + 1) * 128], Rb[:, h * 64:(h + 1) * 64], qdT[:, h, cs], start=False, stop=True)
        psKV = P.ps()
        for h in range(4):
            P.mm(psKV[0:64, h * 64:(h + 1) * 64], Kd[:, c, h * 64:(h + 1) * 64], Vt[:, c, h * 64:(h + 1) * 64])
        P.tt(R, R, cd, ALU.mult)
        P.tt(R, R, psKV[0:64, 0:256], ALU.add)
        P.copy(Rb, R, eng='pool')
        P.act(Osb, psO[0:64, :], AF.Copy)
        psM = P.ps()
        P.mm(psM[0:64, :], ones64, Osb)
        P.tt(dd, Osb, psM[0:64, :], ALU.subtract)
        P.act(dsq, dd, AF.Square)
        psV = P.ps()
        P.mm(psV[0:64, :], ones64, dsq)
        P.ts(rstd, psV[0:64, :], 1e-6, ALU.add)
        P.act(rstd, rstd, AF.Sqrt)
        P.recip(rstd, rstd)
        P.tt(dd, dd, rstd, ALU.mult)
        P.tt(dd.r("p (h q) -> p h q", h=4), dd.r("p (h q) -> p h q", h=4),
             gn.m(lambda x: x.unsqueeze(2).to_broadcast([64, 4, 128])), ALU.mult)
        P.act(sg, g_, AF.Silu)
        y_ = yo[c % 2]
        P.tt(y_, dd.r("p (h q) -> p h q", h=4), sg, ALU.mult)
        P.dma(ydst[:, :, cs], y_)
    P.barrier()


STAGE_FNS['R'] = stage_R
STAGE_FNS['RET'] = stage_RET


def stage_S5(P, l, Dm):
    P.sb_off = SB_BASE
    W = TT
    lam = P.sb([128, 8, 3], F32, "lam")
    bsb = P.sb([128, 8, 2, 16], F32, "bsb")
    csb = P.sb([128, 8, 2, 16], F32, "csb")
    msk = P.sb([128, 8, 8], F32, "msk")
    tau = P.sb([128, W], F32, "tau")
    dsk = P.sb([128, 2], F32, "dsk")
    identf = P.sb([128, 128], F32, "identf")
    P.dma(lam, Dm[f's5lam{l}'])
    P.dma(bsb, Dm[f's5b{l}'])
    P.dma(csb, Dm[f's5c{l}'])
    P.dma(msk, Dm['s5mask'])
    P.dma(tau, Dm['s5tau'])
    P.dma(dsk, Dm[f's5d{l}'])
    P.dma(identf, Dm['ident'])
    wglu = P.sb([128, 2, 256], BF16, "wglu")
    cosT = P.sb([128, 8, W], F32, "cosT")
    sinT = P.sb([128, 8, W], F32, "sinT")
    mag = P.sb([128, 8], F32, "mag")
    BT = P.sb([128, 8, 2, 128], BF16, "BT")
    CX = P.sb([128, 8, 2, 128], BF16, "CX")
    m0 = P.sb_off
    load_weight_bf16(P, wglu, Dm[f's5wglu{l}'], None, 2, 256, blk=256)
    lr = P.sb([128, 8], F32, "lr")
    li = P.sb([128, 8], F32, "li")
    dt = P.sb([128, 8], F32, "dt")
    th = P.sb([128, 8], F32, "th")
    P.ts(lr, lam[:, :, 0], -1e-4, ALU.min)
    P.copy(li, lam[:, :, 1])
    P.act(dt, lam[:, :, 2], AF.Exp)
    P.tt(th, li, dt, ALU.mult)
    P.tt(mag, lr, dt, ALU.mult)
    P.act(mag, mag, AF.Exp)
    ang = P.sb([128, W], F32, "ang")
    kq = P.sb([128, W], F32, "kq")
    ki = P.sb([128, W], I32, "ki")
    m1 = P.sb([128, W], F32, "m1")
    for j in range(8):
        P.ts(ang, tau, th[:, j:j + 1], ALU.mult)
        sin_reduced(P, sinT[:, j, :], ang, kq, ki, m1)
        P.ts(ang, tau, th[:, j:j + 1], ALU.mult, math.pi / 2, ALU.add)
        sin_reduced(P, cosT[:, j, :], ang, kq, ki, m1)
    abr = P.sb([128, 8], F32, "abr")
    abi = P.sb([128, 8], F32, "abi")
    den = P.sb([128, 8], F32, "den")
    t8 = P.sb([128, 8], F32, "t8")
    fre = P.sb([128, 8], F32, "fre")
    fim = P.sb([128, 8], F32, "fim")
    P.tt(abr, mag, cosT[:, :, 0], ALU.mult)
    P.tt(abi, mag, sinT[:, :, 0], ALU.mult)
    P.ts(abr, abr, -1.0, ALU.add)
    P.tt(den, lr, lr, ALU.mult)
    P.tt(t8, li, li, ALU.mult)
    P.tt(den, den, t8, ALU.add)
    P.recip(den, den)
    P.tt(fre, abr, lr, ALU.mult)
    P.tt(t8, abi, li, ALU.mult)
    P.tt(fre, fre, t8, ALU.add)
    P.tt(fre, fre, den, ALU.mult)
    P.tt(fim, abi, lr, ALU.mult)
    P.tt(t8, abr, li, ALU.mult)
    P.tt(fim, fim, t8, ALU.subtract)
    P.tt(fim, fim, den, ALU.mult)
    bb = P.sb([128, 8, 2, 16], F32, "bb")
    tb = P.sb([128, 8, 16], F32, "tb")
    bc16 = lambda v: v.m(lambda x: x.unsqueeze(2).to_broadcast([128, 8, 16]))
    P.tt(bb[:, :, 0, :], bsb[:, :, 0, :], bc16(fre), ALU.mult)
    P.tt(tb, bsb[:, :, 1, :], bc16(fim), ALU.mult)
    P.tt(bb[:, :, 0, :], bb[:, :, 0, :], tb, ALU.subtract)
    P.tt(bb[:, :, 1, :], bsb[:, :, 1, :], bc16(fre), ALU.mult)
    P.tt(tb, bsb[:, :, 0, :], bc16(fim), ALU.mult)
    P.tt(bb[:, :, 1, :], bb[:, :, 1, :], tb, ALU.add)
    P.ts(csb[:, :, 1, :], csb[:, :, 1, :], -1.0, ALU.mult)
    bx = P.sb([128, 8, 16], F32, "bx")
    for j in range(8):
        mj = msk[:, j, :].m(lambda x: x.unsqueeze(2).to_broadcast([128, 8, 16]))
        for ri in range(2):
            P.tt(bx, bb[:, j, ri, :].m(lambda x: x.unsqueeze(1).to_broadcast([128, 8, 16])), mj, ALU.mult)
            ps = P.ps()
            P.transpose(ps[:, 0:128], bx.r("p a b -> p (a b)"), identf)
            P.copy(BT[:, j, ri, :], ps[:, 0:128])
            P.tt(CX[:, j, ri, :].r("p (a b) -> p a b", a=8),
                 csb[:, j, ri, :].m(lambda x: x.unsqueeze(1).to_broadcast([128, 8, 16])), mj, ALU.mult)
    P.barrier()
    P.sb_off = m0
    A = P.sb([128, 8, W], F32, "A")
    B = P.sb([128, 8, W], F32, "B")
    t1 = P.sb([128, 8, W], F32, "t1")
    t2 = P.sb([128, 8, W], F32, "t2")
    wre = P.sb([128, 8, W], F32, "wre")
    wim = P.sb([128, 8, W], F32, "wim")
    xre = P.sb([128, 8, W], BF16, "xre")
    xim = P.sb([128, 8, W], BF16, "xim")
    cre = P.sb([128, 8], F32, "cre")
    cim = P.sb([128, 8], F32, "cim")
    P.memset(cre, 0.0)
    P.memset(cim, 0.0)
    uf = P.sb([128, 2, W], F32, "uf")
    ub = P.sb([128, 2, W], BF16, "ub")
    gf = P.sb([128, 2, W], F32, "gf")
    y = P.sb([128, 2, W], F32, "y")
    y2 = P.sb([128, 2, W], F32, "y2")
    glb = P.sb([128, 2, W], BF16, "glb")
    yo = P.sb([128, 2, W], BF16, "yo")
    csrc = Dm['colsT'].r("(c p) t -> p c t", p=128)
    ydst = Dm['yT_s5'].r("(c p) t -> p c t", p=128)
    for tt in range(NTT):
        sl = slice(tt * W, (tt + 1) * W)
        P.dma(uf, csrc[:, C_SU:C_SU + 2, sl])
        P.dma(gf, csrc[:, C_SG:C_SG + 2, sl])
        P.copy(ub, uf, eng='pool')
        for j in range(8):
            for ri, dst in ((0, A), (1, B)):
                ps = P.ps()
                P.mm(ps, BT[:, j, ri, :], ub[:, j // 4, :])
                P.act(dst[:, j, :], ps, AF.Copy)
        P.tt(t1, A, cosT, ALU.mult)
        P.tt(t2, B, sinT, ALU.mult, eng='pool')
        P.tt(t1, t1, t2, ALU.add)
        P.tt(t2, A, sinT, ALU.mult, eng='pool')
        P.tt(B, B, cosT, ALU.mult)
        P.tt(t2, B, t2, ALU.subtract, eng='pool')
        for j in range(8):
            mb = mag[:, j:j + 1].bc([128, W])
            P.scan(wre[:, j, :], mb, t1[:, j, :], cre[:, j:j + 1], ALU.mult, ALU.add)
            P.scan(wim[:, j, :], mb, t2[:, j, :], cim[:, j:j + 1], ALU.mult, ALU.add)
        P.tt(t1, wre, cosT, ALU.mult)
        P.tt(A, wim, sinT, ALU.mult, eng='pool')
        P.tt(xre, t1, A, ALU.subtract)
        P.tt(cre, t1[:, :, W - 1], A[:, :, W - 1], ALU.subtract)
        P.tt(t2, wre, sinT, ALU.mult, eng='pool')
        P.tt(B, wim, cosT, ALU.mult)
        P.tt(xim, t2, B, ALU.add, eng='pool')
        P.tt(cim, t2[:, :, W - 1], B[:, :, W - 1], ALU.add)
        for jc in range(2):
            ps = P.ps()
            n = 0
            for j in range(4 * jc, 4 * jc + 4):
                for ri, xx in ((0, xre), (1, xim)):
                    P.mm(ps, CX[:, j, ri, :], xx[:, j, :], start=(n == 0), stop=(n == 7))
                    n += 1
            P.stt(y[:, jc, :], uf[:, jc, :], dsk[:, jc:jc + 1], ps, ALU.mult, ALU.add)
        P.tt(y2, y, y, ALU.mult)
        P.ts(y2, y2, 0.044715, ALU.mult, 1.0, ALU.add)
        P.tt(y2, y2, y, ALU.mult)
        P.act(y2, y2, AF.Sigmoid, scale=1.5957691216057308)
        P.tt(y, y, y2, ALU.mult)
        P.copy(glb, y, eng='pool')
        for oc in range(2):
            ps = P.ps()
            for kc in range(2):
                P.mm(ps, wglu[:, kc, oc * 128:(oc + 1) * 128], glb[:, kc, :], start=(kc == 0), stop=(kc == 1))
            P.act(y2[:, oc, :], ps, AF.Sigmoid)
        P.tt(y, y, y2, ALU.mult)
        P.act(y2, gf, AF.Silu)
        P.tt(yo, y, y2, ALU.mult)
        P.dma(ydst[:, :, sl], yo)
    P.barrier()


STAGE_FNS['S5'] = stage_S5


import os
RW_DEBUG = int(os.environ.get('RW_DEBUG', '3'))


def stage_RWKV(P, l, Dm):
    P.sb_off = SB_BASE
    W = 256
    H4 = 4
    NCH = W // 64
    HC = H4 * NCH
    prm = P.sb([64, 8, 4], F32, "prm")
    mu = P.sb([64, 18], F32, "mu")
    w2 = P.sb([64, 256], F32, "w2")
    a2 = P.sb([64, 256], F32, "a2")
    msks = P.sb([64, 3, 64], F32, "msks")
    identf = P.sb([128, 128], F32, "identf")
    ones64 = P.sb([64, 64], F32, "ones64")
    onesw = P.sb([64, 1], F32, "onesw")
    P.dma(prm, Dm[f'rwprm{l}'])
    P.dma(mu, Dm[f'rwmu{l}'])
    P.dma(w2, Dm[f'rww2{l}'])
    P.dma(a2, Dm[f'rwa2{l}'])
    P.dma(msks, Dm['rw_masks'])
    P.dma(identf, Dm['ident'])
    P.memset(ones64, 1.0)
    P.memset(onesw, 1.0)
    id64 = identf[0:64, 0:64]
    hb = lambda v, n=W: v.m(lambda x: x.unsqueeze(2).to_broadcast([64, H4, n]))
    mb = lambda k: msks[:, k, :].m(lambda x: x.unsqueeze(1).to_broadcast([64, H4, 64]))
    idb = id64.m(lambda x: x.unsqueeze(1).to_broadcast([64, H4, 64]))
    cin = P.sb([64, 18, W + 1], F32, "cin")
    cs = P.sb([64, 18, W], F32, "cs")
    f = lambda nm: P.sb([64, H4, W], F32, nm)
    twl = P.sb([64, W], F32, "twl")
    sgz, av, kx, t0, kp, beta = f("sgz"), f("av"), f("kx"), f("t0"), f("kp"), f("beta")
    kkn, lw, cw, e1, e2 = f("kkn"), f("lw"), f("cw"), f("e1"), f("e2")
    rt, at, bt, kt, Bh, Kh = f("rt"), f("at"), f("bt"), f("kt"), f("Bh"), f("Kh")
    bonus, Yt = f("bonus"), f("Yt")
    base = P.sb([64, HC], F32, "base")
    cwC = P.sb([64, HC], F32, "cwC")
    gC = P.sb([64, HC], F32, "gC")
    S0 = P.sb([64, H4, 64], F32, "S0")
    P.memset(S0, 0.0)
    NP2 = NCH // 2
    g8 = lambda nm, dt=F32: [P.sb([64, 2, H4, 64], dt, nm) for _ in range(NP2)]
    X, XT, PaT, AakT, ArbT, ArkT = g8("X", BF16), g8("XT", BF16), g8("PaT", BF16), g8("AakT"), g8("ArbT"), g8("ArkT")
    Vt, BhT, KhT, atT, W2, M2, M1T, KV, Gd = (g8("Vt"), g8("BhT"), g8("KhT"), g8("atT", BF16), g8("W2", BF16), g8("M2"),
                                              g8("M1T"), g8("KV"), g8("Gd"))
    Usb = [P.sb([64, H4, 64], F32, "Usb") for _ in range(2)]
    mb8 = lambda k: msks[:, k, :].m(lambda x: x.unsqueeze(1).unsqueeze(1).to_broadcast([64, 2, H4, 64]))
    id8 = id64.m(lambda x: x.unsqueeze(1).unsqueeze(1).to_broadcast([64, 2, H4, 64]))
    ps8 = lambda ps: ps[0:64, 0:512].r("p (c h x) -> p c h x", c=2, h=H4)
    yo = P.sb([64, H4, W], BF16, "yo")
    src = Dm['colsT'][0:1152, :].r("(g d) t -> d g t", d=64)
    ydst = Dm['yT_rwkv'].r("(h d) t -> d h t", d=64)
    ps4 = lambda ps: ps[0:64, 0:256].r("p (h x) -> p h x", h=H4)
    for tt in range(S // W):
        t_0 = tt * W
        if tt == 0:
            P.dma(cin[:, :, 1:W + 1], src[:, :, t_0:t_0 + W])
            P.memset(cin[:, :, 0:1], 0.0)
        else:
            P.dma(cin, src[:, :, t_0 - 1:t_0 + W])
        P.tt(cs, cin[:, :, 0:W], cin[:, :, 1:W + 1], ALU.subtract)
        P.tt(cs, cs, mu.m(lambda x: x.unsqueeze(2).to_broadcast([64, 18, W])), ALU.mult)
        P.tt(cs, cs, cin[:, :, 1:W + 1], ALU.add)
        Rr, Kk, Vv, G = cs[:, 0:4, :], cs[:, 4:8, :], cs[:, 8:12, :], cs[:, 14:18, :]
        P.act(twl, cs[:, 12, :], AF.Tanh)
        for h in range(H4):
            ps = P.ps()
            P.mm(ps[0:64, 0:W], w2[:, h * 64:(h + 1) * 64], twl)
            P.act(sgz[:, h, :], ps[0:64, 0:W], AF.Sigmoid, bias=prm[:, 0, h:h + 1])
            ps = P.ps()
            P.mm(ps[0:64, 0:W], a2[:, h * 64:(h + 1) * 64], cs[:, 13, :])
            P.act(av[:, h, :], ps[0:64, 0:W], AF.Sigmoid, bias=prm[:, 1, h:h + 1])
        P.tt(kx, Kk, hb(prm[:, 2, :]), ALU.mult)
        P.tt(t0, kx, kx, ALU.mult, eng='pool')
        for h in range(H4):
            ps = P.ps()
            P.mm(ps[0:64, 0:W], ones64, t0[:, h, :])
            P.ts(kkn[:, h, :], ps[0:64, 0:W], 1e-24, ALU.add)
        P.act(kkn, kkn, AF.Sqrt)
        P.recip(kkn, kkn)
        P.tt(kkn, kkn, kx, ALU.mult)
        P.ts(t0, av, -1.0, ALU.add)
        P.tt(t0, t0, hb(prm[:, 3, :]), ALU.mult)
        P.stt(kp, t0, 1.0, Kk, ALU.add, ALU.mult)
        P.tt(beta, kkn, av, ALU.mult, eng='pool')
        P.tt(t0, Rr, kp, ALU.mult)
        P.tt(t0, t0, hb(prm[:, 4, :]), ALU.mult)
        for h in range(H4):
            ps = P.ps()
            P.mm(ps[0:64, 0:W], ones64, t0[:, h, :])
            P.tt(bonus[:, h, :], ps[0:64, 0:W], Vv[:, h, :], ALU.mult)
        P.ts(lw, sgz, -math.exp(-0.5), ALU.mult)
        for h in range(H4):
            P.scan(cw[:, h, :], onesw[:, 0:1].bc([64, W]), lw[:, h, :], 0.0, ALU.mult, ALU.add)
        cw3 = cw.r("p h (c i) -> p (h c) i", i=64)
        P.memset(base, 0.0)
        P.copy(base.r("p (h c) -> p h c", h=H4)[:, :, 1:NCH], cw.r("p h (c i) -> p h c i", i=64)[:, :, 0:NCH - 1, 63])
        P.tt(cw3, cw3, base.m(lambda x: x.unsqueeze(2).to_broadcast([64, HC, 64])), ALU.subtract)
        P.copy(cwC, cw3[:, :, 63])
        P.act(gC, cwC, AF.Exp)
        P.act(e1, cw, AF.Exp)
        P.tt(rt, Rr, e1, ALU.mult)
        P.act(e1, cw, AF.Exp, scale=-1.0)
        P.tt(bt, beta, e1, ALU.mult)
        P.tt(kt, kp, e1, ALU.mult, eng='pool')
        P.tt(e2, cw, lw, ALU.subtract)
        P.act(e2, e2, AF.Exp)
        P.stt(at, kkn, -1.0, e2, ALU.mult, ALU.mult)
        e13 = e1.r("p h (c i) -> p (h c) i", i=64)
        P.tt(e13, cw3, cwC.m(lambda x: x.unsqueeze(2).to_broadcast([64, HC, 64])), ALU.subtract)
        P.act(e1, e1, AF.Exp, scale=-1.0)
        P.tt(Bh, beta, e1, ALU.mult)
        P.tt(Kh, kp, e1, ALU.mult, eng='pool')
        def mm8(p, lhf, rhf):
            ps = P.ps()
            for cl in range(2):
                for h in range(H4):
                    o_ = ps[0:64, (cl * H4 + h) * 64:(cl * H4 + h + 1) * 64]
                    P.mm(o_, lhf(p, cl, h), rhf(p, cl, h))
            return ps8(ps)
        csl = lambda p, cl: slice((2 * p + cl) * 64, (2 * p + cl + 1) * 64)
        tok = lambda t_: (lambda p, cl, h: t_[:, h, csl(p, cl)])
        blk = lambda t_: (lambda p, cl, h: t_[p][:, cl, h, :])
        for p in range(NP2):
            P.tt(X[p], mm8(p, tok(at), tok(bt)), mb8(2), ALU.mult)
            P.tt(XT[p], mm8(p, tok(bt), tok(at)), mb8(0), ALU.mult)
            P.tt(AakT[p], mm8(p, tok(kt), tok(at)), mb8(0), ALU.mult)
            P.tt(ArbT[p], mm8(p, tok(bt), tok(rt)), mb8(1), ALU.mult)
            P.tt(ArkT[p], mm8(p, tok(kt), tok(rt)), mb8(1), ALU.mult)
            P.tt(PaT[p], XT[p], id8, ALU.add)
        for srcT, dstT, eng in ((Vv, Vt, 'act'), (Bh, BhT, 'dve'), (Kh, KhT, 'act'), (at, atT, 'dve')):
            for p in range(NP2):
                ps = P.ps()
                for cl in range(2):
                    for h in range(H4):
                        P.transpose(ps[0:64, (cl * H4 + h) * 64:(cl * H4 + h + 1) * 64], srcT[:, h, csl(p, cl)], id64)
                if eng == 'act':
                    P.act(dstT[p], ps8(ps), AF.Copy)
                else:
                    P.copy(dstT[p], ps8(ps))
        for it in range(5):
            pxs = [(mm8(p, blk(XT), blk(X)), mm8(p, blk(X), blk(XT))) for p in range(NP2)]
            for p in range(NP2):
                P.act(X[p], pxs[p][0], AF.Copy)
                P.copy(XT[p], pxs[p][1])
            pps = [mm8(p, blk(X), blk(PaT)) for p in range(NP2)]
            for p in range(NP2):
                P.tt(PaT[p], PaT[p], pps[p], ALU.add)
        for p in range(NP2):
            P.act(KV[p], mm8(p, blk(KhT), blk(Vt)), AF.Copy)
            P.act(W2[p], mm8(p, blk(AakT), blk(Vt)), AF.Copy)
            P.copy(M1T[p], mm8(p, blk(atT), blk(PaT)))
            for cl in range(2):
                c = 2 * p + cl
                gcb = gC.r("p (h c) -> p h c", h=H4)[:, :, c].m(lambda x: x.unsqueeze(2).to_broadcast([64, H4, 64]))
                P.tt(Gd[p][:, cl], idb, gcb, ALU.mult)
        for p in range(NP2):
            P.act(M2[p], mm8(p, blk(PaT), blk(W2)), AF.Copy)
        for c in range(NCH):
            p, cl = c // 2, c % 2
            sl = slice(c * 64, (c + 1) * 64)
            us = Usb[c % 2]
            psu = P.ps()
            for h in range(H4):
                P.mm(psu[0:64, h * 64:(h + 1) * 64], M1T[p][:, cl, h, :], S0[:, h, :])
            psy = P.ps()
            for h in range(H4):
                o_ = psy[0:64, h * 64:(h + 1) * 64]
                P.mm(o_, S0[:, h, :], rt[:, h, sl], start=True, stop=False)
                P.mm(o_, Vt[p][:, cl, h, :], ArkT[p][:, cl, h, :], start=False, stop=True)
            P.tt(us, ps4(psu), M2[p][:, cl], ALU.add)
            pss = P.ps()
            for h in range(H4):
                o_ = pss[0:64, h * 64:(h + 1) * 64]
                P.mm(o_, Gd[p][:, cl, h, :], S0[:, h, :], start=True, stop=False)
                P.mm(o_, BhT[p][:, cl, h, :], us[:, h, :], start=False, stop=True)
            psy2 = P.ps()
            for h in range(H4):
                P.mm(psy2[0:64, h * 64:(h + 1) * 64], us[:, h, :], ArbT[p][:, cl, h, :])
            P.tt(S0, ps4(pss), KV[p][:, cl], ALU.add)
            P.act(Yt[:, :, sl], ps4(psy), AF.Copy)
            P.tt(Yt[:, :, sl], Yt[:, :, sl], ps4(psy2), ALU.add)
        for h in range(H4):
            ps = P.ps()
            P.mm(ps[0:64, 0:W], ones64, Yt[:, h, :])
            P.stt(e1[:, h, :], ps[0:64, 0:W], -1.0 / 64, Yt[:, h, :], ALU.mult, ALU.add)
        P.tt(e2, e1, e1, ALU.mult, eng='pool')
        for h in range(H4):
            ps = P.ps()
            P.mm(ps[0:64, 0:W], ones64, e2[:, h, :])
            P.ts(t0[:, h, :], ps[0:64, 0:W], 1.0 / 64, ALU.mult, 64e-5, ALU.add)
        P.act(t0, t0, AF.Sqrt)
        P.recip(t0, t0)
        P.tt(e1, e1, t0, ALU.mult)
        P.tt(e1, e1, hb(prm[:, 5, :]), ALU.mult)
        P.tt(e1, e1, hb(prm[:, 6, :]), ALU.add)
        P.tt(e1, e1, bonus, ALU.add)
        P.act(e2, G, AF.Silu)
        P.tt(yo, e1, e2, ALU.mult)
        P.dma(ydst[:, :, t_0:t_0 + W], yo)
    P.barrier()


STAGE_FNS['RWKV'] = stage_RWKV


N_BISECT = 20


def stage_DSA(P, l, Dm):
    P.sb_off = SB_BASE
    kT = P.sb([64, 4, S], BF16, "kT")
    ikT = P.sb([64, 1, S], F32, "ikT")
    m0 = P.sb_off
    ropeC = P.sb([64, S], F32, "ropeC")
    ropeS = P.sb([64, S], F32, "ropeS")
    P.dma(ropeC, Dm['ropeC'])
    P.dma(ropeS, Dm['ropeS'])
    rope_heads(P, None, Dm, C_DQ, C_DQS, ropeC, ropeS, dram_dst=Dm['qR'])
    rope_heads(P, kT, Dm, C_DK, C_DKS, ropeC, ropeS)
    rope_heads(P, None, Dm, C_IQ, C_IQS, ropeC, ropeS, dram_dst=Dm['iqR'])
    rope_heads(P, ikT, Dm, (C_IK * 128,), (C_IK * 128 + 64,), ropeC, ropeS, nheads=1)
    P.barrier()
    P.sb_off = m0
    identf = P.sb([128, 128], F32, "identf")
    identb = P.sb([128, 128], BF16, "identb")
    cb = P.sb([128, 128], F32, "cb")
    P.dma(identf, Dm['ident'])
    P.copy(identb, identf)
    P.dma(cb, Dm['dsa_cb'])
    Vaug = P.sb([128, 32, 4, 65], BF16, "Vaug")
    iwt = P.sb([128, 32, 4], F32, "iwt")
    vst = [P.sb([128, 4, 256], F32, "vst") for _ in range(2)]
    tsrc = Dm['colsTok'].r("(c p) n -> p c n", p=128)
    P.memset(Vaug[:, :, :, 64:65], 1.0)
    for i in range(8):
        v_ = vst[i % 2]
        P.dma(v_, tsrc[:, i * 4:(i + 1) * 4, 0:256])
        P.copy(Vaug[:, i * 4:(i + 1) * 4, :, 0:64], v_.r("p c (h d) -> p c h d", h=4), eng=('dve' if i % 2 else 'pool'))
    P.dma(iwt, tsrc[:, :, 512:516])
    P.ts(iwt, iwt, 1.0 / 16, ALU.mult)
    scores = [P.sb([128, S], F32, "score") for _ in range(2)]
    mask01s = [P.sb([128, S], BF16, "mask01") for _ in range(2)]
    maskTs = [P.sb([128, 32, 128], BF16, "maskT") for _ in range(2)]
    rl = [P.sb([128, 512], F32, "rl") for _ in range(4)]
    E = [P.sb([128, 512], BF16, "E") for _ in range(2)]
    lo = P.sb([128, 1], F32, "lo")
    hi = P.sb([128, 1], F32, "hi")
    mid = P.sb([128, 1], F32, "mid")
    cnt = P.sb([128, 1], F32, "cnt")
    sel = P.sb([128, 1], F32, "sel")
    dlt = P.sb([128, 1], F32, "dlt")
    stp = P.sb([128, 32], F32, "stp")
    pw = P.sb([128, 32], F32, "pw")
    P.dma(pw, Dm['dsa_pw'])
    zt = P.sb([128, S], BF16, "zt")
    cz = P.sb([128, S], F32, "cz")
    junk = cz
    nz = P.sb([128, 1], F32, "nz")
    npos = P.sb([128, 1], F32, "npos")
    flag = P.sb([128, 1], F32, "flag")
    f2 = P.sb([128, 1], F32, "f2")
    rr = P.sb([128, 1], F32, "rr")
    onesw = P.sb([128, 1], F32, "onesw")
    P.memset(onesw, 1.0)
    negbig = P.sb([128, 1], F32, "negbig")
    P.memset(negbig, -1e5)
    osb = P.sb([128, 4, 64], F32, "osb")
    rs = P.sb([128, 4, 1], F32, "rs")
    gt = [P.sb([128, 2, 128], F32, "gt") for _ in range(2)]
    sgs = [P.sb([128, 2, 128], F32, "sg") for _ in range(2)]
    yo = [P.sb([128, 2, 128], BF16, "yo") for _ in range(2)]
    iqt = [P.sb([64, 4, 128], F32, "iqt") for _ in range(2)]
    iqsrc = Dm['iqR'].r("(h d) t -> d h t", d=64)
    qft = [P.sb([64, 4, 128], F32, "qft") for _ in range(2)]
    qbt = [P.sb([64, 4, 128], BF16, "qbt") for _ in range(2)]
    qsrc = Dm['qR'].r("(h d) t -> d h t", d=64)
    gsrc = Dm['colsT'].r("(c p) t -> p c t", p=128)
    ydst = Dm['yT_dsa'].r("(c p) t -> p c t", p=128)
    ne = 0
    pending = None
    for i in range(32):
        qs = slice(i * 128, (i + 1) * 128)
        Nk = 128 * (i + 1)
        g_ = gt[i % 2]
        P.dma(g_, gsrc[:, C_DG:C_DG + 2, qs])
        iq_ = iqt[i % 2]
        P.dma(iq_, iqsrc[:, :, qs])
        P.dma(qft[i % 2], qsrc[:, :, qs])
        qb_ = qbt[i % 2]
        P.copy(qb_, qft[i % 2], eng='pool')
        score = scores[i % 2]
        mask01 = mask01s[i % 2]
        maskT = maskTs[i % 2]
        for k0 in range(0, Nk, 512):
            kw = min(512, Nk - k0)
            pss = []
            for h in range(4):
                ps = P.ps()
                P.mm(ps[:, 0:kw], iq_[:, h, :], ikT[:, 0, k0:k0 + kw], inc=(h == 3))
                pss.append(ps)
            for h in range(4):
                P.act(rl[h][:, 0:kw], pss[h][:, 0:kw], AF.Relu)
            P.ts(score[:, k0:k0 + kw], rl[0][:, 0:kw], iwt[:, i, 0:1], ALU.mult)
            for h in range(1, 4):
                P.stt(score[:, k0:k0 + kw], rl[h][:, 0:kw], iwt[:, i, h:h + 1], score[:, k0:k0 + kw], ALU.mult, ALU.add)
        P.tt(score[:, i * 128:Nk], score[:, i * 128:Nk], cb, ALU.add)
        if Nk > 256:
            P.reduce(hi, score[:, 0:Nk], ALU.max)
            P.reduce(lo, score[:, 0:i * 128], ALU.min)
            P.tt(dlt, hi, lo, ALU.subtract)
            P.ts(dlt, dlt, 2.0, ALU.add)
            P.ts(stp, pw, dlt[:, 0:1], ALU.mult)
            P.stt(mid, dlt, 0.5, lo, ALU.mult, ALU.add)
            P.ts(mid, mid, -1.0, ALU.add)
            for it in range(N_BISECT):
                P.ts(junk[:, 0:Nk], score[:, 0:Nk], mid[:, 0:1], ALU.is_ge, 0.0, ALU.add, accum=cnt)
                if it < N_BISECT - 1:
                    P.ts(sel, cnt, 255.5, ALU.is_ge, 0.5, ALU.subtract)
                    P.stt(mid, sel, stp[:, it:it + 1], mid, ALU.mult, ALU.add)
                else:
                    P.ts(sel, cnt, 255.5, ALU.is_ge, 1.0, ALU.subtract)
                    P.stt(lo, sel, stp[:, it:it + 1], mid, ALU.mult, ALU.add)
        else:
            P.memset(lo, -1e29)
        P.ts(zt[:, 0:Nk], score[:, 0:Nk], 0.0, ALU.is_equal, 0.0, ALU.add, accum=nz)
        P.ts(junk[:, 0:Nk], score[:, 0:Nk], 0.0, ALU.is_gt, 0.0, ALU.add, accum=npos)
        P.ts(flag, npos, 255.5, ALU.is_lt)
        P.tt(f2, npos, nz, ALU.add)
        P.ts(f2, f2, 255.5, ALU.is_ge)
        P.tt(flag, flag, f2, ALU.mult)
        P.ts(rr, npos, -1.0, ALU.mult, 256.0, ALU.add)
        P.scan(cz[:, 0:Nk], onesw[:, 0:1].bc([128, Nk]), zt[:, 0:Nk], 0.0, ALU.mult, ALU.add)
        P.ts(cz[:, 0:Nk], cz[:, 0:Nk], rr[:, 0:1], ALU.is_le, flag[:, 0:1], ALU.mult)
        P.tt(zt[:, 0:Nk], zt[:, 0:Nk], cz[:, 0:Nk], ALU.mult)
        P.ts(f2, flag, -1.0, ALU.mult, 1.0, ALU.add)
        P.tt(lo, lo, f2, ALU.mult)
        P.stt(lo, flag, 1e-30, lo, ALU.mult, ALU.add)
        P.ts(mask01[:, 0:Nk], score[:, 0:Nk], lo[:, 0:1], ALU.is_ge)
        P.tt(mask01[:, 0:Nk], mask01[:, 0:Nk], zt[:, 0:Nk], ALU.add)
        for c0 in range(0, i + 1, 4):
            nc_ = min(4, i + 1 - c0)
            ps = P.ps()
            psb = ps.bitcast(BF16)
            for cl in range(nc_):
                c = c0 + cl
                P.transpose(psb[:, cl * 128:(cl + 1) * 128], mask01[:, c * 128:(c + 1) * 128], identb, inc=(cl == nc_ - 1))
            P.act(maskT[:, c0:c0 + nc_, :], psb[:, 0:nc_ * 128].r("p (c q) -> p c q", q=128), AF.Identity,
                  scale=1e5, bias=negbig[:, 0:1])
        if pending is not None:
            pending()
        psO = P.ps_acc(i)
        for h in range(4):
            for c0 in range(0, i + 1, 4):
                nc_ = min(4, i + 1 - c0)
                ps = P.ps()
                for cl in range(nc_):
                    c = c0 + cl
                    o_ = ps[:, cl * 128:(cl + 1) * 128]
                    P.mm(o_, kT[:, h, c * 128:(c + 1) * 128], qb_[:, h, :], start=True, stop=False)
                    P.mm(o_, identb, maskT[:, c, :], start=False, stop=True)
                e_ = E[ne % 2]
                ne += 1
                P.act(e_[:, 0:nc_ * 128], ps[:, 0:nc_ * 128], AF.Exp, scale=0.125)
                for cl in range(nc_):
                    c = c0 + cl
                    P.mm(psO[:, h * 65:(h + 1) * 65], e_[:, cl * 128:(cl + 1) * 128], Vaug[:, c, h, :],
                         start=(c == 0), stop=(c == i))

        def make_fin(i=i, psO=psO, g_=g_, qs=qs):
            def fin():
                pv = psO[:, 0:260].r("p (h x) -> p h x", h=4)
                P.recip(rs, pv[:, :, 64:65])
                P.tt(osb, pv[:, :, 0:64], rs.m(lambda x: x.to_broadcast([128, 4, 64])), ALU.mult)
                sg = sgs[i % 2]
                P.act(sg, g_, AF.Silu)
                y_ = yo[i % 2]
                for p in range(2):
                    ps = P.ps()
                    P.transpose(ps[:, 0:128], osb[:, 2 * p:2 * p + 2, :].r("p a b -> p (a b)"), identf)
                    P.tt(y_[:, p, :], ps[:, 0:128], sg[:, p, :], ALU.mult)
                P.dma(ydst[:, :, qs], y_)
            return fin
        pending = make_fin()
    pending()
    P.barrier()


STAGE_FNS['DSA'] = stage_DSA


FULL_PLAN = [('R', 0)] + [(st, l) for l in range(2) for st in ('P', 'X', 'RET', 'S5', 'RWKV', 'DSA', 'M')]


def kernel(**inputs):
    inputs = {k: np.asarray(v) for k, v in inputs.items()}
    nb = inputs['x'].shape[0]
    nc, P, used_in = build(FULL_PLAN)
    in_maps = []
    for b in range(nb):
        d = host_inputs(inputs, b)
        in_maps.append({k: v for k, v in d.items() if k in used_in})
    res = run_bass_kernel_spmd(nc, in_maps, core_ids=list(range(nb)))
    out = np.stack([np.ascontiguousarray(np.asarray(r['outT']).T) for r in res.results], 0)
    return out.astype(np.float32)
```

```python
from contextlib import ExitStack
import math
import numpy as np
import ml_dtypes
import concourse.bass as bass
import concourse.mybir as mybir
from concourse.bass_utils import run_bass_kernel_spmd

F32 = mybir.dt.float32
BF16 = mybir.dt.bfloat16
I32 = mybir.dt.int32
AF = mybir.ActivationFunctionType
ALU = mybir.AluOpType
AX = mybir.AxisListType

ENGS = ['sp', 'act', 'dve', 'pool', 'pe']
EPOCH = 16000
NDMASEM = 24
RELAX_SAME_ENGINE = True
SB_BASE = 16640
SBUF_BYTES = 229000


class Buf:
    __slots__ = ('name', 'wev', 'rev', 'tracked')

    def __init__(self, name, tracked=True):
        self.name = name
        self.wev = {}
        self.rev = {}
        self.tracked = tracked


class V:
    __slots__ = ('buf', 'ap')

    def __init__(self, buf, ap):
        self.buf = buf
        self.ap = ap

    def __getitem__(self, k):
        return V(self.buf, self.ap[k])

    def m(self, fn):
        return V(self.buf, fn(self.ap))

    def r(self, s, **kw):
        return V(self.buf, self.ap.rearrange(s, **kw))

    def bc(self, shape):
        return V(self.buf, self.ap.to_broadcast(list(shape)))

    def bitcast(self, dt):
        return V(self.buf, self.ap.bitcast(dt))

    @property
    def shape(self):
        return tuple(self.ap.shape)


def _ap(x):
    return x.ap if isinstance(x, V) else x


class Prog:
    def __init__(self, nc):
        self.nc = nc
        self.q = {e: [] for e in ENGS}
        self.cnt = {e: 0 for e in ENGS}
        self.noinc = {e: False for e in ENGS}
        self.known = {e: {} for e in ENGS}
        self.dma_n = {e: 0 for e in ENGS}
        self.nbar = 0
        self.bufs = []
        self.sb_off = SB_BASE
        self.sb_id = 0
        self.sb_mark = 0
        self.psum = []
        for i in range(8):
            h = nc.alloc_psum_tensor(f"ps{i}", [128, 512], F32)
            self.psum.append(V(self._newbuf(f"ps{i}"), h[:]))
        self.ps_rr = 0

    def _newbuf(self, name, tracked=True):
        b = Buf(name, tracked)
        if tracked:
            self.bufs.append(b)
        return b

    def sb(self, shape, dtype=F32, name="t"):
        esz = {F32: 4, BF16: 2, I32: 4}[dtype]
        per = esz * int(np.prod(shape[1:]))
        per = (per + 63) // 64 * 64
        off = self.sb_off
        assert off + per <= SBUF_BYTES, f"SBUF overflow {name} {off}+{per}"
        self.sb_off += per
        self.sb_id += 1
        nm = f"{name}_{self.sb_id}"
        h = self.nc.alloc_sbuf_tensor_at(nm, list(shape), dtype, offset=off)
        return V(self._newbuf(nm), h[:])

    def mark(self):
        self.sb_mark = self.sb_off

    def release(self):
        self.sb_off = self.sb_mark

    def ps(self):
        v = self.psum[self.ps_rr % 6]
        self.ps_rr += 1
        return v

    def ps_acc(self, i=1):
        return self.psum[6 + (i % 2)]

    def dram(self, name, shape, dtype=F32, kind="Internal"):
        h = self.nc.dram_tensor(name, list(shape), dtype, kind=kind)
        return V(self._newbuf(name, tracked=False), h.ap())

    def _collect(self, eng, reads, writes, extra=None):
        waits = {}

        last = self.cnt.get(eng, 0) if isinstance(eng, str) else 0

        def need(evs, skip_own):
            for sk, v in evs.items():
                if sk == eng:
                    if skip_own or (RELAX_SAME_ENGINE and v < last):
                        continue
                if waits.get(sk, 0) < v:
                    waits[sk] = v
        for x in reads:
            if x.buf.tracked:
                need(x.buf.wev, False)
        for x in writes:
            if x.buf.tracked:
                need(x.buf.wev, True)
                need(x.buf.rev, True)
        if extra:
            need(extra, False)
        kn = self.known[eng]
        wl = []
        for sk, v in waits.items():
            if kn.get(sk, 0) < v:
                kn[sk] = v
                wl.append((sk, v))
        return wl

    def emit(self, eng, fn, reads=(), writes=(), inc=True):
        reads = [x for x in reads if isinstance(x, V)]
        writes = [x for x in writes if isinstance(x, V)]
        wl = self._collect(eng, reads, writes)
        idx = self.cnt[eng] + 1
        self.cnt[eng] = idx
        self.q[eng].append((wl, fn, ('c', idx)))
        for x in reads:
            b = x.buf
            if b.tracked and b.rev.get(eng, 0) < idx:
                b.rev[eng] = idx
        for x in writes:
            b = x.buf
            if b.tracked:
                b.wev = {eng: idx}
                b.rev = {}

    def dma(self, out, in_, eng='sp'):
        n = self.dma_n[eng]
        self.dma_n[eng] = n + 1
        slot, k = n % NDMASEM, n // NDMASEM
        sk = ('dma', eng, slot)
        val = 16 * (k + 1)
        extra = {sk: 16 * k} if k > 0 else None
        wl = self._collect(eng, [in_], [out], extra)
        oa, ia = out.ap, in_.ap
        self.q[eng].append((wl, lambda e: e.dma_start(out=oa, in_=ia), ('d', sk)))
        b = in_.buf
        if b.tracked:
            b.rev[sk] = val
        b = out.buf
        if b.tracked:
            b.wev = {sk: val}
            b.rev = {}

    def barrier(self):
        waits = {}
        for e in ENGS:
            if e != 'sp' and self.cnt[e] > 0:
                waits[e] = self.cnt[e]
            n = self.dma_n[e]
            for slot in range(min(n, NDMASEM)):
                k = (n - 1 - slot) // NDMASEM
                waits[('dma', e, slot)] = 16 * (k + 1)
        kn = self.known['sp']
        wl = []
        for sk, v in waits.items():
            if kn.get(sk, 0) < v:
                kn[sk] = v
                wl.append((sk, v))
        self.nbar += 1
        nb = self.nbar
        bk = ('bar', 0)
        self.q['sp'].append((wl, None, ('b', bk)))
        for e in ENGS:
            if e != 'sp':
                self.q[e].append(([(bk, nb)], None, None))
                for sk, v in waits.items():
                    if self.known[e].get(sk, 0) < v:
                        self.known[e][sk] = v
        for b in self.bufs:
            b.wev = {}
            b.rev = {}

    def finish(self):
        self.barrier()
        nc = self.nc
        targets = {e: set() for e in ENGS}
        for e in ENGS:
            for wl, fn, tag in self.q[e]:
                for sk, v in wl:
                    if isinstance(sk, str):
                        targets[sk].add(v)
        rank = {e: {v: r + 1 for r, v in enumerate(sorted(targets[e]))} for e in ENGS}
        self.n_inc = {e: len(rank[e]) for e in ENGS}
        keys = set()

        def semkey(sk, v):
            if isinstance(sk, str):
                r = rank[sk][v]
                return ((sk, (r - 1) // EPOCH), (r - 1) % EPOCH + 1)
            return (sk, v)
        prog = {e: [] for e in ENGS}
        for e in ENGS:
            for wl, fn, tag in self.q[e]:
                w2 = [semkey(sk, v) for sk, v in wl]
                inc = None
                if tag is not None:
                    if tag[0] == 'c':
                        if tag[1] in rank[e]:
                            r = rank[e][tag[1]]
                            inc = ((e, (r - 1) // EPOCH), 1)
                    elif tag[0] == 'd':
                        inc = (tag[1], 16)
                    elif tag[0] == 'b':
                        inc = (tag[1], 1)
                for k_, _ in w2:
                    keys.add(k_)
                if inc is not None:
                    keys.add(inc[0])
                prog[e].append((w2, fn, inc, tag))
        stack = ExitStack()
        sems = {}
        for i, sk in enumerate(sorted(keys, key=str)):
            sems[sk] = stack.enter_context(nc.semaphore(f"s{i}"))
        self.nsem = len(sems)

        def mk(en):
            def body(e):
                for wl, fn, inc, tag in prog[en]:
                    for sk, v in wl:
                        e.wait_ge(sems[sk], v)
                    if fn is None:
                        if tag is not None and tag[0] == 'b':
                            e.sem_inc(sems[inc[0]], inc[1])
                        continue
                    ins = fn(e)
                    if inc is not None:
                        ins.then_inc(sems[inc[0]], inc[1])
            return body
        with stack:
            with nc.Block() as block:
                block.sync(mk('sp'))
                block.scalar(mk('act'))
                block.vector(mk('dve'))
                block.gpsimd(mk('pool'))
                block.tensor(mk('pe'))

    def act(self, out, in_, func, bias=None, scale=1.0, accum=None):
        o, i, b, s, a = _ap(out), _ap(in_), _ap(bias), _ap(scale), _ap(accum)
        kw = {}
        if b is not None:
            kw['bias'] = b
        if a is not None:
            kw['accum_out'] = a
        self.emit('act', lambda e: e.activation(out=o, in_=i, func=func, scale=s, **kw),
                  [in_, bias, scale], [out, accum])

    def ts(self, out, in0, s1, op0, s2=None, op1=None, accum=None, eng='dve'):
        o, i, a1, a2, ac = _ap(out), _ap(in0), _ap(s1), _ap(s2), _ap(accum)
        kw = {}
        if op1 is not None:
            kw['op1'] = op1
        if ac is not None:
            kw['accum_out'] = ac
        self.emit(eng, lambda e: e.tensor_scalar(out=o, in0=i, scalar1=a1, scalar2=a2, op0=op0, **kw),
                  [in0, s1, s2], [out, accum])

    def tt(self, out, in0, in1, op, eng='dve'):
        o, a, b = _ap(out), _ap(in0), _ap(in1)
        self.emit(eng, lambda e: e.tensor_tensor(out=o, in0=a, in1=b, op=op), [in0, in1], [out])

    def stt(self, out, in0, scalar, in1, op0, op1, eng='dve'):
        o, a, s, b = _ap(out), _ap(in0), _ap(scalar), _ap(in1)
        self.emit(eng, lambda e: e.scalar_tensor_tensor(out=o, in0=a, scalar=s, in1=b, op0=op0, op1=op1),
                  [in0, scalar, in1], [out])

    def copy(self, out, in_, eng='dve'):
        o, i = _ap(out), _ap(in_)
        if eng == 'act':
            self.emit('act', lambda e: e.copy(out=o, in_=i), [in_], [out])
        else:
            self.emit(eng, lambda e: e.tensor_copy(out=o, in_=i), [in_], [out])

    def memset(self, out, val, eng='dve'):
        o = _ap(out)
        self.emit(eng, lambda e: e.memset(o, val), [], [out])

    def recip(self, out, in_):
        o, i = _ap(out), _ap(in_)
        self.emit('dve', lambda e: e.reciprocal(out=o, in_=i), [in_], [out])

    def reduce(self, out, in_, op, axis=AX.X):
        o, i = _ap(out), _ap(in_)
        self.emit('dve', lambda e: e.tensor_reduce(out=o, in_=i, axis=axis, op=op), [in_], [out])

    def scan(self, out, d0, d1, initial, op0, op1):
        o, a, b, ini = _ap(out), _ap(d0), _ap(d1), _ap(initial)
        self.emit('dve', lambda e: e.tensor_tensor_scan(out=o, data0=a, data1=b, initial=ini, op0=op0, op1=op1),
                  [d0, d1, initial], [out])

    def mm(self, out, lhsT, rhs, start=True, stop=True, inc=None):
        o, l, r = _ap(out), _ap(lhsT), _ap(rhs)
        if inc is None:
            inc = stop
        self.emit('pe', lambda e: e.matmul(o, l, r, start=start, stop=stop), [lhsT, rhs], [out], inc=inc)

    def transpose(self, out, in_, ident, inc=True):
        o, i, d = _ap(out), _ap(in_), _ap(ident)
        self.emit('pe', lambda e: e.transpose(o, i, d), [in_, ident], [out], inc=inc)


S = 4096
D = 1024
TT = 512
NTT = S // TT
NP_ROWS = 5376
NT_COLS = 516
B_RWKV, B_DSA, B_RET, B_S5, B_X, B_GATE = 0, 1152, 2500, 3524, 4036, 4548
C_RWKV = 0
C_DQ, C_DQS, C_DK, C_DKS, C_IQ, C_IQS, C_IK, C_DG = 9, 11, 13, 15, 17, 19, 21, 22
C_RQ, C_RQS, C_RK, C_RKS, C_RG = 24, 26, 28, 30, 32
C_SU, C_SG = 34, 36
C_XQ, C_XG = 38, 40


def _swap_idx(base, nheads):
    idx = []
    for h in range(nheads):
        for j in range(64):
            idx.append(base + h * 64 + (j + 32) % 64)
    return idx


def proj_col_indices():
    r = lambda a, n: list(range(a, a + n))
    f = []
    f += r(B_RWKV, 1152)
    f += r(B_DSA, 256) + _swap_idx(B_DSA, 4)
    f += r(B_DSA + 256, 256) + _swap_idx(B_DSA + 256, 4)
    f += r(B_DSA + 768, 256) + _swap_idx(B_DSA + 768, 4)
    f += r(B_DSA + 1024, 64) + _swap_idx(B_DSA + 1024, 1)
    f += r(B_DSA + 1092, 256)
    f += r(B_RET, 256) + _swap_idx(B_RET, 4)
    f += r(B_RET + 256, 256) + _swap_idx(B_RET + 256, 4)
    f += r(B_RET + 768, 256)
    f += r(B_S5, 512)
    f += r(B_X, 512)
    assert len(f) == NP_ROWS
    t = r(B_DSA + 512, 256) + r(B_RET + 512, 256) + r(B_DSA + 1088, 4)
    assert len(t) == NT_COLS
    return np.array(f), np.array(t)


def load_weight_bf16(P, dst, src_dram, gcol, nk, ncols, blk=1344):
    src = src_dram.r("(k p) n -> p k n", p=128)
    stg = [P.sb([128, blk], F32, "wstg") for _ in range(2)]
    i = 0
    for k in range(nk):
        for c0 in range(0, ncols, blk):
            c1 = min(ncols, c0 + blk)
            s = stg[i % 2]
            P.dma(s[:, 0:c1 - c0], src[:, k, c0:c1])
            eng = 'dve' if i % 2 == 0 else 'pool'
            if gcol is not None:
                P.ts(dst[:, k, c0:c1], s[:, 0:c1 - c0], gcol[:, k:k + 1], ALU.mult, eng=eng)
            else:
                P.copy(dst[:, k, c0:c1], s[:, 0:c1 - c0], eng=eng)
            i += 1


def rsqrt_ps(P, out, src, scale, eps):
    P.ts(out, src, scale, ALU.mult, eps, ALU.add)
    P.act(out, out, AF.Sqrt)
    P.recip(out, out)


def rms_tile(P, xt, hT, sq, rstd, ones, nk, n, width):
    P.act(sq, xt, AF.Square)
    ps = P.ps()
    for k in range(nk):
        P.mm(ps[:, 0:width], ones, sq[:, k, :], start=(k == 0), stop=(k == nk - 1))
    rsqrt_ps(P, rstd, ps[:, 0:width], 1.0 / n, 1e-6)
    for k in range(nk):
        P.tt(hT[:, k, :], xt[:, k, :], rstd, ALU.mult)


def stage_P(P, l, Dm, xT):
    P.sb_off = SB_BASE
    npre = P.sb([128, 8], F32, "npre")
    P.dma(npre, Dm[f'npre{l}'])
    ones = P.sb([128, 128], F32, "ones")
    P.memset(ones, 1.0)
    wp = P.sb([128, 8, NP_ROWS], BF16, "wp")
    wt = P.sb([128, 8, NT_COLS], BF16, "wt")
    wf = P.sb([128, 8, 644], F32, "wf")
    wfsrc = Dm[f'wpf{l}'].r("(k p) n -> p k n", p=128)
    for k in range(8):
        P.dma(wf[:, k, :], wfsrc[:, k, :])
    for k in range(8):
        P.ts(wf[:, k, :], wf[:, k, :], npre[:, k:k + 1], ALU.mult, eng=('dve' if k % 2 else 'pool'))
    m0 = P.sb_off
    load_weight_bf16(P, wp, Dm[f'wp{l}'], npre, 8, NP_ROWS)
    load_weight_bf16(P, wt, Dm[f'wt{l}'], npre, 8, NT_COLS, blk=NT_COLS)
    P.barrier()
    P.sb_off = m0
    xts = [P.sb([128, 8, TT], F32, "xt") for _ in range(2)]
    sq = P.sb([128, 8, TT], F32, "sq")
    hTs = [P.sb([128, 8, TT], BF16, "hT") for _ in range(2)]
    rstd = P.sb([128, TT], F32, "rstd")
    ostg = [P.sb([128, 4, TT], F32, "ostg") for _ in range(2)]
    tstg = [P.sb([128, NT_COLS], F32, "tstg") for _ in range(2)]
    xsrc = xT.r("(k p) t -> p k t", p=128)
    cdst = Dm['colsT'].r("(c p) t -> p c t", p=128)
    ctok = Dm['colsTok']
    for tt in range(NTT):
        t0 = tt * TT
        xt, hT = xts[tt % 2], hTs[tt % 2]
        P.dma(xt, xsrc[:, :, t0:t0 + TT])
        rms_tile(P, xt, hT, sq, rstd, ones, 8, D, TT)
        for k in range(8):
            P.tt(sq[:, k, :], xt[:, k, :], rstd, ALU.mult, eng='pool')
        for c in range(42):
            ps = P.ps()
            for k in range(8):
                if C_IQ <= c <= C_IK:
                    P.mm(ps, wf[:, k, (c - C_IQ) * 128:(c - C_IQ + 1) * 128], sq[:, k, :], start=(k == 0), stop=(k == 7))
                else:
                    P.mm(ps, wp[:, k, c * 128:(c + 1) * 128], hT[:, k, :], start=(k == 0), stop=(k == 7))
            stg = ostg[(c // 4) % 2]
            if c % 3 == 2:
                P.copy(stg[:, c % 4, :], ps, eng='dve')
            else:
                P.act(stg[:, c % 4, :], ps, AF.Copy)
            if c % 4 == 3 or c == 41:
                c0 = c - c % 4
                P.dma(cdst[:, c0:c + 1, t0:t0 + TT], stg[:, 0:c % 4 + 1, :])
        for s in range(4):
            ps = P.ps()
            ps2 = P.ps()
            for k in range(8):
                P.mm(ps, hT[:, k, s * 128:(s + 1) * 128], wt[:, k, 0:512], start=(k == 0), stop=(k == 7))
            for k in range(8):
                P.mm(ps2[:, 0:4], sq[:, k, s * 128:(s + 1) * 128], wf[:, k, 640:644], start=(k == 0), stop=(k == 7))
            ts_ = tstg[s % 2]
            P.act(ts_[:, 0:512], ps, AF.Copy)
            P.copy(ts_[:, 512:516], ps2[:, 0:4], eng='dve')
            P.dma(ctok[t0 + s * 128:t0 + (s + 1) * 128, :], ts_)
    P.barrier()


BR_NAMES = ['rwkv', 'dsa', 'ret', 's5', 'xatt']


def stage_M(P, l, Dm, xT, xT_out):
    P.sb_off = SB_BASE
    npre = P.sb([128, 8], F32, "npre")
    npost = P.sb([128, 8], F32, "npost")
    P.dma(npre, Dm[f'npre{l}'])
    P.dma(npost, Dm[f'npost{l}'])
    ones = P.sb([128, 128], F32, "ones")
    P.memset(ones, 1.0)
    wg = P.sb([128, 8, 5120], BF16, "wg")
    wbr = P.sb([128, 10, 1024], BF16, "wbr")
    wout = P.sb([128, 8, 1024], BF16, "wout")
    m0 = P.sb_off
    load_weight_bf16(P, wg, Dm[f'wg{l}'], npre, 8, 5120, blk=1280)
    load_weight_bf16(P, wbr, Dm[f'wbr{l}'], None, 10, 1024, blk=1024)
    load_weight_bf16(P, wout, Dm[f'wout{l}'], None, 8, 1024, blk=1024)
    P.barrier()
    P.sb_off = m0
    xt = P.sb([128, 8, TT], F32, "xt")
    sq = P.sb([128, 8, TT], F32, "sq")
    hT = P.sb([128, 8, TT], BF16, "hT")
    rstd = P.sb([128, TT], F32, "rstd")
    yts = [P.sb([128, 2, TT], BF16, f"y{i}") for i in range(5)]
    sg = [P.sb([128, TT], F32, "sg") for _ in range(2)]
    term = [P.sb([128, TT], F32, "term") for _ in range(2)]
    macc = P.sb([128, TT], F32, "macc")
    mT = P.sb([128, 8, TT], BF16, "mT")
    osb = sq
    osq = P.sb([128, TT], F32, "osq")
    xsrc = xT.r("(k p) t -> p k t", p=128)
    xdst = xT_out.r("(k p) t -> p k t", p=128)
    for tt in range(NTT):
        t0 = tt * TT
        P.dma(xt, xsrc[:, :, t0:t0 + TT])
        for i in range(5):
            P.dma(yts[i], Dm[f'yT_{BR_NAMES[i]}'].r("(c p) t -> p c t", p=128)[:, :, t0:t0 + TT])
        rms_tile(P, xt, hT, sq, rstd, ones, 8, D, TT)
        j = 0
        for dc in range(8):
            for i in range(5):
                psg = P.ps()
                for k in range(8):
                    P.mm(psg, wg[:, k, i * 1024 + dc * 128:i * 1024 + (dc + 1) * 128], hT[:, k, :],
                         start=(k == 0), stop=(k == 7))
                psb = P.ps()
                for kk in range(2):
                    P.mm(psb, wbr[:, i * 2 + kk, dc * 128:(dc + 1) * 128], yts[i][:, kk, :],
                         start=(kk == 0), stop=(kk == 1))
                s_, t_ = sg[j % 2], term[j % 2]
                j += 1
                P.act(s_, psg, AF.Sigmoid)
                if i == 0:
                    P.tt(macc, s_, psb, ALU.mult)
                elif i < 4:
                    P.tt(t_, s_, psb, ALU.mult)
                    P.tt(macc, macc, t_, ALU.add, eng='pool')
                else:
                    P.tt(t_, s_, psb, ALU.mult)
                    P.tt(mT[:, dc, :], macc, t_, ALU.add, eng='pool')
        pss = P.ps_acc()
        for ec in range(8):
            ps = P.ps()
            for k in range(8):
                P.mm(ps, wout[:, k, ec * 128:(ec + 1) * 128], mT[:, k, :], start=(k == 0), stop=(k == 7))
            P.act(osb[:, ec, :], ps, AF.Copy)
            P.act(osq, ps, AF.Square)
            P.mm(pss, ones, osq, start=(ec == 0), stop=(ec == 7))
        rsqrt_ps(P, rstd, pss, 1.0 / D, 1e-6)
        for ec in range(8):
            P.stt(osb[:, ec, :], osb[:, ec, :], npost[:, ec:ec + 1], rstd, ALU.mult, ALU.mult)
            P.tt(xt[:, ec, :], xt[:, ec, :], osb[:, ec, :], ALU.add, eng='pool')
        P.dma(xdst[:, :, t0:t0 + TT], xt)
    P.barrier()


def stage_X(P, l, Dm):
    P.sb_off = SB_BASE
    nmem = P.sb([128, 8], F32, "nmem")
    P.dma(nmem, Dm[f'nmem{l}'])
    ones = P.sb([128, 128], F32, "ones")
    P.memset(ones, 1.0)
    wm = P.sb([128, 8, 512], BF16, "wm")
    m0 = P.sb_off
    load_weight_bf16(P, wm, Dm[f'wmem{l}'], nmem, 8, 512, blk=512)
    P.barrier()
    P.sb_off = m0
    mt = P.sb([128, 8, 256], F32, "mt")
    msq = P.sb([128, 8, 256], F32, "msq")
    mh = P.sb([128, 8, 256], BF16, "mh")
    mr = P.sb([128, 256], F32, "mr")
    P.dma(mt, Dm['memT'].r("(k p) m -> p k m", p=128))
    rms_tile(P, mt, mh, msq, mr, ones, 8, D, 256)
    kmT = [P.sb([128, 256], BF16, "kmT") for _ in range(2)]
    for c in range(2):
        ps = P.ps()
        for k in range(8):
            P.mm(ps[:, 0:256], wm[:, k, c * 128:(c + 1) * 128], mh[:, k, :], start=(k == 0), stop=(k == 7))
        P.copy(kmT[c], ps[:, 0:256])
    vpad = [[P.sb([128, 128], BF16, "vpad") for _ in range(4)] for _ in range(2)]
    opad = [P.sb([128, 128], BF16, "opad") for _ in range(2)]
    for hh in range(2):
        P.memset(opad[hh], 0.0)
        P.memset(opad[hh][:, hh * 64:(hh + 1) * 64], 1.0)
    for mc in range(2):
        ps = P.ps()
        for k in range(8):
            P.mm(ps[:, 0:256], mh[:, k, mc * 128:(mc + 1) * 128], wm[:, k, 256:512], start=(k == 0), stop=(k == 7))
        for h in range(4):
            hh = h % 2
            P.memset(vpad[mc][h], 0.0)
            P.copy(vpad[mc][h][:, hh * 64:(hh + 1) * 64], ps[:, h * 64:(h + 1) * 64])
    qf = P.sb([128, 2, TT], F32, "qf")
    gf = P.sb([128, 2, TT], F32, "gf")
    qb = P.sb([128, 2, TT], BF16, "qb")
    E = [[P.sb([128, TT], BF16, "E") for _ in range(2)] for _ in range(2)]
    rs = P.sb([128, TT], F32, "rs")
    o = P.sb([128, TT], F32, "o")
    sgl = P.sb([128, TT], F32, "sgl")
    yst = P.sb([128, 2, TT], BF16, "yst")
    csrc = Dm['colsT'].r("(c p) t -> p c t", p=128)
    ydst = Dm['yT_xatt'].r("(c p) t -> p c t", p=128)
    for tt in range(NTT):
        t0 = tt * TT
        P.dma(qf, csrc[:, C_XQ:C_XQ + 2, t0:t0 + TT])
        P.dma(gf, csrc[:, C_XG:C_XG + 2, t0:t0 + TT])
        P.copy(qb, qf)
        for p in range(2):
            for hh in range(2):
                for mc in range(2):
                    ps = P.ps()
                    P.mm(ps, kmT[p][hh * 64:(hh + 1) * 64, mc * 128:(mc + 1) * 128],
                         qb[hh * 64:(hh + 1) * 64, p, :])
                    P.act(E[hh][mc], ps, AF.Exp, scale=0.125)
            pso = P.ps()
            pss = P.ps()
            n = 0
            for hh in range(2):
                for mc in range(2):
                    P.mm(pso, vpad[mc][2 * p + hh], E[hh][mc], start=(n == 0), stop=(n == 3))
                    n += 1
            n = 0
            for hh in range(2):
                for mc in range(2):
                    P.mm(pss, opad[hh], E[hh][mc], start=(n == 0), stop=(n == 3))
                    n += 1
            P.recip(rs, pss)
            P.tt(o, pso, rs, ALU.mult)
            P.act(sgl, gf[:, p, :], AF.Silu)
            P.tt(yst[:, p, :], o, sgl, ALU.mult)
        P.dma(ydst[:, :, t0:t0 + TT], yst)
    P.barrier()


def dram_specs():
    sp = {
        'xT': ([D, S], F32, 'in'), 'memT': ([D, 256], F32, 'in'), 'pos': ([1, S], I32, 'in'),
        'colsT': ([NP_ROWS, S], F32, 'scratch'), 'colsTok': ([S, NT_COLS], F32, 'scratch'),
        'xT1': ([D, S], F32, 'scratch'),
    }
    for n in BR_NAMES:
        sp[f'yT_{n}'] = ([256, S], BF16, 'scratch')
    sp['iqR'] = ([256, S], F32, 'scratch')
    sp['qR'] = ([256, S], F32, 'scratch')
    sp['ropeC'] = ([64, S], F32, 'scratch')
    sp['ropeS'] = ([64, S], F32, 'scratch')
    sp['ropeconst'] = ([64, 2], F32, 'in')
    sp['ident'] = ([128, 128], F32, 'in')
    sp['ret_idT'] = ([128, 4, 128], F32, 'in')
    sp['ret_qd'] = ([64, 4, 128], F32, 'in')
    sp['ret_kd'] = ([128, 4], F32, 'in')
    sp['ret_cd'] = ([64, 256], F32, 'in')
    sp['s5mask'] = ([128, 8, 8], F32, 'in')
    sp['rw_masks'] = ([64, 3, 64], F32, 'in')
    sp['dsa_cb'] = ([128, 128], F32, 'in')
    sp['dsa_pw'] = ([128, 32], F32, 'in')
    for l in range(2):
        sp[f'rwprm{l}'] = ([64, 8, 4], F32, 'in')
        sp[f'rwmu{l}'] = ([64, 18], F32, 'in')
        sp[f'rww2{l}'] = ([64, 256], F32, 'in')
        sp[f'rwa2{l}'] = ([64, 256], F32, 'in')
    sp['s5tau'] = ([128, 512], F32, 'in')
    for l in range(2):
        sp[f'retgn{l}'] = ([64, 4], F32, 'in')
        sp[f's5lam{l}'] = ([128, 8, 3], F32, 'in')
        sp[f's5b{l}'] = ([128, 8, 2, 16], F32, 'in')
        sp[f's5c{l}'] = ([128, 8, 2, 16], F32, 'in')
        sp[f's5d{l}'] = ([128, 2], F32, 'in')
        sp[f's5wglu{l}'] = ([256, 256], F32, 'in')
    for l in range(2):
        sp[f'wp{l}'] = ([D, NP_ROWS], F32, 'in')
        sp[f'wt{l}'] = ([D, NT_COLS], F32, 'in')
        sp[f'wpf{l}'] = ([D, 644], F32, 'in')
        sp[f'wg{l}'] = ([D, 5120], F32, 'in')
        sp[f'wbr{l}'] = ([1280, D], F32, 'in')
        sp[f'wout{l}'] = ([D, D], F32, 'in')
        sp[f'wmem{l}'] = ([D, 512], F32, 'in')
        for n in ['npre', 'npost', 'nmem']:
            sp[f'{n}{l}'] = ([128, 8], F32, 'in')
    return sp


def host_inputs(inputs, b):
    f_idx, t_idx = proj_col_indices()
    d = {}
    d['xT'] = np.ascontiguousarray(inputs['x'][b].T)
    d['memT'] = np.ascontiguousarray(inputs['mem'][b].T)
    d['pos'] = np.ascontiguousarray(inputs['positions'][b][None, :]).astype(np.int32)
    pk = lambda v: np.ascontiguousarray(v.reshape(8, 128).T)
    jj = np.arange(64)
    inv = (10000.0 ** (-(np.arange(32, dtype=np.float32)) / 32)).astype(np.float32)
    d['ropeconst'] = np.stack([inv[jj % 32], np.where(jj < 32, -1.0, 1.0)], 1).astype(np.float32)
    d['ident'] = np.eye(128, dtype=np.float32)
    d['ret_idT'], d['ret_qd'], d['ret_kd'], d['ret_cd'] = ret_consts()
    ii = np.arange(64)
    rm = np.zeros((64, 3, 64), np.float32)
    rm[:, 0, :] = (ii[None, :] > ii[:, None])
    rm[:, 1, :] = (ii[None, :] >= ii[:, None])
    rm[:, 2, :] = (ii[None, :] < ii[:, None])
    d['rw_masks'] = rm
    i128 = np.arange(128)
    d['dsa_pw'] = np.ascontiguousarray(np.broadcast_to((0.5 ** np.arange(1, 33, dtype=np.float64)).astype(np.float32)[None, :], (128, 32)))
    d['dsa_cb'] = np.where(i128[None, :] <= i128[:, None], 0.0, -1e30).astype(np.float32)
    for l in range(2):
        hd = lambda v: np.ascontiguousarray(v.reshape(4, 64).T)
        z = np.zeros((64, 4), np.float32)
        d[f'rwprm{l}'] = np.ascontiguousarray(np.stack([hd(inputs['rwkv_w0'][l]), hd(inputs['rwkv_a0'][l]), hd(inputs['rwkv_k_k'][l]),
                                   hd(inputs['rwkv_k_a'][l]), hd(inputs['rwkv_r_k'][l].reshape(256)), hd(inputs['rwkv_lnx_w'][l]),
                                   hd(inputs['rwkv_lnx_b'][l]), z], 1).astype(np.float32))
        d[f'rwmu{l}'] = np.ascontiguousarray(inputs['rwkv_mu'][l].reshape(18, 64).T)
        d[f'rww2{l}'] = np.ascontiguousarray(inputs['rwkv_w2'][l])
        d[f'rwa2{l}'] = np.ascontiguousarray(inputs['rwkv_a2'][l])
    sidx = np.arange(128)
    mk = np.zeros((128, 8, 8), np.float32)
    for j in range(8):
        mk[sidx, j, (2 * j + sidx // 64) % 8] = 1.0
    d['s5mask'] = mk
    d['s5tau'] = np.ascontiguousarray(np.broadcast_to(np.arange(1, 513, dtype=np.float32)[None, :], (128, 512)))
    sj = lambda a: np.ascontiguousarray(a.reshape((8, 128) + a.shape[1:]).swapaxes(0, 1))
    for l in range(2):
        d[f'retgn{l}'] = np.ascontiguousarray(inputs['ret_gn_w'][l].reshape(4, 64).T)
        lam3 = np.stack([inputs['s5_lam_re'][l].reshape(1024), inputs['s5_lam_im'][l].reshape(1024),
                         np.repeat(inputs['s5_log_dt'][l], 64)], 1).astype(np.float32)
        d[f's5lam{l}'] = sj(lam3)
        d[f's5b{l}'] = sj(np.stack([inputs['s5_b_re'][l].reshape(1024, 16), inputs['s5_b_im'][l].reshape(1024, 16)], 1))
        ct = lambda c: np.ascontiguousarray(c.transpose(0, 2, 1)).reshape(1024, 16)
        d[f's5c{l}'] = sj(np.stack([ct(inputs['s5_c_re'][l]), ct(inputs['s5_c_im'][l])], 1))
        d[f's5d{l}'] = np.ascontiguousarray(inputs['s5_d'][l].reshape(2, 128).T)
        d[f's5wglu{l}'] = np.ascontiguousarray(inputs['s5_w_glu'][l])
    for l in range(2):
        w = inputs['w_in'][l]
        d[f'wp{l}'] = np.ascontiguousarray(w[:, f_idx])
        d[f'wt{l}'] = np.ascontiguousarray(w[:, t_idx])
        d[f'wpf{l}'] = np.ascontiguousarray(w[:, np.concatenate([f_idx[C_IQ * 128:(C_IK + 1) * 128], t_idx[512:516]])])
        d[f'wg{l}'] = np.ascontiguousarray(w[:, B_GATE:B_GATE + 5120])
        d[f'wbr{l}'] = np.ascontiguousarray(inputs['w_branch'][l].reshape(1280, D))
        d[f'wout{l}'] = np.ascontiguousarray(inputs['w_out'][l])
        d[f'wmem{l}'] = np.ascontiguousarray(inputs['w_mem_kv'][l])
        d[f'npre{l}'] = pk(inputs['norm_pre'][l])
        d[f'npost{l}'] = pk(inputs['norm_post'][l])
        d[f'nmem{l}'] = pk(inputs['norm_mem'][l])
    return d


STAGE_FNS = {}


def build(plan, ext_in=(), ext_out=()):
    nc = bass.Bass("TRN2", target_bir_lowering=False)
    P = Prog(nc)
    Dm = {}
    used_in = []
    for name, (shape, dtype, role) in dram_specs().items():
        if role == 'in' or name in ext_in:
            kind = "ExternalInput"
            used_in.append(name)
        elif name in ext_out:
            kind = "ExternalOutput"
        else:
            kind = "Internal"
        Dm[name] = P.dram(name, shape, dtype, kind=kind)
    Dm['outT'] = P.dram('outT', [D, S], F32, kind="ExternalOutput")
    for st, l in plan:
        xin = Dm['xT'] if l == 0 else Dm['xT1']
        xout = Dm['xT1'] if l == 0 else Dm['outT']
        if st == 'P':
            stage_P(P, l, Dm, xin)
        elif st == 'M':
            stage_M(P, l, Dm, xin, xout)
        elif st == 'X':
            stage_X(P, l, Dm)
        else:
            STAGE_FNS[st](P, l, Dm)
    P.finish()
    return nc, P, used_in


def sin_reduced(P, out, ang, kq, ki, m1):
    P.ts(kq, ang, 1.0 / (2 * math.pi), ALU.mult)
    P.copy(ki, kq)
    P.copy(kq, ki)
    P.stt(ang, kq, -2 * math.pi, ang, ALU.mult, ALU.add)
    P.ts(m1, ang, math.pi, ALU.is_gt, -2 * math.pi, ALU.mult)
    P.tt(ang, ang, m1, ALU.add)
    P.ts(m1, ang, -math.pi, ALU.is_lt, 2 * math.pi, ALU.mult)
    P.tt(ang, ang, m1, ALU.add)
    P.act(out, ang, AF.Sin)


def stage_R(P, l, Dm):
    P.sb_off = SB_BASE
    W = 2048
    rc = P.sb([64, 2], F32, "rc")
    P.dma(rc, Dm['ropeconst'])
    posi = P.sb([64, W], I32, "posi")
    posf = P.sb([64, W], F32, "posf")
    ang = P.sb([64, W], F32, "ang")
    kq = P.sb([64, W], F32, "kq")
    ki = P.sb([64, W], I32, "ki")
    m1 = P.sb([64, W], F32, "m1")
    o = P.sb([64, W], F32, "o")
    for half in range(S // W):
        sl = slice(half * W, (half + 1) * W)
        P.dma(posi, Dm['pos'][:, sl].m(lambda x: x.to_broadcast([64, W])))
        P.copy(posf, posi)
        P.ts(ang, posf, rc[:, 0:1], ALU.mult)
        sin_reduced(P, o, ang, kq, ki, m1)
        P.ts(o, o, rc[:, 1:2], ALU.mult)
        P.dma(Dm['ropeS'][:, sl], o)
        P.ts(ang, posf, rc[:, 0:1], ALU.mult, math.pi / 2, ALU.add)
        sin_reduced(P, o, ang, kq, ki, m1)
        P.dma(Dm['ropeC'][:, sl], o)
    P.barrier()


def rope_heads(P, dst, Dm, c_base, c_swap, ropeC, ropeS, nheads=4, scale=None, dram_dst=None):
    a = P.sb([64, nheads, TT], F32, "ra")
    b = P.sb([64, nheads, TT], F32, "rb")
    if dram_dst is not None:
        ro = [P.sb([64, nheads, TT], F32, "ro") for _ in range(2)]
    src = Dm['colsT']
    for tt in range(NTT):
        sl = slice(tt * TT, (tt + 1) * TT)
        rb_ = c_base * 128 if isinstance(c_base, int) else c_base[0]
        rs_ = c_swap * 128 if isinstance(c_swap, int) else c_swap[0]
        P.dma(a, src[rb_:rb_ + nheads * 64, sl].r("(h d) t -> d h t", d=64))
        P.dma(b, src[rs_:rs_ + nheads * 64, sl].r("(h d) t -> d h t", d=64))
        cb = ropeC[:, sl].m(lambda x: x.unsqueeze(1).to_broadcast([64, nheads, TT]))
        sb_ = ropeS[:, sl].m(lambda x: x.unsqueeze(1).to_broadcast([64, nheads, TT]))
        P.tt(a, a, cb, ALU.mult)
        P.tt(b, b, sb_, ALU.mult, eng='pool')
        if dram_dst is None:
            P.tt(dst[:, :, sl], a, b, ALU.add)
        else:
            o_ = ro[tt % 2]
            P.tt(o_, a, b, ALU.add)
            P.dma(dram_dst.r("(h d) t -> d h t", d=64)[:, :, sl], o_)


RET_LOGG = [math.log(1.0 - math.exp(v)) for v in np.linspace(math.log(1.0 / 32), math.log(1.0 / 512), 4)]


def ret_consts():
    j = np.arange(128, dtype=np.float64)
    idT = np.zeros((128, 4, 128), np.float32)
    qd = np.zeros((64, 4, 128), np.float32)
    kd = np.zeros((128, 4), np.float32)
    cd = np.zeros((64, 256), np.float32)
    for h in range(4):
        lg = RET_LOGG[h]
        rel = j[None, :] - j[:, None]
        idT[:, h, :] = np.where(rel >= 0, np.exp(lg * np.maximum(rel, 0.0)), 0.0) * 0.125
        qd[:, h, :] = np.exp(lg * (j + 1.0))[None, :]
        kd[:, h] = np.exp(lg * (127.0 - j)) * 0.125
        cd[:, h * 64:(h + 1) * 64] = math.exp(lg * 128)
    return idT, qd, kd, cd


def stage_RET(P, l, Dm):
    P.sb_off = SB_BASE
    ropeC = P.sb([64, S], F32, "ropeC")
    ropeS = P.sb([64, S], F32, "ropeS")
    P.dma(ropeC, Dm['ropeC'])
    P.dma(ropeS, Dm['ropeS'])
    idT = P.sb([128, 4, 128], F32, "idT")
    qd = P.sb([64, 4, 128], F32, "qd")
    kd = P.sb([128, 4], F32, "kd")
    cd = P.sb([64, 256], F32, "cd")
    gn = P.sb([64, 4], F32, "gn")
    identb = P.sb([128, 128], BF16, "identb")
    identf = P.sb([128, 128], F32, "identf")
    ones64 = P.sb([64, 64], F32, "ones64")
    P.dma(idT, Dm['ret_idT'])
    P.dma(qd, Dm['ret_qd'])
    P.dma(kd, Dm['ret_kd'])
    P.dma(cd, Dm['ret_cd'])
    P.dma(gn, Dm[f'retgn{l}'])
    P.dma(identf, Dm['ident'])
    P.copy(identb, identf)
    P.memset(ones64, 1.0 / 64)
    qT = P.sb([64, 4, S], BF16, "qT")
    kT = P.sb([64, 4, S], BF16, "kT")
    qdT = P.sb([64, 4, S], BF16, "qdT")
    m0 = P.sb_off
    rope_heads(P, qT, Dm, C_RQ, C_RQS, ropeC, ropeS)
    rope_heads(P, kT, Dm, C_RK, C_RKS, ropeC, ropeS)
    for c in range(32):
        cs = slice(c * 128, (c + 1) * 128)
        P.tt(qdT[:, :, cs], qT[:, :, cs], qd, ALU.mult, eng=('dve' if c % 2 else 'pool'))
    P.barrier()
    P.sb_off = m0
    Vt = P.sb([128, 32, 256], BF16, "Vt")
    Kd = P.sb([128, 32, 256], BF16, "Kd")
    vst = [P.sb([128, 4, 256], F32, "vst") for _ in range(2)]
    vsrc = Dm['colsTok'].r("(c p) n -> p c n", p=128)
    for i in range(8):
        v_ = vst[i % 2]
        P.dma(v_, vsrc[:, i * 4:(i + 1) * 4, 256:512])
        P.copy(Vt[:, i * 4:(i + 1) * 4, :], v_, eng=('dve' if i % 2 else 'pool'))
    for c in range(32):
        cs = slice(c * 128, (c + 1) * 128)
        ps = P.ps()
        psb = ps.bitcast(BF16)
        for h in range(4):
            P.transpose(psb[:, h * 64:(h + 1) * 64], kT[:, h, cs], identb[0:64, 0:64])
        P.tt(Kd[:, c, :].r("p (h d) -> p h d", h=4), psb[:, 0:256].r("p (h d) -> p h d", h=4),
             kd.m(lambda x: x.unsqueeze(2).to_broadcast([128, 4, 64])), ALU.mult)
    R = P.sb([64, 256], F32, "R")
    Rb = P.sb([64, 256], BF16, "Rb")
    P.memset(R, 0.0)
    P.memset(Rb, 0.0)
    AT = [P.sb([128, 4, 128], BF16, "AT") for _ in range(2)]
    Osb = P.sb([64, 512], F32, "Osb")
    dd = P.sb([64, 512], F32, "dd")
    dsq = P.sb([64, 512], F32, "dsq")
    rstd = P.sb([64, 512], F32, "rstd")
    gt = [P.sb([64, 4, 128], F32, "gt") for _ in range(2)]
    sg = P.sb([64, 4, 128], F32, "sg")
    yo = [P.sb([64, 4, 128], BF16, "yo") for _ in range(2)]
    gsrc = Dm['colsT'][C_RG * 128:C_RG * 128 + 256, :].r("(h d) t -> d h t", d=64)
    ydst = Dm['yT_ret'].r("(h d) t -> d h t", d=64)
    for c in range(32):
        cs = slice(c * 128, (c + 1) * 128)
        g_ = gt[c % 2]
        P.dma(g_, gsrc[:, :, cs])
        psA = P.ps()
        for h in range(4):
            P.mm(psA[:, h * 128:(h + 1) * 128], kT[:, h, cs], qT[:, h, cs])
        at = AT[c % 2]
        P.tt(at, psA.r("p (h q) -> p h q", h=4), idT, ALU.mult)
        psO = P.ps()
        for h in range(4):
            P.mm(psO[0:64, h * 128:(h + 1) * 128], Vt[:, c, h * 64:(h + 1) * 64], at[:, h, :], start=True, stop=False, inc=False)
            P.mm(psO[0:64, h * 128:(h + 1) * 128], Rb[:, h * 64:(h + 1) * 64], qdT[:, h, cs], start=False, stop=True)
        psKV = P.ps()
        for h in range(4):
            P.mm(psKV[0:64, h * 64:(h + 1) * 64], Kd[:, c, h * 64:(h + 1) * 64], Vt[:, c, h * 64:(h + 1) * 64])
        P.tt(R, R, cd, ALU.mult)
        P.tt(R, R, psKV[0:64, 0:256], ALU.add)
        P.copy(Rb, R, eng='pool')
        P.act(Osb, psO[0:64, :], AF.Copy)
        psM = P.ps()
        P.mm(psM[0:64, :], ones64, Osb)
        P.tt(dd, Osb, psM[0:64, :], ALU.subtract)
        P.act(dsq, dd, AF.Square)
        psV = P.ps()
        P.mm(psV[0:64, :], ones64, dsq)
        P.ts(rstd, psV[0:64, :], 1e-6, ALU.add)
        P.act(rstd, rstd, AF.Sqrt)
        P.recip(rstd, rstd)
        P.tt(dd, dd, rstd, ALU.mult)
        P.tt(dd.r("p (h q) -> p h q", h=4), dd.r("p (h q) -> p h q", h=4),
             gn.m(lambda x: x.unsqueeze(2).to_broadcast([64, 4, 128])), ALU.mult)
        P.act(sg, g_, AF.Silu)
        y_ = yo[c % 2]
        P.tt(y_, dd.r("p (h q) -> p h q", h=4), sg, ALU.mult)
        P.dma(ydst[:, :, cs], y_)
    P.barrier()


STAGE_FNS['R'] = stage_R
STAGE_FNS['RET'] = stage_RET


def stage_S5(P, l, Dm):
    P.sb_off = SB_BASE
    W = TT
    lam = P.sb([128, 8, 3], F32, "lam")
    bsb = P.sb([128, 8, 2, 16], F32, "bsb")
    csb = P.sb([128, 8, 2, 16], F32, "csb")
    msk = P.sb([128, 8, 8], F32, "msk")
    tau = P.sb([128, W], F32, "tau")
    dsk = P.sb([128, 2], F32, "dsk")
    identf = P.sb([128, 128], F32, "identf")
    P.dma(lam, Dm[f's5lam{l}'])
    P.dma(bsb, Dm[f's5b{l}'])
    P.dma(csb, Dm[f's5c{l}'])
    P.dma(msk, Dm['s5mask'])
    P.dma(tau, Dm['s5tau'])
    P.dma(dsk, Dm[f's5d{l}'])
    P.dma(identf, Dm['ident'])
    wglu = P.sb([128, 2, 256], BF16, "wglu")
    cosT = P.sb([128, 8, W], F32, "cosT")
    sinT = P.sb([128, 8, W], F32, "sinT")
    mag = P.sb([128, 8], F32, "mag")
    BT = P.sb([128, 8, 2, 128], BF16, "BT")
    CX = P.sb([128, 8, 2, 128], BF16, "CX")
    m0 = P.sb_off
    load_weight_bf16(P, wglu, Dm[f's5wglu{l}'], None, 2, 256, blk=256)
    lr = P.sb([128, 8], F32, "lr")
    li = P.sb([128, 8], F32, "li")
    dt = P.sb([128, 8], F32, "dt")
    th = P.sb([128, 8], F32, "th")
    P.ts(lr, lam[:, :, 0], -1e-4, ALU.min)
    P.copy(li, lam[:, :, 1])
    P.act(dt, lam[:, :, 2], AF.Exp)
    P.tt(th, li, dt, ALU.mult)
    P.tt(mag, lr, dt, ALU.mult)
    P.act(mag, mag, AF.Exp)
    ang = P.sb([128, W], F32, "ang")
    kq = P.sb([128, W], F32, "kq")
    ki = P.sb([128, W], I32, "ki")
    m1 = P.sb([128, W], F32, "m1")
    for j in range(8):
        P.ts(ang, tau, th[:, j:j + 1], ALU.mult)
        sin_reduced(P, sinT[:, j, :], ang, kq, ki, m1)
        P.ts(ang, tau, th[:, j:j + 1], ALU.mult, math.pi / 2, ALU.add)
        sin_reduced(P, cosT[:, j, :], ang, kq, ki, m1)
    abr = P.sb([128, 8], F32, "abr")
    abi = P.sb([128, 8], F32, "abi")
    den = P.sb([128, 8], F32, "den")
    t8 = P.sb([128, 8], F32, "t8")
    fre = P.sb([128, 8], F32, "fre")
    fim = P.sb([128, 8], F32, "fim")
    P.tt(abr, mag, cosT[:, :, 0], ALU.mult)
    P.tt(abi, mag, sinT[:, :, 0], ALU.mult)
    P.ts(abr, abr, -1.0, ALU.add)
    P.tt(den, lr, lr, ALU.mult)
    P.tt(t8, li, li, ALU.mult)
    P.tt(den, den, t8, ALU.add)
    P.recip(den, den)
    P.tt(fre, abr, lr, ALU.mult)
    P.tt(t8, abi, li, ALU.mult)
    P.tt(fre, fre, t8, ALU.add)
    P.tt(fre, fre, den, ALU.mult)
    P.tt(fim, abi, lr, ALU.mult)
    P.tt(t8, abr, li, ALU.mult)
    P.tt(fim, fim, t8, ALU.subtract)
    P.tt(fim, fim, den, ALU.mult)
    bb = P.sb([128, 8, 2, 16], F32, "bb")
    tb = P.sb([128, 8, 16], F32, "tb")
    bc16 = lambda v: v.m(lambda x: x.unsqueeze(2).to_broadcast([128, 8, 16]))
    P.tt(bb[:, :, 0, :], bsb[:, :, 0, :], bc16(fre), ALU.mult)
    P.tt(tb, bsb[:, :, 1, :], bc16(fim), ALU.mult)
    P.tt(bb[:, :, 0, :], bb[:, :, 0, :], tb, ALU.subtract)
    P.tt(bb[:, :, 1, :], bsb[:, :, 1, :], bc16(fre), ALU.mult)
    P.tt(tb, bsb[:, :, 0, :], bc16(fim), ALU.mult)
    P.tt(bb[:, :, 1, :], bb[:, :, 1, :], tb, ALU.add)
    P.ts(csb[:, :, 1, :], csb[:, :, 1, :], -1.0, ALU.mult)
    bx = P.sb([128, 8, 16], F32, "bx")
    for j in range(8):
        mj = msk[:, j, :].m(lambda x: x.unsqueeze(2).to_broadcast([128, 8, 16]))
        for ri in range(2):
            P.tt(bx, bb[:, j, ri, :].m(lambda x: x.unsqueeze(1).to_broadcast([128, 8, 16])), mj, ALU.mult)
            ps = P.ps()
            P.transpose(ps[:, 0:128], bx.r("p a b -> p (a b)"), identf)
            P.copy(BT[:, j, ri, :], ps[:, 0:128])
            P.tt(CX[:, j, ri, :].r("p (a b) -> p a b", a=8),
                 csb[:, j, ri, :].m(lambda x: x.unsqueeze(1).to_broadcast([128, 8, 16])), mj, ALU.mult)
    P.barrier()
    P.sb_off = m0
    A = P.sb([128, 8, W], F32, "A")
    B = P.sb([128, 8, W], F32, "B")
    t1 = P.sb([128, 8, W], F32, "t1")
    t2 = P.sb([128, 8, W], F32, "t2")
    wre = P.sb([128, 8, W], F32, "wre")
    wim = P.sb([128, 8, W], F32, "wim")
    xre = P.sb([128, 8, W], BF16, "xre")
    xim = P.sb([128, 8, W], BF16, "xim")
    cre = P.sb([128, 8], F32, "cre")
    cim = P.sb([128, 8], F32, "cim")
    P.memset(cre, 0.0)
    P.memset(cim, 0.0)
    uf = P.sb([128, 2, W], F32, "uf")
    ub = P.sb([128, 2, W], BF16, "ub")
    gf = P.sb([128, 2, W], F32, "gf")
    y = P.sb([128, 2, W], F32, "y")
    y2 = P.sb([128, 2, W], F32, "y2")
    glb = P.sb([128, 2, W], BF16, "glb")
    yo = P.sb([128, 2, W], BF16, "yo")
    csrc = Dm['colsT'].r("(c p) t -> p c t", p=128)
    ydst = Dm['yT_s5'].r("(c p) t -> p c t", p=128)
    for tt in range(NTT):
        sl = slice(tt * W, (tt + 1) * W)
        P.dma(uf, csrc[:, C_SU:C_SU + 2, sl])
        P.dma(gf, csrc[:, C_SG:C_SG + 2, sl])
        P.copy(ub, uf, eng='pool')
        for j in range(8):
            for ri, dst in ((0, A), (1, B)):
                ps = P.ps()
                P.mm(ps, BT[:, j, ri, :], ub[:, j // 4, :])
                P.act(dst[:, j, :], ps, AF.Copy)
        P.tt(t1, A, cosT, ALU.mult)
        P.tt(t2, B, sinT, ALU.mult, eng='pool')
        P.tt(t1, t1, t2, ALU.add)
        P.tt(t2, A, sinT, ALU.mult, eng='pool')
        P.tt(B, B, cosT, ALU.mult)
        P.tt(t2, B, t2, ALU.subtract, eng='pool')
        for j in range(8):
            mb = mag[:, j:j + 1].bc([128, W])
            P.scan(wre[:, j, :], mb, t1[:, j, :], cre[:, j:j + 1], ALU.mult, ALU.add)
            P.scan(wim[:, j, :], mb, t2[:, j, :], cim[:, j:j + 1], ALU.mult, ALU.add)
        P.tt(t1, wre, cosT, ALU.mult)
        P.tt(A, wim, sinT, ALU.mult, eng='pool')
        P.tt(xre, t1, A, ALU.subtract)
        P.tt(cre, t1[:, :, W - 1], A[:, :, W - 1], ALU.subtract)
        P.tt(t2, wre, sinT, ALU.mult, eng='pool')
        P.tt(B, wim, cosT, ALU.mult)
        P.tt(xim, t2, B, ALU.add, eng='pool')
        P.tt(cim, t2[:, :, W - 1], B[:, :, W - 1], ALU.add)
        for jc in range(2):
            ps = P.ps()
            n = 0
            for j in range(4 * jc, 4 * jc + 4):
                for ri, xx in ((0, xre), (1, xim)):
                    P.mm(ps, CX[:, j, ri, :], xx[:, j, :], start=(n == 0), stop=(n == 7))
                    n += 1
            P.stt(y[:, jc, :], uf[:, jc, :], dsk[:, jc:jc + 1], ps, ALU.mult, ALU.add)
        P.tt(y2, y, y, ALU.mult)
        P.ts(y2, y2, 0.044715, ALU.mult, 1.0, ALU.add)
        P.tt(y2, y2, y, ALU.mult)
        P.act(y2, y2, AF.Sigmoid, scale=1.5957691216057308)
        P.tt(y, y, y2, ALU.mult)
        P.copy(glb, y, eng='pool')
        for oc in range(2):
            ps = P.ps()
            for kc in range(2):
                P.mm(ps, wglu[:, kc, oc * 128:(oc + 1) * 128], glb[:, kc, :], start=(kc == 0), stop=(kc == 1))
            P.act(y2[:, oc, :], ps, AF.Sigmoid)
        P.tt(y, y, y2, ALU.mult)
        P.act(y2, gf, AF.Silu)
        P.tt(yo, y, y2, ALU.mult)
        P.dma(ydst[:, :, sl], yo)
    P.barrier()


STAGE_FNS['S5'] = stage_S5


import os
RW_DEBUG = int(os.environ.get('RW_DEBUG', '3'))


def stage_RWKV(P, l, Dm):
    P.sb_off = SB_BASE
    W = 256
    H4 = 4
    NCH = W // 64
    HC = H4 * NCH
    prm = P.sb([64, 8, 4], F32, "prm")
    mu = P.sb([64, 18], F32, "mu")
    w2 = P.sb([64, 256], F32, "w2")
    a2 = P.sb([64, 256], F32, "a2")
    msks = P.sb([64, 3, 64], F32, "msks")
    identf = P.sb([128, 128], F32, "identf")
    ones64 = P.sb([64, 64], F32, "ones64")
    onesw = P.sb([64, 1], F32, "onesw")
    P.dma(prm, Dm[f'rwprm{l}'])
    P.dma(mu, Dm[f'rwmu{l}'])
    P.dma(w2, Dm[f'rww2{l}'])
    P.dma(a2, Dm[f'rwa2{l}'])
    P.dma(msks, Dm['rw_masks'])
    P.dma(identf, Dm['ident'])
    P.memset(ones64, 1.0)
    P.memset(onesw, 1.0)
    id64 = identf[0:64, 0:64]
    hb = lambda v, n=W: v.m(lambda x: x.unsqueeze(2).to_broadcast([64, H4, n]))
    mb = lambda k: msks[:, k, :].m(lambda x: x.unsqueeze(1).to_broadcast([64, H4, 64]))
    idb = id64.m(lambda x: x.unsqueeze(1).to_broadcast([64, H4, 64]))
    cin = P.sb([64, 18, W + 1], F32, "cin")
    cs = P.sb([64, 18, W], F32, "cs")
    f = lambda nm: P.sb([64, H4, W], F32, nm)
    twl = P.sb([64, W], F32, "twl")
    sgz, av, kx, t0, kp, beta = f("sgz"), f("av"), f("kx"), f("t0"), f("kp"), f("beta")
    kkn, lw, cw, e1, e2 = f("kkn"), f("lw"), f("cw"), f("e1"), f("e2")
    rt, at, bt, kt, Bh, Kh = f("rt"), f("at"), f("bt"), f("kt"), f("Bh"), f("Kh")
    bonus, Yt = f("bonus"), f("Yt")
    base = P.sb([64, HC], F32, "base")
    cwC = P.sb([64, HC], F32, "cwC")
    gC = P.sb([64, HC], F32, "gC")
    S0 = P.sb([64, H4, 64], F32, "S0")
    P.memset(S0, 0.0)
    NP2 = NCH // 2
    g8 = lambda nm, dt=F32: [P.sb([64, 2, H4, 64], dt, nm) for _ in range(NP2)]
    X, XT, PaT, AakT, ArbT, ArkT = g8("X", BF16), g8("XT", BF16), g8("PaT", BF16), g8("AakT"), g8("ArbT"), g8("ArkT")
    Vt, BhT, KhT, atT, W2, M2, M1T, KV, Gd = (g8("Vt"), g8("BhT"), g8("KhT"), g8("atT", BF16), g8("W2", BF16), g8("M2"),
                                              g8("M1T"), g8("KV"), g8("Gd"))
    Usb = [P.sb([64, H4, 64], F32, "Usb") for _ in range(2)]
    mb8 = lambda k: msks[:, k, :].m(lambda x: x.unsqueeze(1).unsqueeze(1).to_broadcast([64, 2, H4, 64]))
    id8 = id64.m(lambda x: x.unsqueeze(1).unsqueeze(1).to_broadcast([64, 2, H4, 64]))
    ps8 = lambda ps: ps[0:64, 0:512].r("p (c h x) -> p c h x", c=2, h=H4)
    yo = P.sb([64, H4, W], BF16, "yo")
    src = Dm['colsT'][0:1152, :].r("(g d) t -> d g t", d=64)
    ydst = Dm['yT_rwkv'].r("(h d) t -> d h t", d=64)
    ps4 = lambda ps: ps[0:64, 0:256].r("p (h x) -> p h x", h=H4)
    for tt in range(S // W):
        t_0 = tt * W
        if tt == 0:
            P.dma(cin[:, :, 1:W + 1], src[:, :, t_0:t_0 + W])
            P.memset(cin[:, :, 0:1], 0.0)
        else:
            P.dma(cin, src[:, :, t_0 - 1:t_0 + W])
        P.tt(cs, cin[:, :, 0:W], cin[:, :, 1:W + 1], ALU.subtract)
        P.tt(cs, cs, mu.m(lambda x: x.unsqueeze(2).to_broadcast([64, 18, W])), ALU.mult)
        P.tt(cs, cs, cin[:, :, 1:W + 1], ALU.add)
        Rr, Kk, Vv, G = cs[:, 0:4, :], cs[:, 4:8, :], cs[:, 8:12, :], cs[:, 14:18, :]
        P.act(twl, cs[:, 12, :], AF.Tanh)
        for h in range(H4):
            ps = P.ps()
            P.mm(ps[0:64, 0:W], w2[:, h * 64:(h + 1) * 64], twl)
            P.act(sgz[:, h, :], ps[0:64, 0:W], AF.Sigmoid, bias=prm[:, 0, h:h + 1])
            ps = P.ps()
            P.mm(ps[0:64, 0:W], a2[:, h * 64:(h + 1) * 64], cs[:, 13, :])
            P.act(av[:, h, :], ps[0:64, 0:W], AF.Sigmoid, bias=prm[:, 1, h:h + 1])
        P.tt(kx, Kk, hb(prm[:, 2, :]), ALU.mult)
        P.tt(t0, kx, kx, ALU.mult, eng='pool')
        for h in range(H4):
            ps = P.ps()
            P.mm(ps[0:64, 0:W], ones64, t0[:, h, :])
            P.ts(kkn[:, h, :], ps[0:64, 0:W], 1e-24, ALU.add)
        P.act(kkn, kkn, AF.Sqrt)
        P.recip(kkn, kkn)
        P.tt(kkn, kkn, kx, ALU.mult)
        P.ts(t0, av, -1.0, ALU.add)
        P.tt(t0, t0, hb(prm[:, 3, :]), ALU.mult)
        P.stt(kp, t0, 1.0, Kk, ALU.add, ALU.mult)
        P.tt(beta, kkn, av, ALU.mult, eng='pool')
        P.tt(t0, Rr, kp, ALU.mult)
        P.tt(t0, t0, hb(prm[:, 4, :]), ALU.mult)
        for h in range(H4):
            ps = P.ps()
            P.mm(ps[0:64, 0:W], ones64, t0[:, h, :])
            P.tt(bonus[:, h, :], ps[0:64, 0:W], Vv[:, h, :], ALU.mult)
        P.ts(lw, sgz, -math.exp(-0.5), ALU.mult)
        for h in range(H4):
            P.scan(cw[:, h, :], onesw[:, 0:1].bc([64, W]), lw[:, h, :], 0.0, ALU.mult, ALU.add)
        cw3 = cw.r("p h (c i) -> p (h c) i", i=64)
        P.memset(base, 0.0)
        P.copy(base.r("p (h c) -> p h c", h=H4)[:, :, 1:NCH], cw.r("p h (c i) -> p h c i", i=64)[:, :, 0:NCH - 1, 63])
        P.tt(cw3, cw3, base.m(lambda x: x.unsqueeze(2).to_broadcast([64, HC, 64])), ALU.subtract)
        P.copy(cwC, cw3[:, :, 63])
        P.act(gC, cwC, AF.Exp)
        P.act(e1, cw, AF.Exp)
        P.tt(rt, Rr, e1, ALU.mult)
        P.act(e1, cw, AF.Exp, scale=-1.0)
        P.tt(bt, beta, e1, ALU.mult)
        P.tt(kt, kp, e1, ALU.mult, eng='pool')
        P.tt(e2, cw, lw, ALU.subtract)
        P.act(e2, e2, AF.Exp)
        P.stt(at, kkn, -1.0, e2, ALU.mult, ALU.mult)
        e13 = e1.r("p h (c i) -> p (h c) i", i=64)
        P.tt(e13, cw3, cwC.m(lambda x: x.unsqueeze(2).to_broadcast([64, HC, 64])), ALU.subtract)
        P.act(e1, e1, AF.Exp, scale=-1.0)
        P.tt(Bh, beta, e1, ALU.mult)
        P.tt(Kh, kp, e1, ALU.mult, eng='pool')
        def mm8(p, lhf, rhf):
            ps = P.ps()
            for cl in range(2):
                for h in range(H4):
                    o_ = ps[0:64, (cl * H4 + h) * 64:(cl * H4 + h + 1) * 64]
                    P.mm(o_, lhf(p, cl, h), rhf(p, cl, h))
            return ps8(ps)
        csl = lambda p, cl: slice((2 * p + cl) * 64, (2 * p + cl + 1) * 64)
        tok = lambda t_: (lambda p, cl, h: t_[:, h, csl(p, cl)])
        blk = lambda t_: (lambda p, cl, h: t_[p][:, cl, h, :])
        for p in range(NP2):
            P.tt(X[p], mm8(p, tok(at), tok(bt)), mb8(2), ALU.mult)
            P.tt(XT[p], mm8(p, tok(bt), tok(at)), mb8(0), ALU.mult)
            P.tt(AakT[p], mm8(p, tok(kt), tok(at)), mb8(0), ALU.mult)
            P.tt(ArbT[p], mm8(p, tok(bt), tok(rt)), mb8(1), ALU.mult)
            P.tt(ArkT[p], mm8(p, tok(kt), tok(rt)), mb8(1), ALU.mult)
            P.tt(PaT[p], XT[p], id8, ALU.add)
        for srcT, dstT, eng in ((Vv, Vt, 'act'), (Bh, BhT, 'dve'), (Kh, KhT, 'act'), (at, atT, 'dve')):
            for p in range(NP2):
                ps = P.ps()
                for cl in range(2):
                    for h in range(H4):
                        P.transpose(ps[0:64, (cl * H4 + h) * 64:(cl * H4 + h + 1) * 64], srcT[:, h, csl(p, cl)], id64)
                if eng == 'act':
                    P.act(dstT[p], ps8(ps), AF.Copy)
                else:
                    P.copy(dstT[p], ps8(ps))
        for it in range(5):
            pxs = [(mm8(p, blk(XT), blk(X)), mm8(p, blk(X), blk(XT))) for p in range(NP2)]
            for p in range(NP2):
                P.act(X[p], pxs[p][0], AF.Copy)
                P.copy(XT[p], pxs[p][1])
            pps = [mm8(p, blk(X), blk(PaT)) for p in range(NP2)]
            for p in range(NP2):
                P.tt(PaT[p], PaT[p], pps[p], ALU.add)
        for p in range(NP2):
            P.act(KV[p], mm8(p, blk(KhT), blk(Vt)), AF.Copy)
            P.act(W2[p], mm8(p, blk(AakT), blk(Vt)), AF.Copy)
            P.copy(M1T[p], mm8(p, blk(atT), blk(PaT)))
            for cl in range(2):
                c = 2 * p + cl
                gcb = gC.r("p (h c) -> p h c", h=H4)[:, :, c].m(lambda x: x.unsqueeze(2).to_broadcast([64, H4, 64]))
                P.tt(Gd[p][:, cl], idb, gcb, ALU.mult)
        for p in range(NP2):
            P.act(M2[p], mm8(p, blk(PaT), blk(W2)), AF.Copy)
        for c in range(NCH):
            p, cl = c // 2, c % 2
            sl = slice(c * 64, (c + 1) * 64)
            us = Usb[c % 2]
            psu = P.ps()
            for h in range(H4):
                P.mm(psu[0:64, h * 64:(h + 1) * 64], M1T[p][:, cl, h, :], S0[:, h, :])
            psy = P.ps()
            for h in range(H4):
                o_ = psy[0:64, h * 64:(h + 1) * 64]
                P.mm(o_, S0[:, h, :], rt[:, h, sl], start=True, stop=False)
                P.mm(o_, Vt[p][:, cl, h, :], ArkT[p][:, cl, h, :], start=False, stop=True)
            P.tt(us, ps4(psu), M2[p][:, cl], ALU.add)
            pss = P.ps()
            for h in range(H4):
                o_ = pss[0:64, h * 64:(h + 1) * 64]
                P.mm(o_, Gd[p][:, cl, h, :], S0[:, h, :], start=True, stop=False)
                P.mm(o_, BhT[p][:, cl, h, :], us[:, h, :], start=False, stop=True)
            psy2 = P.ps()
            for h in range(H4):
                P.mm(psy2[0:64, h * 64:(h + 1) * 64], us[:, h, :], ArbT[p][:, cl, h, :])
            P.tt(S0, ps4(pss), KV[p][:, cl], ALU.add)
            P.act(Yt[:, :, sl], ps4(psy), AF.Copy)
            P.tt(Yt[:, :, sl], Yt[:, :, sl], ps4(psy2), ALU.add)
        for h in range(H4):
            ps = P.ps()
            P.mm(ps[0:64, 0:W], ones64, Yt[:, h, :])
            P.stt(e1[:, h, :], ps[0:64, 0:W], -1.0 / 64, Yt[:, h, :], ALU.mult, ALU.add)
        P.tt(e2, e1, e1, ALU.mult, eng='pool')
        for h in range(H4):
            ps = P.ps()
            P.mm(ps[0:64, 0:W], ones64, e2[:, h, :])
            P.ts(t0[:, h, :], ps[0:64, 0:W], 1.0 / 64, ALU.mult, 64e-5, ALU.add)
        P.act(t0, t0, AF.Sqrt)
        P.recip(t0, t0)
        P.tt(e1, e1, t0, ALU.mult)
        P.tt(e1, e1, hb(prm[:, 5, :]), ALU.mult)
        P.tt(e1, e1, hb(prm[:, 6, :]), ALU.add)
        P.tt(e1, e1, bonus, ALU.add)
        P.act(e2, G, AF.Silu)
        P.tt(yo, e1, e2, ALU.mult)
        P.dma(ydst[:, :, t_0:t_0 + W], yo)
    P.barrier()


STAGE_FNS['RWKV'] = stage_RWKV


N_BISECT = int(os.environ.get("N_BISECT", "20"))


def stage_DSA(P, l, Dm):
    P.sb_off = SB_BASE
    kT = P.sb([64, 4, S], BF16, "kT")
    ikT = P.sb([64, 1, S], F32, "ikT")
    m0 = P.sb_off
    ropeC = P.sb([64, S], F32, "ropeC")
    ropeS = P.sb([64, S], F32, "ropeS")
    P.dma(ropeC, Dm['ropeC'])
    P.dma(ropeS, Dm['ropeS'])
    rope_heads(P, None, Dm, C_DQ, C_DQS, ropeC, ropeS, dram_dst=Dm['qR'])
    rope_heads(P, kT, Dm, C_DK, C_DKS, ropeC, ropeS)
    rope_heads(P, None, Dm, C_IQ, C_IQS, ropeC, ropeS, dram_dst=Dm['iqR'])
    rope_heads(P, ikT, Dm, (C_IK * 128,), (C_IK * 128 + 64,), ropeC, ropeS, nheads=1)
    P.barrier()
    P.sb_off = m0
    identf = P.sb([128, 128], F32, "identf")
    identb = P.sb([128, 128], BF16, "identb")
    cb = P.sb([128, 128], F32, "cb")
    P.dma(identf, Dm['ident'])
    P.copy(identb, identf)
    P.dma(cb, Dm['dsa_cb'])
    Vaug = P.sb([128, 32, 4, 65], BF16, "Vaug")
    iwt = P.sb([128, 32, 4], F32, "iwt")
    vst = [P.sb([128, 4, 256], F32, "vst") for _ in range(2)]
    tsrc = Dm['colsTok'].r("(c p) n -> p c n", p=128)
    P.memset(Vaug[:, :, :, 64:65], 1.0)
    for i in range(8):
        v_ = vst[i % 2]
        P.dma(v_, tsrc[:, i * 4:(i + 1) * 4, 0:256])
        P.copy(Vaug[:, i * 4:(i + 1) * 4, :, 0:64], v_.r("p c (h d) -> p c h d", h=4), eng=('dve' if i % 2 else 'pool'))
    P.dma(iwt, tsrc[:, :, 512:516])
    P.ts(iwt, iwt, 1.0 / 16, ALU.mult)
    scores = [P.sb([128, S], F32, "score") for _ in range(2)]
    mask01s = [P.sb([128, S], BF16, "mask01") for _ in range(2)]
    maskTs = [P.sb([128, 32, 128], BF16, "maskT") for _ in range(2)]
    rl = [P.sb([128, 512], F32, "rl") for _ in range(4)]
    E = [P.sb([128, 512], BF16, "E") for _ in range(2)]
    lo = P.sb([128, 1], F32, "lo")
    hi = P.sb([128, 1], F32, "hi")
    mid = P.sb([128, 1], F32, "mid")
    cnt = P.sb([128, 1], F32, "cnt")
    sel = P.sb([128, 1], F32, "sel")
    dlt = P.sb([128, 1], F32, "dlt")
    stp = P.sb([128, 32], F32, "stp")
    pw = P.sb([128, 32], F32, "pw")
    P.dma(pw, Dm['dsa_pw'])
    zt = P.sb([128, S], BF16, "zt")
    cz = P.sb([128, S], F32, "cz")
    junk = cz
    nz = P.sb([128, 1], F32, "nz")
    npos = P.sb([128, 1], F32, "npos")
    flag = P.sb([128, 1], F32, "flag")
    f2 = P.sb([128, 1], F32, "f2")
    rr = P.sb([128, 1], F32, "rr")
    onesw = P.sb([128, 1], F32, "onesw")
    P.memset(onesw, 1.0)
    negbig = P.sb([128, 1], F32, "negbig")
    P.memset(negbig, -1e5)
    osb = P.sb([128, 4, 64], F32, "osb")
    rs = P.sb([128, 4, 1], F32, "rs")
    gt = [P.sb([128, 2, 128], F32, "gt") for _ in range(2)]
    sgs = [P.sb([128, 2, 128], F32, "sg") for _ in range(2)]
    yo = [P.sb([128, 2, 128], BF16, "yo") for _ in range(2)]
    iqt = [P.sb([64, 4, 128], F32, "iqt") for _ in range(2)]
    iqsrc = Dm['iqR'].r("(h d) t -> d h t", d=64)
    qft = [P.sb([64, 4, 128], F32, "qft") for _ in range(2)]
    qbt = [P.sb([64, 4, 128], BF16, "qbt") for _ in range(2)]
    qsrc = Dm['qR'].r("(h d) t -> d h t", d=64)
    gsrc = Dm['colsT'].r("(c p) t -> p c t", p=128)
    ydst = Dm['yT_dsa'].r("(c p) t -> p c t", p=128)
    ne = [0]
    st = {}

    def score_phase(i):
        qs = slice(i * 128, (i + 1) * 128)
        Nk = 128 * (i + 1)
        g_ = gt[i % 2]
        P.dma(g_, gsrc[:, C_DG:C_DG + 2, qs])
        iq_ = iqt[i % 2]
        P.dma(iq_, iqsrc[:, :, qs])
        P.dma(qft[i % 2], qsrc[:, :, qs])
        qb_ = qbt[i % 2]
        P.copy(qb_, qft[i % 2], eng='pool')
        score = scores[i % 2]
        mask01 = mask01s[i % 2]
        maskT = maskTs[i % 2]
        for k0 in range(0, Nk, 512):
            kw = min(512, Nk - k0)
            pss = []
            for h in range(4):
                ps = P.ps()
                P.mm(ps[:, 0:kw], iq_[:, h, :], ikT[:, 0, k0:k0 + kw], inc=(h == 3))
                pss.append(ps)
            for h in range(4):
                P.act(rl[h][:, 0:kw], pss[h][:, 0:kw], AF.Relu)
            P.ts(score[:, k0:k0 + kw], rl[0][:, 0:kw], iwt[:, i, 0:1], ALU.mult)
            for h in range(1, 4):
                P.stt(score[:, k0:k0 + kw], rl[h][:, 0:kw], iwt[:, i, h:h + 1], score[:, k0:k0 + kw], ALU.mult, ALU.add)
        P.tt(score[:, i * 128:Nk], score[:, i * 128:Nk], cb, ALU.add)
        st[i] = (qs, Nk, g_, iq_, qb_, score, mask01, maskT)

    def select_phase(i):
        qs, Nk, g_, iq_, qb_, score, mask01, maskT = st[i]
        if Nk > 256:
            P.reduce(hi, score[:, 0:Nk], ALU.max)
            P.reduce(lo, score[:, 0:i * 128], ALU.min)
            P.tt(dlt, hi, lo, ALU.subtract)
            P.ts(dlt, dlt, 2.0, ALU.add)
            P.ts(stp, pw, dlt[:, 0:1], ALU.mult)
            P.stt(mid, dlt, 0.5, lo, ALU.mult, ALU.add)
            P.ts(mid, mid, -1.0, ALU.add)
            for it in range(N_BISECT):
                P.ts(junk[:, 0:Nk], score[:, 0:Nk], mid[:, 0:1], ALU.is_ge, 0.0, ALU.add, accum=cnt)
                if it < N_BISECT - 1:
                    P.ts(sel, cnt, 255.5, ALU.is_ge, 0.5, ALU.subtract)
                    P.stt(mid, sel, stp[:, it:it + 1], mid, ALU.mult, ALU.add)
                else:
                    P.ts(sel, cnt, 255.5, ALU.is_ge, 1.0, ALU.subtract)
                    P.stt(lo, sel, stp[:, it:it + 1], mid, ALU.mult, ALU.add)
        else:
            P.memset(lo, -1e29)
        P.ts(zt[:, 0:Nk], score[:, 0:Nk], 0.0, ALU.is_equal, 0.0, ALU.add, accum=nz)
        P.ts(junk[:, 0:Nk], score[:, 0:Nk], 0.0, ALU.is_gt, 0.0, ALU.add, accum=npos)
        P.ts(flag, npos, 255.5, ALU.is_lt)
        P.tt(f2, npos, nz, ALU.add)
        P.ts(f2, f2, 255.5, ALU.is_ge)
        P.tt(flag, flag, f2, ALU.mult)
        P.ts(rr, npos, -1.0, ALU.mult, 256.0, ALU.add)
        P.scan(cz[:, 0:Nk], onesw[:, 0:1].bc([128, Nk]), zt[:, 0:Nk], 0.0, ALU.mult, ALU.add)
        P.ts(cz[:, 0:Nk], cz[:, 0:Nk], rr[:, 0:1], ALU.is_le, flag[:, 0:1], ALU.mult)
        P.tt(zt[:, 0:Nk], zt[:, 0:Nk], cz[:, 0:Nk], ALU.mult)
        P.ts(f2, flag, -1.0, ALU.mult, 1.0, ALU.add)
        P.tt(lo, lo, f2, ALU.mult)
        P.stt(lo, flag, 1e-30, lo, ALU.mult, ALU.add)
        P.ts(mask01[:, 0:Nk], score[:, 0:Nk], lo[:, 0:1], ALU.is_ge)
        P.tt(mask01[:, 0:Nk], mask01[:, 0:Nk], zt[:, 0:Nk], ALU.add)
        for c0 in range(0, i + 1, 4):
            nc_ = min(4, i + 1 - c0)
            ps = P.ps()
            psb = ps.bitcast(BF16)
            for cl in range(nc_):
                c = c0 + cl
                P.transpose(psb[:, cl * 128:(cl + 1) * 128], mask01[:, c * 128:(c + 1) * 128], identb, inc=(cl == nc_ - 1))
            P.act(maskT[:, c0:c0 + nc_, :], psb[:, 0:nc_ * 128].r("p (c q) -> p c q", q=128), AF.Identity,
                  scale=1e5, bias=negbig[:, 0:1])

    def attention(i):
        qs, Nk, g_, iq_, qb_, score, mask01, maskT = st[i]
        psO = P.ps_acc(i)
        groups = [(h, c0, min(4, i + 1 - c0)) for h in range(4) for c0 in range(0, i + 1, 4)]

        def logits(g):
            h, c0, nc_ = g
            ps = P.ps()
            for cl in range(nc_):
                c = c0 + cl
                o_ = ps[:, cl * 128:(cl + 1) * 128]
                P.mm(o_, kT[:, h, c * 128:(c + 1) * 128], qb_[:, h, :], start=True, stop=False)
                P.mm(o_, identb, maskT[:, c, :], start=False, stop=True)
            return ps
        nxt = logits(groups[0])
        for gi, (h, c0, nc_) in enumerate(groups):
            ps = nxt
            if gi + 1 < len(groups):
                nxt = logits(groups[gi + 1])
            e_ = E[ne[0] % 2]
            ne[0] += 1
            P.act(e_[:, 0:nc_ * 128], ps[:, 0:nc_ * 128], AF.Exp, scale=0.125)
            for cl in range(nc_):
                c = c0 + cl
                P.mm(psO[:, h * 65:(h + 1) * 65], e_[:, cl * 128:(cl + 1) * 128], Vaug[:, c, h, :],
                     start=(c == 0), stop=(c == i))

    def fin(i):
        qs, Nk, g_, iq_, qb_, score, mask01, maskT = st[i]
        psO = P.ps_acc(i)
        pv = psO[:, 0:260].r("p (h x) -> p h x", h=4)
        P.recip(rs, pv[:, :, 64:65])
        P.tt(osb, pv[:, :, 0:64], rs.m(lambda x: x.to_broadcast([128, 4, 64])), ALU.mult)
        sg = sgs[i % 2]
        P.act(sg, g_, AF.Silu)
        y_ = yo[i % 2]
        for p in range(2):
            ps = P.ps()
            P.transpose(ps[:, 0:128], osb[:, 2 * p:2 * p + 2, :].r("p a b -> p (a b)"), identf)
            P.tt(y_[:, p, :], ps[:, 0:128], sg[:, p, :], ALU.mult)
        P.dma(ydst[:, :, qs], y_)

    score_phase(0)
    for i in range(32):
        select_phase(i)
        if i > 0:
            fin(i - 1)
        if i + 1 < 32:
            score_phase(i + 1)
        attention(i)
    fin(31)
    P.barrier()


STAGE_FNS['DSA'] = stage_DSA


FULL_PLAN = [('R', 0)] + [(st, l) for l in range(2) for st in ('P', 'X', 'RET', 'S5', 'RWKV', 'DSA', 'M')]


def kernel(**inputs):
    inputs = {k: np.asarray(v) for k, v in inputs.items()}
    nb = inputs['x'].shape[0]
    nc, P, used_in = build(FULL_PLAN)
    in_maps = []
    for b in range(nb):
        d = host_inputs(inputs, b)
        in_maps.append({k: v for k, v in d.items() if k in used_in})
    res = run_bass_kernel_spmd(nc, in_maps, core_ids=list(range(nb)))
    out = np.stack([np.ascontiguousarray(np.asarray(r['outT']).T) for r in res.results], 0)
    return out.astype(np.float32)
```

```python
from contextlib import ExitStack
import math
import numpy as np
import ml_dtypes
import concourse.bass as bass
import concourse.mybir as mybir
from concourse.bass_utils import run_bass_kernel_spmd

F32 = mybir.dt.float32
BF16 = mybir.dt.bfloat16
I32 = mybir.dt.int32
AF = mybir.ActivationFunctionType
ALU = mybir.AluOpType
AX = mybir.AxisListType

ENGS = ['sp', 'act', 'dve', 'pool', 'pe']
EPOCH = 16000
NDMASEM = 24
RELAX_SAME_ENGINE = True
SB_BASE = 16640
SBUF_BYTES = 229000


class Buf:
    __slots__ = ('name', 'wev', 'rev', 'tracked')

    def __init__(self, name, tracked=True):
        self.name = name
        self.wev = {}
        self.rev = {}
        self.tracked = tracked


class V:
    __slots__ = ('buf', 'ap')

    def __init__(self, buf, ap):
        self.buf = buf
        self.ap = ap

    def __getitem__(self, k):
        return V(self.buf, self.ap[k])

    def m(self, fn):
        return V(self.buf, fn(self.ap))

    def r(self, s, **kw):
        return V(self.buf, self.ap.rearrange(s, **kw))

    def bc(self, shape):
        return V(self.buf, self.ap.to_broadcast(list(shape)))

    def bitcast(self, dt):
        return V(self.buf, self.ap.bitcast(dt))

    @property
    def shape(self):
        return tuple(self.ap.shape)


def _ap(x):
    return x.ap if isinstance(x, V) else x


class Prog:
    def __init__(self, nc):
        self.nc = nc
        self.q = {e: [] for e in ENGS}
        self.cnt = {e: 0 for e in ENGS}
        self.noinc = {e: False for e in ENGS}
        self.known = {e: {} for e in ENGS}
        self.dma_n = {e: 0 for e in ENGS}
        self.nbar = 0
        self.bufs = []
        self.sb_off = SB_BASE
        self.sb_id = 0
        self.sb_mark = 0
        self.psum = []
        for i in range(8):
            h = nc.alloc_psum_tensor(f"ps{i}", [128, 512], F32)
            self.psum.append(V(self._newbuf(f"ps{i}"), h[:]))
        self.ps_rr = 0

    def _newbuf(self, name, tracked=True):
        b = Buf(name, tracked)
        if tracked:
            self.bufs.append(b)
        return b

    def sb(self, shape, dtype=F32, name="t"):
        esz = {F32: 4, BF16: 2, I32: 4}[dtype]
        per = esz * int(np.prod(shape[1:]))
        per = (per + 63) // 64 * 64
        off = self.sb_off
        assert off + per <= SBUF_BYTES, f"SBUF overflow {name} {off}+{per}"
        self.sb_off += per
        self.sb_id += 1
        nm = f"{name}_{self.sb_id}"
        h = self.nc.alloc_sbuf_tensor_at(nm, list(shape), dtype, offset=off)
        return V(self._newbuf(nm), h[:])

    def mark(self):
        self.sb_mark = self.sb_off

    def release(self):
        self.sb_off = self.sb_mark

    def ps(self):
        v = self.psum[self.ps_rr % 6]
        self.ps_rr += 1
        return v

    def ps_acc(self, i=1):
        return self.psum[6 + (i % 2)]

    def dram(self, name, shape, dtype=F32, kind="Internal"):
        h = self.nc.dram_tensor(name, list(shape), dtype, kind=kind)
        return V(self._newbuf(name, tracked=False), h.ap())

    def _collect(self, eng, reads, writes, extra=None):
        waits = {}

        last = self.cnt.get(eng, 0) if isinstance(eng, str) else 0

        def need(evs, skip_own):
            for sk, v in evs.items():
                if sk == eng:
                    if skip_own or (RELAX_SAME_ENGINE and v < last):
                        continue
                if waits.get(sk, 0) < v:
                    waits[sk] = v
        for x in reads:
            if x.buf.tracked:
                need(x.buf.wev, False)
        for x in writes:
            if x.buf.tracked:
                need(x.buf.wev, True)
                need(x.buf.rev, True)
        if extra:
            need(extra, False)
        kn = self.known[eng]
        wl = []
        for sk, v in waits.items():
            if kn.get(sk, 0) < v:
                kn[sk] = v
                wl.append((sk, v))
        return wl

    def emit(self, eng, fn, reads=(), writes=(), inc=True):
        reads = [x for x in reads if isinstance(x, V)]
        writes = [x for x in writes if isinstance(x, V)]
        wl = self._collect(eng, reads, writes)
        idx = self.cnt[eng] + 1
        self.cnt[eng] = idx
        self.q[eng].append((wl, fn, ('c', idx)))
        for x in reads:
            b = x.buf
            if b.tracked and b.rev.get(eng, 0) < idx:
                b.rev[eng] = idx
        for x in writes:
            b = x.buf
            if b.tracked:
                b.wev = {eng: idx}
                b.rev = {}

    def dma(self, out, in_, eng='sp'):
        n = self.dma_n[eng]
        self.dma_n[eng] = n + 1
        slot, k = n % NDMASEM, n // NDMASEM
        sk = ('dma', eng, slot)
        val = 16 * (k + 1)
        extra = {sk: 16 * k} if k > 0 else None
        wl = self._collect(eng, [in_], [out], extra)
        oa, ia = out.ap, in_.ap
        self.q[eng].append((wl, lambda e: e.dma_start(out=oa, in_=ia), ('d', sk)))
        b = in_.buf
        if b.tracked:
            b.rev[sk] = val
        b = out.buf
        if b.tracked:
            b.wev = {sk: val}
            b.rev = {}

    def barrier(self):
        waits = {}
        for e in ENGS:
            if e != 'sp' and self.cnt[e] > 0:
                waits[e] = self.cnt[e]
            n = self.dma_n[e]
            for slot in range(min(n, NDMASEM)):
                k = (n - 1 - slot) // NDMASEM
                waits[('dma', e, slot)] = 16 * (k + 1)
        kn = self.known['sp']
        wl = []
        for sk, v in waits.items():
            if kn.get(sk, 0) < v:
                kn[sk] = v
                wl.append((sk, v))
        self.nbar += 1
        nb = self.nbar
        bk = ('bar', 0)
        self.q['sp'].append((wl, None, ('b', bk)))
        for e in ENGS:
            if e != 'sp':
                self.q[e].append(([(bk, nb)], None, None))
                for sk, v in waits.items():
                    if self.known[e].get(sk, 0) < v:
                        self.known[e][sk] = v
        for b in self.bufs:
            b.wev = {}
            b.rev = {}

    def finish(self):
        self.barrier()
        nc = self.nc
        targets = {e: set() for e in ENGS}
        for e in ENGS:
            for wl, fn, tag in self.q[e]:
                for sk, v in wl:
                    if isinstance(sk, str):
                        targets[sk].add(v)
        rank = {e: {v: r + 1 for r, v in enumerate(sorted(targets[e]))} for e in ENGS}
        self.n_inc = {e: len(rank[e]) for e in ENGS}
        keys = set()

        def semkey(sk, v):
            if isinstance(sk, str):
                r = rank[sk][v]
                return ((sk, (r - 1) // EPOCH), (r - 1) % EPOCH + 1)
            return (sk, v)
        prog = {e: [] for e in ENGS}
        for e in ENGS:
            for wl, fn, tag in self.q[e]:
                w2 = [semkey(sk, v) for sk, v in wl]
                inc = None
                if tag is not None:
                    if tag[0] == 'c':
                        if tag[1] in rank[e]:
                            r = rank[e][tag[1]]
                            inc = ((e, (r - 1) // EPOCH), 1)
                    elif tag[0] == 'd':
                        inc = (tag[1], 16)
                    elif tag[0] == 'b':
                        inc = (tag[1], 1)
                for k_, _ in w2:
                    keys.add(k_)
                if inc is not None:
                    keys.add(inc[0])
                prog[e].append((w2, fn, inc, tag))
        stack = ExitStack()
        sems = {}
        for i, sk in enumerate(sorted(keys, key=str)):
            sems[sk] = stack.enter_context(nc.semaphore(f"s{i}"))
        self.nsem = len(sems)

        def mk(en):
            def body(e):
                for wl, fn, inc, tag in prog[en]:
                    for sk, v in wl:
                        e.wait_ge(sems[sk], v)
                    if fn is None:
                        if tag is not None and tag[0] == 'b':
                            e.sem_inc(sems[inc[0]], inc[1])
                        continue
                    ins = fn(e)
                    if inc is not None:
                        ins.then_inc(sems[inc[0]], inc[1])
            return body
        with stack:
            with nc.Block() as block:
                block.sync(mk('sp'))
                block.scalar(mk('act'))
                block.vector(mk('dve'))
                block.gpsimd(mk('pool'))
                block.tensor(mk('pe'))

    def act(self, out, in_, func, bias=None, scale=1.0, accum=None):
        o, i, b, s, a = _ap(out), _ap(in_), _ap(bias), _ap(scale), _ap(accum)
        kw = {}
        if b is not None:
            kw['bias'] = b
        if a is not None:
            kw['accum_out'] = a
        self.emit('act', lambda e: e.activation(out=o, in_=i, func=func, scale=s, **kw),
                  [in_, bias, scale], [out, accum])

    def ts(self, out, in0, s1, op0, s2=None, op1=None, accum=None, eng='dve'):
        o, i, a1, a2, ac = _ap(out), _ap(in0), _ap(s1), _ap(s2), _ap(accum)
        kw = {}
        if op1 is not None:
            kw['op1'] = op1
        if ac is not None:
            kw['accum_out'] = ac
        self.emit(eng, lambda e: e.tensor_scalar(out=o, in0=i, scalar1=a1, scalar2=a2, op0=op0, **kw),
                  [in0, s1, s2], [out, accum])

    def tt(self, out, in0, in1, op, eng='dve'):
        o, a, b = _ap(out), _ap(in0), _ap(in1)
        self.emit(eng, lambda e: e.tensor_tensor(out=o, in0=a, in1=b, op=op), [in0, in1], [out])

    def stt(self, out, in0, scalar, in1, op0, op1, eng='dve'):
        o, a, s, b = _ap(out), _ap(in0), _ap(scalar), _ap(in1)
        self.emit(eng, lambda e: e.scalar_tensor_tensor(out=o, in0=a, scalar=s, in1=b, op0=op0, op1=op1),
                  [in0, scalar, in1], [out])

    def copy(self, out, in_, eng='dve'):
        o, i = _ap(out), _ap(in_)
        if eng == 'act':
            self.emit('act', lambda e: e.copy(out=o, in_=i), [in_], [out])
        else:
            self.emit(eng, lambda e: e.tensor_copy(out=o, in_=i), [in_], [out])

    def memset(self, out, val, eng='dve'):
        o = _ap(out)
        self.emit(eng, lambda e: e.memset(o, val), [], [out])

    def recip(self, out, in_):
        o, i = _ap(out), _ap(in_)
        self.emit('dve', lambda e: e.reciprocal(out=o, in_=i), [in_], [out])

    def reduce(self, out, in_, op, axis=AX.X):
        o, i = _ap(out), _ap(in_)
        self.emit('dve', lambda e: e.tensor_reduce(out=o, in_=i, axis=axis, op=op), [in_], [out])

    def scan(self, out, d0, d1, initial, op0, op1):
        o, a, b, ini = _ap(out), _ap(d0), _ap(d1), _ap(initial)
        self.emit('dve', lambda e: e.tensor_tensor_scan(out=o, data0=a, data1=b, initial=ini, op0=op0, op1=op1),
                  [d0, d1, initial], [out])

    def mm(self, out, lhsT, rhs, start=True, stop=True, inc=None):
        o, l, r = _ap(out), _ap(lhsT), _ap(rhs)
        if inc is None:
            inc = stop
        self.emit('pe', lambda e: e.matmul(o, l, r, start=start, stop=stop), [lhsT, rhs], [out], inc=inc)

    def transpose(self, out, in_, ident, inc=True):
        o, i, d = _ap(out), _ap(in_), _ap(ident)
        self.emit('pe', lambda e: e.transpose(o, i, d), [in_, ident], [out], inc=inc)


S = 4096
D = 1024
TT = 512
NTT = S // TT
NP_ROWS = 5376
NT_COLS = 516
B_RWKV, B_DSA, B_RET, B_S5, B_X, B_GATE = 0, 1152, 2500, 3524, 4036, 4548
C_RWKV = 0
C_DQ, C_DQS, C_DK, C_DKS, C_IQ, C_IQS, C_IK, C_DG = 9, 11, 13, 15, 17, 19, 21, 22
C_RQ, C_RQS, C_RK, C_RKS, C_RG = 24, 26, 28, 30, 32
C_SU, C_SG = 34, 36
C_XQ, C_XG = 38, 40


def _swap_idx(base, nheads):
    idx = []
    for h in range(nheads):
        for j in range(64):
            idx.append(base + h * 64 + (j + 32) % 64)
    return idx


def proj_col_indices():
    r = lambda a, n: list(range(a, a + n))
    f = []
    f += r(B_RWKV, 1152)
    f += r(B_DSA, 256) + _swap_idx(B_DSA, 4)
    f += r(B_DSA + 256, 256) + _swap_idx(B_DSA + 256, 4)
    f += r(B_DSA + 768, 256) + _swap_idx(B_DSA + 768, 4)
    f += r(B_DSA + 1024, 64) + _swap_idx(B_DSA + 1024, 1)
    f += r(B_DSA + 1092, 256)
    f += r(B_RET, 256) + _swap_idx(B_RET, 4)
    f += r(B_RET + 256, 256) + _swap_idx(B_RET + 256, 4)
    f += r(B_RET + 768, 256)
    f += r(B_S5, 512)
    f += r(B_X, 512)
    assert len(f) == NP_ROWS
    t = r(B_DSA + 512, 256) + r(B_RET + 512, 256) + r(B_DSA + 1088, 4)
    assert len(t) == NT_COLS
    return np.array(f), np.array(t)


def load_weight_bf16(P, dst, src_dram, gcol, nk, ncols, blk=1344):
    src = src_dram.r("(k p) n -> p k n", p=128)
    stg = [P.sb([128, blk], F32, "wstg") for _ in range(2)]
    i = 0
    for k in range(nk):
        for c0 in range(0, ncols, blk):
            c1 = min(ncols, c0 + blk)
            s = stg[i % 2]
            P.dma(s[:, 0:c1 - c0], src[:, k, c0:c1])
            eng = 'dve' if i % 2 == 0 else 'pool'
            if gcol is not None:
                P.ts(dst[:, k, c0:c1], s[:, 0:c1 - c0], gcol[:, k:k + 1], ALU.mult, eng=eng)
            else:
                P.copy(dst[:, k, c0:c1], s[:, 0:c1 - c0], eng=eng)
            i += 1


def stream_weight_blocks(P, dst, src_dram, gcol, nk, blocks, stg):
    src = src_dram.r("(k p) n -> p k n", p=128)
    views = []
    i = 0
    for (c0, c1) in blocks:
        v = V(P._newbuf("wblk"), dst.ap[:, :, c0:c1])
        views.append(v)
        for k in range(nk):
            s_ = stg[i % len(stg)]
            P.dma(s_[:, 0:c1 - c0], src[:, k, c0:c1])
            eng = 'dve' if i % 2 == 0 else 'pool'
            if gcol is not None:
                P.ts(v[:, k, :], s_[:, 0:c1 - c0], gcol[:, k:k + 1], ALU.mult, eng=eng)
            else:
                P.copy(v[:, k, :], s_[:, 0:c1 - c0], eng=eng)
            i += 1
    return views


def rsqrt_ps(P, out, src, scale, eps):
    P.ts(out, src, scale, ALU.mult, eps, ALU.add)
    P.act(out, out, AF.Sqrt)
    P.recip(out, out)


def rms_tile(P, xt, hT, sq, rstd, ones, nk, n, width):
    P.act(sq, xt, AF.Square)
    ps = P.ps()
    for k in range(nk):
        P.mm(ps[:, 0:width], ones, sq[:, k, :], start=(k == 0), stop=(k == nk - 1))
    rsqrt_ps(P, rstd, ps[:, 0:width], 1.0 / n, 1e-6)
    for k in range(nk):
        P.tt(hT[:, k, :], xt[:, k, :], rstd, ALU.mult)


def stage_P(P, l, Dm, xT):
    P.sb_off = SB_BASE
    npre = P.sb([128, 8], F32, "npre")
    P.dma(npre, Dm[f'npre{l}'])
    ones = P.sb([128, 128], F32, "ones")
    P.memset(ones, 1.0)
    wp = P.sb([128, 8, NP_ROWS], BF16, "wp")
    wt = P.sb([128, 8, NT_COLS], BF16, "wt")
    wf = P.sb([128, 8, 644], F32, "wf")
    wfsrc = Dm[f'wpf{l}'].r("(k p) n -> p k n", p=128)
    for k in range(8):
        P.dma(wf[:, k, :], wfsrc[:, k, :])
    for k in range(8):
        P.ts(wf[:, k, :], wf[:, k, :], npre[:, k:k + 1], ALU.mult, eng=('dve' if k % 2 else 'pool'))
    WB = 640
    stg = [P.sb([128, WB], F32, "wstg") for _ in range(3)]
    wtb = stream_weight_blocks(P, wt, Dm[f'wt{l}'], npre, 8, [(0, NT_COLS)], stg)[0]
    wpb = stream_weight_blocks(P, wp, Dm[f'wp{l}'], npre, 8,
                               [(c0, min(c0 + WB, NP_ROWS)) for c0 in range(0, NP_ROWS, WB)], stg)
    xts = [P.sb([128, 8, TT], F32, "xt") for _ in range(2)]
    sq = P.sb([128, 8, TT], F32, "sq")
    hTs = [P.sb([128, 8, TT], BF16, "hT") for _ in range(2)]
    rstd = P.sb([128, TT], F32, "rstd")
    ostg = [P.sb([128, 2, TT], F32, "ostg") for _ in range(2)]
    tstg = [P.sb([128, NT_COLS], F32, "tstg") for _ in range(2)]
    xsrc = xT.r("(k p) t -> p k t", p=128)
    cdst = Dm['colsT'].r("(c p) t -> p c t", p=128)
    ctok = Dm['colsTok']
    for tt in range(NTT):
        t0 = tt * TT
        xt, hT = xts[tt % 2], hTs[tt % 2]
        P.dma(xt, xsrc[:, :, t0:t0 + TT])
        rms_tile(P, xt, hT, sq, rstd, ones, 8, D, TT)
        for k in range(8):
            P.tt(sq[:, k, :], xt[:, k, :], rstd, ALU.mult, eng='pool')
        for c in range(42):
            ps = P.ps()
            for k in range(8):
                if C_IQ <= c <= C_IK:
                    P.mm(ps, wf[:, k, (c - C_IQ) * 128:(c - C_IQ + 1) * 128], sq[:, k, :], start=(k == 0), stop=(k == 7))
                else:
                    P.mm(ps, wpb[c // 5][:, k, (c % 5) * 128:(c % 5 + 1) * 128], hT[:, k, :], start=(k == 0), stop=(k == 7))
            stg_ = ostg[(c // 2) % 2]
            if c % 3 == 2:
                P.copy(stg_[:, c % 2, :], ps, eng='dve')
            else:
                P.act(stg_[:, c % 2, :], ps, AF.Copy)
            if c % 2 == 1:
                P.dma(cdst[:, c - 1:c + 1, t0:t0 + TT], stg_)
        for s in range(4):
            ps = P.ps()
            ps2 = P.ps()
            for k in range(8):
                P.mm(ps, hT[:, k, s * 128:(s + 1) * 128], wtb[:, k, 0:512], start=(k == 0), stop=(k == 7))
            for k in range(8):
                P.mm(ps2[:, 0:4], sq[:, k, s * 128:(s + 1) * 128], wf[:, k, 640:644], start=(k == 0), stop=(k == 7))
            ts_ = tstg[s % 2]
            P.act(ts_[:, 0:512], ps, AF.Copy)
            P.copy(ts_[:, 512:516], ps2[:, 0:4], eng='dve')
            P.dma(ctok[t0 + s * 128:t0 + (s + 1) * 128, :], ts_)
    P.barrier()


BR_NAMES = ['rwkv', 'dsa', 'ret', 's5', 'xatt']


def stage_M(P, l, Dm, xT, xT_out):
    P.sb_off = SB_BASE
    npre = P.sb([128, 8], F32, "npre")
    npost = P.sb([128, 8], F32, "npost")
    P.dma(npre, Dm[f'npre{l}'])
    P.dma(npost, Dm[f'npost{l}'])
    ones = P.sb([128, 128], F32, "ones")
    P.memset(ones, 1.0)
    wg = P.sb([128, 8, 5120], BF16, "wg")
    wbr = P.sb([128, 10, 1024], BF16, "wbr")
    wout = P.sb([128, 8, 1024], BF16, "wout")
    stg = [P.sb([128, 1024], F32, "wstg") for _ in range(3)]
    wbrb = stream_weight_blocks(P, wbr, Dm[f'wbr{l}'], None, 10, [(0, 1024)], stg)[0]
    wgb = stream_weight_blocks(P, wg, Dm[f'wg{l}'], npre, 8, [(i * 1024, (i + 1) * 1024) for i in range(5)], stg)
    woutb = stream_weight_blocks(P, wout, Dm[f'wout{l}'], None, 8, [(0, 1024)], stg)[0]
    xt = P.sb([128, 8, TT], F32, "xt")
    sq = P.sb([128, 8, TT], F32, "sq")
    hT = P.sb([128, 8, TT], BF16, "hT")
    rstd = P.sb([128, TT], F32, "rstd")
    yts = [P.sb([128, 2, TT], BF16, f"y{i}") for i in range(5)]
    sg = [P.sb([128, TT], F32, "sg") for _ in range(2)]
    term = [P.sb([128, TT], F32, "term") for _ in range(2)]
    macc = P.sb([128, TT], F32, "macc")
    mT = P.sb([128, 8, TT], BF16, "mT")
    osb = sq
    osq = P.sb([128, TT], F32, "osq")
    xsrc = xT.r("(k p) t -> p k t", p=128)
    xdst = xT_out.r("(k p) t -> p k t", p=128)
    for tt in range(NTT):
        t0 = tt * TT
        P.dma(xt, xsrc[:, :, t0:t0 + TT])
        for i in range(5):
            P.dma(yts[i], Dm[f'yT_{BR_NAMES[i]}'].r("(c p) t -> p c t", p=128)[:, :, t0:t0 + TT])
        rms_tile(P, xt, hT, sq, rstd, ones, 8, D, TT)
        j = 0
        for i in range(5):
            for dc in range(8):
                psg = P.ps()
                for k in range(8):
                    P.mm(psg, wgb[i][:, k, dc * 128:(dc + 1) * 128], hT[:, k, :], start=(k == 0), stop=(k == 7))
                psb = P.ps()
                for kk in range(2):
                    P.mm(psb, wbrb[:, i * 2 + kk, dc * 128:(dc + 1) * 128], yts[i][:, kk, :],
                         start=(kk == 0), stop=(kk == 1))
                s_, t_ = sg[j % 2], term[j % 2]
                j += 1
                P.act(s_, psg, AF.Sigmoid)
                if i == 0:
                    P.tt(sq[:, dc, :], s_, psb, ALU.mult)
                elif i < 4:
                    P.tt(t_, s_, psb, ALU.mult)
                    P.tt(sq[:, dc, :], sq[:, dc, :], t_, ALU.add, eng='pool')
                else:
                    P.tt(t_, s_, psb, ALU.mult)
                    P.tt(mT[:, dc, :], sq[:, dc, :], t_, ALU.add, eng='pool')
        pss = P.ps_acc()
        for ec in range(8):
            ps = P.ps()
            for k in range(8):
                P.mm(ps, woutb[:, k, ec * 128:(ec + 1) * 128], mT[:, k, :], start=(k == 0), stop=(k == 7))
            P.act(osb[:, ec, :], ps, AF.Copy)
            P.act(osq, ps, AF.Square)
            P.mm(pss, ones, osq, start=(ec == 0), stop=(ec == 7))
        rsqrt_ps(P, rstd, pss, 1.0 / D, 1e-6)
        for ec in range(8):
            P.stt(osb[:, ec, :], osb[:, ec, :], npost[:, ec:ec + 1], rstd, ALU.mult, ALU.mult)
            P.tt(xt[:, ec, :], xt[:, ec, :], osb[:, ec, :], ALU.add, eng='pool')
        P.dma(xdst[:, :, t0:t0 + TT], xt)
    P.barrier()


def stage_X(P, l, Dm):
    P.sb_off = SB_BASE
    nmem = P.sb([128, 8], F32, "nmem")
    P.dma(nmem, Dm[f'nmem{l}'])
    ones = P.sb([128, 128], F32, "ones")
    P.memset(ones, 1.0)
    wm = P.sb([128, 8, 512], BF16, "wm")
    m0 = P.sb_off
    load_weight_bf16(P, wm, Dm[f'wmem{l}'], nmem, 8, 512, blk=512)
    P.barrier()
    P.sb_off = m0
    mt = P.sb([128, 8, 256], F32, "mt")
    msq = P.sb([128, 8, 256], F32, "msq")
    mh = P.sb([128, 8, 256], BF16, "mh")
    mr = P.sb([128, 256], F32, "mr")
    P.dma(mt, Dm['memT'].r("(k p) m -> p k m", p=128))
    rms_tile(P, mt, mh, msq, mr, ones, 8, D, 256)
    kmT = [P.sb([128, 256], BF16, "kmT") for _ in range(2)]
    for c in range(2):
        ps = P.ps()
        for k in range(8):
            P.mm(ps[:, 0:256], wm[:, k, c * 128:(c + 1) * 128], mh[:, k, :], start=(k == 0), stop=(k == 7))
        P.copy(kmT[c], ps[:, 0:256])
    vpad = [[P.sb([128, 128], BF16, "vpad") for _ in range(4)] for _ in range(2)]
    opad = [P.sb([128, 128], BF16, "opad") for _ in range(2)]
    for hh in range(2):
        P.memset(opad[hh], 0.0)
        P.memset(opad[hh][:, hh * 64:(hh + 1) * 64], 1.0)
    for mc in range(2):
        ps = P.ps()
        for k in range(8):
            P.mm(ps[:, 0:256], mh[:, k, mc * 128:(mc + 1) * 128], wm[:, k, 256:512], start=(k == 0), stop=(k == 7))
        for h in range(4):
            hh = h % 2
            P.memset(vpad[mc][h], 0.0)
            P.copy(vpad[mc][h][:, hh * 64:(hh + 1) * 64], ps[:, h * 64:(h + 1) * 64])
    qf = P.sb([128, 2, TT], F32, "qf")
    gf = P.sb([128, 2, TT], F32, "gf")
    qb = P.sb([128, 2, TT], BF16, "qb")
    E = [[P.sb([128, TT], BF16, "E") for _ in range(2)] for _ in range(2)]
    rs = P.sb([128, TT], F32, "rs")
    o = P.sb([128, TT], F32, "o")
    sgl = P.sb([128, TT], F32, "sgl")
    yst = P.sb([128, 2, TT], BF16, "yst")
    csrc = Dm['colsT'].r("(c p) t -> p c t", p=128)
    ydst = Dm['yT_xatt'].r("(c p) t -> p c t", p=128)
    for tt in range(NTT):
        t0 = tt * TT
        P.dma(qf, csrc[:, C_XQ:C_XQ + 2, t0:t0 + TT])
        P.dma(gf, csrc[:, C_XG:C_XG + 2, t0:t0 + TT])
        P.copy(qb, qf)
        for p in range(2):
            for hh in range(2):
                for mc in range(2):
                    ps = P.ps()
                    P.mm(ps, kmT[p][hh * 64:(hh + 1) * 64, mc * 128:(mc + 1) * 128],
                         qb[hh * 64:(hh + 1) * 64, p, :])
                    P.act(E[hh][mc], ps, AF.Exp, scale=0.125)
            pso = P.ps()
            pss = P.ps()
            n = 0
            for hh in range(2):
                for mc in range(2):
                    P.mm(pso, vpad[mc][2 * p + hh], E[hh][mc], start=(n == 0), stop=(n == 3))
                    n += 1
            n = 0
            for hh in range(2):
                for mc in range(2):
                    P.mm(pss, opad[hh], E[hh][mc], start=(n == 0), stop=(n == 3))
                    n += 1
            P.recip(rs, pss)
            P.tt(o, pso, rs, ALU.mult)
            P.act(sgl, gf[:, p, :], AF.Silu)
            P.tt(yst[:, p, :], o, sgl, ALU.mult)
        P.dma(ydst[:, :, t0:t0 + TT], yst)
    P.barrier()


def dram_specs():
    sp = {
        'xT': ([D, S], F32, 'in'), 'memT': ([D, 256], F32, 'in'), 'pos': ([1, S], I32, 'in'),
        'colsT': ([NP_ROWS, S], F32, 'scratch'), 'colsTok': ([S, NT_COLS], F32, 'scratch'),
        'xT1': ([D, S], F32, 'scratch'),
    }
    for n in BR_NAMES:
        sp[f'yT_{n}'] = ([256, S], BF16, 'scratch')
    sp['iqR'] = ([256, S], F32, 'scratch')
    sp['qR'] = ([256, S], F32, 'scratch')
    sp['ropeC'] = ([64, S], F32, 'scratch')
    sp['ropeS'] = ([64, S], F32, 'scratch')
    sp['ropeconst'] = ([64, 2], F32, 'in')
    sp['ident'] = ([128, 128], F32, 'in')
    sp['ret_idT'] = ([128, 4, 128], F32, 'in')
    sp['ret_qd'] = ([64, 4, 128], F32, 'in')
    sp['ret_kd'] = ([128, 4], F32, 'in')
    sp['ret_cd'] = ([64, 256], F32, 'in')
    sp['s5mask'] = ([128, 8, 8], F32, 'in')
    sp['rw_masks'] = ([64, 3, 64], F32, 'in')
    sp['dsa_cb'] = ([128, 128], F32, 'in')
    sp['dsa_pw'] = ([128, 32], F32, 'in')
    for l in range(2):
        sp[f'rwprm{l}'] = ([64, 8, 4], F32, 'in')
        sp[f'rwmu{l}'] = ([64, 18], F32, 'in')
        sp[f'rww2{l}'] = ([64, 256], F32, 'in')
        sp[f'rwa2{l}'] = ([64, 256], F32, 'in')
    sp['s5tau'] = ([128, 512], F32, 'in')
    for l in range(2):
        sp[f'retgn{l}'] = ([64, 4], F32, 'in')
        sp[f's5lam{l}'] = ([128, 8, 3], F32, 'in')
        sp[f's5b{l}'] = ([128, 8, 2, 16], F32, 'in')
        sp[f's5c{l}'] = ([128, 8, 2, 16], F32, 'in')
        sp[f's5d{l}'] = ([128, 2], F32, 'in')
        sp[f's5wglu{l}'] = ([256, 256], F32, 'in')
    for l in range(2):
        sp[f'wp{l}'] = ([D, NP_ROWS], F32, 'in')
        sp[f'wt{l}'] = ([D, NT_COLS], F32, 'in')
        sp[f'wpf{l}'] = ([D, 644], F32, 'in')
        sp[f'wg{l}'] = ([D, 5120], F32, 'in')
        sp[f'wbr{l}'] = ([1280, D], F32, 'in')
        sp[f'wout{l}'] = ([D, D], F32, 'in')
        sp[f'wmem{l}'] = ([D, 512], F32, 'in')
        for n in ['npre', 'npost', 'nmem']:
            sp[f'{n}{l}'] = ([128, 8], F32, 'in')
    return sp


def host_inputs(inputs, b):
    f_idx, t_idx = proj_col_indices()
    d = {}
    d['xT'] = np.ascontiguousarray(inputs['x'][b].T)
    d['memT'] = np.ascontiguousarray(inputs['mem'][b].T)
    d['pos'] = np.ascontiguousarray(inputs['positions'][b][None, :]).astype(np.int32)
    pk = lambda v: np.ascontiguousarray(v.reshape(8, 128).T)
    jj = np.arange(64)
    inv = (10000.0 ** (-(np.arange(32, dtype=np.float32)) / 32)).astype(np.float32)
    d['ropeconst'] = np.stack([inv[jj % 32], np.where(jj < 32, -1.0, 1.0)], 1).astype(np.float32)
    d['ident'] = np.eye(128, dtype=np.float32)
    d['ret_idT'], d['ret_qd'], d['ret_kd'], d['ret_cd'] = ret_consts()
    ii = np.arange(64)
    rm = np.zeros((64, 3, 64), np.float32)
    rm[:, 0, :] = (ii[None, :] > ii[:, None])
    rm[:, 1, :] = (ii[None, :] >= ii[:, None])
    rm[:, 2, :] = (ii[None, :] < ii[:, None])
    d['rw_masks'] = rm
    i128 = np.arange(128)
    d['dsa_pw'] = np.ascontiguousarray(np.broadcast_to((0.5 ** np.arange(1, 33, dtype=np.float64)).astype(np.float32)[None, :], (128, 32)))
    d['dsa_cb'] = np.where(i128[None, :] <= i128[:, None], 0.0, -1e30).astype(np.float32)
    for l in range(2):
        hd = lambda v: np.ascontiguousarray(v.reshape(4, 64).T)
        z = np.zeros((64, 4), np.float32)
        d[f'rwprm{l}'] = np.ascontiguousarray(np.stack([hd(inputs['rwkv_w0'][l]), hd(inputs['rwkv_a0'][l]), hd(inputs['rwkv_k_k'][l]),
                                   hd(inputs['rwkv_k_a'][l]), hd(inputs['rwkv_r_k'][l].reshape(256)), hd(inputs['rwkv_lnx_w'][l]),
                                   hd(inputs['rwkv_lnx_b'][l]), z], 1).astype(np.float32))
        d[f'rwmu{l}'] = np.ascontiguousarray(inputs['rwkv_mu'][l].reshape(18, 64).T)
        d[f'rww2{l}'] = np.ascontiguousarray(inputs['rwkv_w2'][l])
        d[f'rwa2{l}'] = np.ascontiguousarray(inputs['rwkv_a2'][l])
    sidx = np.arange(128)
    mk = np.zeros((128, 8, 8), np.float32)
    for j in range(8):
        mk[sidx, j, (2 * j + sidx // 64) % 8] = 1.0
    d['s5mask'] = mk
    d['s5tau'] = np.ascontiguousarray(np.broadcast_to(np.arange(1, 513, dtype=np.float32)[None, :], (128, 512)))
    sj = lambda a: np.ascontiguousarray(a.reshape((8, 128) + a.shape[1:]).swapaxes(0, 1))
    for l in range(2):
        d[f'retgn{l}'] = np.ascontiguousarray(inputs['ret_gn_w'][l].reshape(4, 64).T)
        lam3 = np.stack([inputs['s5_lam_re'][l].reshape(1024), inputs['s5_lam_im'][l].reshape(1024),
                         np.repeat(inputs['s5_log_dt'][l], 64)], 1).astype(np.float32)
        d[f's5lam{l}'] = sj(lam3)
        d[f's5b{l}'] = sj(np.stack([inputs['s5_b_re'][l].reshape(1024, 16), inputs['s5_b_im'][l].reshape(1024, 16)], 1))
        ct = lambda c: np.ascontiguousarray(c.transpose(0, 2, 1)).reshape(1024, 16)
        d[f's5c{l}'] = sj(np.stack([ct(inputs['s5_c_re'][l]), ct(inputs['s5_c_im'][l])], 1))
        d[f's5d{l}'] = np.ascontiguousarray(inputs['s5_d'][l].reshape(2, 128).T)
        d[f's5wglu{l}'] = np.ascontiguousarray(inputs['s5_w_glu'][l])
    for l in range(2):
        w = inputs['w_in'][l]
        d[f'wp{l}'] = np.ascontiguousarray(w[:, f_idx])
        d[f'wt{l}'] = np.ascontiguousarray(w[:, t_idx])
        d[f'wpf{l}'] = np.ascontiguousarray(w[:, np.concatenate([f_idx[C_IQ * 128:(C_IK + 1) * 128], t_idx[512:516]])])
        d[f'wg{l}'] = np.ascontiguousarray(w[:, B_GATE:B_GATE + 5120])
        d[f'wbr{l}'] = np.ascontiguousarray(inputs['w_branch'][l].reshape(1280, D))
        d[f'wout{l}'] = np.ascontiguousarray(inputs['w_out'][l])
        d[f'wmem{l}'] = np.ascontiguousarray(inputs['w_mem_kv'][l])
        d[f'npre{l}'] = pk(inputs['norm_pre'][l])
        d[f'npost{l}'] = pk(inputs['norm_post'][l])
        d[f'nmem{l}'] = pk(inputs['norm_mem'][l])
    return d


STAGE_FNS = {}


def build(plan, ext_in=(), ext_out=()):
    nc = bass.Bass("TRN2", target_bir_lowering=False)
    P = Prog(nc)
    Dm = {}
    used_in = []
    for name, (shape, dtype, role) in dram_specs().items():
        if role == 'in' or name in ext_in:
            kind = "ExternalInput"
            used_in.append(name)
        elif name in ext_out:
            kind = "ExternalOutput"
        else:
            kind = "Internal"
        Dm[name] = P.dram(name, shape, dtype, kind=kind)
    Dm['outT'] = P.dram('outT', [D, S], F32, kind="ExternalOutput")
    for st, l in plan:
        xin = Dm['xT'] if l == 0 else Dm['xT1']
        xout = Dm['xT1'] if l == 0 else Dm['outT']
        if st == 'P':
            stage_P(P, l, Dm, xin)
        elif st == 'M':
            stage_M(P, l, Dm, xin, xout)
        elif st == 'X':
            stage_X(P, l, Dm)
        else:
            STAGE_FNS[st](P, l, Dm)
    P.finish()
    return nc, P, used_in


def sin_reduced(P, out, ang, kq, ki, m1):
    P.ts(kq, ang, 1.0 / (2 * math.pi), ALU.mult)
    P.copy(ki, kq)
    P.copy(kq, ki)
    P.stt(ang, kq, -2 * math.pi, ang, ALU.mult, ALU.add)
    P.ts(m1, ang, math.pi, ALU.is_gt, -2 * math.pi, ALU.mult)
    P.tt(ang, ang, m1, ALU.add)
    P.ts(m1, ang, -math.pi, ALU.is_lt, 2 * math.pi, ALU.mult)
    P.tt(ang, ang, m1, ALU.add)
    P.act(out, ang, AF.Sin)


def stage_R(P, l, Dm):
    P.sb_off = SB_BASE
    W = 2048
    rc = P.sb([64, 2], F32, "rc")
    P.dma(rc, Dm['ropeconst'])
    posi = P.sb([64, W], I32, "posi")
    posf = P.sb([64, W], F32, "posf")
    ang = P.sb([64, W], F32, "ang")
    kq = P.sb([64, W], F32, "kq")
    ki = P.sb([64, W], I32, "ki")
    m1 = P.sb([64, W], F32, "m1")
    o = P.sb([64, W], F32, "o")
    for half in range(S // W):
        sl = slice(half * W, (half + 1) * W)
        P.dma(posi, Dm['pos'][:, sl].m(lambda x: x.to_broadcast([64, W])))
        P.copy(posf, posi)
        P.ts(ang, posf, rc[:, 0:1], ALU.mult)
        sin_reduced(P, o, ang, kq, ki, m1)
        P.ts(o, o, rc[:, 1:2], ALU.mult)
        P.dma(Dm['ropeS'][:, sl], o)
        P.ts(ang, posf, rc[:, 0:1], ALU.mult, math.pi / 2, ALU.add)
        sin_reduced(P, o, ang, kq, ki, m1)
        P.dma(Dm['ropeC'][:, sl], o)
    P.barrier()


def rope_heads(P, dst, Dm, c_base, c_swap, ropeC, ropeS, nheads=4, scale=None, dram_dst=None):
    a = P.sb([64, nheads, TT], F32, "ra")
    b = P.sb([64, nheads, TT], F32, "rb")
    if dram_dst is not None:
        ro = [P.sb([64, nheads, TT], F32, "ro") for _ in range(2)]
    src = Dm['colsT']
    for tt in range(NTT):
        sl = slice(tt * TT, (tt + 1) * TT)
        rb_ = c_base * 128 if isinstance(c_base, int) else c_base[0]
        rs_ = c_swap * 128 if isinstance(c_swap, int) else c_swap[0]
        P.dma(a, src[rb_:rb_ + nheads * 64, sl].r("(h d) t -> d h t", d=64))
        P.dma(b, src[rs_:rs_ + nheads * 64, sl].r("(h d) t -> d h t", d=64))
        cb = ropeC[:, sl].m(lambda x: x.unsqueeze(1).to_broadcast([64, nheads, TT]))
        sb_ = ropeS[:, sl].m(lambda x: x.unsqueeze(1).to_broadcast([64, nheads, TT]))
        P.tt(a, a, cb, ALU.mult)
        P.tt(b, b, sb_, ALU.mult, eng='pool')
        if dram_dst is None:
            P.tt(dst[:, :, sl], a, b, ALU.add)
        else:
            o_ = ro[tt % 2]
            P.tt(o_, a, b, ALU.add)
            P.dma(dram_dst.r("(h d) t -> d h t", d=64)[:, :, sl], o_)


RET_LOGG = [math.log(1.0 - math.exp(v)) for v in np.linspace(math.log(1.0 / 32), math.log(1.0 / 512), 4)]


def ret_consts():
    j = np.arange(128, dtype=np.float64)
    idT = np.zeros((128, 4, 128), np.float32)
    qd = np.zeros((64, 4, 128), np.float32)
    kd = np.zeros((128, 4), np.float32)
    cd = np.zeros((64, 256), np.float32)
    for h in range(4):
        lg = RET_LOGG[h]
        rel = j[None, :] - j[:, None]
        idT[:, h, :] = np.where(rel >= 0, np.exp(lg * np.maximum(rel, 0.0)), 0.0) * 0.125
        qd[:, h, :] = np.exp(lg * (j + 1.0))[None, :]
        kd[:, h] = np.exp(lg * (127.0 - j)) * 0.125
        cd[:, h * 64:(h + 1) * 64] = math.exp(lg * 128)
    return idT, qd, kd, cd


def stage_RET(P, l, Dm):
    P.sb_off = SB_BASE
    ropeC = P.sb([64, S], F32, "ropeC")
    ropeS = P.sb([64, S], F32, "ropeS")
    P.dma(ropeC, Dm['ropeC'])
    P.dma(ropeS, Dm['ropeS'])
    idT = P.sb([128, 4, 128], F32, "idT")
    qd = P.sb([64, 4, 128], F32, "qd")
    kd = P.sb([128, 4], F32, "kd")
    cd = P.sb([64, 256], F32, "cd")
    gn = P.sb([64, 4], F32, "gn")
    identb = P.sb([128, 128], BF16, "identb")
    identf = P.sb([128, 128], F32, "identf")
    ones64 = P.sb([64, 64], F32, "ones64")
    P.dma(idT, Dm['ret_idT'])
    P.dma(qd, Dm['ret_qd'])
    P.dma(kd, Dm['ret_kd'])
    P.dma(cd, Dm['ret_cd'])
    P.dma(gn, Dm[f'retgn{l}'])
    P.dma(identf, Dm['ident'])
    P.copy(identb, identf)
    P.memset(ones64, 1.0 / 64)
    qT = P.sb([64, 4, S], BF16, "qT")
    kT = P.sb([64, 4, S], BF16, "kT")
    qdT = P.sb([64, 4, S], BF16, "qdT")
    m0 = P.sb_off
    rope_heads(P, qT, Dm, C_RQ, C_RQS, ropeC, ropeS)
    rope_heads(P, kT, Dm, C_RK, C_RKS, ropeC, ropeS)
    for c in range(32):
        cs = slice(c * 128, (c + 1) * 128)
        P.tt(qdT[:, :, cs], qT[:, :, cs], qd, ALU.mult, eng=('dve' if c % 2 else 'pool'))
    P.barrier()
    P.sb_off = m0
    Vt = P.sb([128, 32, 256], BF16, "Vt")
    Kd = P.sb([128, 32, 256], BF16, "Kd")
    vst = [P.sb([128, 4, 256], F32, "vst") for _ in range(2)]
    vsrc = Dm['colsTok'].r("(c p) n -> p c n", p=128)
    for i in range(8):
        v_ = vst[i % 2]
        P.dma(v_, vsrc[:, i * 4:(i + 1) * 4, 256:512])
        P.copy(Vt[:, i * 4:(i + 1) * 4, :], v_, eng=('dve' if i % 2 else 'pool'))
    for c in range(32):
        cs = slice(c * 128, (c + 1) * 128)
        ps = P.ps()
        psb = ps.bitcast(BF16)
        for h in range(4):
            P.transpose(psb[:, h * 64:(h + 1) * 64], kT[:, h, cs], identb[0:64, 0:64])
        P.tt(Kd[:, c, :].r("p (h d) -> p h d", h=4), psb[:, 0:256].r("p (h d) -> p h d", h=4),
             kd.m(lambda x: x.unsqueeze(2).to_broadcast([128, 4, 64])), ALU.mult)
    R = P.sb([64, 256], F32, "R")
    Rb = P.sb([64, 256], BF16, "Rb")
    P.memset(R, 0.0)
    P.memset(Rb, 0.0)
    AT = [P.sb([128, 4, 128], BF16, "AT") for _ in range(2)]
    Osb = P.sb([64, 512], F32, "Osb")
    dd = P.sb([64, 512], F32, "dd")
    dsq = P.sb([64, 512], F32, "dsq")
    rstd = P.sb([64, 512], F32, "rstd")
    gt = [P.sb([64, 4, 128], F32, "gt") for _ in range(2)]
    sg = P.sb([64, 4, 128], F32, "sg")
    yo = [P.sb([64, 4, 128], BF16, "yo") for _ in range(2)]
    gsrc = Dm['colsT'][C_RG * 128:C_RG * 128 + 256, :].r("(h d) t -> d h t", d=64)
    ydst = Dm['yT_ret'].r("(h d) t -> d h t", d=64)
    for c in range(32):
        cs = slice(c * 128, (c + 1) * 128)
        g_ = gt[c % 2]
        P.dma(g_, gsrc[:, :, cs])
        psA = P.ps()
        for h in range(4):
            P.mm(psA[:, h * 128:(h + 1) * 128], kT[:, h, cs], qT[:, h, cs])
        at = AT[c % 2]
        P.tt(at, psA.r("p (h q) -> p h q", h=4), idT, ALU.mult)
        psO = P.ps()
        for h in range(4):
            P.mm(psO[0:64, h * 128:(h + 1) * 128], Vt[:, c, h * 64:(h + 1) * 64], at[:, h, :], start=True, stop=False, inc=False)
            P.mm(psO[0:64, h * 128:(h + 1) * 128], Rb[:, h * 64:(h + 1) * 64], qdT[:, h, cs], start=False, stop=True)
        psKV = P.ps()
        for h in range(4):
            P.mm(psKV[0:64, h * 64:(h + 1) * 64], Kd[:, c, h * 64:(h + 1) * 64], Vt[:, c, h * 64:(h + 1) * 64])
        P.tt(R, R, cd, ALU.mult)
        P.tt(R, R, psKV[0:64, 0:256], ALU.add)
        P.copy(Rb, R, eng='pool')
        P.act(Osb, psO[0:64, :], AF.Copy)
        psM = P.ps()
        P.mm(psM[0:64, :], ones64, Osb)
        P.tt(dd, Osb, psM[0:64, :], ALU.subtract)
        P.act(dsq, dd, AF.Square)
        psV = P.ps()
        P.mm(psV[0:64, :], ones64, dsq)
        P.ts(rstd, psV[0:64, :], 1e-6, ALU.add)
        P.act(rstd, rstd, AF.Sqrt)
        P.recip(rstd, rstd)
        P.tt(dd, dd, rstd, ALU.mult)
        P.tt(dd.r("p (h q) -> p h q", h=4), dd.r("p (h q) -> p h q", h=4),
             gn.m(lambda x: x.unsqueeze(2).to_broadcast([64, 4, 128])), ALU.mult)
        P.act(sg, g_, AF.Silu)
        y_ = yo[c % 2]
        P.tt(y_, dd.r("p (h q) -> p h q", h=4), sg, ALU.mult)
        P.dma(ydst[:, :, cs], y_)
    P.barrier()


STAGE_FNS['R'] = stage_R
STAGE_FNS['RET'] = stage_RET


def stage_S5(P, l, Dm):
    P.sb_off = SB_BASE
    W = TT
    lam = P.sb([128, 8, 3], F32, "lam")
    bsb = P.sb([128, 8, 2, 16], F32, "bsb")
    csb = P.sb([128, 8, 2, 16], F32, "csb")
    msk = P.sb([128, 8, 8], F32, "msk")
    tau = P.sb([128, W], F32, "tau")
    dsk = P.sb([128, 2], F32, "dsk")
    identf = P.sb([128, 128], F32, "identf")
    P.dma(lam, Dm[f's5lam{l}'])
    P.dma(bsb, Dm[f's5b{l}'])
    P.dma(csb, Dm[f's5c{l}'])
    P.dma(msk, Dm['s5mask'])
    P.dma(tau, Dm['s5tau'])
    P.dma(dsk, Dm[f's5d{l}'])
    P.dma(identf, Dm['ident'])
    wglu = P.sb([128, 2, 256], BF16, "wglu")
    cosT = P.sb([128, 8, W], F32, "cosT")
    sinT = P.sb([128, 8, W], F32, "sinT")
    mag = P.sb([128, 8], F32, "mag")
    BT = P.sb([128, 8, 2, 128], BF16, "BT")
    CX = P.sb([128, 8, 2, 128], BF16, "CX")
    m0 = P.sb_off
    load_weight_bf16(P, wglu, Dm[f's5wglu{l}'], None, 2, 256, blk=256)
    lr = P.sb([128, 8], F32, "lr")
    li = P.sb([128, 8], F32, "li")
    dt = P.sb([128, 8], F32, "dt")
    th = P.sb([128, 8], F32, "th")
    P.ts(lr, lam[:, :, 0], -1e-4, ALU.min)
    P.copy(li, lam[:, :, 1])
    P.act(dt, lam[:, :, 2], AF.Exp)
    P.tt(th, li, dt, ALU.mult)
    P.tt(mag, lr, dt, ALU.mult)
    P.act(mag, mag, AF.Exp)
    ang = P.sb([128, W], F32, "ang")
    kq = P.sb([128, W], F32, "kq")
    ki = P.sb([128, W], I32, "ki")
    m1 = P.sb([128, W], F32, "m1")
    for j in range(8):
        P.ts(ang, tau, th[:, j:j + 1], ALU.mult)
        sin_reduced(P, sinT[:, j, :], ang, kq, ki, m1)
        P.ts(ang, tau, th[:, j:j + 1], ALU.mult, math.pi / 2, ALU.add)
        sin_reduced(P, cosT[:, j, :], ang, kq, ki, m1)
    abr = P.sb([128, 8], F32, "abr")
    abi = P.sb([128, 8], F32, "abi")
    den = P.sb([128, 8], F32, "den")
    t8 = P.sb([128, 8], F32, "t8")
    fre = P.sb([128, 8], F32, "fre")
    fim = P.sb([128, 8], F32, "fim")
    P.tt(abr, mag, cosT[:, :, 0], ALU.mult)
    P.tt(abi, mag, sinT[:, :, 0], ALU.mult)
    P.ts(abr, abr, -1.0, ALU.add)
    P.tt(den, lr, lr, ALU.mult)
    P.tt(t8, li, li, ALU.mult)
    P.tt(den, den, t8, ALU.add)
    P.recip(den, den)
    P.tt(fre, abr, lr, ALU.mult)
    P.tt(t8, abi, li, ALU.mult)
    P.tt(fre, fre, t8, ALU.add)
    P.tt(fre, fre, den, ALU.mult)
    P.tt(fim, abi, lr, ALU.mult)
    P.tt(t8, abr, li, ALU.mult)
    P.tt(fim, fim, t8, ALU.subtract)
    P.tt(fim, fim, den, ALU.mult)
    bb = P.sb([128, 8, 2, 16], F32, "bb")
    tb = P.sb([128, 8, 16], F32, "tb")
    bc16 = lambda v: v.m(lambda x: x.unsqueeze(2).to_broadcast([128, 8, 16]))
    P.tt(bb[:, :, 0, :], bsb[:, :, 0, :], bc16(fre), ALU.mult)
    P.tt(tb, bsb[:, :, 1, :], bc16(fim), ALU.mult)
    P.tt(bb[:, :, 0, :], bb[:, :, 0, :], tb, ALU.subtract)
    P.tt(bb[:, :, 1, :], bsb[:, :, 1, :], bc16(fre), ALU.mult)
    P.tt(tb, bsb[:, :, 0, :], bc16(fim), ALU.mult)
    P.tt(bb[:, :, 1, :], bb[:, :, 1, :], tb, ALU.add)
    P.ts(csb[:, :, 1, :], csb[:, :, 1, :], -1.0, ALU.mult)
    bx = P.sb([128, 8, 16], F32, "bx")
    for j in range(8):
        mj = msk[:, j, :].m(lambda x: x.unsqueeze(2).to_broadcast([128, 8, 16]))
        for ri in range(2):
            P.tt(bx, bb[:, j, ri, :].m(lambda x: x.unsqueeze(1).to_broadcast([128, 8, 16])), mj, ALU.mult)
            ps = P.ps()
            P.transpose(ps[:, 0:128], bx.r("p a b -> p (a b)"), identf)
            P.copy(BT[:, j, ri, :], ps[:, 0:128])
            P.tt(CX[:, j, ri, :].r("p (a b) -> p a b", a=8),
                 csb[:, j, ri, :].m(lambda x: x.unsqueeze(1).to_broadcast([128, 8, 16])), mj, ALU.mult)
    P.barrier()
    P.sb_off = m0
    A = P.sb([128, 8, W], F32, "A")
    B = P.sb([128, 8, W], F32, "B")
    t1 = P.sb([128, 8, W], F32, "t1")
    t2 = P.sb([128, 8, W], F32, "t2")
    wre = P.sb([128, 8, W], F32, "wre")
    wim = P.sb([128, 8, W], F32, "wim")
    xre = P.sb([128, 8, W], BF16, "xre")
    xim = P.sb([128, 8, W], BF16, "xim")
    cre = P.sb([128, 8], F32, "cre")
    cim = P.sb([128, 8], F32, "cim")
    P.memset(cre, 0.0)
    P.memset(cim, 0.0)
    uf = P.sb([128, 2, W], F32, "uf")
    ub = P.sb([128, 2, W], BF16, "ub")
    gf = P.sb([128, 2, W], F32, "gf")
    y = P.sb([128, 2, W], F32, "y")
    y2 = P.sb([128, 2, W], F32, "y2")
    glb = P.sb([128, 2, W], BF16, "glb")
    yo = P.sb([128, 2, W], BF16, "yo")
    csrc = Dm['colsT'].r("(c p) t -> p c t", p=128)
    ydst = Dm['yT_s5'].r("(c p) t -> p c t", p=128)
    for tt in range(NTT):
        sl = slice(tt * W, (tt + 1) * W)
        P.dma(uf, csrc[:, C_SU:C_SU + 2, sl])
        P.dma(gf, csrc[:, C_SG:C_SG + 2, sl])
        P.copy(ub, uf, eng='pool')
        for j in range(8):
            for ri, dst in ((0, A), (1, B)):
                ps = P.ps()
                P.mm(ps, BT[:, j, ri, :], ub[:, j // 4, :])
                P.act(dst[:, j, :], ps, AF.Copy)
        P.tt(t1, A, cosT, ALU.mult)
        P.tt(t2, B, sinT, ALU.mult, eng='pool')
        P.tt(t1, t1, t2, ALU.add)
        P.tt(t2, A, sinT, ALU.mult, eng='pool')
        P.tt(B, B, cosT, ALU.mult)
        P.tt(t2, B, t2, ALU.subtract, eng='pool')
        for j in range(8):
            mb = mag[:, j:j + 1].bc([128, W])
            P.scan(wre[:, j, :], mb, t1[:, j, :], cre[:, j:j + 1], ALU.mult, ALU.add)
            P.scan(wim[:, j, :], mb, t2[:, j, :], cim[:, j:j + 1], ALU.mult, ALU.add)
        P.tt(t1, wre, cosT, ALU.mult)
        P.tt(A, wim, sinT, ALU.mult, eng='pool')
        P.tt(xre, t1, A, ALU.subtract)
        P.tt(cre, t1[:, :, W - 1], A[:, :, W - 1], ALU.subtract)
        P.tt(t2, wre, sinT, ALU.mult, eng='pool')
        P.tt(B, wim, cosT, ALU.mult)
        P.tt(xim, t2, B, ALU.add, eng='pool')
        P.tt(cim, t2[:, :, W - 1], B[:, :, W - 1], ALU.add)
        for jc in range(2):
            ps = P.ps()
            n = 0
            for j in range(4 * jc, 4 * jc + 4):
                for ri, xx in ((0, xre), (1, xim)):
                    P.mm(ps, CX[:, j, ri, :], xx[:, j, :], start=(n == 0), stop=(n == 7))
                    n += 1
            P.stt(y[:, jc, :], uf[:, jc, :], dsk[:, jc:jc + 1], ps, ALU.mult, ALU.add)
        P.tt(y2, y, y, ALU.mult)
        P.ts(y2, y2, 0.044715, ALU.mult, 1.0, ALU.add)
        P.tt(y2, y2, y, ALU.mult)
        P.act(y2, y2, AF.Sigmoid, scale=1.5957691216057308)
        P.tt(y, y, y2, ALU.mult)
        P.copy(glb, y, eng='pool')
        for oc in range(2):
            ps = P.ps()
            for kc in range(2):
                P.mm(ps, wglu[:, kc, oc * 128:(oc + 1) * 128], glb[:, kc, :], start=(kc == 0), stop=(kc == 1))
            P.act(y2[:, oc, :], ps, AF.Sigmoid)
        P.tt(y, y, y2, ALU.mult)
        P.act(y2, gf, AF.Silu)
        P.tt(yo, y, y2, ALU.mult)
        P.dma(ydst[:, :, sl], yo)
    P.barrier()


STAGE_FNS['S5'] = stage_S5


import os
RW_DEBUG = int(os.environ.get('RW_DEBUG', '3'))


def stage_RWKV(P, l, Dm):
    P.sb_off = SB_BASE
    W = 256
    H4 = 4
    NCH = W // 64
    HC = H4 * NCH
    prm = P.sb([64, 8, 4], F32, "prm")
    mu = P.sb([64, 18], F32, "mu")
    w2 = P.sb([64, 256], F32, "w2")
    a2 = P.sb([64, 256], F32, "a2")
    msks = P.sb([64, 3, 64], F32, "msks")
    identf = P.sb([128, 128], F32, "identf")
    ones64 = P.sb([64, 64], F32, "ones64")
    onesw = P.sb([64, 1], F32, "onesw")
    P.dma(prm, Dm[f'rwprm{l}'])
    P.dma(mu, Dm[f'rwmu{l}'])
    P.dma(w2, Dm[f'rww2{l}'])
    P.dma(a2, Dm[f'rwa2{l}'])
    P.dma(msks, Dm['rw_masks'])
    P.dma(identf, Dm['ident'])
    P.memset(ones64, 1.0)
    P.memset(onesw, 1.0)
    id64 = identf[0:64, 0:64]
    hb = lambda v, n=W: v.m(lambda x: x.unsqueeze(2).to_broadcast([64, H4, n]))
    mb = lambda k: msks[:, k, :].m(lambda x: x.unsqueeze(1).to_broadcast([64, H4, 64]))
    idb = id64.m(lambda x: x.unsqueeze(1).to_broadcast([64, H4, 64]))
    cin = P.sb([64, 18, W + 1], F32, "cin")
    cs = P.sb([64, 18, W], F32, "cs")
    f = lambda nm: P.sb([64, H4, W], F32, nm)
    twl = P.sb([64, W], F32, "twl")
    sgz, av, kx, t0, kp, beta = f("sgz"), f("av"), f("kx"), f("t0"), f("kp"), f("beta")
    kkn, lw, cw, e1, e2 = f("kkn"), f("lw"), f("cw"), f("e1"), f("e2")
    rt, at, bt, kt, Bh, Kh = f("rt"), f("at"), f("bt"), f("kt"), f("Bh"), f("Kh")
    bonus, Yt = f("bonus"), f("Yt")
    base = P.sb([64, HC], F32, "base")
    cwC = P.sb([64, HC], F32, "cwC")
    gC = P.sb([64, HC], F32, "gC")
    S0 = P.sb([64, H4, 64], F32, "S0")
    P.memset(S0, 0.0)
    NP2 = NCH // 2
    g8 = lambda nm, dt=F32: [P.sb([64, 2, H4, 64], dt, nm) for _ in range(NP2)]
    X, XT, PaT, AakT, ArbT, ArkT = g8("X", BF16), g8("XT", BF16), g8("PaT", BF16), g8("AakT"), g8("ArbT"), g8("ArkT")
    Vt, BhT, KhT, atT, W2, M2, M1T, KV, Gd = (g8("Vt"), g8("BhT"), g8("KhT"), g8("atT", BF16), g8("W2", BF16), g8("M2"),
                                              g8("M1T"), g8("KV"), g8("Gd"))
    Usb = [P.sb([64, H4, 64], F32, "Usb") for _ in range(2)]
    mb8 = lambda k: msks[:, k, :].m(lambda x: x.unsqueeze(1).unsqueeze(1).to_broadcast([64, 2, H4, 64]))
    id8 = id64.m(lambda x: x.unsqueeze(1).unsqueeze(1).to_broadcast([64, 2, H4, 64]))
    ps8 = lambda ps: ps[0:64, 0:512].r("p (c h x) -> p c h x", c=2, h=H4)
    yo = P.sb([64, H4, W], BF16, "yo")
    src = Dm['colsT'][0:1152, :].r("(g d) t -> d g t", d=64)
    ydst = Dm['yT_rwkv'].r("(h d) t -> d h t", d=64)
    ps4 = lambda ps: ps[0:64, 0:256].r("p (h x) -> p h x", h=H4)
    for tt in range(S // W):
        t_0 = tt * W
        if tt == 0:
            P.dma(cin[:, :, 1:W + 1], src[:, :, t_0:t_0 + W])
            P.memset(cin[:, :, 0:1], 0.0)
        else:
            P.dma(cin, src[:, :, t_0 - 1:t_0 + W])
        P.tt(cs, cin[:, :, 0:W], cin[:, :, 1:W + 1], ALU.subtract)
        P.tt(cs, cs, mu.m(lambda x: x.unsqueeze(2).to_broadcast([64, 18, W])), ALU.mult)
        P.tt(cs, cs, cin[:, :, 1:W + 1], ALU.add)
        Rr, Kk, Vv, G = cs[:, 0:4, :], cs[:, 4:8, :], cs[:, 8:12, :], cs[:, 14:18, :]
        P.act(twl, cs[:, 12, :], AF.Tanh)
        for h in range(H4):
            ps = P.ps()
            P.mm(ps[0:64, 0:W], w2[:, h * 64:(h + 1) * 64], twl)
            P.act(sgz[:, h, :], ps[0:64, 0:W], AF.Sigmoid, bias=prm[:, 0, h:h + 1])
            ps = P.ps()
            P.mm(ps[0:64, 0:W], a2[:, h * 64:(h + 1) * 64], cs[:, 13, :])
            P.act(av[:, h, :], ps[0:64, 0:W], AF.Sigmoid, bias=prm[:, 1, h:h + 1])
        P.tt(kx, Kk, hb(prm[:, 2, :]), ALU.mult)
        P.tt(t0, kx, kx, ALU.mult, eng='pool')
        for h in range(H4):
            ps = P.ps()
            P.mm(ps[0:64, 0:W], ones64, t0[:, h, :])
            P.ts(kkn[:, h, :], ps[0:64, 0:W], 1e-24, ALU.add)
        P.act(kkn, kkn, AF.Sqrt)
        P.recip(kkn, kkn)
        P.tt(kkn, kkn, kx, ALU.mult)
        P.ts(t0, av, -1.0, ALU.add)
        P.tt(t0, t0, hb(prm[:, 3, :]), ALU.mult)
        P.stt(kp, t0, 1.0, Kk, ALU.add, ALU.mult)
        P.tt(beta, kkn, av, ALU.mult, eng='pool')
        P.tt(t0, Rr, kp, ALU.mult)
        P.tt(t0, t0, hb(prm[:, 4, :]), ALU.mult)
        for h in range(H4):
            ps = P.ps()
            P.mm(ps[0:64, 0:W], ones64, t0[:, h, :])
            P.tt(bonus[:, h, :], ps[0:64, 0:W], Vv[:, h, :], ALU.mult)
        P.ts(lw, sgz, -math.exp(-0.5), ALU.mult)
        for h in range(H4):
            P.scan(cw[:, h, :], onesw[:, 0:1].bc([64, W]), lw[:, h, :], 0.0, ALU.mult, ALU.add)
        cw3 = cw.r("p h (c i) -> p (h c) i", i=64)
        P.memset(base, 0.0)
        P.copy(base.r("p (h c) -> p h c", h=H4)[:, :, 1:NCH], cw.r("p h (c i) -> p h c i", i=64)[:, :, 0:NCH - 1, 63])
        P.tt(cw3, cw3, base.m(lambda x: x.unsqueeze(2).to_broadcast([64, HC, 64])), ALU.subtract)
        P.copy(cwC, cw3[:, :, 63])
        P.act(gC, cwC, AF.Exp)
        P.act(e1, cw, AF.Exp)
        P.tt(rt, Rr, e1, ALU.mult)
        P.act(e1, cw, AF.Exp, scale=-1.0)
        P.tt(bt, beta, e1, ALU.mult)
        P.tt(kt, kp, e1, ALU.mult, eng='pool')
        P.tt(e2, cw, lw, ALU.subtract)
        P.act(e2, e2, AF.Exp)
        P.stt(at, kkn, -1.0, e2, ALU.mult, ALU.mult)
        e13 = e1.r("p h (c i) -> p (h c) i", i=64)
        P.tt(e13, cw3, cwC.m(lambda x: x.unsqueeze(2).to_broadcast([64, HC, 64])), ALU.subtract)
        P.act(e1, e1, AF.Exp, scale=-1.0)
        P.tt(Bh, beta, e1, ALU.mult)
        P.tt(Kh, kp, e1, ALU.mult, eng='pool')
        def mm8(p, lhf, rhf):
            ps = P.ps()
            for cl in range(2):
                for h in range(H4):
                    o_ = ps[0:64, (cl * H4 + h) * 64:(cl * H4 + h + 1) * 64]
                    P.mm(o_, lhf(p, cl, h), rhf(p, cl, h))
            return ps8(ps)
        csl = lambda p, cl: slice((2 * p + cl) * 64, (2 * p + cl + 1) * 64)
        tok = lambda t_: (lambda p, cl, h: t_[:, h, csl(p, cl)])
        blk = lambda t_: (lambda p, cl, h: t_[p][:, cl, h, :])
        for p in range(NP2):
            P.tt(X[p], mm8(p, tok(at), tok(bt)), mb8(2), ALU.mult)
            P.tt(XT[p], mm8(p, tok(bt), tok(at)), mb8(0), ALU.mult)
            P.tt(AakT[p], mm8(p, tok(kt), tok(at)), mb8(0), ALU.mult)
            P.tt(ArbT[p], mm8(p, tok(bt), tok(rt)), mb8(1), ALU.mult)
            P.tt(ArkT[p], mm8(p, tok(kt), tok(rt)), mb8(1), ALU.mult)
            P.tt(PaT[p], XT[p], id8, ALU.add)
        for srcT, dstT, eng in ((Vv, Vt, 'act'), (Bh, BhT, 'dve'), (Kh, KhT, 'act'), (at, atT, 'dve')):
            for p in range(NP2):
                ps = P.ps()
                for cl in range(2):
                    for h in range(H4):
                        P.transpose(ps[0:64, (cl * H4 + h) * 64:(cl * H4 + h + 1) * 64], srcT[:, h, csl(p, cl)], id64)
                if eng == 'act':
                    P.act(dstT[p], ps8(ps), AF.Copy)
                else:
                    P.copy(dstT[p], ps8(ps))
        for it in range(5):
            pxs = [(mm8(p, blk(XT), blk(X)), mm8(p, blk(X), blk(XT))) for p in range(NP2)]
            for p in range(NP2):
                P.act(X[p], pxs[p][0], AF.Copy)
                P.copy(XT[p], pxs[p][1])
            pps = [mm8(p, blk(X), blk(PaT)) for p in range(NP2)]
            for p in range(NP2):
                P.tt(PaT[p], PaT[p], pps[p], ALU.add)
        for p in range(NP2):
            P.act(KV[p], mm8(p, blk(KhT), blk(Vt)), AF.Copy)
            P.act(W2[p], mm8(p, blk(AakT), blk(Vt)), AF.Copy)
            P.copy(M1T[p], mm8(p, blk(atT), blk(PaT)))
            for cl in range(2):
                c = 2 * p + cl
                gcb = gC.r("p (h c) -> p h c", h=H4)[:, :, c].m(lambda x: x.unsqueeze(2).to_broadcast([64, H4, 64]))
                P.tt(Gd[p][:, cl], idb, gcb, ALU.mult)
        for p in range(NP2):
            P.act(M2[p], mm8(p, blk(PaT), blk(W2)), AF.Copy)
        for c in range(NCH):
            p, cl = c // 2, c % 2
            sl = slice(c * 64, (c + 1) * 64)
            us = Usb[c % 2]
            psu = P.ps()
            for h in range(H4):
                P.mm(psu[0:64, h * 64:(h + 1) * 64], M1T[p][:, cl, h, :], S0[:, h, :])
            psy = P.ps()
            for h in range(H4):
                o_ = psy[0:64, h * 64:(h + 1) * 64]
                P.mm(o_, S0[:, h, :], rt[:, h, sl], start=True, stop=False)
                P.mm(o_, Vt[p][:, cl, h, :], ArkT[p][:, cl, h, :], start=False, stop=True)
            P.tt(us, ps4(psu), M2[p][:, cl], ALU.add)
            pss = P.ps()
            for h in range(H4):
                o_ = pss[0:64, h * 64:(h + 1) * 64]
                P.mm(o_, Gd[p][:, cl, h, :], S0[:, h, :], start=True, stop=False)
                P.mm(o_, BhT[p][:, cl, h, :], us[:, h, :], start=False, stop=True)
            psy2 = P.ps()
            for h in range(H4):
                P.mm(psy2[0:64, h * 64:(h + 1) * 64], us[:, h, :], ArbT[p][:, cl, h, :])
            P.tt(S0, ps4(pss), KV[p][:, cl], ALU.add)
            P.act(Yt[:, :, sl], ps4(psy), AF.Copy)
            P.tt(Yt[:, :, sl], Yt[:, :, sl], ps4(psy2), ALU.add)
        for h in range(H4):
            ps = P.ps()
            P.mm(ps[0:64, 0:W], ones64, Yt[:, h, :])
            P.stt(e1[:, h, :], ps[0:64, 0:W], -1.0 / 64, Yt[:, h, :], ALU.mult, ALU.add)
        P.tt(e2, e1, e1, ALU.mult, eng='pool')
        for h in range(H4):
            ps = P.ps()
            P.mm(ps[0:64, 0:W], ones64, e2[:, h, :])
            P.ts(t0[:, h, :], ps[0:64, 0:W], 1.0 / 64, ALU.mult, 64e-5, ALU.add)
        P.act(t0, t0, AF.Sqrt)
        P.recip(t0, t0)
        P.tt(e1, e1, t0, ALU.mult)
        P.tt(e1, e1, hb(prm[:, 5, :]), ALU.mult)
        P.tt(e1, e1, hb(prm[:, 6, :]), ALU.add)
        P.tt(e1, e1, bonus, ALU.add)
        P.act(e2, G, AF.Silu)
        P.tt(yo, e1, e2, ALU.mult)
        P.dma(ydst[:, :, t_0:t_0 + W], yo)
    P.barrier()


STAGE_FNS['RWKV'] = stage_RWKV


N_BISECT = int(os.environ.get("N_BISECT", "20"))


def stage_DSA(P, l, Dm):
    P.sb_off = SB_BASE
    kT = P.sb([64, 4, S], BF16, "kT")
    ikT = P.sb([64, 1, S], F32, "ikT")
    m0 = P.sb_off
    ropeC = P.sb([64, S], F32, "ropeC")
    ropeS = P.sb([64, S], F32, "ropeS")
    P.dma(ropeC, Dm['ropeC'])
    P.dma(ropeS, Dm['ropeS'])
    rope_heads(P, None, Dm, C_DQ, C_DQS, ropeC, ropeS, dram_dst=Dm['qR'])
    rope_heads(P, kT, Dm, C_DK, C_DKS, ropeC, ropeS)
    rope_heads(P, None, Dm, C_IQ, C_IQS, ropeC, ropeS, dram_dst=Dm['iqR'])
    rope_heads(P, ikT, Dm, (C_IK * 128,), (C_IK * 128 + 64,), ropeC, ropeS, nheads=1)
    P.barrier()
    P.sb_off = m0
    identf = P.sb([128, 128], F32, "identf")
    identb = P.sb([128, 128], BF16, "identb")
    cb = P.sb([128, 128], F32, "cb")
    P.dma(identf, Dm['ident'])
    P.copy(identb, identf)
    P.dma(cb, Dm['dsa_cb'])
    Vaug = P.sb([128, 32, 4, 65], BF16, "Vaug")
    iwt = P.sb([128, 32, 4], F32, "iwt")
    vst = [P.sb([128, 4, 256], F32, "vst") for _ in range(2)]
    tsrc = Dm['colsTok'].r("(c p) n -> p c n", p=128)
    P.memset(Vaug[:, :, :, 64:65], 1.0)
    for i in range(8):
        v_ = vst[i % 2]
        P.dma(v_, tsrc[:, i * 4:(i + 1) * 4, 0:256])
        P.copy(Vaug[:, i * 4:(i + 1) * 4, :, 0:64], v_.r("p c (h d) -> p c h d", h=4), eng=('dve' if i % 2 else 'pool'))
    P.dma(iwt, tsrc[:, :, 512:516])
    P.ts(iwt, iwt, 1.0 / 16, ALU.mult)
    scores = [P.sb([128, S], F32, "score") for _ in range(2)]
    mask01s = [P.sb([128, S], BF16, "mask01") for _ in range(2)]
    maskTs = [P.sb([128, 32, 128], BF16, "maskT") for _ in range(2)]
    rl = [P.sb([128, 512], F32, "rl") for _ in range(4)]
    E = [P.sb([128, 512], BF16, "E") for _ in range(2)]
    lo = P.sb([128, 1], F32, "lo")
    hi = P.sb([128, 1], F32, "hi")
    mid = P.sb([128, 1], F32, "mid")
    cnt = P.sb([128, 1], F32, "cnt")
    sel = P.sb([128, 1], F32, "sel")
    dlt = P.sb([128, 1], F32, "dlt")
    stp = P.sb([128, 32], F32, "stp")
    pw = P.sb([128, 32], F32, "pw")
    P.dma(pw, Dm['dsa_pw'])
    zt = P.sb([128, S], BF16, "zt")
    cz = P.sb([128, S], F32, "cz")
    junk = cz
    nz = P.sb([128, 1], F32, "nz")
    npos = P.sb([128, 1], F32, "npos")
    flag = P.sb([128, 1], F32, "flag")
    f2 = P.sb([128, 1], F32, "f2")
    rr = P.sb([128, 1], F32, "rr")
    onesw = P.sb([128, 1], F32, "onesw")
    P.memset(onesw, 1.0)
    negbig = P.sb([128, 1], F32, "negbig")
    P.memset(negbig, -1e5)
    osb = P.sb([128, 4, 64], F32, "osb")
    rs = P.sb([128, 4, 1], F32, "rs")
    gt = [P.sb([128, 2, 128], F32, "gt") for _ in range(2)]
    sgs = [P.sb([128, 2, 128], F32, "sg") for _ in range(2)]
    yo = [P.sb([128, 2, 128], BF16, "yo") for _ in range(2)]
    iqt = [P.sb([64, 4, 128], F32, "iqt") for _ in range(2)]
    iqsrc = Dm['iqR'].r("(h d) t -> d h t", d=64)
    qft = [P.sb([64, 4, 128], F32, "qft") for _ in range(2)]
    qbt = [P.sb([64, 4, 128], BF16, "qbt") for _ in range(2)]
    qsrc = Dm['qR'].r("(h d) t -> d h t", d=64)
    gsrc = Dm['colsT'].r("(c p) t -> p c t", p=128)
    ydst = Dm['yT_dsa'].r("(c p) t -> p c t", p=128)
    ne = [0]
    st = {}

    def score_phase(i):
        qs = slice(i * 128, (i + 1) * 128)
        Nk = 128 * (i + 1)
        g_ = gt[i % 2]
        P.dma(g_, gsrc[:, C_DG:C_DG + 2, qs])
        iq_ = iqt[i % 2]
        P.dma(iq_, iqsrc[:, :, qs])
        P.dma(qft[i % 2], qsrc[:, :, qs])
        qb_ = qbt[i % 2]
        P.copy(qb_, qft[i % 2], eng='pool')
        score = scores[i % 2]
        mask01 = mask01s[i % 2]
        maskT = maskTs[i % 2]
        for k0 in range(0, Nk, 512):
            kw = min(512, Nk - k0)
            pss = []
            for h in range(4):
                ps = P.ps()
                P.mm(ps[:, 0:kw], iq_[:, h, :], ikT[:, 0, k0:k0 + kw], inc=(h == 3))
                pss.append(ps)
            for h in range(4):
                P.act(rl[h][:, 0:kw], pss[h][:, 0:kw], AF.Relu)
            P.ts(score[:, k0:k0 + kw], rl[0][:, 0:kw], iwt[:, i, 0:1], ALU.mult)
            for h in range(1, 4):
                P.stt(score[:, k0:k0 + kw], rl[h][:, 0:kw], iwt[:, i, h:h + 1], score[:, k0:k0 + kw], ALU.mult, ALU.add)
        P.tt(score[:, i * 128:Nk], score[:, i * 128:Nk], cb, ALU.add)
        st[i] = (qs, Nk, g_, iq_, qb_, score, mask01, maskT)

    def select_phase(i):
        qs, Nk, g_, iq_, qb_, score, mask01, maskT = st[i]
        if Nk > 256:
            P.reduce(hi, score[:, 0:Nk], ALU.max)
            P.reduce(lo, score[:, 0:i * 128], ALU.min)
            P.tt(dlt, hi, lo, ALU.subtract)
            P.ts(dlt, dlt, 2.0, ALU.add)
            P.ts(stp, pw, dlt[:, 0:1], ALU.mult)
            P.stt(mid, dlt, 0.5, lo, ALU.mult, ALU.add)
            P.ts(mid, mid, -1.0, ALU.add)
            for it in range(N_BISECT):
                P.ts(junk[:, 0:Nk], score[:, 0:Nk], mid[:, 0:1], ALU.is_ge, 0.0, ALU.add, accum=cnt)
                if it < N_BISECT - 1:
                    P.ts(sel, cnt, 255.5, ALU.is_ge, 0.5, ALU.subtract)
                    P.stt(mid, sel, stp[:, it:it + 1], mid, ALU.mult, ALU.add)
                else:
                    P.ts(sel, cnt, 255.5, ALU.is_ge, 1.0, ALU.subtract)
                    P.stt(lo, sel, stp[:, it:it + 1], mid, ALU.mult, ALU.add)
        else:
            P.memset(lo, -1e29)
        P.ts(zt[:, 0:Nk], score[:, 0:Nk], 0.0, ALU.is_equal, 0.0, ALU.add, accum=nz)
        P.ts(junk[:, 0:Nk], score[:, 0:Nk], 0.0, ALU.is_gt, 0.0, ALU.add, accum=npos)
        P.ts(flag, npos, 255.5, ALU.is_lt)
        P.tt(f2, npos, nz, ALU.add)
        P.ts(f2, f2, 255.5, ALU.is_ge)
        P.tt(flag, flag, f2, ALU.mult)
        P.ts(rr, npos, -1.0, ALU.mult, 256.0, ALU.add)
        P.scan(cz[:, 0:Nk], onesw[:, 0:1].bc([128, Nk]), zt[:, 0:Nk], 0.0, ALU.mult, ALU.add)
        P.ts(cz[:, 0:Nk], cz[:, 0:Nk], rr[:, 0:1], ALU.is_le, flag[:, 0:1], ALU.mult)
        P.tt(zt[:, 0:Nk], zt[:, 0:Nk], cz[:, 0:Nk], ALU.mult)
        P.ts(f2, flag, -1.0, ALU.mult, 1.0, ALU.add)
        P.tt(lo, lo, f2, ALU.mult)
        P.stt(lo, flag, 1e-30, lo, ALU.mult, ALU.add)
        P.ts(mask01[:, 0:Nk], score[:, 0:Nk], lo[:, 0:1], ALU.is_ge)
        P.tt(mask01[:, 0:Nk], mask01[:, 0:Nk], zt[:, 0:Nk], ALU.add)
        for c0 in range(0, i + 1, 4):
            nc_ = min(4, i + 1 - c0)
            ps = P.ps()
            psb = ps.bitcast(BF16)
            for cl in range(nc_):
                c = c0 + cl
                P.transpose(psb[:, cl * 128:(cl + 1) * 128], mask01[:, c * 128:(c + 1) * 128], identb, inc=(cl == nc_ - 1))
            P.act(maskT[:, c0:c0 + nc_, :], psb[:, 0:nc_ * 128].r("p (c q) -> p c q", q=128), AF.Identity,
                  scale=1e5, bias=negbig[:, 0:1])

    def attention(i):
        qs, Nk, g_, iq_, qb_, score, mask01, maskT = st[i]
        psO = P.ps_acc(i)
        groups = [(h, c0, min(4, i + 1 - c0)) for h in range(4) for c0 in range(0, i + 1, 4)]

        def logits(g):
            h, c0, nc_ = g
            ps = P.ps()
            for cl in range(nc_):
                c = c0 + cl
                o_ = ps[:, cl * 128:(cl + 1) * 128]
                P.mm(o_, kT[:, h, c * 128:(c + 1) * 128], qb_[:, h, :], start=True, stop=False)
                P.mm(o_, identb, maskT[:, c, :], start=False, stop=True)
            return ps
        nxt = logits(groups[0])
        for gi, (h, c0, nc_) in enumerate(groups):
            ps = nxt
            if gi + 1 < len(groups):
                nxt = logits(groups[gi + 1])
            e_ = E[ne[0] % 2]
            ne[0] += 1
            P.act(e_[:, 0:nc_ * 128], ps[:, 0:nc_ * 128], AF.Exp, scale=0.125)
            for cl in range(nc_):
                c = c0 + cl
                P.mm(psO[:, h * 65:(h + 1) * 65], e_[:, cl * 128:(cl + 1) * 128], Vaug[:, c, h, :],
                     start=(c == 0), stop=(c == i))

    def fin(i):
        qs, Nk, g_, iq_, qb_, score, mask01, maskT = st[i]
        psO = P.ps_acc(i)
        pv = psO[:, 0:260].r("p (h x) -> p h x", h=4)
        P.recip(rs, pv[:, :, 64:65])
        P.tt(osb, pv[:, :, 0:64], rs.m(lambda x: x.to_broadcast([128, 4, 64])), ALU.mult)
        sg = sgs[i % 2]
        P.act(sg, g_, AF.Silu)
        y_ = yo[i % 2]
        for p in range(2):
            ps = P.ps()
            P.transpose(ps[:, 0:128], osb[:, 2 * p:2 * p + 2, :].r("p a b -> p (a b)"), identf)
            P.tt(y_[:, p, :], ps[:, 0:128], sg[:, p, :], ALU.mult)
        P.dma(ydst[:, :, qs], y_)

    score_phase(0)
    for i in range(32):
        select_phase(i)
        if i > 0:
            fin(i - 1)
        if i + 1 < 32:
            score_phase(i + 1)
        attention(i)
    fin(31)
    P.barrier()


STAGE_FNS['DSA'] = stage_DSA


FULL_PLAN = [('R', 0)] + [(st, l) for l in range(2) for st in ('P', 'X', 'RET', 'S5', 'RWKV', 'DSA', 'M')]


def kernel(**inputs):
    inputs = {k: np.asarray(v) for k, v in inputs.items()}
    nb = inputs['x'].shape[0]
    nc, P, used_in = build(FULL_PLAN)
    in_maps = []
    for b in range(nb):
        d = host_inputs(inputs, b)
        in_maps.append({k: v for k, v in d.items() if k in used_in})
    res = run_bass_kernel_spmd(nc, in_maps, core_ids=list(range(nb)))
    out = np.stack([np.ascontiguousarray(np.asarray(r['outT']).T) for r in res.results], 0)
    return out.astype(np.float32)
```

```python
from contextlib import ExitStack
import math
import numpy as np
import ml_dtypes
import concourse.bass as bass
import concourse.mybir as mybir
from concourse.bass_utils import run_bass_kernel_spmd

F32 = mybir.dt.float32
BF16 = mybir.dt.bfloat16
I32 = mybir.dt.int32
AF = mybir.ActivationFunctionType
ALU = mybir.AluOpType
AX = mybir.AxisListType

ENGS = ['sp', 'act', 'dve', 'pool', 'pe']
EPOCH = 16000
NDMASEM = 24
RELAX_SAME_ENGINE = True
SB_BASE = 16640
SBUF_BYTES = 229000


class Buf:
    __slots__ = ('name', 'wev', 'rev', 'tracked')

    def __init__(self, name, tracked=True):
        self.name = name
        self.wev = {}
        self.rev = {}
        self.tracked = tracked


class V:
    __slots__ = ('buf', 'ap')

    def __init__(self, buf, ap):
        self.buf = buf
        self.ap = ap

    def __getitem__(self, k):
        return V(self.buf, self.ap[k])

    def m(self, fn):
        return V(self.buf, fn(self.ap))

    def r(self, s, **kw):
        return V(self.buf, self.ap.rearrange(s, **kw))

    def bc(self, shape):
        return V(self.buf, self.ap.to_broadcast(list(shape)))

    def bitcast(self, dt):
        return V(self.buf, self.ap.bitcast(dt))

    @property
    def shape(self):
        return tuple(self.ap.shape)


def _ap(x):
    return x.ap if isinstance(x, V) else x


class Prog:
    def __init__(self, nc):
        self.nc = nc
        self.q = {e: [] for e in ENGS}
        self.cnt = {e: 0 for e in ENGS}
        self.noinc = {e: False for e in ENGS}
        self.known = {e: {} for e in ENGS}
        self.dma_n = {e: 0 for e in ENGS}
        self.nbar = 0
        self.bufs = []
        self.sb_off = SB_BASE
        self.sb_id = 0
        self.sb_mark = 0
        self.psum = []
        for i in range(8):
            h = nc.alloc_psum_tensor(f"ps{i}", [128, 512], F32)
            self.psum.append(V(self._newbuf(f"ps{i}"), h[:]))
        self.ps_rr = 0

    def _newbuf(self, name, tracked=True):
        b = Buf(name, tracked)
        if tracked:
            self.bufs.append(b)
        return b

    def sb(self, shape, dtype=F32, name="t"):
        esz = {F32: 4, BF16: 2, I32: 4}[dtype]
        per = esz * int(np.prod(shape[1:]))
        per = (per + 63) // 64 * 64
        off = self.sb_off
        assert off + per <= SBUF_BYTES, f"SBUF overflow {name} {off}+{per}"
        self.sb_off += per
        self.sb_id += 1
        nm = f"{name}_{self.sb_id}"
        h = self.nc.alloc_sbuf_tensor_at(nm, list(shape), dtype, offset=off)
        return V(self._newbuf(nm), h[:])

    def mark(self):
        self.sb_mark = self.sb_off

    def release(self):
        self.sb_off = self.sb_mark

    def ps(self):
        v = self.psum[self.ps_rr % 6]
        self.ps_rr += 1
        return v

    def ps_acc(self, i=1):
        return self.psum[6 + (i % 2)]

    def dram(self, name, shape, dtype=F32, kind="Internal"):
        h = self.nc.dram_tensor(name, list(shape), dtype, kind=kind)
        return V(self._newbuf(name, tracked=False), h.ap())

    def _collect(self, eng, reads, writes, extra=None):
        waits = {}

        last = self.cnt.get(eng, 0) if isinstance(eng, str) else 0

        def need(evs, skip_own):
            for sk, v in evs.items():
                if sk == eng:
                    if skip_own or (RELAX_SAME_ENGINE and v < last):
                        continue
                if waits.get(sk, 0) < v:
                    waits[sk] = v
        for x in reads:
            if x.buf.tracked:
                need(x.buf.wev, False)
        for x in writes:
            if x.buf.tracked:
                need(x.buf.wev, True)
                need(x.buf.rev, True)
        if extra:
            need(extra, False)
        kn = self.known[eng]
        wl = []
        for sk, v in waits.items():
            if kn.get(sk, 0) < v:
                kn[sk] = v
                wl.append((sk, v))
        return wl

    def emit(self, eng, fn, reads=(), writes=(), inc=True):
        reads = [x for x in reads if isinstance(x, V)]
        writes = [x for x in writes if isinstance(x, V)]
        wl = self._collect(eng, reads, writes)
        idx = self.cnt[eng] + 1
        self.cnt[eng] = idx
        self.q[eng].append((wl, fn, ('c', idx)))
        for x in reads:
            b = x.buf
            if b.tracked and b.rev.get(eng, 0) < idx:
                b.rev[eng] = idx
        for x in writes:
            b = x.buf
            if b.tracked:
                b.wev = {eng: idx}
                b.rev = {}

    def dma(self, out, in_, eng='sp'):
        n = self.dma_n[eng]
        self.dma_n[eng] = n + 1
        slot, k = n % NDMASEM, n // NDMASEM
        sk = ('dma', eng, slot)
        val = 16 * (k + 1)
        extra = {sk: 16 * k} if k > 0 else None
        wl = self._collect(eng, [in_], [out], extra)
        oa, ia = out.ap, in_.ap
        self.q[eng].append((wl, lambda e: e.dma_start(out=oa, in_=ia), ('d', sk)))
        b = in_.buf
        if b.tracked:
            b.rev[sk] = val
        b = out.buf
        if b.tracked:
            b.wev = {sk: val}
            b.rev = {}

    def barrier(self):
        waits = {}
        for e in ENGS:
            if e != 'sp' and self.cnt[e] > 0:
                waits[e] = self.cnt[e]
            n = self.dma_n[e]
            for slot in range(min(n, NDMASEM)):
                k = (n - 1 - slot) // NDMASEM
                waits[('dma', e, slot)] = 16 * (k + 1)
        kn = self.known['sp']
        wl = []
        for sk, v in waits.items():
            if kn.get(sk, 0) < v:
                kn[sk] = v
                wl.append((sk, v))
        self.nbar += 1
        nb = self.nbar
        bk = ('bar', 0)
        self.q['sp'].append((wl, None, ('b', bk)))
        for e in ENGS:
            if e != 'sp':
                self.q[e].append(([(bk, nb)], None, None))
                for sk, v in waits.items():
                    if self.known[e].get(sk, 0) < v:
                        self.known[e][sk] = v
        for b in self.bufs:
            b.wev = {}
            b.rev = {}

    def finish(self):
        self.barrier()
        nc = self.nc
        targets = {e: set() for e in ENGS}
        for e in ENGS:
            for wl, fn, tag in self.q[e]:
                for sk, v in wl:
                    if isinstance(sk, str):
                        targets[sk].add(v)
        rank = {e: {v: r + 1 for r, v in enumerate(sorted(targets[e]))} for e in ENGS}
        self.n_inc = {e: len(rank[e]) for e in ENGS}
        keys = set()

        def semkey(sk, v):
            if isinstance(sk, str):
                r = rank[sk][v]
                return ((sk, (r - 1) // EPOCH), (r - 1) % EPOCH + 1)
            return (sk, v)
        prog = {e: [] for e in ENGS}
        for e in ENGS:
            for wl, fn, tag in self.q[e]:
                w2 = [semkey(sk, v) for sk, v in wl]
                inc = None
                if tag is not None:
                    if tag[0] == 'c':
                        if tag[1] in rank[e]:
                            r = rank[e][tag[1]]
                            inc = ((e, (r - 1) // EPOCH), 1)
                    elif tag[0] == 'd':
                        inc = (tag[1], 16)
                    elif tag[0] == 'b':
                        inc = (tag[1], 1)
                for k_, _ in w2:
                    keys.add(k_)
                if inc is not None:
                    keys.add(inc[0])
                prog[e].append((w2, fn, inc, tag))
        stack = ExitStack()
        sems = {}
        for i, sk in enumerate(sorted(keys, key=str)):
            sems[sk] = stack.enter_context(nc.semaphore(f"s{i}"))
        self.nsem = len(sems)

        def mk(en):
            def body(e):
                for wl, fn, inc, tag in prog[en]:
                    for sk, v in wl:
                        e.wait_ge(sems[sk], v)
                    if fn is None:
                        if tag is not None and tag[0] == 'b':
                            e.sem_inc(sems[inc[0]], inc[1])
                        continue
                    ins = fn(e)
                    if inc is not None:
                        ins.then_inc(sems[inc[0]], inc[1])
            return body
        with stack:
            with nc.Block() as block:
                block.sync(mk('sp'))
                block.scalar(mk('act'))
                block.vector(mk('dve'))
                block.gpsimd(mk('pool'))
                block.tensor(mk('pe'))

    def act(self, out, in_, func, bias=None, scale=1.0, accum=None):
        o, i, b, s, a = _ap(out), _ap(in_), _ap(bias), _ap(scale), _ap(accum)
        kw = {}
        if b is not None:
            kw['bias'] = b
        if a is not None:
            kw['accum_out'] = a
        self.emit('act', lambda e: e.activation(out=o, in_=i, func=func, scale=s, **kw),
                  [in_, bias, scale], [out, accum])

    def ts(self, out, in0, s1, op0, s2=None, op1=None, accum=None, eng='dve'):
        o, i, a1, a2, ac = _ap(out), _ap(in0), _ap(s1), _ap(s2), _ap(accum)
        kw = {}
        if op1 is not None:
            kw['op1'] = op1
        if ac is not None:
            kw['accum_out'] = ac
        self.emit(eng, lambda e: e.tensor_scalar(out=o, in0=i, scalar1=a1, scalar2=a2, op0=op0, **kw),
                  [in0, s1, s2], [out, accum])

    def tt(self, out, in0, in1, op, eng='dve'):
        o, a, b = _ap(out), _ap(in0), _ap(in1)
        self.emit(eng, lambda e: e.tensor_tensor(out=o, in0=a, in1=b, op=op), [in0, in1], [out])

    def stt(self, out, in0, scalar, in1, op0, op1, eng='dve'):
        o, a, s, b = _ap(out), _ap(in0), _ap(scalar), _ap(in1)
        self.emit(eng, lambda e: e.scalar_tensor_tensor(out=o, in0=a, scalar=s, in1=b, op0=op0, op1=op1),
                  [in0, scalar, in1], [out])

    def copy(self, out, in_, eng='dve'):
        o, i = _ap(out), _ap(in_)
        if eng == 'act':
            self.emit('act', lambda e: e.copy(out=o, in_=i), [in_], [out])
        else:
            self.emit(eng, lambda e: e.tensor_copy(out=o, in_=i), [in_], [out])

    def memset(self, out, val, eng='dve'):
        o = _ap(out)
        self.emit(eng, lambda e: e.memset(o, val), [], [out])

    def recip(self, out, in_):
        o, i = _ap(out), _ap(in_)
        self.emit('dve', lambda e: e.reciprocal(out=o, in_=i), [in_], [out])

    def reduce(self, out, in_, op, axis=AX.X):
        o, i = _ap(out), _ap(in_)
        self.emit('dve', lambda e: e.tensor_reduce(out=o, in_=i, axis=axis, op=op), [in_], [out])

    def scan(self, out, d0, d1, initial, op0, op1):
        o, a, b, ini = _ap(out), _ap(d0), _ap(d1), _ap(initial)
        self.emit('dve', lambda e: e.tensor_tensor_scan(out=o, data0=a, data1=b, initial=ini, op0=op0, op1=op1),
                  [d0, d1, initial], [out])

    def mm(self, out, lhsT, rhs, start=True, stop=True, inc=None):
        o, l, r = _ap(out), _ap(lhsT), _ap(rhs)
        if inc is None:
            inc = stop
        self.emit('pe', lambda e: e.matmul(o, l, r, start=start, stop=stop), [lhsT, rhs], [out], inc=inc)

    def transpose(self, out, in_, ident, inc=True):
        o, i, d = _ap(out), _ap(in_), _ap(ident)
        self.emit('pe', lambda e: e.transpose(o, i, d), [in_, ident], [out], inc=inc)


S = 4096
D = 1024
TT = 512
NTT = S // TT
NP_ROWS = 5376
NT_COLS = 516
B_RWKV, B_DSA, B_RET, B_S5, B_X, B_GATE = 0, 1152, 2500, 3524, 4036, 4548
C_RWKV = 0
C_DQ, C_DQS, C_DK, C_DKS, C_IQ, C_IQS, C_IK, C_DG = 9, 11, 13, 15, 17, 19, 21, 22
C_RQ, C_RQS, C_RK, C_RKS, C_RG = 24, 26, 28, 30, 32
C_SU, C_SG = 34, 36
C_XQ, C_XG = 38, 40


def _swap_idx(base, nheads):
    idx = []
    for h in range(nheads):
        for j in range(64):
            idx.append(base + h * 64 + (j + 32) % 64)
    return idx


def proj_col_indices():
    r = lambda a, n: list(range(a, a + n))
    f = []
    f += r(B_RWKV, 1152)
    f += r(B_DSA, 256) + _swap_idx(B_DSA, 4)
    f += r(B_DSA + 256, 256) + _swap_idx(B_DSA + 256, 4)
    f += r(B_DSA + 768, 256) + _swap_idx(B_DSA + 768, 4)
    f += r(B_DSA + 1024, 64) + _swap_idx(B_DSA + 1024, 1)
    f += r(B_DSA + 1092, 256)
    f += r(B_RET, 256) + _swap_idx(B_RET, 4)
    f += r(B_RET + 256, 256) + _swap_idx(B_RET + 256, 4)
    f += r(B_RET + 768, 256)
    f += r(B_S5, 512)
    f += r(B_X, 512)
    assert len(f) == NP_ROWS
    t = r(B_DSA + 512, 256) + r(B_RET + 512, 256) + r(B_DSA + 1088, 4)
    assert len(t) == NT_COLS
    return np.array(f), np.array(t)


def load_weight_bf16(P, dst, src_dram, gcol, nk, ncols, blk=1344):
    src = src_dram.r("(k p) n -> p k n", p=128)
    stg = [P.sb([128, blk], F32, "wstg") for _ in range(2)]
    i = 0
    for k in range(nk):
        for c0 in range(0, ncols, blk):
            c1 = min(ncols, c0 + blk)
            s = stg[i % 2]
            P.dma(s[:, 0:c1 - c0], src[:, k, c0:c1])
            eng = 'dve' if i % 2 == 0 else 'pool'
            if gcol is not None:
                P.ts(dst[:, k, c0:c1], s[:, 0:c1 - c0], gcol[:, k:k + 1], ALU.mult, eng=eng)
            else:
                P.copy(dst[:, k, c0:c1], s[:, 0:c1 - c0], eng=eng)
            i += 1


def stream_weight_blocks(P, dst, src_dram, gcol, nk, blocks, stg):
    src = src_dram.r("(k p) n -> p k n", p=128)
    views = []
    i = 0
    for (c0, c1) in blocks:
        v = V(P._newbuf("wblk"), dst.ap[:, :, c0:c1])
        views.append(v)
        for k in range(nk):
            s_ = stg[i % len(stg)]
            P.dma(s_[:, 0:c1 - c0], src[:, k, c0:c1])
            eng = 'dve' if i % 2 == 0 else 'pool'
            if gcol is not None:
                P.ts(v[:, k, :], s_[:, 0:c1 - c0], gcol[:, k:k + 1], ALU.mult, eng=eng)
            else:
                P.copy(v[:, k, :], s_[:, 0:c1 - c0], eng=eng)
            i += 1
    return views


def rsqrt_ps(P, out, src, scale, eps):
    P.ts(out, src, scale, ALU.mult, eps, ALU.add)
    P.act(out, out, AF.Sqrt)
    P.recip(out, out)


def rms_tile(P, xt, hT, sq, rstd, ones, nk, n, width):
    P.act(sq, xt, AF.Square)
    ps = P.ps()
    for k in range(nk):
        P.mm(ps[:, 0:width], ones, sq[:, k, :], start=(k == 0), stop=(k == nk - 1))
    rsqrt_ps(P, rstd, ps[:, 0:width], 1.0 / n, 1e-6)
    for k in range(nk):
        P.tt(hT[:, k, :], xt[:, k, :], rstd, ALU.mult)


def stage_P(P, l, Dm, xT):
    P.sb_off = SB_BASE
    npre = P.sb([128, 8], F32, "npre")
    P.dma(npre, Dm[f'npre{l}'])
    ones = P.sb([128, 128], F32, "ones")
    P.memset(ones, 1.0)
    wp = P.sb([128, 8, NP_ROWS], BF16, "wp")
    wt = P.sb([128, 8, NT_COLS], BF16, "wt")
    wf = P.sb([128, 8, 644], F32, "wf")
    wfsrc = Dm[f'wpf{l}'].r("(k p) n -> p k n", p=128)
    for k in range(8):
        P.dma(wf[:, k, :], wfsrc[:, k, :])
    for k in range(8):
        P.ts(wf[:, k, :], wf[:, k, :], npre[:, k:k + 1], ALU.mult, eng=('dve' if k % 2 else 'pool'))
    WB = 640
    stg = [P.sb([128, WB], F32, "wstg") for _ in range(3)]
    wtb = stream_weight_blocks(P, wt, Dm[f'wt{l}'], npre, 8, [(0, NT_COLS)], stg)[0]
    wpb = stream_weight_blocks(P, wp, Dm[f'wp{l}'], npre, 8,
                               [(c0, min(c0 + WB, NP_ROWS)) for c0 in range(0, NP_ROWS, WB)], stg)
    xts = [P.sb([128, 8, TT], F32, "xt") for _ in range(2)]
    sq = P.sb([128, 8, TT], F32, "sq")
    hTs = [P.sb([128, 8, TT], BF16, "hT") for _ in range(2)]
    rstd = P.sb([128, TT], F32, "rstd")
    ostg = [P.sb([128, 2, TT], F32, "ostg") for _ in range(2)]
    tstg = [P.sb([128, NT_COLS], F32, "tstg") for _ in range(2)]
    xsrc = xT.r("(k p) t -> p k t", p=128)
    cdst = Dm['colsT'].r("(c p) t -> p c t", p=128)
    ctok = Dm['colsTok']
    for tt in range(NTT):
        t0 = tt * TT
        xt, hT = xts[tt % 2], hTs[tt % 2]
        P.dma(xt, xsrc[:, :, t0:t0 + TT])
        rms_tile(P, xt, hT, sq, rstd, ones, 8, D, TT)
        for k in range(8):
            P.tt(sq[:, k, :], xt[:, k, :], rstd, ALU.mult, eng='pool')
        for c in range(42):
            ps = P.ps()
            for k in range(8):
                if C_IQ <= c <= C_IK:
                    P.mm(ps, wf[:, k, (c - C_IQ) * 128:(c - C_IQ + 1) * 128], sq[:, k, :], start=(k == 0), stop=(k == 7))
                else:
                    P.mm(ps, wpb[c // 5][:, k, (c % 5) * 128:(c % 5 + 1) * 128], hT[:, k, :], start=(k == 0), stop=(k == 7))
            stg_ = ostg[(c // 2) % 2]
            if c % 3 == 2:
                P.copy(stg_[:, c % 2, :], ps, eng='dve')
            else:
                P.act(stg_[:, c % 2, :], ps, AF.Copy)
            if c % 2 == 1:
                P.dma(cdst[:, c - 1:c + 1, t0:t0 + TT], stg_)
        for s in range(4):
            ps = P.ps()
            ps2 = P.ps()
            for k in range(8):
                P.mm(ps, hT[:, k, s * 128:(s + 1) * 128], wtb[:, k, 0:512], start=(k == 0), stop=(k == 7))
            for k in range(8):
                P.mm(ps2[:, 0:4], sq[:, k, s * 128:(s + 1) * 128], wf[:, k, 640:644], start=(k == 0), stop=(k == 7))
            ts_ = tstg[s % 2]
            P.act(ts_[:, 0:512], ps, AF.Copy)
            P.copy(ts_[:, 512:516], ps2[:, 0:4], eng='dve')
            P.dma(ctok[t0 + s * 128:t0 + (s + 1) * 128, :], ts_)
    P.barrier()


BR_NAMES = ['rwkv', 'dsa', 'ret', 's5', 'xatt']


def stage_M(P, l, Dm, xT, xT_out):
    P.sb_off = SB_BASE
    npre = P.sb([128, 8], F32, "npre")
    npost = P.sb([128, 8], F32, "npost")
    P.dma(npre, Dm[f'npre{l}'])
    P.dma(npost, Dm[f'npost{l}'])
    ones = P.sb([128, 128], F32, "ones")
    P.memset(ones, 1.0)
    wg = P.sb([128, 8, 5120], BF16, "wg")
    wbr = P.sb([128, 10, 1024], BF16, "wbr")
    wout = P.sb([128, 8, 1024], BF16, "wout")
    stg = [P.sb([128, 1024], F32, "wstg") for _ in range(3)]
    wbrb = stream_weight_blocks(P, wbr, Dm[f'wbr{l}'], None, 10, [(0, 1024)], stg)[0]
    wgb = stream_weight_blocks(P, wg, Dm[f'wg{l}'], npre, 8, [(i * 1024, (i + 1) * 1024) for i in range(5)], stg)
    woutb = stream_weight_blocks(P, wout, Dm[f'wout{l}'], None, 8, [(0, 1024)], stg)[0]
    xt = P.sb([128, 8, TT], F32, "xt")
    sq = P.sb([128, 8, TT], F32, "sq")
    hT = P.sb([128, 8, TT], BF16, "hT")
    rstd = P.sb([128, TT], F32, "rstd")
    yts = [P.sb([128, 2, TT], BF16, f"y{i}") for i in range(5)]
    sg = [P.sb([128, TT], F32, "sg") for _ in range(2)]
    term = [P.sb([128, TT], F32, "term") for _ in range(2)]
    macc = P.sb([128, TT], F32, "macc")
    mT = P.sb([128, 8, TT], BF16, "mT")
    osb = sq
    osq = P.sb([128, TT], F32, "osq")
    xsrc = xT.r("(k p) t -> p k t", p=128)
    xdst = xT_out.r("(k p) t -> p k t", p=128)
    for tt in range(NTT):
        t0 = tt * TT
        P.dma(xt, xsrc[:, :, t0:t0 + TT])
        for i in range(5):
            P.dma(yts[i], Dm[f'yT_{BR_NAMES[i]}'].r("(c p) t -> p c t", p=128)[:, :, t0:t0 + TT])
        rms_tile(P, xt, hT, sq, rstd, ones, 8, D, TT)
        j = 0
        for i in range(5):
            for dc in range(8):
                psg = P.ps()
                for k in range(8):
                    P.mm(psg, wgb[i][:, k, dc * 128:(dc + 1) * 128], hT[:, k, :], start=(k == 0), stop=(k == 7))
                psb = P.ps()
                for kk in range(2):
                    P.mm(psb, wbrb[:, i * 2 + kk, dc * 128:(dc + 1) * 128], yts[i][:, kk, :],
                         start=(kk == 0), stop=(kk == 1))
                s_, t_ = sg[j % 2], term[j % 2]
                j += 1
                P.act(s_, psg, AF.Sigmoid)
                if i == 0:
                    P.tt(sq[:, dc, :], s_, psb, ALU.mult)
                elif i < 4:
                    P.tt(t_, s_, psb, ALU.mult)
                    P.tt(sq[:, dc, :], sq[:, dc, :], t_, ALU.add, eng='pool')
                else:
                    P.tt(t_, s_, psb, ALU.mult)
                    P.tt(mT[:, dc, :], sq[:, dc, :], t_, ALU.add, eng='pool')
        pss = P.ps_acc()
        for ec in range(8):
            ps = P.ps()
            for k in range(8):
                P.mm(ps, woutb[:, k, ec * 128:(ec + 1) * 128], mT[:, k, :], start=(k == 0), stop=(k == 7))
            P.act(osb[:, ec, :], ps, AF.Copy)
            P.act(osq, ps, AF.Square)
            P.mm(pss, ones, osq, start=(ec == 0), stop=(ec == 7))
        rsqrt_ps(P, rstd, pss, 1.0 / D, 1e-6)
        for ec in range(8):
            P.stt(osb[:, ec, :], osb[:, ec, :], npost[:, ec:ec + 1], rstd, ALU.mult, ALU.mult)
            P.tt(xt[:, ec, :], xt[:, ec, :], osb[:, ec, :], ALU.add, eng='pool')
        P.dma(xdst[:, :, t0:t0 + TT], xt)
    P.barrier()


def stage_X(P, l, Dm):
    P.sb_off = SB_BASE
    nmem = P.sb([128, 8], F32, "nmem")
    P.dma(nmem, Dm[f'nmem{l}'])
    ones = P.sb([128, 128], F32, "ones")
    P.memset(ones, 1.0)
    wm = P.sb([128, 8, 512], BF16, "wm")
    m0 = P.sb_off
    load_weight_bf16(P, wm, Dm[f'wmem{l}'], nmem, 8, 512, blk=512)
    P.barrier()
    P.sb_off = m0
    mt = P.sb([128, 8, 256], F32, "mt")
    msq = P.sb([128, 8, 256], F32, "msq")
    mh = P.sb([128, 8, 256], BF16, "mh")
    mr = P.sb([128, 256], F32, "mr")
    P.dma(mt, Dm['memT'].r("(k p) m -> p k m", p=128))
    rms_tile(P, mt, mh, msq, mr, ones, 8, D, 256)
    kmT = [P.sb([128, 256], BF16, "kmT") for _ in range(2)]
    for c in range(2):
        ps = P.ps()
        for k in range(8):
            P.mm(ps[:, 0:256], wm[:, k, c * 128:(c + 1) * 128], mh[:, k, :], start=(k == 0), stop=(k == 7))
        P.copy(kmT[c], ps[:, 0:256])
    vpad = [[P.sb([128, 128], BF16, "vpad") for _ in range(4)] for _ in range(2)]
    opad = [P.sb([128, 128], BF16, "opad") for _ in range(2)]
    for hh in range(2):
        P.memset(opad[hh], 0.0)
        P.memset(opad[hh][:, hh * 64:(hh + 1) * 64], 1.0)
    for mc in range(2):
        ps = P.ps()
        for k in range(8):
            P.mm(ps[:, 0:256], mh[:, k, mc * 128:(mc + 1) * 128], wm[:, k, 256:512], start=(k == 0), stop=(k == 7))
        for h in range(4):
            hh = h % 2
            P.memset(vpad[mc][h], 0.0)
            P.copy(vpad[mc][h][:, hh * 64:(hh + 1) * 64], ps[:, h * 64:(h + 1) * 64])
    qf = P.sb([128, 2, TT], F32, "qf")
    gf = P.sb([128, 2, TT], F32, "gf")
    qb = P.sb([128, 2, TT], BF16, "qb")
    E = [[P.sb([128, TT], BF16, "E") for _ in range(2)] for _ in range(2)]
    rs = P.sb([128, TT], F32, "rs")
    o = P.sb([128, TT], F32, "o")
    sgl = P.sb([128, TT], F32, "sgl")
    yst = P.sb([128, 2, TT], BF16, "yst")
    csrc = Dm['colsT'].r("(c p) t -> p c t", p=128)
    ydst = Dm['yT_xatt'].r("(c p) t -> p c t", p=128)
    for tt in range(NTT):
        t0 = tt * TT
        P.dma(qf, csrc[:, C_XQ:C_XQ + 2, t0:t0 + TT])
        P.dma(gf, csrc[:, C_XG:C_XG + 2, t0:t0 + TT])
        P.copy(qb, qf)
        for p in range(2):
            for hh in range(2):
                for mc in range(2):
                    ps = P.ps()
                    P.mm(ps, kmT[p][hh * 64:(hh + 1) * 64, mc * 128:(mc + 1) * 128],
                         qb[hh * 64:(hh + 1) * 64, p, :])
                    P.act(E[hh][mc], ps, AF.Exp, scale=0.125)
            pso = P.ps()
            pss = P.ps()
            n = 0
            for hh in range(2):
                for mc in range(2):
                    P.mm(pso, vpad[mc][2 * p + hh], E[hh][mc], start=(n == 0), stop=(n == 3))
                    n += 1
            n = 0
            for hh in range(2):
                for mc in range(2):
                    P.mm(pss, opad[hh], E[hh][mc], start=(n == 0), stop=(n == 3))
                    n += 1
            P.recip(rs, pss)
            P.tt(o, pso, rs, ALU.mult)
            P.act(sgl, gf[:, p, :], AF.Silu)
            P.tt(yst[:, p, :], o, sgl, ALU.mult)
        P.dma(ydst[:, :, t0:t0 + TT], yst)
    P.barrier()


def dram_specs():
    sp = {
        'xT': ([D, S], F32, 'in'), 'memT': ([D, 256], F32, 'in'), 'pos': ([1, S], I32, 'in'),
        'colsT': ([NP_ROWS, S], F32, 'scratch'), 'colsTok': ([S, NT_COLS], F32, 'scratch'),
        'xT1': ([D, S], F32, 'scratch'),
    }
    for n in BR_NAMES:
        sp[f'yT_{n}'] = ([256, S], BF16, 'scratch')
    sp['iqR'] = ([256, S], F32, 'scratch')
    sp['qR'] = ([256, S], F32, 'scratch')
    sp['ropeC'] = ([64, S], F32, 'scratch')
    sp['ropeS'] = ([64, S], F32, 'scratch')
    sp['ropeconst'] = ([64, 2], F32, 'in')
    sp['ident'] = ([128, 128], F32, 'in')
    sp['ret_idT'] = ([128, 4, 128], F32, 'in')
    sp['ret_qd'] = ([64, 4, 128], F32, 'in')
    sp['ret_kd'] = ([128, 4], F32, 'in')
    sp['ret_cd'] = ([64, 256], F32, 'in')
    sp['s5mask'] = ([128, 8, 8], F32, 'in')
    sp['rw_masks'] = ([64, 3, 64], F32, 'in')
    sp['dsa_cb'] = ([128, 128], F32, 'in')
    sp['dsa_pw'] = ([128, 32], F32, 'in')
    for l in range(2):
        sp[f'rwprm{l}'] = ([64, 8, 4], F32, 'in')
        sp[f'rwmu{l}'] = ([64, 18], F32, 'in')
        sp[f'rww2{l}'] = ([64, 256], F32, 'in')
        sp[f'rwa2{l}'] = ([64, 256], F32, 'in')
    sp['s5tau'] = ([128, 512], F32, 'in')
    for l in range(2):
        sp[f'retgn{l}'] = ([64, 4], F32, 'in')
        sp[f's5lam{l}'] = ([128, 8, 3], F32, 'in')
        sp[f's5b{l}'] = ([128, 8, 2, 16], F32, 'in')
        sp[f's5c{l}'] = ([128, 8, 2, 16], F32, 'in')
        sp[f's5d{l}'] = ([128, 2], F32, 'in')
        sp[f's5wglu{l}'] = ([256, 256], F32, 'in')
    for l in range(2):
        sp[f'wp{l}'] = ([D, NP_ROWS], F32, 'in')
        sp[f'wt{l}'] = ([D, NT_COLS], F32, 'in')
        sp[f'wpf{l}'] = ([D, 644], F32, 'in')
        sp[f'wg{l}'] = ([D, 5120], F32, 'in')
        sp[f'wbr{l}'] = ([1280, D], F32, 'in')
        sp[f'wout{l}'] = ([D, D], F32, 'in')
        sp[f'wmem{l}'] = ([D, 512], F32, 'in')
        for n in ['npre', 'npost', 'nmem']:
            sp[f'{n}{l}'] = ([128, 8], F32, 'in')
    return sp


def host_inputs(inputs, b):
    f_idx, t_idx = proj_col_indices()
    d = {}
    d['xT'] = np.ascontiguousarray(inputs['x'][b].T)
    d['memT'] = np.ascontiguousarray(inputs['mem'][b].T)
    d['pos'] = np.ascontiguousarray(inputs['positions'][b][None, :]).astype(np.int32)
    pk = lambda v: np.ascontiguousarray(v.reshape(8, 128).T)
    jj = np.arange(64)
    inv = (10000.0 ** (-(np.arange(32, dtype=np.float32)) / 32)).astype(np.float32)
    d['ropeconst'] = np.stack([inv[jj % 32], np.where(jj < 32, -1.0, 1.0)], 1).astype(np.float32)
    d['ident'] = np.eye(128, dtype=np.float32)
    d['ret_idT'], d['ret_qd'], d['ret_kd'], d['ret_cd'] = ret_consts()
    ii = np.arange(64)
    rm = np.zeros((64, 3, 64), np.float32)
    rm[:, 0, :] = (ii[None, :] > ii[:, None])
    rm[:, 1, :] = (ii[None, :] >= ii[:, None])
    rm[:, 2, :] = (ii[None, :] < ii[:, None])
    d['rw_masks'] = rm
    i128 = np.arange(128)
    d['dsa_pw'] = np.ascontiguousarray(np.broadcast_to((0.5 ** np.arange(1, 33, dtype=np.float64)).astype(np.float32)[None, :], (128, 32)))
    d['dsa_cb'] = np.where(i128[None, :] <= i128[:, None], 0.0, -1e30).astype(np.float32)
    for l in range(2):
        hd = lambda v: np.ascontiguousarray(v.reshape(4, 64).T)
        z = np.zeros((64, 4), np.float32)
        d[f'rwprm{l}'] = np.ascontiguousarray(np.stack([hd(inputs['rwkv_w0'][l]), hd(inputs['rwkv_a0'][l]), hd(inputs['rwkv_k_k'][l]),
                                   hd(inputs['rwkv_k_a'][l]), hd(inputs['rwkv_r_k'][l].reshape(256)), hd(inputs['rwkv_lnx_w'][l]),
                                   hd(inputs['rwkv_lnx_b'][l]), z], 1).astype(np.float32))
        d[f'rwmu{l}'] = np.ascontiguousarray(inputs['rwkv_mu'][l].reshape(18, 64).T)
        d[f'rww2{l}'] = np.ascontiguousarray(inputs['rwkv_w2'][l])
        d[f'rwa2{l}'] = np.ascontiguousarray(inputs['rwkv_a2'][l])
    sidx = np.arange(128)
    mk = np.zeros((128, 8, 8), np.float32)
    for j in range(8):
        mk[sidx, j, (2 * j + sidx // 64) % 8] = 1.0
    d['s5mask'] = mk
    d['s5tau'] = np.ascontiguousarray(np.broadcast_to(np.arange(1, 513, dtype=np.float32)[None, :], (128, 512)))
    sj = lambda a: np.ascontiguousarray(a.reshape((8, 128) + a.shape[1:]).swapaxes(0, 1))
    for l in range(2):
        d[f'retgn{l}'] = np.ascontiguousarray(inputs['ret_gn_w'][l].reshape(4, 64).T)
        lam3 = np.stack([inputs['s5_lam_re'][l].reshape(1024), inputs['s5_lam_im'][l].reshape(1024),
                         np.repeat(inputs['s5_log_dt'][l], 64)], 1).astype(np.float32)
        d[f's5lam{l}'] = sj(lam3)
        d[f's5b{l}'] = sj(np.stack([inputs['s5_b_re'][l].reshape(1024, 16), inputs['s5_b_im'][l].reshape(1024, 16)], 1))
        ct = lambda c: np.ascontiguousarray(c.transpose(0, 2, 1)).reshape(1024, 16)
        d[f's5c{l}'] = sj(np.stack([ct(inputs['s5_c_re'][l]), ct(inputs['s5_c_im'][l])], 1))
        d[f's5d{l}'] = np.ascontiguousarray(inputs['s5_d'][l].reshape(2, 128).T)
        d[f's5wglu{l}'] = np.ascontiguousarray(inputs['s5_w_glu'][l])
    for l in range(2):
        w = inputs['w_in'][l]
        d[f'wp{l}'] = np.ascontiguousarray(w[:, f_idx])
        d[f'wt{l}'] = np.ascontiguousarray(w[:, t_idx])
        d[f'wpf{l}'] = np.ascontiguousarray(w[:, np.concatenate([f_idx[C_IQ * 128:(C_IK + 1) * 128], t_idx[512:516]])])
        d[f'wg{l}'] = np.ascontiguousarray(w[:, B_GATE:B_GATE + 5120])
        d[f'wbr{l}'] = np.ascontiguousarray(inputs['w_branch'][l].reshape(1280, D))
        d[f'wout{l}'] = np.ascontiguousarray(inputs['w_out'][l])
        d[f'wmem{l}'] = np.ascontiguousarray(inputs['w_mem_kv'][l])
        d[f'npre{l}'] = pk(inputs['norm_pre'][l])
        d[f'npost{l}'] = pk(inputs['norm_post'][l])
        d[f'nmem{l}'] = pk(inputs['norm_mem'][l])
    return d


STAGE_FNS = {}


def build(plan, ext_in=(), ext_out=()):
    nc = bass.Bass("TRN2", target_bir_lowering=False)
    P = Prog(nc)
    Dm = {}
    used_in = []
    for name, (shape, dtype, role) in dram_specs().items():
        if role == 'in' or name in ext_in:
            kind = "ExternalInput"
            used_in.append(name)
        elif name in ext_out:
            kind = "ExternalOutput"
        else:
            kind = "Internal"
        Dm[name] = P.dram(name, shape, dtype, kind=kind)
    Dm['outT'] = P.dram('outT', [D, S], F32, kind="ExternalOutput")
    for st, l in plan:
        xin = Dm['xT'] if l == 0 else Dm['xT1']
        xout = Dm['xT1'] if l == 0 else Dm['outT']
        if st == 'P':
            stage_P(P, l, Dm, xin)
        elif st == 'M':
            stage_M(P, l, Dm, xin, xout)
        elif st == 'X':
            stage_X(P, l, Dm)
        else:
            STAGE_FNS[st](P, l, Dm)
    P.finish()
    return nc, P, used_in


def sin_reduced(P, out, ang, kq, ki, m1):
    P.ts(kq, ang, 1.0 / (2 * math.pi), ALU.mult)
    P.copy(ki, kq)
    P.copy(kq, ki)
    P.stt(ang, kq, -2 * math.pi, ang, ALU.mult, ALU.add)
    P.ts(m1, ang, math.pi, ALU.is_gt, -2 * math.pi, ALU.mult)
    P.tt(ang, ang, m1, ALU.add)
    P.ts(m1, ang, -math.pi, ALU.is_lt, 2 * math.pi, ALU.mult)
    P.tt(ang, ang, m1, ALU.add)
    P.act(out, ang, AF.Sin)


def stage_R(P, l, Dm):
    P.sb_off = SB_BASE
    W = 2048
    rc = P.sb([64, 2], F32, "rc")
    P.dma(rc, Dm['ropeconst'])
    posi = P.sb([64, W], I32, "posi")
    posf = P.sb([64, W], F32, "posf")
    ang = P.sb([64, W], F32, "ang")
    kq = P.sb([64, W], F32, "kq")
    ki = P.sb([64, W], I32, "ki")
    m1 = P.sb([64, W], F32, "m1")
    o = P.sb([64, W], F32, "o")
    for half in range(S // W):
        sl = slice(half * W, (half + 1) * W)
        P.dma(posi, Dm['pos'][:, sl].m(lambda x: x.to_broadcast([64, W])))
        P.copy(posf, posi)
        P.ts(ang, posf, rc[:, 0:1], ALU.mult)
        sin_reduced(P, o, ang, kq, ki, m1)
        P.ts(o, o, rc[:, 1:2], ALU.mult)
        P.dma(Dm['ropeS'][:, sl], o)
        P.ts(ang, posf, rc[:, 0:1], ALU.mult, math.pi / 2, ALU.add)
        sin_reduced(P, o, ang, kq, ki, m1)
        P.dma(Dm['ropeC'][:, sl], o)
    P.barrier()


def rope_heads(P, dst, Dm, c_base, c_swap, ropeC, ropeS, nheads=4, scale=None, dram_dst=None):
    a = P.sb([64, nheads, TT], F32, "ra")
    b = P.sb([64, nheads, TT], F32, "rb")
    if dram_dst is not None:
        ro = [P.sb([64, nheads, TT], F32, "ro") for _ in range(2)]
    src = Dm['colsT']
    for tt in range(NTT):
        sl = slice(tt * TT, (tt + 1) * TT)
        rb_ = c_base * 128 if isinstance(c_base, int) else c_base[0]
        rs_ = c_swap * 128 if isinstance(c_swap, int) else c_swap[0]
        P.dma(a, src[rb_:rb_ + nheads * 64, sl].r("(h d) t -> d h t", d=64))
        P.dma(b, src[rs_:rs_ + nheads * 64, sl].r("(h d) t -> d h t", d=64))
        cb = ropeC[:, sl].m(lambda x: x.unsqueeze(1).to_broadcast([64, nheads, TT]))
        sb_ = ropeS[:, sl].m(lambda x: x.unsqueeze(1).to_broadcast([64, nheads, TT]))
        P.tt(a, a, cb, ALU.mult)
        P.tt(b, b, sb_, ALU.mult, eng='pool')
        if dram_dst is None:
            P.tt(dst[:, :, sl], a, b, ALU.add)
        else:
            o_ = ro[tt % 2]
            P.tt(o_, a, b, ALU.add)
            P.dma(dram_dst.r("(h d) t -> d h t", d=64)[:, :, sl], o_)


RET_LOGG = [math.log(1.0 - math.exp(v)) for v in np.linspace(math.log(1.0 / 32), math.log(1.0 / 512), 4)]


def ret_consts():
    j = np.arange(128, dtype=np.float64)
    idT = np.zeros((128, 4, 128), np.float32)
    qd = np.zeros((64, 4, 128), np.float32)
    kd = np.zeros((128, 4), np.float32)
    cd = np.zeros((64, 256), np.float32)
    for h in range(4):
        lg = RET_LOGG[h]
        rel = j[None, :] - j[:, None]
        idT[:, h, :] = np.where(rel >= 0, np.exp(lg * np.maximum(rel, 0.0)), 0.0) * 0.125
        qd[:, h, :] = np.exp(lg * (j + 1.0))[None, :]
        kd[:, h] = np.exp(lg * (127.0 - j)) * 0.125
        cd[:, h * 64:(h + 1) * 64] = math.exp(lg * 128)
    return idT, qd, kd, cd


def stage_RET(P, l, Dm):
    P.sb_off = SB_BASE
    ropeC = P.sb([64, S], F32, "ropeC")
    ropeS = P.sb([64, S], F32, "ropeS")
    P.dma(ropeC, Dm['ropeC'])
    P.dma(ropeS, Dm['ropeS'])
    idT = P.sb([128, 4, 128], F32, "idT")
    qd = P.sb([64, 4, 128], F32, "qd")
    kd = P.sb([128, 4], F32, "kd")
    cd = P.sb([64, 256], F32, "cd")
    gn = P.sb([64, 4], F32, "gn")
    identb = P.sb([128, 128], BF16, "identb")
    identf = P.sb([128, 128], F32, "identf")
    ones64 = P.sb([64, 64], F32, "ones64")
    P.dma(idT, Dm['ret_idT'])
    P.dma(qd, Dm['ret_qd'])
    P.dma(kd, Dm['ret_kd'])
    P.dma(cd, Dm['ret_cd'])
    P.dma(gn, Dm[f'retgn{l}'])
    P.dma(identf, Dm['ident'])
    P.copy(identb, identf)
    P.memset(ones64, 1.0 / 64)
    qT = P.sb([64, 4, S], BF16, "qT")
    kT = P.sb([64, 4, S], BF16, "kT")
    qdT = P.sb([64, 4, S], BF16, "qdT")
    m0 = P.sb_off
    rope_heads(P, qT, Dm, C_RQ, C_RQS, ropeC, ropeS)
    rope_heads(P, kT, Dm, C_RK, C_RKS, ropeC, ropeS)
    for c in range(32):
        cs = slice(c * 128, (c + 1) * 128)
        P.tt(qdT[:, :, cs], qT[:, :, cs], qd, ALU.mult, eng=('dve' if c % 2 else 'pool'))
    P.barrier()
    P.sb_off = m0
    Vt = P.sb([128, 32, 256], BF16, "Vt")
    Kd = P.sb([128, 32, 256], BF16, "Kd")
    vst = [P.sb([128, 4, 256], F32, "vst") for _ in range(2)]
    vsrc = Dm['colsTok'].r("(c p) n -> p c n", p=128)
    for i in range(8):
        v_ = vst[i % 2]
        P.dma(v_, vsrc[:, i * 4:(i + 1) * 4, 256:512])
        P.copy(Vt[:, i * 4:(i + 1) * 4, :], v_, eng=('dve' if i % 2 else 'pool'))
    for c in range(32):
        cs = slice(c * 128, (c + 1) * 128)
        ps = P.ps()
        psb = ps.bitcast(BF16)
        for h in range(4):
            P.transpose(psb[:, h * 64:(h + 1) * 64], kT[:, h, cs], identb[0:64, 0:64])
        P.tt(Kd[:, c, :].r("p (h d) -> p h d", h=4), psb[:, 0:256].r("p (h d) -> p h d", h=4),
             kd.m(lambda x: x.unsqueeze(2).to_broadcast([128, 4, 64])), ALU.mult)
    R = P.sb([64, 256], F32, "R")
    Rb = P.sb([64, 256], BF16, "Rb")
    P.memset(R, 0.0)
    P.memset(Rb, 0.0)
    AT = [P.sb([128, 4, 128], BF16, "AT") for _ in range(2)]
    Osb = P.sb([64, 512], F32, "Osb")
    dd = P.sb([64, 512], F32, "dd")
    dsq = P.sb([64, 512], F32, "dsq")
    rstd = P.sb([64, 512], F32, "rstd")
    gt = [P.sb([64, 4, 128], F32, "gt") for _ in range(2)]
    sg = P.sb([64, 4, 128], F32, "sg")
    yo = [P.sb([64, 4, 128], BF16, "yo") for _ in range(2)]
    gsrc = Dm['colsT'][C_RG * 128:C_RG * 128 + 256, :].r("(h d) t -> d h t", d=64)
    ydst = Dm['yT_ret'].r("(h d) t -> d h t", d=64)
    for c in range(32):
        cs = slice(c * 128, (c + 1) * 128)
        g_ = gt[c % 2]
        P.dma(g_, gsrc[:, :, cs])
        psA = P.ps()
        for h in range(4):
            P.mm(psA[:, h * 128:(h + 1) * 128], kT[:, h, cs], qT[:, h, cs])
        at = AT[c % 2]
        P.tt(at, psA.r("p (h q) -> p h q", h=4), idT, ALU.mult)
        psO = P.ps()
        for h in range(4):
            P.mm(psO[0:64, h * 128:(h + 1) * 128], Vt[:, c, h * 64:(h + 1) * 64], at[:, h, :], start=True, stop=False, inc=False)
            P.mm(psO[0:64, h * 128:(h + 1) * 128], Rb[:, h * 64:(h + 1) * 64], qdT[:, h, cs], start=False, stop=True)
        psKV = P.ps()
        for h in range(4):
            P.mm(psKV[0:64, h * 64:(h + 1) * 64], Kd[:, c, h * 64:(h + 1) * 64], Vt[:, c, h * 64:(h + 1) * 64])
        P.tt(R, R, cd, ALU.mult)
        P.tt(R, R, psKV[0:64, 0:256], ALU.add)
        P.copy(Rb, R, eng='pool')
        P.act(Osb, psO[0:64, :], AF.Copy)
        psM = P.ps()
        P.mm(psM[0:64, :], ones64, Osb)
        P.tt(dd, Osb, psM[0:64, :], ALU.subtract)
        P.act(dsq, dd, AF.Square)
        psV = P.ps()
        P.mm(psV[0:64, :], ones64, dsq)
        P.ts(rstd, psV[0:64, :], 1e-6, ALU.add)
        P.act(rstd, rstd, AF.Sqrt)
        P.recip(rstd, rstd)
        P.tt(dd, dd, rstd, ALU.mult)
        P.tt(dd.r("p (h q) -> p h q", h=4), dd.r("p (h q) -> p h q", h=4),
             gn.m(lambda x: x.unsqueeze(2).to_broadcast([64, 4, 128])), ALU.mult)
        P.act(sg, g_, AF.Silu)
        y_ = yo[c % 2]
        P.tt(y_, dd.r("p (h q) -> p h q", h=4), sg, ALU.mult)
        P.dma(ydst[:, :, cs], y_)
    P.barrier()


STAGE_FNS['R'] = stage_R
STAGE_FNS['RET'] = stage_RET


def stage_S5(P, l, Dm):
    P.sb_off = SB_BASE
    W = TT
    lam = P.sb([128, 8, 3], F32, "lam")
    bsb = P.sb([128, 8, 2, 16], F32, "bsb")
    csb = P.sb([128, 8, 2, 16], F32, "csb")
    msk = P.sb([128, 8, 8], F32, "msk")
    tau = P.sb([128, W], F32, "tau")
    dsk = P.sb([128, 2], F32, "dsk")
    identf = P.sb([128, 128], F32, "identf")
    P.dma(lam, Dm[f's5lam{l}'])
    P.dma(bsb, Dm[f's5b{l}'])
    P.dma(csb, Dm[f's5c{l}'])
    P.dma(msk, Dm['s5mask'])
    P.dma(tau, Dm['s5tau'])
    P.dma(dsk, Dm[f's5d{l}'])
    P.dma(identf, Dm['ident'])
    wglu = P.sb([128, 2, 256], BF16, "wglu")
    cosT = P.sb([128, 8, W], F32, "cosT")
    sinT = P.sb([128, 8, W], F32, "sinT")
    mag = P.sb([128, 8], F32, "mag")
    BT = P.sb([128, 8, 2, 128], BF16, "BT")
    CX = P.sb([128, 8, 2, 128], BF16, "CX")
    m0 = P.sb_off
    load_weight_bf16(P, wglu, Dm[f's5wglu{l}'], None, 2, 256, blk=256)
    lr = P.sb([128, 8], F32, "lr")
    li = P.sb([128, 8], F32, "li")
    dt = P.sb([128, 8], F32, "dt")
    th = P.sb([128, 8], F32, "th")
    P.ts(lr, lam[:, :, 0], -1e-4, ALU.min)
    P.copy(li, lam[:, :, 1])
    P.act(dt, lam[:, :, 2], AF.Exp)
    P.tt(th, li, dt, ALU.mult)
    P.tt(mag, lr, dt, ALU.mult)
    P.act(mag, mag, AF.Exp)
    ang = P.sb([128, W], F32, "ang")
    kq = P.sb([128, W], F32, "kq")
    ki = P.sb([128, W], I32, "ki")
    m1 = P.sb([128, W], F32, "m1")
    for j in range(8):
        P.ts(ang, tau, th[:, j:j + 1], ALU.mult)
        sin_reduced(P, sinT[:, j, :], ang, kq, ki, m1)
        P.ts(ang, tau, th[:, j:j + 1], ALU.mult, math.pi / 2, ALU.add)
        sin_reduced(P, cosT[:, j, :], ang, kq, ki, m1)
    abr = P.sb([128, 8], F32, "abr")
    abi = P.sb([128, 8], F32, "abi")
    den = P.sb([128, 8], F32, "den")
    t8 = P.sb([128, 8], F32, "t8")
    fre = P.sb([128, 8], F32, "fre")
    fim = P.sb([128, 8], F32, "fim")
    P.tt(abr, mag, cosT[:, :, 0], ALU.mult)
    P.tt(abi, mag, sinT[:, :, 0], ALU.mult)
    P.ts(abr, abr, -1.0, ALU.add)
    P.tt(den, lr, lr, ALU.mult)
    P.tt(t8, li, li, ALU.mult)
    P.tt(den, den, t8, ALU.add)
    P.recip(den, den)
    P.tt(fre, abr, lr, ALU.mult)
    P.tt(t8, abi, li, ALU.mult)
    P.tt(fre, fre, t8, ALU.add)
    P.tt(fre, fre, den, ALU.mult)
    P.tt(fim, abi, lr, ALU.mult)
    P.tt(t8, abr, li, ALU.mult)
    P.tt(fim, fim, t8, ALU.subtract)
    P.tt(fim, fim, den, ALU.mult)
    bb = P.sb([128, 8, 2, 16], F32, "bb")
    tb = P.sb([128, 8, 16], F32, "tb")
    bc16 = lambda v: v.m(lambda x: x.unsqueeze(2).to_broadcast([128, 8, 16]))
    P.tt(bb[:, :, 0, :], bsb[:, :, 0, :], bc16(fre), ALU.mult)
    P.tt(tb, bsb[:, :, 1, :], bc16(fim), ALU.mult)
    P.tt(bb[:, :, 0, :], bb[:, :, 0, :], tb, ALU.subtract)
    P.tt(bb[:, :, 1, :], bsb[:, :, 1, :], bc16(fre), ALU.mult)
    P.tt(tb, bsb[:, :, 0, :], bc16(fim), ALU.mult)
    P.tt(bb[:, :, 1, :], bb[:, :, 1, :], tb, ALU.add)
    P.ts(csb[:, :, 1, :], csb[:, :, 1, :], -1.0, ALU.mult)
    bx = P.sb([128, 8, 16], F32, "bx")
    for j in range(8):
        mj = msk[:, j, :].m(lambda x: x.unsqueeze(2).to_broadcast([128, 8, 16]))
        for ri in range(2):
            P.tt(bx, bb[:, j, ri, :].m(lambda x: x.unsqueeze(1).to_broadcast([128, 8, 16])), mj, ALU.mult)
            ps = P.ps()
            P.transpose(ps[:, 0:128], bx.r("p a b -> p (a b)"), identf)
            P.copy(BT[:, j, ri, :], ps[:, 0:128])
            P.tt(CX[:, j, ri, :].r("p (a b) -> p a b", a=8),
                 csb[:, j, ri, :].m(lambda x: x.unsqueeze(1).to_broadcast([128, 8, 16])), mj, ALU.mult)
    P.barrier()
    P.sb_off = m0
    A = P.sb([128, 8, W], F32, "A")
    B = P.sb([128, 8, W], F32, "B")
    t1 = P.sb([128, 8, W], F32, "t1")
    t2 = P.sb([128, 8, W], F32, "t2")
    wre = P.sb([128, 8, W], F32, "wre")
    wim = P.sb([128, 8, W], F32, "wim")
    xre = P.sb([128, 8, W], BF16, "xre")
    xim = P.sb([128, 8, W], BF16, "xim")
    cre = P.sb([128, 8], F32, "cre")
    cim = P.sb([128, 8], F32, "cim")
    P.memset(cre, 0.0)
    P.memset(cim, 0.0)
    uf = P.sb([128, 2, W], F32, "uf")
    ub = P.sb([128, 2, W], BF16, "ub")
    gf = P.sb([128, 2, W], F32, "gf")
    y = P.sb([128, 2, W], F32, "y")
    y2 = P.sb([128, 2, W], F32, "y2")
    glb = P.sb([128, 2, W], BF16, "glb")
    yo = P.sb([128, 2, W], BF16, "yo")
    csrc = Dm['colsT'].r("(c p) t -> p c t", p=128)
    ydst = Dm['yT_s5'].r("(c p) t -> p c t", p=128)
    for tt in range(NTT):
        sl = slice(tt * W, (tt + 1) * W)
        P.dma(uf, csrc[:, C_SU:C_SU + 2, sl])
        P.dma(gf, csrc[:, C_SG:C_SG + 2, sl])
        P.copy(ub, uf, eng='pool')
        for j in range(8):
            for ri, dst in ((0, A), (1, B)):
                ps = P.ps()
                P.mm(ps, BT[:, j, ri, :], ub[:, j // 4, :])
                P.act(dst[:, j, :], ps, AF.Copy)
        P.tt(t1, A, cosT, ALU.mult)
        P.tt(t2, B, sinT, ALU.mult, eng='pool')
        P.tt(t1, t1, t2, ALU.add)
        P.tt(t2, A, sinT, ALU.mult, eng='pool')
        P.tt(B, B, cosT, ALU.mult)
        P.tt(t2, B, t2, ALU.subtract, eng='pool')
        for j in range(8):
            mb = mag[:, j:j + 1].bc([128, W])
            P.scan(wre[:, j, :], mb, t1[:, j, :], cre[:, j:j + 1], ALU.mult, ALU.add)
            P.scan(wim[:, j, :], mb, t2[:, j, :], cim[:, j:j + 1], ALU.mult, ALU.add)
        P.tt(t1, wre, cosT, ALU.mult)
        P.tt(A, wim, sinT, ALU.mult, eng='pool')
        P.tt(xre, t1, A, ALU.subtract)
        P.tt(cre, t1[:, :, W - 1], A[:, :, W - 1], ALU.subtract)
        P.tt(t2, wre, sinT, ALU.mult, eng='pool')
        P.tt(B, wim, cosT, ALU.mult)
        P.tt(xim, t2, B, ALU.add, eng='pool')
        P.tt(cim, t2[:, :, W - 1], B[:, :, W - 1], ALU.add)
        for jc in range(2):
            ps = P.ps()
            n = 0
            for j in range(4 * jc, 4 * jc + 4):
                for ri, xx in ((0, xre), (1, xim)):
                    P.mm(ps, CX[:, j, ri, :], xx[:, j, :], start=(n == 0), stop=(n == 7))
                    n += 1
            P.stt(y[:, jc, :], uf[:, jc, :], dsk[:, jc:jc + 1], ps, ALU.mult, ALU.add)
        P.tt(y2, y, y, ALU.mult)
        P.ts(y2, y2, 0.044715, ALU.mult, 1.0, ALU.add)
        P.tt(y2, y2, y, ALU.mult)
        P.act(y2, y2, AF.Sigmoid, scale=1.5957691216057308)
        P.tt(y, y, y2, ALU.mult)
        P.copy(glb, y, eng='pool')
        for oc in range(2):
            ps = P.ps()
            for kc in range(2):
                P.mm(ps, wglu[:, kc, oc * 128:(oc + 1) * 128], glb[:, kc, :], start=(kc == 0), stop=(kc == 1))
            P.act(y2[:, oc, :], ps, AF.Sigmoid)
        P.tt(y, y, y2, ALU.mult)
        P.act(y2, gf, AF.Silu)
        P.tt(yo, y, y2, ALU.mult)
        P.dma(ydst[:, :, sl], yo)
    P.barrier()


STAGE_FNS['S5'] = stage_S5


import os
RW_DEBUG = int(os.environ.get('RW_DEBUG', '3'))


def stage_RWKV(P, l, Dm):
    P.sb_off = SB_BASE
    W = 256
    H4 = 4
    NCH = W // 64
    HC = H4 * NCH
    prm = P.sb([64, 8, 4], F32, "prm")
    mu = P.sb([64, 18], F32, "mu")
    w2 = P.sb([64, 256], F32, "w2")
    a2 = P.sb([64, 256], F32, "a2")
    msks = P.sb([64, 3, 64], F32, "msks")
    identf = P.sb([128, 128], F32, "identf")
    ones64 = P.sb([64, 64], F32, "ones64")
    onesw = P.sb([64, 1], F32, "onesw")
    P.dma(prm, Dm[f'rwprm{l}'])
    P.dma(mu, Dm[f'rwmu{l}'])
    P.dma(w2, Dm[f'rww2{l}'])
    P.dma(a2, Dm[f'rwa2{l}'])
    P.dma(msks, Dm['rw_masks'])
    P.dma(identf, Dm['ident'])
    P.memset(ones64, 1.0)
    P.memset(onesw, 1.0)
    id64 = identf[0:64, 0:64]
    hb = lambda v, n=W: v.m(lambda x: x.unsqueeze(2).to_broadcast([64, H4, n]))
    mb = lambda k: msks[:, k, :].m(lambda x: x.unsqueeze(1).to_broadcast([64, H4, 64]))
    idb = id64.m(lambda x: x.unsqueeze(1).to_broadcast([64, H4, 64]))
    cin = P.sb([64, 18, W + 1], F32, "cin")
    cs = P.sb([64, 18, W], F32, "cs")
    f = lambda nm: P.sb([64, H4, W], F32, nm)
    twl = P.sb([64, W], F32, "twl")
    sgz, av, kx, t0, kp, beta = f("sgz"), f("av"), f("kx"), f("t0"), f("kp"), f("beta")
    kkn, lw, cw, e1, e2 = f("kkn"), f("lw"), f("cw"), f("e1"), f("e2")
    rt, at, bt, kt, Bh, Kh = f("rt"), f("at"), f("bt"), f("kt"), f("Bh"), f("Kh")
    bonus, Yt = f("bonus"), f("Yt")
    fb = lambda nm: P.sb([64, H4, W], BF16, nm)
    rtb, atb, btb, ktb = fb("rtb"), fb("atb"), fb("btb"), fb("ktb")
    base = P.sb([64, HC], F32, "base")
    cwC = P.sb([64, HC], F32, "cwC")
    gC = P.sb([64, HC], F32, "gC")
    S0 = P.sb([64, H4, 64], F32, "S0")
    P.memset(S0, 0.0)
    NP2 = NCH // 2
    g8 = lambda nm, dt=F32: [P.sb([64, 2, H4, 64], dt, nm) for _ in range(NP2)]
    X, XT, PaT, AakT, ArbT, ArkT = g8("X", BF16), g8("XT", BF16), g8("PaT", BF16), g8("AakT"), g8("ArbT"), g8("ArkT")
    Vt, BhT, KhT, atT, W2, M2, M1T, KV, Gd = (g8("Vt"), g8("BhT"), g8("KhT"), g8("atT", BF16), g8("W2", BF16), g8("M2"),
                                              g8("M1T"), g8("KV"), g8("Gd"))
    Usb = [P.sb([64, H4, 64], F32, "Usb") for _ in range(2)]
    mb8 = lambda k: msks[:, k, :].m(lambda x: x.unsqueeze(1).unsqueeze(1).to_broadcast([64, 2, H4, 64]))
    id8 = id64.m(lambda x: x.unsqueeze(1).unsqueeze(1).to_broadcast([64, 2, H4, 64]))
    ps8 = lambda ps: ps[0:64, 0:512].r("p (c h x) -> p c h x", c=2, h=H4)
    yo = P.sb([64, H4, W], BF16, "yo")
    src = Dm['colsT'][0:1152, :].r("(g d) t -> d g t", d=64)
    ydst = Dm['yT_rwkv'].r("(h d) t -> d h t", d=64)
    ps4 = lambda ps: ps[0:64, 0:256].r("p (h x) -> p h x", h=H4)
    for tt in range(S // W):
        t_0 = tt * W
        if tt == 0:
            P.dma(cin[:, :, 1:W + 1], src[:, :, t_0:t_0 + W])
            P.memset(cin[:, :, 0:1], 0.0)
        else:
            P.dma(cin, src[:, :, t_0 - 1:t_0 + W])
        P.tt(cs, cin[:, :, 0:W], cin[:, :, 1:W + 1], ALU.subtract)
        P.tt(cs, cs, mu.m(lambda x: x.unsqueeze(2).to_broadcast([64, 18, W])), ALU.mult)
        P.tt(cs, cs, cin[:, :, 1:W + 1], ALU.add)
        Rr, Kk, Vv, G = cs[:, 0:4, :], cs[:, 4:8, :], cs[:, 8:12, :], cs[:, 14:18, :]
        P.act(twl, cs[:, 12, :], AF.Tanh)
        for h in range(H4):
            ps = P.ps()
            P.mm(ps[0:64, 0:W], w2[:, h * 64:(h + 1) * 64], twl)
            P.act(sgz[:, h, :], ps[0:64, 0:W], AF.Sigmoid, bias=prm[:, 0, h:h + 1])
            ps = P.ps()
            P.mm(ps[0:64, 0:W], a2[:, h * 64:(h + 1) * 64], cs[:, 13, :])
            P.act(av[:, h, :], ps[0:64, 0:W], AF.Sigmoid, bias=prm[:, 1, h:h + 1])
        P.tt(kx, Kk, hb(prm[:, 2, :]), ALU.mult)
        P.tt(t0, kx, kx, ALU.mult, eng='pool')
        for h in range(H4):
            ps = P.ps()
            P.mm(ps[0:64, 0:W], ones64, t0[:, h, :])
            P.ts(kkn[:, h, :], ps[0:64, 0:W], 1e-24, ALU.add)
        P.act(kkn, kkn, AF.Sqrt)
        P.recip(kkn, kkn)
        P.tt(kkn, kkn, kx, ALU.mult)
        P.ts(t0, av, -1.0, ALU.add)
        P.tt(t0, t0, hb(prm[:, 3, :]), ALU.mult)
        P.stt(kp, t0, 1.0, Kk, ALU.add, ALU.mult)
        P.tt(beta, kkn, av, ALU.mult, eng='pool')
        P.tt(t0, Rr, kp, ALU.mult)
        P.tt(t0, t0, hb(prm[:, 4, :]), ALU.mult)
        for h in range(H4):
            ps = P.ps()
            P.mm(ps[0:64, 0:W], ones64, t0[:, h, :])
            P.tt(bonus[:, h, :], ps[0:64, 0:W], Vv[:, h, :], ALU.mult)
        P.ts(lw, sgz, -math.exp(-0.5), ALU.mult)
        for h in range(H4):
            P.scan(cw[:, h, :], onesw[:, 0:1].bc([64, W]), lw[:, h, :], 0.0, ALU.mult, ALU.add)
        cw3 = cw.r("p h (c i) -> p (h c) i", i=64)
        P.memset(base, 0.0)
        P.copy(base.r("p (h c) -> p h c", h=H4)[:, :, 1:NCH], cw.r("p h (c i) -> p h c i", i=64)[:, :, 0:NCH - 1, 63])
        P.tt(cw3, cw3, base.m(lambda x: x.unsqueeze(2).to_broadcast([64, HC, 64])), ALU.subtract)
        P.copy(cwC, cw3[:, :, 63])
        P.act(gC, cwC, AF.Exp)
        P.act(e1, cw, AF.Exp)
        P.tt(rt, Rr, e1, ALU.mult)
        P.act(e1, cw, AF.Exp, scale=-1.0)
        P.tt(bt, beta, e1, ALU.mult)
        P.tt(kt, kp, e1, ALU.mult, eng='pool')
        P.tt(e2, cw, lw, ALU.subtract)
        P.act(e2, e2, AF.Exp)
        P.stt(at, kkn, -1.0, e2, ALU.mult, ALU.mult)
        e13 = e1.r("p h (c i) -> p (h c) i", i=64)
        P.tt(e13, cw3, cwC.m(lambda x: x.unsqueeze(2).to_broadcast([64, HC, 64])), ALU.subtract)
        P.act(e1, e1, AF.Exp, scale=-1.0)
        P.tt(Bh, beta, e1, ALU.mult)
        P.tt(Kh, kp, e1, ALU.mult, eng='pool')
        def mm8(p, lhf, rhf):
            ps = P.ps()
            for cl in range(2):
                for h in range(H4):
                    o_ = ps[0:64, (cl * H4 + h) * 64:(cl * H4 + h + 1) * 64]
                    P.mm(o_, lhf(p, cl, h), rhf(p, cl, h))
            return ps8(ps)
        csl = lambda p, cl: slice((2 * p + cl) * 64, (2 * p + cl + 1) * 64)
        tok = lambda t_: (lambda p, cl, h: t_[:, h, csl(p, cl)])
        blk = lambda t_: (lambda p, cl, h: t_[p][:, cl, h, :])
        for src_, dst_ in ((rt, rtb), (at, atb), (bt, btb), (kt, ktb)):
            P.copy(dst_, src_, eng='pool')
        for p in range(NP2):
            P.tt(X[p], mm8(p, tok(atb), tok(btb)), mb8(2), ALU.mult)
            P.tt(XT[p], mm8(p, tok(btb), tok(atb)), mb8(0), ALU.mult)
            P.tt(AakT[p], mm8(p, tok(ktb), tok(atb)), mb8(0), ALU.mult)
            P.tt(ArbT[p], mm8(p, tok(btb), tok(rtb)), mb8(1), ALU.mult)
            P.tt(ArkT[p], mm8(p, tok(ktb), tok(rtb)), mb8(1), ALU.mult)
            P.tt(PaT[p], XT[p], id8, ALU.add)
        for srcT, dstT, eng in ((Vv, Vt, 'act'), (Bh, BhT, 'dve'), (Kh, KhT, 'act'), (at, atT, 'dve')):
            for p in range(NP2):
                ps = P.ps()
                for cl in range(2):
                    for h in range(H4):
                        P.transpose(ps[0:64, (cl * H4 + h) * 64:(cl * H4 + h + 1) * 64], srcT[:, h, csl(p, cl)], id64)
                if eng == 'act':
                    P.act(dstT[p], ps8(ps), AF.Copy)
                else:
                    P.copy(dstT[p], ps8(ps))
        for it in range(5):
            pxs = [(mm8(p, blk(XT), blk(X)), mm8(p, blk(X), blk(XT))) for p in range(NP2)]
            for p in range(NP2):
                P.act(X[p], pxs[p][0], AF.Copy)
                P.copy(XT[p], pxs[p][1])
            pps = [mm8(p, blk(X), blk(PaT)) for p in range(NP2)]
            for p in range(NP2):
                P.tt(PaT[p], PaT[p], pps[p], ALU.add)
        for p in range(NP2):
            P.act(KV[p], mm8(p, blk(KhT), blk(Vt)), AF.Copy)
            P.act(W2[p], mm8(p, blk(AakT), blk(Vt)), AF.Copy)
            P.copy(M1T[p], mm8(p, blk(atT), blk(PaT)))
            for cl in range(2):
                c = 2 * p + cl
                gcb = gC.r("p (h c) -> p h c", h=H4)[:, :, c].m(lambda x: x.unsqueeze(2).to_broadcast([64, H4, 64]))
                P.tt(Gd[p][:, cl], idb, gcb, ALU.mult)
        for p in range(NP2):
            P.act(M2[p], mm8(p, blk(PaT), blk(W2)), AF.Copy)
        for c in range(NCH):
            p, cl = c // 2, c % 2
            sl = slice(c * 64, (c + 1) * 64)
            us = Usb[c % 2]
            psu = P.ps()
            for h in range(H4):
                P.mm(psu[0:64, h * 64:(h + 1) * 64], M1T[p][:, cl, h, :], S0[:, h, :])
            psy = P.ps()
            for h in range(H4):
                o_ = psy[0:64, h * 64:(h + 1) * 64]
                P.mm(o_, S0[:, h, :], rt[:, h, sl], start=True, stop=False)
                P.mm(o_, Vt[p][:, cl, h, :], ArkT[p][:, cl, h, :], start=False, stop=True)
            P.tt(us, ps4(psu), M2[p][:, cl], ALU.add)
            pss = P.ps()
            for h in range(H4):
                o_ = pss[0:64, h * 64:(h + 1) * 64]
                P.mm(o_, Gd[p][:, cl, h, :], S0[:, h, :], start=True, stop=False)
                P.mm(o_, BhT[p][:, cl, h, :], us[:, h, :], start=False, stop=True)
            psy2 = P.ps()
            for h in range(H4):
                P.mm(psy2[0:64, h * 64:(h + 1) * 64], us[:, h, :], ArbT[p][:, cl, h, :])
            P.tt(S0, ps4(pss), KV[p][:, cl], ALU.add)
            P.act(Yt[:, :, sl], ps4(psy), AF.Copy)
            P.tt(Yt[:, :, sl], Yt[:, :, sl], ps4(psy2), ALU.add)
        for h in range(H4):
            ps = P.ps()
            P.mm(ps[0:64, 0:W], ones64, Yt[:, h, :])
            P.stt(e1[:, h, :], ps[0:64, 0:W], -1.0 / 64, Yt[:, h, :], ALU.mult, ALU.add)
        P.tt(e2, e1, e1, ALU.mult, eng='pool')
        for h in range(H4):
            ps = P.ps()
            P.mm(ps[0:64, 0:W], ones64, e2[:, h, :])
            P.ts(t0[:, h, :], ps[0:64, 0:W], 1.0 / 64, ALU.mult, 64e-5, ALU.add)
        P.act(t0, t0, AF.Sqrt)
        P.recip(t0, t0)
        P.tt(e1, e1, t0, ALU.mult)
        P.tt(e1, e1, hb(prm[:, 5, :]), ALU.mult)
        P.tt(e1, e1, hb(prm[:, 6, :]), ALU.add)
        P.tt(e1, e1, bonus, ALU.add)
        P.act(e2, G, AF.Silu)
        P.tt(yo, e1, e2, ALU.mult)
        P.dma(ydst[:, :, t_0:t_0 + W], yo)
    P.barrier()


STAGE_FNS['RWKV'] = stage_RWKV


N_BISECT = int(os.environ.get("N_BISECT", "16"))


def stage_DSA(P, l, Dm):
    P.sb_off = SB_BASE
    kT = P.sb([64, 4, S], BF16, "kT")
    ikT = P.sb([64, 1, S], F32, "ikT")
    m0 = P.sb_off
    ropeC = P.sb([64, S], F32, "ropeC")
    ropeS = P.sb([64, S], F32, "ropeS")
    P.dma(ropeC, Dm['ropeC'])
    P.dma(ropeS, Dm['ropeS'])
    rope_heads(P, None, Dm, C_DQ, C_DQS, ropeC, ropeS, dram_dst=Dm['qR'])
    rope_heads(P, kT, Dm, C_DK, C_DKS, ropeC, ropeS)
    rope_heads(P, None, Dm, C_IQ, C_IQS, ropeC, ropeS, dram_dst=Dm['iqR'])
    rope_heads(P, ikT, Dm, (C_IK * 128,), (C_IK * 128 + 64,), ropeC, ropeS, nheads=1)
    P.barrier()
    P.sb_off = m0
    identf = P.sb([128, 128], F32, "identf")
    identb = P.sb([128, 128], BF16, "identb")
    cb = P.sb([128, 128], F32, "cb")
    P.dma(identf, Dm['ident'])
    P.copy(identb, identf)
    P.dma(cb, Dm['dsa_cb'])
    Vaug = P.sb([128, 32, 4, 65], BF16, "Vaug")
    iwt = P.sb([128, 32, 4], F32, "iwt")
    vst = [P.sb([128, 4, 256], F32, "vst") for _ in range(2)]
    tsrc = Dm['colsTok'].r("(c p) n -> p c n", p=128)
    P.memset(Vaug[:, :, :, 64:65], 1.0)
    for i in range(8):
        v_ = vst[i % 2]
        P.dma(v_, tsrc[:, i * 4:(i + 1) * 4, 0:256])
        P.copy(Vaug[:, i * 4:(i + 1) * 4, :, 0:64], v_.r("p c (h d) -> p c h d", h=4), eng=('dve' if i % 2 else 'pool'))
    P.dma(iwt, tsrc[:, :, 512:516])
    P.ts(iwt, iwt, 1.0 / 16, ALU.mult)
    scores = [P.sb([128, S], F32, "score") for _ in range(2)]
    mask01s = [P.sb([128, S], BF16, "mask01") for _ in range(2)]
    maskTs = [P.sb([128, 32, 128], BF16, "maskT") for _ in range(2)]
    rl = [P.sb([128, 512], F32, "rl") for _ in range(4)]
    E = [P.sb([128, 512], BF16, "E") for _ in range(2)]
    lo = P.sb([128, 1], F32, "lo")
    hi = P.sb([128, 1], F32, "hi")
    mid = P.sb([128, 1], F32, "mid")
    cnt = P.sb([128, 1], F32, "cnt")
    sel = P.sb([128, 1], F32, "sel")
    dlt = P.sb([128, 1], F32, "dlt")
    stp = P.sb([128, 32], F32, "stp")
    pw = P.sb([128, 32], F32, "pw")
    P.dma(pw, Dm['dsa_pw'])
    zt = P.sb([128, S], BF16, "zt")
    cz = P.sb([128, S], F32, "cz")
    junk = cz
    nz = P.sb([128, 1], F32, "nz")
    npos = P.sb([128, 1], F32, "npos")
    flag = P.sb([128, 1], F32, "flag")
    f2 = P.sb([128, 1], F32, "f2")
    rr = P.sb([128, 1], F32, "rr")
    onesw = P.sb([128, 1], F32, "onesw")
    P.memset(onesw, 1.0)
    negbig = P.sb([128, 1], F32, "negbig")
    P.memset(negbig, -1e5)
    osb = P.sb([128, 4, 64], F32, "osb")
    rs = P.sb([128, 4, 1], F32, "rs")
    gt = [P.sb([128, 2, 128], F32, "gt") for _ in range(2)]
    sgs = [P.sb([128, 2, 128], F32, "sg") for _ in range(2)]
    yo = [P.sb([128, 2, 128], BF16, "yo") for _ in range(2)]
    iqt = [P.sb([64, 4, 128], F32, "iqt") for _ in range(2)]
    iqsrc = Dm['iqR'].r("(h d) t -> d h t", d=64)
    qft = [P.sb([64, 4, 128], F32, "qft") for _ in range(2)]
    qbt = [P.sb([64, 4, 128], BF16, "qbt") for _ in range(2)]
    qsrc = Dm['qR'].r("(h d) t -> d h t", d=64)
    gsrc = Dm['colsT'].r("(c p) t -> p c t", p=128)
    ydst = Dm['yT_dsa'].r("(c p) t -> p c t", p=128)
    ne = [0]
    st = {}

    def score_phase(i):
        qs = slice(i * 128, (i + 1) * 128)
        Nk = 128 * (i + 1)
        g_ = gt[i % 2]
        P.dma(g_, gsrc[:, C_DG:C_DG + 2, qs])
        iq_ = iqt[i % 2]
        P.dma(iq_, iqsrc[:, :, qs])
        P.dma(qft[i % 2], qsrc[:, :, qs])
        qb_ = qbt[i % 2]
        P.copy(qb_, qft[i % 2], eng='pool')
        score = scores[i % 2]
        mask01 = mask01s[i % 2]
        maskT = maskTs[i % 2]
        for k0 in range(0, Nk, 512):
            kw = min(512, Nk - k0)
            pss = []
            for h in range(4):
                ps = P.ps()
                P.mm(ps[:, 0:kw], iq_[:, h, :], ikT[:, 0, k0:k0 + kw], inc=(h == 3))
                pss.append(ps)
            for h in range(4):
                P.act(rl[h][:, 0:kw], pss[h][:, 0:kw], AF.Relu)
            P.ts(score[:, k0:k0 + kw], rl[0][:, 0:kw], iwt[:, i, 0:1], ALU.mult)
            for h in range(1, 4):
                P.stt(score[:, k0:k0 + kw], rl[h][:, 0:kw], iwt[:, i, h:h + 1], score[:, k0:k0 + kw], ALU.mult, ALU.add)
        P.tt(score[:, i * 128:Nk], score[:, i * 128:Nk], cb, ALU.add)
        st[i] = (qs, Nk, g_, iq_, qb_, score, mask01, maskT)

    def select_phase(i):
        qs, Nk, g_, iq_, qb_, score, mask01, maskT = st[i]
        if Nk > 256:
            P.reduce(hi, score[:, 0:Nk], ALU.max)
            P.reduce(lo, score[:, 0:i * 128], ALU.min)
            P.tt(dlt, hi, lo, ALU.subtract)
            P.ts(dlt, dlt, 2.0, ALU.add)
            P.ts(stp, pw, dlt[:, 0:1], ALU.mult)
            P.stt(mid, dlt, 0.5, lo, ALU.mult, ALU.add)
            P.ts(mid, mid, -1.0, ALU.add)
            for it in range(N_BISECT):
                P.ts(junk[:, 0:Nk], score[:, 0:Nk], mid[:, 0:1], ALU.is_ge, 0.0, ALU.add, accum=cnt)
                if it < N_BISECT - 1:
                    P.ts(sel, cnt, 255.5, ALU.is_ge, 0.5, ALU.subtract)
                    P.stt(mid, sel, stp[:, it:it + 1], mid, ALU.mult, ALU.add)
                else:
                    P.ts(sel, cnt, 255.5, ALU.is_ge, 1.0, ALU.subtract)
                    P.stt(lo, sel, stp[:, it:it + 1], mid, ALU.mult, ALU.add)
        else:
            P.memset(lo, -1e29)
        P.ts(zt[:, 0:Nk], score[:, 0:Nk], 0.0, ALU.is_equal, 0.0, ALU.add, accum=nz)
        P.ts(junk[:, 0:Nk], score[:, 0:Nk], 0.0, ALU.is_gt, 0.0, ALU.add, accum=npos)
        P.ts(flag, npos, 255.5, ALU.is_lt)
        P.tt(f2, npos, nz, ALU.add)
        P.ts(f2, f2, 255.5, ALU.is_ge)
        P.tt(flag, flag, f2, ALU.mult)
        P.ts(rr, npos, -1.0, ALU.mult, 256.0, ALU.add)
        P.scan(cz[:, 0:Nk], onesw[:, 0:1].bc([128, Nk]), zt[:, 0:Nk], 0.0, ALU.mult, ALU.add)
        P.ts(cz[:, 0:Nk], cz[:, 0:Nk], rr[:, 0:1], ALU.is_le, flag[:, 0:1], ALU.mult)
        P.tt(zt[:, 0:Nk], zt[:, 0:Nk], cz[:, 0:Nk], ALU.mult)
        P.ts(f2, flag, -1.0, ALU.mult, 1.0, ALU.add)
        P.tt(lo, lo, f2, ALU.mult)
        P.stt(lo, flag, 1e-30, lo, ALU.mult, ALU.add)
        P.ts(mask01[:, 0:Nk], score[:, 0:Nk], lo[:, 0:1], ALU.is_ge)
        P.tt(mask01[:, 0:Nk], mask01[:, 0:Nk], zt[:, 0:Nk], ALU.add)
        for c0 in range(0, i + 1, 4):
            nc_ = min(4, i + 1 - c0)
            ps = P.ps()
            psb = ps.bitcast(BF16)
            for cl in range(nc_):
                c = c0 + cl
                P.transpose(psb[:, cl * 128:(cl + 1) * 128], mask01[:, c * 128:(c + 1) * 128], identb, inc=(cl == nc_ - 1))
            P.act(maskT[:, c0:c0 + nc_, :], psb[:, 0:nc_ * 128].r("p (c q) -> p c q", q=128), AF.Identity,
                  scale=1e5, bias=negbig[:, 0:1])

    def attention(i):
        qs, Nk, g_, iq_, qb_, score, mask01, maskT = st[i]
        psO = P.ps_acc(i)
        groups = [(h, c0, min(4, i + 1 - c0)) for h in range(4) for c0 in range(0, i + 1, 4)]

        def logits(g):
            h, c0, nc_ = g
            ps = P.ps()
            for cl in range(nc_):
                c = c0 + cl
                o_ = ps[:, cl * 128:(cl + 1) * 128]
                P.mm(o_, kT[:, h, c * 128:(c + 1) * 128], qb_[:, h, :], start=True, stop=False)
                P.mm(o_, identb, maskT[:, c, :], start=False, stop=True)
            return ps
        nxt = logits(groups[0])
        for gi, (h, c0, nc_) in enumerate(groups):
            ps = nxt
            if gi + 1 < len(groups):
                nxt = logits(groups[gi + 1])
            e_ = E[ne[0] % 2]
            ne[0] += 1
            P.act(e_[:, 0:nc_ * 128], ps[:, 0:nc_ * 128], AF.Exp, scale=0.125)
            for cl in range(nc_):
                c = c0 + cl
                P.mm(psO[:, h * 65:(h + 1) * 65], e_[:, cl * 128:(cl + 1) * 128], Vaug[:, c, h, :],
                     start=(c == 0), stop=(c == i))

    def fin(i):
        qs, Nk, g_, iq_, qb_, score, mask01, maskT = st[i]
        psO = P.ps_acc(i)
        pv = psO[:, 0:260].r("p (h x) -> p h x", h=4)
        P.recip(rs, pv[:, :, 64:65])
        P.tt(osb, pv[:, :, 0:64], rs.m(lambda x: x.to_broadcast([128, 4, 64])), ALU.mult)
        sg = sgs[i % 2]
        P.act(sg, g_, AF.Silu)
        y_ = yo[i % 2]
        for p in range(2):
            ps = P.ps()
            P.transpose(ps[:, 0:128], osb[:, 2 * p:2 * p + 2, :].r("p a b -> p (a b)"), identf)
            P.tt(y_[:, p, :], ps[:, 0:128], sg[:, p, :], ALU.mult)
        P.dma(ydst[:, :, qs], y_)

    score_phase(0)
    for i in range(32):
        select_phase(i)
        if i > 0:
            fin(i - 1)
        if i + 1 < 32:
            score_phase(i + 1)
        attention(i)
    fin(31)
    P.barrier()


STAGE_FNS['DSA'] = stage_DSA


FULL_PLAN = [('R', 0)] + [(st, l) for l in range(2) for st in ('P', 'X', 'RET', 'S5', 'RWKV', 'DSA', 'M')]


def kernel(**inputs):
    inputs = {k: np.asarray(v) for k, v in inputs.items()}
    nb = inputs['x'].shape[0]
    nc, P, used_in = build(FULL_PLAN)
    in_maps = []
    for b in range(nb):
        d = host_inputs(inputs, b)
        in_maps.append({k: v for k, v in d.items() if k in used_in})
    res = run_bass_kernel_spmd(nc, in_maps, core_ids=list(range(nb)))
    out = np.stack([np.ascontiguousarray(np.asarray(r['outT']).T) for r in res.results], 0)
    return out.astype(np.float32)
```
